# Optimizing a Trainium2 kernel written in Bass

```python
import math
import jax
import jax.numpy as jnp
from jax import lax
import numpy as np

D_MODEL = 1024
BATCH = 4
SEQ = 8192
DEPTH = 4

GRID_W = 64
CTX_LEN = 256
N_EVEN = (DEPTH + 1) // 2
N_ODD = DEPTH // 2
EPS = 1e-6
NEG_INF = -1e30
N_MOD = 6

HY_CH = D_MODEL // 2
HY_ORDER = 2
SHORT_CONV = 3
HY_BANDS = 16
HY_EMB = 1 + 2 * HY_BANDS
HY_FFN = 64
HY_WINDOW_SHIFT = 0.05
HY_DECAY_TARGET = 1e-2
HY_FAST_PCT = 0.3
HY_SLOW_PCT = 1.5
S5_CH = D_MODEL // 2
S5_GROUP = 16
S5_NG = S5_CH // S5_GROUP
S5_P = 64
S5_DT_MIN = 1e-3
S5_DT_MAX = 1e-1
EV_IN = (HY_ORDER + 1) * HY_CH + S5_CH
EV_MIX = HY_CH + S5_CH

MLA_HEADS = 8
MLA_NOPE = 64
MLA_ROPE = 32
MLA_V = 64
Q_LORA = 256
KV_LORA = 128
GQA_HEADS = 8
GQA_KV = 2
GQA_HD = 64
WINDOW = 128
BLK = 128
Q_BLK = 128
ROPE_BASE = 10000.0
OFF_KR = Q_LORA + KV_LORA
OFF_GQ = OFF_KR + MLA_ROPE
OFF_GK = OFF_GQ + GQA_HEADS * GQA_HD
OFF_GV = OFF_GK + GQA_KV * GQA_HD
OD_IN = OFF_GV + GQA_KV * GQA_HD
OD_MIX = MLA_HEADS * MLA_V + GQA_HEADS * GQA_HD
MLA_SCALE = (MLA_NOPE + MLA_ROPE) ** -0.5
GQA_SCALE = GQA_HD ** -0.5

D_FF = 2816
N_EXP = 8
TOP_K = 2
EXP_FF = 3584

kernel_name = 'hybrid_flow_backbone'


def rmsnorm(x, g):
    xf = x.astype(jnp.float32)
    y = xf * lax.rsqrt(jnp.mean(xf * xf, axis=-1, keepdims=True) + EPS)
    return (y * g.astype(jnp.float32)).astype(x.dtype)


def ada_norm(x, g, shift, scale):
    return rmsnorm(x, g) * (1 + scale) + shift


def swiglu(y, wg, wu, wd):
    return (jax.nn.silu(y @ wg) * (y @ wu)) @ wd


def moe_ffn(y, router, wg, wu, wd):
    logits = (y @ router).astype(jnp.float32)
    top_val, top_idx = lax.top_k(logits, TOP_K)
    weights = jax.nn.softmax(top_val, axis=-1)
    gates = jnp.sum(jax.nn.one_hot(top_idx, N_EXP, dtype=jnp.float32) * weights[..., None], axis=-2).astype(y.dtype)
    out = jnp.zeros_like(y)
    for e in range(N_EXP):
        out = out + gates[..., e:e + 1] * swiglu(y, wg[e], wu[e], wd[e])
    return out


def axial_rope_tables(L, dim, dtype):
    rows = L // GRID_W
    row = jnp.repeat(jnp.arange(rows), GRID_W).astype(jnp.float32)
    col = jnp.tile(jnp.arange(GRID_W), rows).astype(jnp.float32)
    nf = dim // 4
    inv = ROPE_BASE ** (-jnp.arange(nf, dtype=jnp.float32) / nf)
    ar = row[:, None] * inv[None, :]
    ac = col[:, None] * inv[None, :]
    return ((jnp.cos(ar).astype(dtype), jnp.sin(ar).astype(dtype)),
            (jnp.cos(ac).astype(dtype), jnp.sin(ac).astype(dtype)))


def _rotate(x, cos, sin):
    x1, x2 = jnp.split(x, 2, axis=-1)
    return jnp.concatenate([x1 * cos - x2 * sin, x2 * cos + x1 * sin], axis=-1)


def axial_rope(x, tables):
    (cr, sr), (cc, sc) = tables
    e = lambda t: t[None, :, None, :]
    xr, xc = jnp.split(x, 2, axis=-1)
    return jnp.concatenate([_rotate(xr, e(cr), e(sr)), _rotate(xc, e(cc), e(sc))], axis=-1)


def short_conv(u, w, b):
    L = u.shape[1]
    pad = SHORT_CONV // 2
    up = jnp.pad(u, ((0, 0), (pad, SHORT_CONV - 1 - pad), (0, 0)))
    out = b
    for j in range(SHORT_CONV):
        out = out + up[:, j:j + L] * w[j]
    return out


def hyena_filters(L, w1, b1, w2, b2, w3, freq, decay):
    f32 = jnp.float32
    t = jnp.arange(L, dtype=f32)
    t01 = t / L
    bands = jnp.linspace(1e-4, HY_BANDS - 1, HY_BANDS, dtype=f32)
    ang = (2.0 * math.pi / L) * t[:, None] * bands[None, :]
    feats = jnp.concatenate([t01[:, None], jnp.cos(ang), -jnp.sin(ang)], axis=-1)
    fr = freq.astype(f32)
    hid = jnp.sin(fr * (feats @ w1.astype(f32) + b1.astype(f32)))
    hid = jnp.sin(fr * (hid @ w2.astype(f32) + b2.astype(f32)))
    h = (hid @ w3.astype(f32)).reshape(L, 2, HY_ORDER, HY_CH)
    window = jnp.exp(-t01[:, None, None] * jnp.abs(decay.astype(f32))[None]) + HY_WINDOW_SHIFT
    h = h * window[:, None]
    fwd, bwd = h[:, 0], h[:, 1]
    k = jnp.concatenate([fwd, jnp.zeros((1, HY_ORDER, HY_CH), f32), bwd[:0:-1]], axis=0)
    return k / jnp.sum(jnp.abs(k), axis=0, keepdims=True)


def fft_long_conv(u, k):
    n = k.shape[0]
    L = u.shape[1]
    spec = jnp.fft.rfft(u, n=n, axis=1) * jnp.fft.rfft(k, axis=0)[None]
    return jnp.fft.irfft(spec, n=n, axis=1)[:, :L]


def hyena_sequence(z, conv_w, conv_b, w1, b1, w2, b2, w3, freq, decay, bias):
    L = z.shape[1]
    zc = short_conv(z, conv_w, conv_b).astype(jnp.float32)
    v, *gates = jnp.split(zc, HY_ORDER + 1, axis=-1)
    k = hyena_filters(L, w1, b1, w2, b2, w3, freq, decay)
    bias = bias.astype(jnp.float32)
    y = v
    for o, gate in enumerate(gates):
        y = gate * (fft_long_conv(y, k[:, o]) + y * bias[o])
    return y.astype(z.dtype)


def s5_discretize(a_re, a_im, log_dt, b_re, b_im):
    f32 = jnp.float32
    lam = lax.complex(jnp.minimum(a_re.astype(f32), -1e-4), a_im.astype(f32))
    dt = jnp.exp(log_dt.astype(f32))[:, None]
    abar = jnp.exp(lam * dt)
    bbar = ((abar - 1.0) / lam)[..., None] * lax.complex(b_re.astype(f32), b_im.astype(f32))
    return abar, bbar


def _linear_recurrence(e1, e2):
    a1, b1 = e1
    a2, b2 = e2
    return a1 * a2, a2 * b1 + b2


def s5_scan(abar, bu, s0):
    if s0 is not None:
        bu = bu.at[:, 0].add(abar * s0)
    a = jnp.broadcast_to(abar, (1, bu.shape[1]) + abar.shape)
    _, states = lax.associative_scan(_linear_recurrence, (a, bu), axis=1)
    return states


def s5_mixer(u_lat, u_ctx, a_re, a_im, log_dt, b_re, b_im, c_re, c_im, d_skip, w_glu, need_ctx):
    f32 = jnp.float32
    grouped = lambda u: u.astype(f32).reshape(u.shape[0], u.shape[1], S5_NG, S5_GROUP)
    ul, uc = grouped(u_lat), grouped(u_ctx)
    y_lat = jnp.zeros(ul.shape, f32)
    y_ctx = jnp.zeros(uc.shape, f32)
    for d in range(2):
        abar, bbar = s5_discretize(a_re[d], a_im[d], log_dt[d], b_re[d], b_im[d])
        cmat = lax.complex(c_re[d].astype(f32), c_im[d].astype(f32))
        orient = (lambda s: s) if d == 0 else (lambda s: jnp.flip(s, axis=1))
        st_c = s5_scan(abar, jnp.einsum('gpc,blgc->blgp', bbar, orient(uc)), None)
        st_l = s5_scan(abar, jnp.einsum('gpc,blgc->blgp', bbar, orient(ul)), st_c[:, -1])
        y_lat = y_lat + orient(jnp.real(jnp.einsum('gcp,blgp->blgc', cmat, st_l)))
        if need_ctx:
            y_ctx = y_ctx + orient(jnp.real(jnp.einsum('gcp,blgp->blgc', cmat, st_c)))
    dsk = d_skip.astype(f32).reshape(S5_NG, S5_GROUP)
    wg = w_glu.astype(f32)

    def readout(y, u):
        B, L = y.shape[:2]
        y = jax.nn.gelu((y + dsk * u).reshape(B, L, S5_CH))
        return y * jax.nn.sigmoid(y @ wg)

    out_l = readout(y_lat, ul).astype(u_lat.dtype)
    out_c = readout(y_ctx, uc).astype(u_ctx.dtype) if need_ctx else None
    return out_l, out_c


def even_mixer(yl, yc, w_in, conv_w, conv_b, hy_params, s5_params, w_out, need_ctx):
    hy_cols = (HY_ORDER + 1) * HY_CH
    zl = yl @ w_in
    zc = yc @ w_in
    hl = hyena_sequence(zl[..., :hy_cols], conv_w, conv_b, *hy_params)
    sl, sc = s5_mixer(zl[..., hy_cols:], zc[..., hy_cols:], *s5_params, need_ctx)
    out_l = jnp.concatenate([hl, sl], axis=-1) @ w_out
    out_c = None
    if need_ctx:
        hc = hyena_sequence(zc[..., :hy_cols], conv_w, conv_b, *hy_params)
        out_c = jnp.concatenate([hc, sc], axis=-1) @ w_out
    return out_l, out_c


def attend(q, k, v, scale):
    s = jnp.einsum('bqhd,bkhd->bhqk', q, k).astype(jnp.float32) * scale
    p = jax.nn.softmax(s, axis=-1).astype(v.dtype)
    return jnp.einsum('bhqk,bkhd->bqhd', p, v)


def blocked_attend(q, k, v, scale):
    B, L, H, dk = q.shape
    nb = L // Q_BLK
    qb = q.reshape(B, nb, Q_BLK, H, dk).transpose(1, 0, 2, 3, 4)
    out = lax.map(lambda qi: attend(qi, k, v, scale), qb)
    return out.transpose(1, 0, 2, 3, 4).reshape(B, L, H, v.shape[-1])


def window_sink_attention(q, k, v, kc, vc, sink, scale):
    B, L, H, hd = q.shape
    KVH = k.shape[2]
    G = H // KVH
    nb = L // BLK
    f32 = jnp.float32
    qb = q.reshape(B, nb, BLK, KVH, G, hd)
    pad = ((0, 0), (BLK, BLK), (0, 0), (0, 0))
    kp = jnp.pad(k, pad).reshape(B, nb + 2, BLK, KVH, hd)
    vp = jnp.pad(v, pad).reshape(B, nb + 2, BLK, KVH, hd)
    kband = jnp.concatenate([kp[:, :-2], kp[:, 1:-1], kp[:, 2:]], axis=2)
    vband = jnp.concatenate([vp[:, :-2], vp[:, 1:-1], vp[:, 2:]], axis=2)
    qpos = jnp.arange(nb)[:, None] * BLK + jnp.arange(BLK)[None, :]
    kpos = jnp.arange(nb)[:, None] * BLK - BLK + jnp.arange(3 * BLK)[None, :]
    valid = ((jnp.abs(qpos[:, :, None] - kpos[:, None, :]) <= WINDOW)
             & (kpos[:, None, :] >= 0) & (kpos[:, None, :] < L))
    s_band = jnp.einsum('bnqkgd,bnjkd->bnkgqj', qb, kband).astype(f32) * scale
    s_band = jnp.where(valid[None, :, None, None], s_band, NEG_INF)
    s_ctx = jnp.einsum('bnqkgd,bckd->bnkgqc', qb, kc).astype(f32) * scale
    s_sink = jnp.broadcast_to(sink.astype(f32).reshape(KVH, G)[None, None, :, :, None, None], s_band.shape[:-1] + (1,))
    p = jax.nn.softmax(jnp.concatenate([s_band, s_ctx, s_sink], axis=-1), axis=-1)
    nband = 3 * BLK
    nctx = kc.shape[1]
    p_band = p[..., :nband].astype(v.dtype)
    p_ctx = p[..., nband:nband + nctx].astype(v.dtype)
    out = (jnp.einsum('bnkgqj,bnjkd->bnqkgd', p_band, vband)
           + jnp.einsum('bnkgqc,bckd->bnqkgd', p_ctx, vc))
    return out.reshape(B, L, H, hd)


def ctx_sink_attention(q, k, v, sink, scale):
    B, C, H, hd = q.shape
    KVH = k.shape[2]
    G = H // KVH
    f32 = jnp.float32
    qg = q.reshape(B, C, KVH, G, hd)
    s = jnp.einsum('bqkgd,bckd->bkgqc', qg, k).astype(f32) * scale
    s_sink = jnp.broadcast_to(sink.astype(f32).reshape(KVH, G)[None, :, :, None, None], s.shape[:-1] + (1,))
    p = jax.nn.softmax(jnp.concatenate([s, s_sink], axis=-1), axis=-1)[..., :-1].astype(v.dtype)
    return jnp.einsum('bkgqc,bckd->bqkgd', p, v).reshape(B, C, H, hd)


def odd_mixer(yl, yc, w_in, q_norm, w_uq, kv_norm, w_ukv, sink, w_out, rope_mla, rope_gqa, need_ctx):
    zl = yl @ w_in
    zc = yc @ w_in

    def mla_q(z):
        B, L = z.shape[:2]
        return (rmsnorm(z[..., :Q_LORA], q_norm) @ w_uq).reshape(B, L, MLA_HEADS, MLA_NOPE + MLA_ROPE)

    def mla_kv(z):
        B, L = z.shape[:2]
        kv = (rmsnorm(z[..., Q_LORA:OFF_KR], kv_norm) @ w_ukv).reshape(B, L, MLA_HEADS, MLA_NOPE + MLA_V)
        k_rope = z[..., OFF_KR:OFF_GQ].reshape(B, L, 1, MLA_ROPE)
        return kv[..., :MLA_NOPE], k_rope, kv[..., MLA_NOPE:]

    def mla_keys(k_nope, k_rope):
        return jnp.concatenate([k_nope, jnp.broadcast_to(k_rope, k_nope.shape[:3] + (MLA_ROPE,))], axis=-1)

    def gqa_q(z):
        B, L = z.shape[:2]
        return z[..., OFF_GQ:OFF_GK].reshape(B, L, GQA_HEADS, GQA_HD)

    def gqa_kv(z):
        B, L = z.shape[:2]
        return (z[..., OFF_GK:OFF_GV].reshape(B, L, GQA_KV, GQA_HD),
                z[..., OFF_GV:].reshape(B, L, GQA_KV, GQA_HD))

    B, L = yl.shape[:2]
    ql = mla_q(zl)
    ql = jnp.concatenate([ql[..., :MLA_NOPE], axial_rope(ql[..., MLA_NOPE:], rope_mla)], axis=-1)
    kl_nope, krl, vl = mla_kv(zl)
    kc_nope, krc, vc = mla_kv(zc)
    kl = mla_keys(kl_nope, axial_rope(krl, rope_mla))
    kc = mla_keys(kc_nope, krc)
    mla_l = blocked_attend(ql, jnp.concatenate([kc, kl], axis=1), jnp.concatenate([vc, vl], axis=1), MLA_SCALE)
    gkl, gvl = gqa_kv(zl)
    gkc, gvc = gqa_kv(zc)
    gql = axial_rope(gqa_q(zl), rope_gqa)
    gqa_l = window_sink_attention(gql, axial_rope(gkl, rope_gqa), gvl, gkc, gvc, sink, GQA_SCALE)
    out_l = jnp.concatenate([mla_l.reshape(B, L, -1), gqa_l.reshape(B, L, -1)], axis=-1) @ w_out
    out_c = None
    if need_ctx:
        C = yc.shape[1]
        mla_c = attend(mla_q(zc), kc, vc, MLA_SCALE)
        gqa_c = ctx_sink_attention(gqa_q(zc), gkc, gvc, sink, GQA_SCALE)
        out_c = jnp.concatenate([mla_c.reshape(yc.shape[0], C, -1), gqa_c.reshape(yc.shape[0], C, -1)], axis=-1) @ w_out
    return out_l, out_c


def setup_inputs(seed: int = 0) -> dict:
    key = jax.random.key(seed)
    ks = iter(jax.random.split(key, 64))
    f32 = jnp.float32
    D = D_MODEL

    def nrm(shape, scale=1.0):
        return jax.random.normal(next(ks), shape, f32) * scale

    def gain(shape):
        return 1.0 + nrm(shape, 0.02)

    decay0 = jnp.linspace(-math.log(HY_DECAY_TARGET) / HY_SLOW_PCT, -math.log(HY_DECAY_TARGET) / HY_FAST_PCT, HY_CH, dtype=f32)
    return {
        'x': nrm((BATCH, SEQ, D)),
        'c': nrm((BATCH, D)),
        'ctx': nrm((BATCH, CTX_LEN, D)),
        'c_ctx': nrm((D,)),
        'mod_w': nrm((DEPTH, D, N_MOD * D), 0.5 * D ** -0.5),
        'mod_b': nrm((DEPTH, N_MOD * D), 0.02),
        'norm_mix': gain((DEPTH, D)),
        'norm_ffn': gain((DEPTH, D)),
        'final_norm': gain((D,)),
        'ev_w_in': nrm((N_EVEN, D, EV_IN), D ** -0.5),
        'ev_conv_w': nrm((N_EVEN, SHORT_CONV, (HY_ORDER + 1) * HY_CH), SHORT_CONV ** -0.5),
        'ev_conv_b': nrm((N_EVEN, (HY_ORDER + 1) * HY_CH), 0.02),
        'hy_w1': nrm((N_EVEN, HY_EMB, HY_FFN), HY_EMB ** -0.5),
        'hy_b1': nrm((N_EVEN, HY_FFN), 0.1),
        'hy_w2': nrm((N_EVEN, HY_FFN, HY_FFN), HY_FFN ** -0.5),
        'hy_b2': nrm((N_EVEN, HY_FFN), 0.1),
        'hy_w3': nrm((N_EVEN, HY_FFN, 2 * HY_ORDER * HY_CH), HY_FFN ** -0.5),
        'hy_freq': 1.0 + nrm((N_EVEN, HY_FFN), 0.1),
        'hy_decay': decay0 + nrm((N_EVEN, HY_ORDER, HY_CH), 0.1),
        'hy_bias': nrm((N_EVEN, HY_ORDER, HY_CH)),
        's5_a_re': -0.5 + nrm((N_EVEN, 2, S5_NG, S5_P), 0.01),
        's5_a_im': math.pi * jnp.arange(S5_P, dtype=f32) + nrm((N_EVEN, 2, S5_NG, S5_P), 0.01),
        's5_log_dt': jax.random.uniform(next(ks), (N_EVEN, 2, S5_NG), f32, math.log(S5_DT_MIN), math.log(S5_DT_MAX)),
        's5_b_re': nrm((N_EVEN, 2, S5_NG, S5_P, S5_GROUP), (2 * S5_GROUP) ** -0.5),
        's5_b_im': nrm((N_EVEN, 2, S5_NG, S5_P, S5_GROUP), (2 * S5_GROUP) ** -0.5),
        's5_c_re': nrm((N_EVEN, 2, S5_NG, S5_GROUP, S5_P), (2 * S5_P) ** -0.5),
        's5_c_im': nrm((N_EVEN, 2, S5_NG, S5_GROUP, S5_P), (2 * S5_P) ** -0.5),
        's5_d': nrm((N_EVEN, S5_CH)),
        's5_w_glu': nrm((N_EVEN, S5_CH, S5_CH), S5_CH ** -0.5),
        'ev_w_out': nrm((N_EVEN, EV_MIX, D), EV_MIX ** -0.5),
        'ff_w_gate': nrm((N_EVEN, D, D_FF), D ** -0.5),
        'ff_w_up': nrm((N_EVEN, D, D_FF), D ** -0.5),
        'ff_w_down': nrm((N_EVEN, D_FF, D), D_FF ** -0.5),
        'od_w_in': nrm((N_ODD, D, OD_IN), D ** -0.5),
        'mla_q_norm': gain((N_ODD, Q_LORA)),
        'mla_w_uq': nrm((N_ODD, Q_LORA, MLA_HEADS * (MLA_NOPE + MLA_ROPE)), Q_LORA ** -0.5),
        'mla_kv_norm': gain((N_ODD, KV_LORA)),
        'mla_w_ukv': nrm((N_ODD, KV_LORA, MLA_HEADS * (MLA_NOPE + MLA_V)), KV_LORA ** -0.5),
        'gqa_sink': nrm((N_ODD, GQA_HEADS), 0.5),
        'od_w_out': nrm((N_ODD, OD_MIX, D), OD_MIX ** -0.5),
        'moe_router': nrm((N_ODD, D, N_EXP), D ** -0.5),
        'moe_w_gate': nrm((N_ODD, N_EXP, D, EXP_FF), D ** -0.5),
        'moe_w_up': nrm((N_ODD, N_EXP, D, EXP_FF), D ** -0.5),
        'moe_w_down': nrm((N_ODD, N_EXP, EXP_FF, D), EXP_FF ** -0.5),
    }


def reference(x, c, ctx, c_ctx, mod_w, mod_b, norm_mix, norm_ffn, final_norm,
              ev_w_in, ev_conv_w, ev_conv_b, hy_w1, hy_b1, hy_w2, hy_b2, hy_w3, hy_freq, hy_decay, hy_bias,
              s5_a_re, s5_a_im, s5_log_dt, s5_b_re, s5_b_im, s5_c_re, s5_c_im, s5_d, s5_w_glu, ev_w_out,
              ff_w_gate, ff_w_up, ff_w_down,
              od_w_in, mla_q_norm, mla_w_uq, mla_kv_norm, mla_w_ukv, gqa_sink, od_w_out,
              moe_router, moe_w_gate, moe_w_up, moe_w_down):
    L = x.shape[1]
    rope_mla = axial_rope_tables(L, MLA_ROPE, x.dtype)
    rope_gqa = axial_rope_tables(L, GQA_HD, x.dtype)
    s_lat = jax.nn.silu(c)
    s_ctx = jax.nn.silu(c_ctx)
    h, g = x, ctx
    for l in range(DEPTH):
        i = l // 2
        need_ctx = l < DEPTH - 1
        m_lat = jnp.split((s_lat @ mod_w[l] + mod_b[l])[:, None, :], N_MOD, axis=-1)
        m_ctx = jnp.split((s_ctx @ mod_w[l] + mod_b[l])[None, None, :], N_MOD, axis=-1)
        yl = ada_norm(h, norm_mix[l], m_lat[0], m_lat[1])
        yc = ada_norm(g, norm_mix[l], m_ctx[0], m_ctx[1])
        if l % 2 == 0:
            hy_params = (hy_w1[i], hy_b1[i], hy_w2[i], hy_b2[i], hy_w3[i], hy_freq[i], hy_decay[i], hy_bias[i])
            s5_params = (s5_a_re[i], s5_a_im[i], s5_log_dt[i], s5_b_re[i], s5_b_im[i],
                         s5_c_re[i], s5_c_im[i], s5_d[i], s5_w_glu[i])
            ol, oc = even_mixer(yl, yc, ev_w_in[i], ev_conv_w[i], ev_conv_b[i], hy_params, s5_params,
                                ev_w_out[i], need_ctx)

            def ffn(y):
                return swiglu(y, ff_w_gate[i], ff_w_up[i], ff_w_down[i])
        else:
            ol, oc = odd_mixer(yl, yc, od_w_in[i], mla_q_norm[i], mla_w_uq[i], mla_kv_norm[i], mla_w_ukv[i],
                               gqa_sink[i], od_w_out[i], rope_mla, rope_gqa, need_ctx)

            def ffn(y):
                return moe_ffn(y, moe_router[i], moe_w_gate[i], moe_w_up[i], moe_w_down[i])
        h = h + m_lat[2] * ol
        h = h + m_lat[5] * ffn(ada_norm(h, norm_ffn[l], m_lat[3], m_lat[4]))
        if need_ctx:
            g = g + m_ctx[2] * oc
            g = g + m_ctx[5] * ffn(ada_norm(g, norm_ffn[l], m_ctx[3], m_ctx[4]))
    return rmsnorm(h, final_norm)
```

```python
import math
import numpy as np
import concourse.bass as bass
import concourse.mybir as mybir
from concourse.bass_utils import run_bass_kernel_spmd

F32 = mybir.dt.float32
BF16 = mybir.dt.bfloat16
I32 = mybir.dt.int32
AF = mybir.ActivationFunctionType
ALU = mybir.AluOpType
AX = mybir.AxisListType

D = 1024
KC = D // 128
EPS = 1e-6
HY_WINDOW_SHIFT = 0.05
HY_DT = BF16


class Trk:
    __slots__ = ("w", "r")

    def __init__(self):
        self.w = None
        self.r = {}


class T:
    def __init__(self, t, ap=None):
        self.t = t
        self.ap = ap if ap is not None else t
        self.whole = Trk()
        self.parts = {}

    def __getitem__(self, idx):
        return self.ap[idx]

    def p(self, key):
        return (self, key)


def _trks(x, for_write):
    if isinstance(x, tuple):
        t, key = x
        if key not in t.parts:
            t.parts[key] = Trk()
        return [t.whole, t.parts[key]], [t.parts[key]]
    return [x.whole] + list(x.parts.values()), [x.whole]


class Prog:
    NSLOT = 6

    def __init__(self):
        nc = bass.Bass("TRN2", target_bir_lowering=False)
        self.nc = nc
        self.E = dict(pe=nc.tensor, dve=nc.vector, act=nc.scalar, pool=nc.gpsimd, sp=nc.sync)
        self.sem = {}
        self.val = {}
        for e in ("pe", "dve", "act", "pool"):
            self.sem[e] = nc.semaphore("sem_" + e).__enter__()
            self.val[e] = 0
        self.slots = {}
        self.rr = {}
        for q in ("sp", "act", "pool"):
            self.slots[q] = []
            self.rr[q] = 0
            for i in range(self.NSLOT):
                k = f"dma_{q}{i}"
                self.sem[k] = nc.semaphore(k).__enter__()
                self.val[k] = 0
                self.slots[q].append(k)
        self.seen = {e: {} for e in self.E}
        self.n_ins = 0
        self._names = 0
        self.out_events = []
        self._stack = []

    def name(self, base):
        self._names += 1
        return f"{base}_{self._names}"

    def sb(self, shape, dt=F32, name="sb"):
        cm = self.nc.sbuf_tensor(self.name(name), list(shape), dt)
        t = T(cm.__enter__())
        self._stack.append(cm)
        return t

    def mark(self):
        return len(self._stack)

    def release_to(self, mark):
        self.barrier()
        while len(self._stack) > mark:
            self._stack.pop().__exit__(None, None, None)

    def barrier(self):
        for eng in self.E:
            for k in self.sem:
                if self.val[k] > 0:
                    self._wait(eng, (k, self.val[k]))

    def psum(self, shape, dt=F32, name="ps"):
        return T(self.nc.psum_tensor(self.name(name), list(shape), dt).__enter__())

    def dram(self, name, shape, dt=F32, kind="Internal"):
        return T(self.nc.dram_tensor(name, list(shape), dt, kind=kind).ap())

    def _wait(self, eng, ev):
        if ev is None:
            return
        key, v = ev
        if key == eng and eng == "pe":
            return
        if self.seen[eng].get(key, 0) >= v:
            return
        self.E[eng].wait_ge(self.sem[key], v)
        self.seen[eng][key] = v

    def _deps(self, eng, reads, writes):
        for x in reads:
            chk, _ = _trks(x, False)
            for tr in chk:
                self._wait(eng, tr.w)
        for x in writes:
            chk, _ = _trks(x, True)
            for tr in chk:
                self._wait(eng, tr.w)
                for k, v in tr.r.items():
                    self._wait(eng, (k, v))

    def _record(self, ev, reads, writes):
        for x in reads:
            _, upd = _trks(x, False)
            for tr in upd:
                tr.r[ev[0]] = ev[1]
        for x in writes:
            chk, upd = _trks(x, True)
            if not isinstance(x, tuple):
                x.parts.clear()
            for tr in upd:
                tr.w = ev
                tr.r = {}

    def op(self, eng, fn, reads=(), writes=()):
        self._deps(eng, reads, writes)
        ins = fn(self.E[eng])
        self.val[eng] += 1
        ins.then_inc(self.sem[eng], 1)
        ev = (eng, self.val[eng])
        self._record(ev, reads, writes)
        self.n_ins += 1
        return ev

    def dma(self, q, out, in_, reads=(), writes=(), **kw):
        self._deps(q, reads, writes)
        slot = self.slots[q][self.rr[q] % self.NSLOT]
        self.rr[q] += 1
        if self.val[slot] > 0:
            self._wait(q, (slot, self.val[slot]))
        ins = self.E[q].dma_start(out=out, in_=in_, **kw)
        self.val[slot] += 16
        ins.then_inc(self.sem[slot], 16)
        ev = (slot, self.val[slot])
        self._record(ev, reads, writes)
        self.n_ins += 1
        return ev

    def finish(self):
        for ev in self.out_events:
            self._wait("sp", ev)
        for k in self.sem:
            if self.val[k] > 0:
                self._wait("sp", (k, self.val[k]))

    def mm(self, ps, out_ap, lhsT, rhs, start, stop, reads, eng="pe"):
        return self.op("pe", lambda e: e.matmul(out_ap, lhsT, rhs, start=start, stop=stop),
                       reads=reads, writes=[ps])

    def act(self, out_t, out_ap, in_t, in_ap, func, bias=None, scale=1.0, extra_reads=(), accum_out=None):
        kw = {}
        if bias is not None:
            kw["bias"] = bias
        if accum_out is not None:
            kw["accum_out"] = accum_out
        return self.op("act", lambda e: e.activation(out=out_ap, in_=in_ap, func=func, scale=scale, **kw),
                       reads=[in_t] + list(extra_reads), writes=[out_t])

    def tt(self, eng, out_t, out_ap, a_t, a_ap, b_t, b_ap, op):
        return self.op(eng, lambda e: e.tensor_tensor(out=out_ap, in0=a_ap, in1=b_ap, op=op),
                       reads=[a_t, b_t], writes=[out_t])

    def ts(self, eng, out_t, out_ap, a_t, a_ap, s1, op0, s2=None, op1=None, extra_reads=()):
        if op1 is None:
            return self.op(eng, lambda e: e.tensor_scalar(out=out_ap, in0=a_ap, scalar1=s1, scalar2=None, op0=op0),
                           reads=[a_t] + list(extra_reads), writes=[out_t])
        return self.op(eng, lambda e: e.tensor_scalar(out=out_ap, in0=a_ap, scalar1=s1, scalar2=s2, op0=op0, op1=op1),
                       reads=[a_t] + list(extra_reads), writes=[out_t])

    def stt(self, out_t, out_ap, a_t, a_ap, scalar, b_t, b_ap, op0, op1, extra_reads=()):
        return self.op("dve", lambda e: e.scalar_tensor_tensor(out=out_ap, in0=a_ap, scalar=scalar, in1=b_ap, op0=op0, op1=op1),
                       reads=[a_t, b_t] + list(extra_reads), writes=[out_t])

    def copy(self, eng, out_t, out_ap, in_t, in_ap):
        if eng == "act":
            return self.op("act", lambda e: e.copy(out=out_ap, in_=in_ap), reads=[in_t], writes=[out_t])
        return self.op(eng, lambda e: e.tensor_copy(out=out_ap, in_=in_ap), reads=[in_t], writes=[out_t])

    def memset(self, eng, out_t, out_ap, v):
        return self.op(eng, lambda e: e.memset(out_ap, v), reads=[], writes=[out_t])


class Ctx:
    pass


def setup_common(P, NT_CTX, NT_LAT):
    K = Ctx()
    K.P = P
    K.NC = NT_CTX
    K.NL = NT_LAT
    K.NT = NT_CTX + NT_LAT
    nc = P.nc
    K.ps = [P.psum([128, 512], F32, name=f"bank{i}") for i in range(8)]
    K.ps_i = 0
    K.ps_reserved = set()
    K.ones = P.sb([128, 128], F32, "ones")
    P.memset("pool", K.ones, K.ones[:], 1.0)
    K.onesb = P.sb([128, 128], BF16, "onesb")
    P.memset("pool", K.onesb, K.onesb[:], 1.0)
    K.hT = P.dram("hT", [D, K.NT], F32)
    return K


def next_ps(K):
    while True:
        b = K.ps[K.ps_i % 8]
        K.ps_i += 1
        if (K.ps_i - 1) % 8 not in K.ps_reserved:
            return b


def dvec(ap1d, k):
    return ap1d.rearrange("(k p) -> p k", p=128)


def mods_all(K, layers, c_in, cctx_in, mod_w, mod_b, norm_mix, norm_ffn):
    P = K.P
    nc = P.nc
    res = {}
    pers = {}
    for l in layers:
        pers[l] = dict(M=P.sb([128, 48, 2], F32, f"modM{l}"), A_mix=P.sb([128, KC, 2], F32, f"A_mix{l}"),
                       A_ffn=P.sb([128, KC, 2], F32, f"A_ffn{l}"))
    mark = P.mark()
    craw = P.sb([128, KC, 2], F32, "craw")
    sT = P.sb([128, KC, 2], F32, "sT")
    modw_buf = [P.sb([128, KC, 256], F32, f"modw{i}") for i in range(2)]
    bT = P.sb([128, 48], F32, "modb")
    gm = P.sb([128, KC], F32, "gmix")
    gf = P.sb([128, KC], F32, "gffn")
    with nc.allow_non_contiguous_dma(reason="tiny vector load"):
        P.dma("sp", craw[:, :, 0], dvec(c_in.ap, KC), reads=[c_in], writes=[craw])
        P.dma("sp", craw[:, :, 1], dvec(cctx_in.ap, KC), reads=[cctx_in], writes=[craw])
    P.act(sT, sT[:], craw, craw[:], AF.Silu)
    wi = 0
    for l in layers:
        M = pers[l]["M"]
        with nc.allow_non_contiguous_dma(reason="tiny vector load"):
            P.dma("sp", bT[:], dvec(mod_b.ap[l], 48), reads=[mod_b], writes=[bT])
            P.dma("sp", gm[:], dvec(norm_mix.ap[l], KC), reads=[norm_mix], writes=[gm])
            P.dma("sp", gf[:], dvec(norm_ffn.ap[l], KC), reads=[norm_ffn], writes=[gf])
        for s in range(24):
            wb = modw_buf[wi % 2]
            wi += 1
            P.dma("sp", wb[:], mod_w.ap[l, :, s * 256:(s + 1) * 256].rearrange("(k p) n -> p k n", p=128),
                  reads=[mod_w], writes=[wb])
            ps = next_ps(K)
            for j in range(2):
                for k in range(KC):
                    P.mm(ps, ps[:, j * 2:j * 2 + 2], wb[:, k, j * 128:(j + 1) * 128], sT[:, k, :],
                         start=(k == 0), stop=(k == KC - 1), reads=[wb, sT])
            for j in range(2):
                jj = s * 2 + j
                P.ts("dve", M, M[:, jj, :], ps, ps[:, j * 2:j * 2 + 2], bT[:, jj:jj + 1], ALU.add, extra_reads=[bT])
        out = dict(pers[l])
        for nm, sc_i, g in (("mix", 1, gm), ("ffn", 4, gf)):
            A = pers[l]["A_" + nm]
            for col in range(2):
                P.stt(A, A[:, :, col], M, M[:, sc_i * 8:(sc_i + 1) * 8, col], 1.0, g, g[:], ALU.add, ALU.mult)
        out["B_mix"] = (lambda M: (lambda k, col: M[:, 0 * 8 + k, col:col + 1]))(M)
        out["G_mix"] = (lambda M: (lambda k, col: M[:, 2 * 8 + k, col:col + 1]))(M)
        out["B_ffn"] = (lambda M: (lambda k, col: M[:, 3 * 8 + k, col:col + 1]))(M)
        out["G_ffn"] = (lambda M: (lambda k, col: M[:, 5 * 8 + k, col:col + 1]))(M)
        res[l] = out
    P.release_to(mark)
    return res


def norm_tile(K, h_t, hc0, n, A, Bf, M, col, y_bf, yc0, y_f32=None, sq=None, rstd=None):
    P = K.P
    ps = next_ps(K)
    for k in range(KC):
        P.act(sq, sq[:, k, :n], h_t, h_t[:, k, hc0:hc0 + n], AF.Square)
    for k in range(KC):
        P.mm(ps, ps[:, :n], K.ones[:], sq[:, k, :n], start=(k == 0), stop=(k == KC - 1), reads=[K.ones, sq])
    P.ts("dve", rstd, rstd[:, :n], ps, ps[:, :n], 1.0 / D, ALU.mult, EPS, ALU.add)
    P.op("act", lambda e: e.sqrt(out=rstd[:, :n], in_=rstd[:, :n]), reads=[rstd], writes=[rstd])
    P.op("dve", lambda e: e.reciprocal(out=rstd[:, :n], in_=rstd[:, :n]), reads=[rstd], writes=[rstd])
    for k in range(KC):
        P.stt(sq, sq[:, k, :n], h_t, h_t[:, k, hc0:hc0 + n], A[:, k, col:col + 1], rstd, rstd[:, :n], ALU.mult, ALU.mult,
              extra_reads=[A])
        if y_f32 is not None:
            P.act(y_f32, y_f32[:, k, :n], sq, sq[:, k, :n], AF.Identity, bias=Bf(k, col), extra_reads=[M])
            P.copy("pool", y_bf, y_bf[:, k, yc0:yc0 + n], y_f32, y_f32[:, k, :n])
        else:
            P.act(y_bf, y_bf[:, k, yc0:yc0 + n], sq, sq[:, k, :n], AF.Identity, bias=Bf(k, col), extra_reads=[M])


def ffn_stage(K, mods, experts, router=None, tok_ranges=None, ST=1024, tag="ffn"):
    P = K.P
    nc = P.nc
    FF = experts[0][0].ap.shape[1]
    NE = len(experts)
    slabs = []
    f0 = 0
    while f0 < FF:
        fs = min(512, FF - f0)
        slabs.append((f0, fs))
        f0 += fs
    A, Bf, Gf, M = mods["A_ffn"], mods["B_ffn"], mods["G_ffn"], mods["M"]
    if tok_ranges is None:
        tok_ranges = [(0, K.NC, 1)] + [(K.NC + i * ST, min(ST, K.NL - i * ST), 0) for i in range((K.NL + ST - 1) // ST)]
    mark = P.mark()
    if True:
        B = Ctx()
        B.acc = P.sb([128, KC, ST], F32, "ffn_acc")
        B.y = P.sb([128, KC, ST], BF16, "ffn_y")
        B.sq = P.sb([128, KC, 512], F32, "ffn_sq")
        B.yf = P.sb([128, KC, 512], F32, "ffn_yf")
        B.rstd = P.sb([128, 512], F32, "ffn_rstd")
        B.hid = P.sb([128, 4, ST], BF16, "ffn_hid")
        B.wg = [P.sb([128, KC, 512], BF16, f"ffn_wg{i}") for i in range(2)]
        B.wu = [P.sb([128, KC, 512], BF16, f"ffn_wu{i}") for i in range(2)]
        B.wd = [P.sb([128, 4, D], BF16, f"ffn_wd{i}") for i in range(2)]
        B.sg = [P.sb([128, 512], F32, f"ffn_sg{i}") for i in range(2)]
        B.tmp = [P.sb([128, 512], F32, f"ffn_tmp{i}") for i in range(2)]
        B.gate_bc = P.sb([128, 8, ST], F32, "ffn_gatebc")
        B.rt = P.sb([128, KC, 8], F32, "ffn_router")
        B.lg = P.sb([128, 8], F32, "ffn_lg")
        B.mx = P.sb([128, 8], F32, "ffn_mx")
        B.gt = P.sb([128, 8], F32, "ffn_gt")
        B.m1 = P.sb([128, 8], F32, "ffn_m1")
        B.wv = P.sb([128, 4], F32, "ffn_wv")
        B.ident = P.sb([128, 128], F32, "ffn_ident")
        B.gcol = P.sb([128, 128], F32, "ffn_gcol")
        B.i = 0
        P.memset("pool", B.ident, B.ident[:], 0.0)
        P.op("pool", lambda e: e.affine_select(out=B.ident[:], in_=B.ident[:], pattern=[[-1, 128]], base=0,
                                                channel_multiplier=1, compare_op=ALU.not_equal, fill=1.0),
             reads=[B.ident], writes=[B.ident])
    if router is not None:
        P.dma("sp", B.rt[:], router.ap.rearrange("(k p) e -> p k e", p=128), reads=[router], writes=[B.rt])
    for (t0, n, col) in tok_ranges:
        ntile = (n + 511) // 512
        for j in range(ntile):
            c0 = j * 512
            w = min(512, n - c0)
            P.dma("sp", B.acc[:, :, c0:c0 + w], K.hT.ap[:, t0 + c0:t0 + c0 + w].rearrange("(k p) t -> p k t", p=128),
                  reads=[K.hT], writes=[B.acc])
            norm_tile(K, B.acc, c0, w, A, Bf, M, col, B.y, c0,
                      y_f32=(B.yf if router is not None else None), sq=B.sq, rstd=B.rstd)
            if router is not None:
                for q in range((w + 127) // 128):
                    qw = min(128, w - q * 128)
                    ps = next_ps(K)
                    for k in range(KC):
                        P.mm(ps, ps[:qw, 0:8], B.yf[:, k, q * 128:q * 128 + qw], B.rt[:, k, :],
                             start=(k == 0), stop=(k == KC - 1), reads=[B.yf, B.rt])
                    P.copy("dve", B.lg, B.lg[:qw, :], ps, ps[:qw, 0:8])
                    P.op("dve", lambda e: e.max(out=B.mx[:qw, :], in_=B.lg[:qw, :]), reads=[B.lg], writes=[B.mx])
                    P.tt("dve", B.wv, B.wv[:qw, 0:1], B.mx, B.mx[:qw, 1:2], B.mx, B.mx[:qw, 0:1], ALU.subtract)
                    P.act(B.wv, B.wv[:qw, 1:2], B.wv, B.wv[:qw, 0:1], AF.Exp)
                    P.ts("dve", B.wv, B.wv[:qw, 1:2], B.wv, B.wv[:qw, 1:2], 1.0, ALU.add)
                    P.op("dve", lambda e: e.reciprocal(out=B.wv[:qw, 2:3], in_=B.wv[:qw, 1:2]), reads=[B.wv], writes=[B.wv])
                    P.ts("dve", B.wv, B.wv[:qw, 3:4], B.wv, B.wv[:qw, 2:3], -1.0, ALU.mult, 1.0, ALU.add)
                    P.ts("dve", B.gt, B.gt[:qw, :], B.lg, B.lg[:qw, :], B.mx[:qw, 0:1], ALU.is_equal,
                         B.wv[:qw, 2:3], ALU.mult, extra_reads=[B.mx, B.wv])
                    P.ts("dve", B.m1, B.m1[:qw, :], B.lg, B.lg[:qw, :], B.mx[:qw, 1:2], ALU.is_equal,
                         B.wv[:qw, 3:4], ALU.mult, extra_reads=[B.mx, B.wv])
                    P.tt("dve", B.gt, B.gt[:qw, :], B.gt, B.gt[:qw, :], B.m1, B.m1[:qw, :], ALU.add)
                    for e_i in range(NE):
                        P.ts("dve", B.gcol, B.gcol[:qw, :], K.ones, K.ones[:qw, :], B.gt[:qw, e_i:e_i + 1], ALU.mult,
                             extra_reads=[B.gt])
                        ps2 = next_ps(K)
                        P.mm(ps2, ps2[:, :qw], B.gcol[:qw, :], B.ident[:qw, :qw], start=True, stop=True,
                             reads=[B.gcol, B.ident])
                        P.copy("act", B.gate_bc, B.gate_bc[:, e_i, c0 + q * 128:c0 + q * 128 + qw], ps2, ps2[:, :qw])
        for e_i, (wg, wu, wd) in enumerate(experts):
            for (f0, fs) in slabs:
                i = B.i % 2
                B.i += 1
                nfc = fs // 128
                P.dma("pool", B.wg[i][:, :, :fs], wg.ap[:, f0:f0 + fs].rearrange("(k p) f -> p k f", p=128),
                      reads=[wg], writes=[B.wg[i]])
                P.dma("pool", B.wu[i][:, :, :fs], wu.ap[:, f0:f0 + fs].rearrange("(k p) f -> p k f", p=128),
                      reads=[wu], writes=[B.wu[i]])
                P.dma("pool", B.wd[i][:, :nfc, :], wd.ap[f0:f0 + fs, :].rearrange("(c p) d -> p c d", p=128),
                      reads=[wd], writes=[B.wd[i]])
                for j in range(ntile):
                    c0 = j * 512
                    w = min(512, n - c0)
                    for fc in range(nfc):
                        pg = next_ps(K)
                        pu = next_ps(K)
                        for k in range(KC):
                            P.mm(pg, pg[:, :w], B.wg[i][:, k, fc * 128:(fc + 1) * 128], B.y[:, k, c0:c0 + w],
                                 start=(k == 0), stop=(k == KC - 1), reads=[B.wg[i], B.y])
                        for k in range(KC):
                            P.mm(pu, pu[:, :w], B.wu[i][:, k, fc * 128:(fc + 1) * 128], B.y[:, k, c0:c0 + w],
                                 start=(k == 0), stop=(k == KC - 1), reads=[B.wu[i], B.y])
                        sg = B.sg[(j * 4 + fc) % 2]
                        P.act(sg, sg[:, :w], pg, pg[:, :w], AF.Silu)
                        P.tt("dve", B.hid, B.hid[:, fc, c0:c0 + w], sg, sg[:, :w], pu, pu[:, :w], ALU.mult)
                    for oc in range(KC):
                        po = next_ps(K)
                        for fc in range(nfc):
                            P.mm(po, po[:, :w], B.wd[i][:, fc, oc * 128:(oc + 1) * 128], B.hid[:, fc, c0:c0 + w],
                                 start=(fc == 0), stop=(fc == nfc - 1), reads=[B.wd[i], B.hid])
                        if router is not None:
                            tmp = B.tmp[oc % 2]
                            P.stt(tmp, tmp[:, :w], po, po[:, :w], Gf(oc, col), B.gate_bc, B.gate_bc[:, e_i, c0:c0 + w],
                                  ALU.mult, ALU.mult, extra_reads=[M])
                            P.tt("pool", B.acc, B.acc[:, oc, c0:c0 + w], B.acc, B.acc[:, oc, c0:c0 + w], tmp, tmp[:, :w], ALU.add)
                        else:
                            P.stt(B.acc, B.acc[:, oc, c0:c0 + w], po, po[:, :w], Gf(oc, col), B.acc, B.acc[:, oc, c0:c0 + w],
                                  ALU.mult, ALU.add, extra_reads=[M])
        for j in range(ntile):
            c0 = j * 512
            w = min(512, n - c0)
            P.dma("sp", K.hT.ap[:, t0 + c0:t0 + c0 + w].rearrange("(k p) t -> p k t", p=128), B.acc[:, :, c0:c0 + w],
                  reads=[B.acc], writes=[K.hT])
    if hasattr(K, "dbg") and router is not None:
        P.dma("sp", K.dbg["gate_bc"].ap[:, :, :], B.gate_bc[:, :, :], reads=[B.gate_bc], writes=[K.dbg["gate_bc"]])
        P.dma("sp", K.dbg["ident"].ap[:, :], B.ident[:, :], reads=[B.ident], writes=[K.dbg["ident"]])
        for i_, t_ in enumerate((B.lg, B.mx, B.gt, B.m1)):
            P.dma("sp", K.dbg["small"].ap[:, i_ * 8:(i_ + 1) * 8], t_[:, :], reads=[t_], writes=[K.dbg["small"]])
        P.dma("sp", K.dbg["small"].ap[:, 32:36], B.wv[:, :], reads=[B.wv], writes=[K.dbg["small"]])
    P.release_to(mark)


def inproj_stage(K, mods, w_in, zT, segs=None):
    P = K.P
    nc = P.nc
    if segs is None:
        segs = [(0, w_in.ap.shape[1])]
    N = sum(n for _, n in segs)
    A, Bf, M = mods["A_mix"], mods["B_mix"], mods["M"]
    mark = P.mark()
    nch = (N + 127) // 128
    wst = [P.sb([128, KC, 512], F32, f"ip_wst{i}") for i in range(2)]
    wb = P.sb([128, KC, N], BF16, "ip_w")
    o0 = 0
    si = 0
    for (c0, ncol) in segs:
        for s0 in range(0, ncol, 512):
            cw = min(512, ncol - s0)
            st = wst[si % 2]
            si += 1
            with nc.allow_non_contiguous_dma(reason="weight column segments"):
                P.dma("sp", st[:, :, :cw], w_in.ap[:, c0 + s0:c0 + s0 + cw].rearrange("(k p) n -> p k n", p=128),
                      reads=[w_in], writes=[st])
            P.copy("pool", wb, wb[:, :, o0 + s0:o0 + s0 + cw], st, st[:, :, :cw])
        o0 += ncol
    h = [P.sb([128, KC, 512], F32, f"ip_h{i}") for i in range(2)]
    y = [P.sb([128, KC, 512], BF16, f"ip_y{i}") for i in range(2)]
    sq = P.sb([128, KC, 512], F32, "ip_sq")
    rstd = P.sb([128, 512], F32, "ip_rstd")
    zo = [P.sb([128, 512], F32, f"ip_zo{i}") for i in range(4)]
    ranges = []
    if K.NC:
        ranges.append((0, K.NC, 1))
    ranges += [(K.NC + i * 512, min(512, K.NL - i * 512), 0) for i in range((K.NL + 511) // 512)]
    for it, (t0, n, col) in enumerate(ranges):
        hb, yb = h[it % 2], y[it % 2]
        P.dma("sp", hb[:, :, :n], K.hT.ap[:, t0:t0 + n].rearrange("(k p) t -> p k t", p=128), reads=[K.hT], writes=[hb])
        norm_tile(K, hb, 0, n, A, Bf, M, col, yb, 0, sq=sq, rstd=rstd)
        for c in range(nch):
            m = min(128, N - c * 128)
            ps = next_ps(K)
            for k in range(KC):
                P.mm(ps, ps[:m, :n], wb[:, k, c * 128:c * 128 + m], yb[:, k, :n], start=(k == 0), stop=(k == KC - 1),
                     reads=[wb, yb])
            o = zo[c % 4]
            P.copy("act" if c % 2 else "dve", o, o[:m, :n], ps, ps[:m, :n])
            P.dma("pool" if c % 2 else "sp", zT.ap[c * 128:c * 128 + m, t0:t0 + n], o[:m, :n], reads=[o], writes=[zT])
    P.release_to(mark)


def outproj_stage(K, mods, w_out, mixT):
    P = K.P
    Gm, M = mods["G_mix"], mods["M"]
    mark = P.mark()
    wst = [P.sb([128, KC, 512], F32, f"op_wst{i}") for i in range(2)]
    wb = P.sb([128, KC, D], BF16, "op_w")
    for s in range(2):
        st = wst[s % 2]
        P.dma("sp", st[:], w_out.ap[:, s * 512:(s + 1) * 512].rearrange("(k p) n -> p k n", p=128), reads=[w_out], writes=[st])
        P.copy("pool", wb, wb[:, :, s * 512:(s + 1) * 512], st, st[:])
    mx = [P.sb([128, KC, 512], F32, f"op_m{i}") for i in range(2)]
    mb = [P.sb([128, KC, 512], BF16, f"op_mb{i}") for i in range(2)]
    h = [P.sb([128, KC, 512], F32, f"op_h{i}") for i in range(2)]
    ranges = [(0, K.NC, 1)] + [(K.NC + i * 512, min(512, K.NL - i * 512), 0) for i in range((K.NL + 511) // 512)]
    for it, (t0, n, col) in enumerate(ranges):
        m_, b_, h_ = mx[it % 2], mb[it % 2], h[it % 2]
        P.dma("sp", m_[:, :, :n], mixT.ap[:, t0:t0 + n].rearrange("(k p) t -> p k t", p=128), reads=[mixT], writes=[m_])
        P.dma("sp", h_[:, :, :n], K.hT.ap[:, t0:t0 + n].rearrange("(k p) t -> p k t", p=128), reads=[K.hT], writes=[h_])
        P.copy("pool", b_, b_[:, :, :n], m_, m_[:, :, :n])
        for c in range(KC):
            ps = next_ps(K)
            for k in range(KC):
                P.mm(ps, ps[:, :n], wb[:, k, c * 128:(c + 1) * 128], b_[:, k, :n], start=(k == 0), stop=(k == KC - 1),
                     reads=[wb, b_])
            P.stt(h_, h_[:, c, :n], ps, ps[:, :n], Gm(c, col), h_, h_[:, c, :n], ALU.mult, ALU.add, extra_reads=[M])
        P.dma("pool", K.hT.ap[:, t0:t0 + n].rearrange("(k p) t -> p k t", p=128), h_[:, :, :n], reads=[h_], writes=[K.hT])
    P.release_to(mark)


def final_norm_stage(K, gain, outT, t0_all, n_all):
    P = K.P
    nc = P.nc
    mark = P.mark()
    g = P.sb([128, KC], F32, "fn_g")
    with nc.allow_non_contiguous_dma(reason="tiny vector load"):
        P.dma("sp", g[:], dvec(gain.ap, KC), reads=[gain], writes=[g])
    h = [P.sb([128, KC, 512], F32, f"fn_h{i}") for i in range(2)]
    o = [P.sb([128, KC, 512], F32, f"fn_o{i}") for i in range(2)]
    sq = P.sb([128, KC, 512], F32, "fn_sq")
    rstd = P.sb([128, 512], F32, "fn_rstd")
    for it in range((n_all + 511) // 512):
        n = min(512, n_all - it * 512)
        t0 = t0_all + it * 512
        hb, ob = h[it % 2], o[it % 2]
        P.dma("sp", hb[:, :, :n], K.hT.ap[:, t0:t0 + n].rearrange("(k p) t -> p k t", p=128), reads=[K.hT], writes=[hb])
        ps = next_ps(K)
        for k in range(KC):
            P.act(sq, sq[:, k, :n], hb, hb[:, k, :n], AF.Square)
        for k in range(KC):
            P.mm(ps, ps[:, :n], K.ones[:], sq[:, k, :n], start=(k == 0), stop=(k == KC - 1), reads=[K.ones, sq])
        P.ts("dve", rstd, rstd[:, :n], ps, ps[:, :n], 1.0 / D, ALU.mult, EPS, ALU.add)
        P.op("act", lambda e: e.sqrt(out=rstd[:, :n], in_=rstd[:, :n]), reads=[rstd], writes=[rstd])
        P.op("dve", lambda e: e.reciprocal(out=rstd[:, :n], in_=rstd[:, :n]), reads=[rstd], writes=[rstd])
        for k in range(KC):
            P.stt(ob, ob[:, k, :n], hb, hb[:, k, :n], g[:, k:k + 1], rstd, rstd[:, :n], ALU.mult, ALU.mult, extra_reads=[g])
        ev = P.dma("pool", outT.ap[:, it * 512:it * 512 + n].rearrange("(k p) t -> p k t", p=128), ob[:, :, :n],
                   reads=[ob], writes=[outT])
        P.out_events.append(ev)
    P.release_to(mark)


MAGIC = 12582912.0
TWO_PI = 2.0 * math.pi


def host_consts():
    c = {}
    c["k_tau"] = np.tile(np.arange(128, dtype=np.float32)[None, :], (128, 1))
    q = np.arange(128)
    mC = np.zeros((128, 4, 2, 64), np.float32)
    for jj in range(4):
        for gl in range(2):
            sel = (q // 32 == jj) & ((q % 32) // 16 == gl)
            mC[sel, jj, gl, :] = 1.0
    c["k_maskC"] = mC.reshape(128, 512)
    mB = np.zeros((128, 4, 128), np.float32)
    for jj in range(4):
        for gl in range(2):
            rows = np.arange(64 * gl, 64 * gl + 64)
            cols = np.arange(32 * jj + 16 * gl, 32 * jj + 16 * gl + 16)
            mB[np.ix_(rows, [jj], cols)] = 1.0
    c["k_maskB"] = mB.reshape(128, 512)
    c["k_ident"] = np.eye(128, dtype=np.float32)
    return c


def range_reduce(P, out_t, out_ap, in_t, in_ap, tmp_t, tmp_ap, shift=0.0):
    P.ts("dve", tmp_t, tmp_ap, in_t, in_ap, 1.0 / TWO_PI, ALU.mult, shift / TWO_PI + MAGIC, ALU.add)
    P.ts("dve", tmp_t, tmp_ap, tmp_t, tmp_ap, MAGIC, ALU.subtract, -TWO_PI, ALU.mult)
    P.stt(out_t, out_ap, in_t, in_ap, shift, tmp_t, tmp_ap, ALU.add, ALU.add)


def s5_stage(K, i, I, zT, mixT, C, u_row0=1536, out_row0=512):
    P = K.P
    nc = P.nc
    TT = 128
    mark = P.mark()
    tau, maskB, maskC, ident = C["k_tau"], C["k_maskB"], C["k_maskC"], C["k_ident"]
    yS = K.yS5
    sb = P.sb
    lr = sb([128, 16], F32, "s5_lr"); li = sb([128, 16], F32, "s5_li"); dtv = sb([128, 16], F32, "s5_dt")
    wdt = sb([128, 16], F32, "s5_wdt"); rdt = sb([128, 16], F32, "s5_rdt"); nrdt = sb([128, 16], F32, "s5_nrdt")
    Er = sb([128, 16, TT], F32, "s5_Er"); Ei = sb([128, 16, TT], F32, "s5_Ei")
    Gr = sb([128, 16, TT], F32, "s5_Gr"); Gi = sb([128, 16, TT], F32, "s5_Gi")
    Hr = sb([128, 16], F32, "s5_Hr"); Hi = sb([128, 16], F32, "s5_Hi")
    BTr = sb([128, 16, 128], F32, "s5_BTr"); BTi = sb([128, 16, 128], F32, "s5_BTi")
    CTr = sb([128, 16, 128], F32, "s5_CTr"); CTin = sb([128, 16, 128], F32, "s5_CTin")
    bre = sb([128, 16, 16], F32, "s5_bre"); bim = sb([128, 16, 16], F32, "s5_bim")
    bbr = sb([128, 16, 16], F32, "s5_bbr"); bbi = sb([128, 16, 16], F32, "s5_bbi")
    cre = sb([128, 4, 64], F32, "s5_cre"); cim = sb([128, 4, 64], F32, "s5_cim")
    ex = sb([128, 128], F32, "s5_ex")
    th = sb([128, TT], F32, "s5_th"); tmp = sb([128, TT], F32, "s5_tmp"); sn = sb([128, TT], F32, "s5_sn")
    cs = sb([128, TT], F32, "s5_cs"); mg = sb([128, TT], F32, "s5_mg")
    s16 = [sb([128, 16], F32, f"s5_s16_{k}") for k in range(8)]
    u = [sb([128, 4, TT], F32, f"s5_u{k}") for k in range(2)]
    SL = 2
    t1s = [sb([128, 4, TT], F32, f"s5_t1{k}") for k in range(SL)]; t2s = [sb([128, 4, TT], F32, f"s5_t2{k}") for k in range(SL)]
    t3s = [sb([128, 4, TT], F32, f"s5_t3{k}") for k in range(SL)]; t4s = [sb([128, 4, TT], F32, f"s5_t4{k}") for k in range(SL)]
    wrs = [sb([128, 4, TT], F32, f"s5_wr{k}") for k in range(SL)]; wis = [sb([128, 4, TT], F32, f"s5_wi{k}") for k in range(SL)]
    zrs = [sb([128, 4, TT], F32, f"s5_zr{k}") for k in range(SL)]; zis = [sb([128, 4, TT], F32, f"s5_zi{k}") for k in range(SL)]
    xrs = [sb([128, 4, TT], F32, f"s5_xr{k}") for k in range(SL)]; xis = [sb([128, 4, TT], F32, f"s5_xi{k}") for k in range(SL)]
    cars = [sb([128, 4], F32, f"s5_car{k}") for k in range(4)]; cais = [sb([128, 4], F32, f"s5_cai{k}") for k in range(4)]
    c4s = [[sb([128, 4], F32, f"s5_c4_{q}{k}") for k in range(2)] for q in range(SL)]
    g1s = [sb([128, TT], F32, f"s5_g1{k}") for k in range(SL)]; g2s = [sb([128, TT], F32, f"s5_g2{k}") for k in range(SL)]
    yo = [sb([128, TT], F32, f"s5_yo{k}") for k in range(2)]
    yf = sb([128, 4, TT], F32, "s5_yf")
    vq = [sb([128, TT], F32, f"s5_v{k}") for k in range(4)]
    oo = [sb([128, TT], F32, f"s5_oo{k}") for k in range(2)]
    wglu = sb([128, 4, 512], F32, "s5_wglu"); dsk = sb([128, 4], F32, "s5_dsk")
    onesT = sb([128, TT], F32, "s5_ones")
    P.memset("pool", onesT, onesT[:], 1.0)
    P.dma("sp", wglu[:], I["s5_w_glu"].ap[i].rearrange("(k p) n -> p k n", p=128), reads=[I["s5_w_glu"]], writes=[wglu])
    with nc.allow_non_contiguous_dma(reason="tiny vector load"):
        P.dma("sp", dsk[:], I["s5_d"].ap[i].rearrange("(k p) -> p k", p=128), reads=[I["s5_d"]], writes=[dsk])

    def sincos(angle_t, angle_ap, sin_t, sin_ap, cos_t, cos_ap, tmp_a, tmp_a_ap, tmp_b, tmp_b_ap):
        range_reduce(P, tmp_b, tmp_b_ap, angle_t, angle_ap, tmp_a, tmp_a_ap, 0.0)
        P.act(sin_t, sin_ap, tmp_b, tmp_b_ap, AF.Sin)
        range_reduce(P, tmp_b, tmp_b_ap, angle_t, angle_ap, tmp_a, tmp_a_ap, math.pi / 2)
        P.act(cos_t, cos_ap, tmp_b, tmp_b_ap, AF.Sin)

    chunks_ctx = [(k * TT, 1) for k in range(K.NC // TT)]
    chunks_lat = [(K.NC + k * TT, 0) for k in range(K.NL // TT)]
    for d in range(2):
        rev = (d == 1)
        with nc.allow_non_contiguous_dma(reason="small parameter loads"):
            P.dma("sp", lr[:], I["s5_a_re"].ap[i, d].rearrange("g p -> (g p)").rearrange("(j q) -> q j", q=128),
                  reads=[I["s5_a_re"]], writes=[lr])
            P.dma("sp", li[:], I["s5_a_im"].ap[i, d].rearrange("g p -> (g p)").rearrange("(j q) -> q j", q=128),
                  reads=[I["s5_a_im"]], writes=[li])
            for gl in range(2):
                P.dma("sp", dtv[64 * gl:64 * gl + 64, :],
                      I["s5_log_dt"].ap[i, d].rearrange("(j g) -> g j", g=2)[gl:gl + 1, :].partition_broadcast(64),
                      reads=[I["s5_log_dt"]], writes=[dtv])
            P.dma("sp", bre[:], I["s5_b_re"].ap[i, d].rearrange("g p c -> (g p) c").rearrange("(j q) c -> q j c", q=128),
                  reads=[I["s5_b_re"]], writes=[bre])
            P.dma("sp", bim[:], I["s5_b_im"].ap[i, d].rearrange("g p c -> (g p) c").rearrange("(j q) c -> q j c", q=128),
                  reads=[I["s5_b_im"]], writes=[bim])
            P.dma("sp", cre[:], I["s5_c_re"].ap[i, d].rearrange("g c p -> (g c) p").rearrange("(k q) p -> q k p", q=128),
                  reads=[I["s5_c_re"]], writes=[cre])
            P.dma("sp", cim[:], I["s5_c_im"].ap[i, d].rearrange("g c p -> (g c) p").rearrange("(k q) p -> q k p", q=128),
                  reads=[I["s5_c_im"]], writes=[cim])
        P.ts("dve", lr, lr[:], lr, lr[:], -1e-4, ALU.min)
        P.act(dtv, dtv[:], dtv, dtv[:], AF.Exp)
        P.tt("dve", wdt, wdt[:], li, li[:], dtv, dtv[:], ALU.mult)
        P.tt("dve", rdt, rdt[:], lr, lr[:], dtv, dtv[:], ALU.mult)
        P.ts("dve", nrdt, nrdt[:], rdt, rdt[:], -1.0, ALU.mult)
        ar, ai, a_s, a_c, a_m, ta, tb, den = s16
        sincos(wdt, wdt[:], a_s, a_s[:], a_c, a_c[:], ta, ta[:], tb, tb[:])
        P.act(a_m, a_m[:], rdt, rdt[:], AF.Exp)
        P.tt("dve", ar, ar[:], a_m, a_m[:], a_c, a_c[:], ALU.mult)
        P.tt("dve", ai, ai[:], a_m, a_m[:], a_s, a_s[:], ALU.mult)
        P.ts("dve", ar, ar[:], ar, ar[:], -1.0, ALU.add)
        P.tt("dve", den, den[:], lr, lr[:], lr, lr[:], ALU.mult)
        P.tt("dve", ta, ta[:], li, li[:], li, li[:], ALU.mult)
        P.tt("dve", den, den[:], den, den[:], ta, ta[:], ALU.add)
        P.op("dve", lambda e: e.reciprocal(out=den[:], in_=den[:]), reads=[den], writes=[den])
        P.tt("dve", ta, ta[:], ar, ar[:], lr, lr[:], ALU.mult)
        P.tt("dve", tb, tb[:], ai, ai[:], li, li[:], ALU.mult)
        P.tt("dve", ta, ta[:], ta, ta[:], tb, tb[:], ALU.add)
        P.tt("dve", a_c, a_c[:], ta, ta[:], den, den[:], ALU.mult)
        P.tt("dve", ta, ta[:], ai, ai[:], lr, lr[:], ALU.mult)
        P.tt("dve", tb, tb[:], ar, ar[:], li, li[:], ALU.mult)
        P.tt("dve", ta, ta[:], ta, ta[:], tb, tb[:], ALU.subtract)
        P.tt("dve", a_s, a_s[:], ta, ta[:], den, den[:], ALU.mult)
        P.ts("dve", a_m, a_m[:], a_s, a_s[:], -1.0, ALU.mult)
        coef_r, coef_i, ncoef_i = a_c, a_s, a_m
        P.ts("dve", ta, ta[:], wdt, wdt[:], float(TT), ALU.mult)
        sincos(ta, ta[:], Hi, Hi[:], Hr, Hr[:], tb, tb[:], den, den[:])
        P.act(ta, ta[:], rdt, rdt[:], AF.Exp, scale=float(TT))
        P.tt("dve", Hr, Hr[:], Hr, Hr[:], ta, ta[:], ALU.mult)
        P.tt("dve", Hi, Hi[:], Hi, Hi[:], ta, ta[:], ALU.mult)
        for j in range(16):
            jj = j % 4
            cc = j // 4
            P.ts("dve", th, th[:], tau, tau[:], wdt[:, j:j + 1], ALU.mult, extra_reads=[wdt])
            sincos(th, th[:], sn, sn[:], cs, cs[:], tmp, tmp[:], mg, mg[:])
            P.act(mg, mg[:], tau, tau[:], AF.Exp, scale=rdt[:, j:j + 1], extra_reads=[rdt])
            P.tt("dve", Gr, Gr[:, j, :], mg, mg[:], cs, cs[:], ALU.mult)
            P.tt("dve", Gi, Gi[:, j, :], mg, mg[:], sn, sn[:], ALU.mult)
            P.act(mg, mg[:], tau, tau[:], AF.Exp, scale=nrdt[:, j:j + 1], extra_reads=[nrdt])
            P.tt("dve", Er, Er[:, j, :], mg, mg[:], cs, cs[:], ALU.mult)
            P.stt(Ei, Ei[:, j, :], mg, mg[:], -1.0, sn, sn[:], ALU.mult, ALU.mult)
            P.ts("dve", bbr, bbr[:, j, :], bre, bre[:, j, :], coef_r[:, j:j + 1], ALU.mult, extra_reads=[coef_r])
            P.stt(bbr, bbr[:, j, :], bim, bim[:, j, :], ncoef_i[:, j:j + 1], bbr, bbr[:, j, :], ALU.mult, ALU.add,
                  extra_reads=[ncoef_i])
            P.ts("dve", bbi, bbi[:, j, :], bim, bim[:, j, :], coef_r[:, j:j + 1], ALU.mult, extra_reads=[coef_r])
            P.stt(bbi, bbi[:, j, :], bre, bre[:, j, :], coef_i[:, j:j + 1], bbi, bbi[:, j, :], ALU.mult, ALU.add,
                  extra_reads=[coef_i])
            for (src, dst, neg) in ((bbr, BTr, False), (bbi, BTi, False)):
                P.tt("dve", ex, ex[:].rearrange("q (a c) -> q a c", c=16),
                     src, src[:, j, :].unsqueeze(1).broadcast_to([128, 8, 16]),
                     maskB, maskB[:, jj * 128:(jj + 1) * 128].rearrange("q (a c) -> q a c", c=16), ALU.mult)
                ps = next_ps(K)
                P.op("pe", lambda e: e.transpose(ps[:, 0:128], ex[:], ident[:]), reads=[ex, ident], writes=[ps])
                P.copy("act", dst, dst[:, j, :], ps, ps[:, 0:128])
            for (src, dst, neg) in ((cre, CTr, False), (cim, CTin, True)):
                P.tt("dve", ex, ex[:].rearrange("q (a p) -> q a p", p=64),
                     src, src[:, cc, :].unsqueeze(1).broadcast_to([128, 2, 64]),
                     maskC, maskC[:, jj * 128:(jj + 1) * 128].rearrange("q (a p) -> q a p", p=64), ALU.mult)
                ps = next_ps(K)
                P.op("pe", lambda e: e.transpose(ps[:, 0:128], ex[:], ident[:]), reads=[ex, ident], writes=[ps])
                if neg:
                    P.ts("dve", dst, dst[:, j, :], ps, ps[:, 0:128], -1.0, ALU.mult)
                else:
                    P.copy("act", dst, dst[:, j, :], ps, ps[:, 0:128])
        for q4 in range(4):
            P.memset("pool", cars[q4], cars[q4][:], 0.0)
            P.memset("pool", cais[q4], cais[q4][:], 0.0)
        order = (chunks_ctx + chunks_lat) if not rev else (chunks_ctx[::-1] + chunks_lat[::-1])
        K.ps_reserved = {0, 1, 2, 3, 4, 5}
        for ci_, (t0, is_ctx) in enumerate(order):
            ub = u[ci_ % 2]
            P.dma("sp", ub[:], zT.ap[u_row0:u_row0 + 512, t0:t0 + TT].rearrange("(k q) t -> q k t", q=128),
                  reads=[zT], writes=[ub])
            if rev:
                P.dma("sp", yf[:], yS.ap[:, t0:t0 + TT].rearrange("(k q) t -> q k t", q=128), reads=[yS], writes=[yf])

            def quad(cc, ub=ub, t0=t0):
                s_ = cc % SL
                t1, t2, t3, t4, wr, wi, zr, zi, xr, xi = (t1s[s_], t2s[s_], t3s[s_], t4s[s_], wrs[s_], wis[s_], zrs[s_],
                                                           zis[s_], xrs[s_], xis[s_])
                car, cai, c4, g1, g2 = cars[cc], cais[cc], c4s[s_], g1s[s_], g2s[s_]
                pa, pb, py = K.ps[3 * s_], K.ps[3 * s_ + 1], K.ps[3 * s_ + 2]
                urhs = ub[:, cc, ::-1] if rev else ub[:, cc, :]
                for jj in range(4):
                    j = 4 * cc + jj
                    P.mm(pa, pa[:, jj * TT:(jj + 1) * TT], BTr[:, j, :], urhs, True, True, reads=[BTr, ub])
                    P.mm(pb, pb[:, jj * TT:(jj + 1) * TT], BTi[:, j, :], urhs, True, True, reads=[BTi, ub])
                yield
                Wr = pa[:, :].rearrange("q (a t) -> q a t", t=TT)
                Wi = pb[:, :].rearrange("q (a t) -> q a t", t=TT)
                sl = slice(4 * cc, 4 * cc + 4)
                P.tt("dve", t1, t1[:], pa, Wr, Er, Er[:, sl, :], ALU.mult)
                P.tt("dve", t2, t2[:], pb, Wi, Ei, Ei[:, sl, :], ALU.mult)
                P.tt("pool", wr, wr[:], t1, t1[:], t2, t2[:], ALU.subtract)
                P.tt("dve", t3, t3[:], pa, Wr, Ei, Ei[:, sl, :], ALU.mult)
                P.tt("dve", t4, t4[:], pb, Wi, Er, Er[:, sl, :], ALU.mult)
                P.tt("pool", wi, wi[:], t3, t3[:], t4, t4[:], ALU.add)
                yield
                for jj in range(4):
                    P.op("dve", lambda e: e.tensor_tensor_scan(out=zr[:, jj, :], data0=onesT[:], data1=wr[:, jj, :],
                                                               initial=car[:, jj:jj + 1], op0=ALU.mult, op1=ALU.add),
                         reads=[onesT, wr, car], writes=[zr])
                    P.op("dve", lambda e: e.tensor_tensor_scan(out=zi[:, jj, :], data0=onesT[:], data1=wi[:, jj, :],
                                                               initial=cai[:, jj:jj + 1], op0=ALU.mult, op1=ALU.add),
                         reads=[onesT, wi, cai], writes=[zi])
                yield
                P.tt("dve", t1, t1[:], zr, zr[:], Gr, Gr[:, sl, :], ALU.mult)
                P.tt("pool", t2, t2[:], zi, zi[:], Gi, Gi[:, sl, :], ALU.mult)
                P.tt("pool", t4, t4[:], zr, zr[:], Gi, Gi[:, sl, :], ALU.mult)
                P.tt("dve", t3, t3[:], zi, zi[:], Gr, Gr[:, sl, :], ALU.mult)
                P.tt("dve", xr, xr[:], t1, t1[:], t2, t2[:], ALU.subtract)
                P.tt("pool", xi, xi[:], t3, t3[:], t4, t4[:], ALU.add)
                zlr = zr[:, :, TT - 1]
                zli = zi[:, :, TT - 1]
                P.tt("dve", c4[0], c4[0][:], zr, zlr, Hr, Hr[:, sl], ALU.mult)
                P.tt("dve", c4[1], c4[1][:], zi, zli, Hi, Hi[:, sl], ALU.mult)
                P.tt("dve", car, car[:], c4[0], c4[0][:], c4[1], c4[1][:], ALU.subtract)
                P.tt("dve", c4[0], c4[0][:], zi, zli, Hr, Hr[:, sl], ALU.mult)
                P.tt("dve", c4[1], c4[1][:], zr, zlr, Hi, Hi[:, sl], ALU.mult)
                P.tt("dve", cai, cai[:], c4[0], c4[0][:], c4[1], c4[1][:], ALU.add)
                yield
                for jj in range(4):
                    j = 4 * cc + jj
                    P.mm(py, py[:, :TT], CTr[:, j, :], xr[:, jj, :], jj == 0, False, reads=[CTr, xr])
                    P.mm(py, py[:, :TT], CTin[:, j, :], xi[:, jj, :], False, jj == 3, reads=[CTin, xi])
                yield
                if not rev:
                    yb = yo[cc % 2]
                    P.copy("act", yb, yb[:], py, py[:, :TT])
                    P.dma("pool", yS.ap[128 * cc:128 * cc + 128, t0:t0 + TT], yb[:], reads=[yb], writes=[yS])
                else:
                    P.tt("dve", g1, g1[:], py, py[:, :TT][:, ::-1], yf, yf[:, cc, :], ALU.add)
                    P.stt(g1, g1[:], ub, ub[:, cc, :], dsk[:, cc:cc + 1], g1, g1[:], ALU.mult, ALU.add, extra_reads=[dsk])
                    P.tt("pool", g2, g2[:], g1, g1[:], g1, g1[:], ALU.mult)
                    P.ts("dve", g2, g2[:], g2, g2[:], 0.044715, ALU.mult, 1.0, ALU.add)
                    P.tt("dve", g2, g2[:], g2, g2[:], g1, g1[:], ALU.mult)
                    P.act(g2, g2[:], g2, g2[:], AF.Sigmoid, scale=1.5957691216057308)
                    P.tt("dve", vq[cc], vq[cc][:], g1, g1[:], g2, g2[:], ALU.mult)
                yield

            run_interleaved((quad(cc) for cc in range(4)), width=SL)
            if rev:
                for mc in range(4):
                    pg = next_ps(K)
                    for kc in range(4):
                        P.mm(pg, pg[:, :TT], wglu[:, kc, mc * 128:(mc + 1) * 128], vq[kc][:], kc == 0, kc == 3,
                             reads=[wglu, vq[kc]])
                    ob = oo[mc % 2]
                    P.act(ob, ob[:], pg, pg[:, :TT], AF.Sigmoid)
                    P.tt("dve", ob, ob[:], ob, ob[:], vq[mc], vq[mc][:], ALU.mult)
                    P.dma("pool", mixT.ap[out_row0 + 128 * mc:out_row0 + 128 * mc + 128, t0:t0 + TT], ob[:],
                          reads=[ob], writes=[mixT])
        K.ps_reserved = set()
    P.release_to(mark)


HY_BANDS = 16
HY_EMB = 33


def hyena_consts(L):
    N = 2 * L
    N1 = N // 128
    R = L // 128
    c = {}
    f64 = np.float64
    n1 = np.arange(R, dtype=f64)[:, None]; k1 = np.arange(N1, dtype=f64)[None, :]
    a = 2 * np.pi * n1 * k1 / N1
    c["F1c"] = np.cos(a); c["F1ns"] = -np.sin(a)
    n2 = np.arange(128, dtype=f64)[:, None]
    a = 2 * np.pi * n2 * k1 / N
    c["Twc"] = np.cos(a); c["Tws"] = np.sin(a)
    c["TwcT"] = np.cos(a).T.copy(); c["TwsT"] = np.sin(a).T.copy()
    k1c = np.arange(N1, dtype=f64)[:, None]; n1r = np.arange(R, dtype=f64)[None, :]
    a = 2 * np.pi * k1c * n1r / N1
    c["I2c"] = np.cos(a) / N; c["I2ns"] = -np.sin(a) / N
    t = np.arange(L, dtype=np.float32)
    t01 = t / np.float32(L)
    bands = np.linspace(1e-4, HY_BANDS - 1, HY_BANDS, dtype=np.float32)
    ang = (np.float32(2.0 * math.pi / L) * t[:, None]) * bands[None, :]
    feats = np.concatenate([t01[:, None], np.cos(ang), -np.sin(ang)], axis=-1)
    c["featsT"] = feats.T.copy()
    return {f"hy{L}_{k}": np.ascontiguousarray(v, dtype=np.float32) for k, v in c.items()}


def hyena_shared_consts():
    n2 = np.arange(128, dtype=np.float64)[:, None]; k2 = np.arange(128, dtype=np.float64)[None, :]
    a = 2 * np.pi * n2 * k2 / 128
    return {"hy_F3c": np.cos(a).astype(np.float32), "hy_F3s": np.sin(a).astype(np.float32),
            "hy_F3ns": (-np.sin(a)).astype(np.float32),
            "hy_tau512": np.tile(np.arange(512, dtype=np.float32)[None, :], (128, 1))}


def run_interleaved(gens, width=2):
    gens = list(gens)
    active = []
    while gens or active:
        while gens and len(active) < width:
            active.append(gens.pop(0))
        for g in list(active):
            try:
                next(g)
            except StopIteration:
                active.remove(g)


def hyena_filters_td(K, i, I, C, L, emit):
    P = K.P
    nc = P.nc
    sb = P.sb
    tau512 = C["hy_tau512"]
    featsD = C[f"hy{L}_featsT_dram"]
    mark1 = P.mark()
    w1 = sb([HY_EMB, 64], F32, "hy_w1"); w2 = sb([64, 64], F32, "hy_w2"); w3 = sb([64, 2048], F32, "hy_w3")
    b1 = sb([64, 1], F32, "hy_b1"); b2 = sb([64, 1], F32, "hy_b2"); fq = sb([64, 1], F32, "hy_fq")
    fb1 = sb([64, 1], F32, "hy_fb1"); fb2 = sb([64, 1], F32, "hy_fb2")
    dec = sb([128, 8], F32, "hy_dec"); decb = sb([128, 8], F32, "hy_decb")
    P.dma("sp", w1[:], I["hy_w1"].ap[i], reads=[I["hy_w1"]], writes=[w1])
    P.dma("sp", w2[:], I["hy_w2"].ap[i], reads=[I["hy_w2"]], writes=[w2])
    P.dma("sp", w3[:], I["hy_w3"].ap[i], reads=[I["hy_w3"]], writes=[w3])
    with nc.allow_non_contiguous_dma(reason="tiny vector load"):
        P.dma("sp", b1[:], I["hy_b1"].ap[i].rearrange("(p o) -> p o", o=1), reads=[I["hy_b1"]], writes=[b1])
        P.dma("sp", b2[:], I["hy_b2"].ap[i].rearrange("(p o) -> p o", o=1), reads=[I["hy_b2"]], writes=[b2])
        P.dma("sp", fq[:], I["hy_freq"].ap[i].rearrange("(p o) -> p o", o=1), reads=[I["hy_freq"]], writes=[fq])
        P.dma("sp", dec[:], I["hy_decay"].ap[i].rearrange("o (k p) -> p (o k)", p=128), reads=[I["hy_decay"]], writes=[dec])
    P.tt("dve", fb1, fb1[:], b1, b1[:], fq, fq[:], ALU.mult)
    P.tt("dve", fb2, fb2[:], b2, b2[:], fq, fq[:], ALU.mult)
    P.act(dec, dec[:], dec, dec[:], AF.Abs)
    P.ts("dve", dec, dec[:], dec, dec[:], -1.0 / L, ALU.mult)
    TC = min(512, L)
    nTC = L // TC
    hid1 = sb([64, L], F32, "hy_hid1"); hid2 = sb([64, L], F32, "hy_hid2")
    ft = sb([HY_EMB, TC], F32, "hy_ft")
    pre = sb([64, 512], F32, "hy_pre"); rr = sb([64, 512], F32, "hy_rr"); rt = sb([64, 512], F32, "hy_rt")
    for tcn in range(nTC):
        P.dma("sp", ft[:], featsD.ap[:, tcn * TC:(tcn + 1) * TC], reads=[featsD], writes=[ft])
        ps = next_ps(K)
        P.mm(ps, ps[:64, :TC], w1[:], ft[:], True, True, reads=[w1, ft])
        P.ts("dve", pre, pre[:, :TC], ps, ps[:64, :TC], fq[:, 0:1], ALU.mult, fb1[:, 0:1], ALU.add, extra_reads=[fq, fb1])
        range_reduce(P, rr, rr[:, :TC], pre, pre[:, :TC], rt, rt[:, :TC])
        P.act(hid1, hid1[:, tcn * TC:(tcn + 1) * TC], rr, rr[:, :TC], AF.Sin)
        ps = next_ps(K)
        P.mm(ps, ps[:64, :TC], w2[:], hid1[:, tcn * TC:(tcn + 1) * TC], True, True, reads=[w2, hid1])
        P.ts("dve", pre, pre[:, :TC], ps, ps[:64, :TC], fq[:, 0:1], ALU.mult, fb2[:, 0:1], ALU.add, extra_reads=[fq, fb2])
        range_reduce(P, rr, rr[:, :TC], pre, pre[:, :TC], rt, rt[:, :TC])
        P.act(hid2, hid2[:, tcn * TC:(tcn + 1) * TC], rr, rr[:, :TC], AF.Sin)
    kf = sb([128, L], F32, "hy_kf"); kb = sb([128, L], F32, "hy_kb")
    win = sb([128, 512], F32, "hy_win"); wb_ = sb([128, 1], F32, "hy_wb")
    sums = sb([128, 2 * nTC + 1], F32, "hy_sums"); tot = sb([128, 1], F32, "hy_tot")
    for o in range(2):
        for cq in range(4):
            oc = o * 4 + cq
            for tcn in range(nTC):
                P.ts("dve", wb_, wb_[:], dec, dec[:, oc:oc + 1], float(tcn * TC), ALU.mult)
                P.act(win, win[:, :TC], tau512, tau512[:, :TC], AF.Exp, bias=wb_[:, 0:1], scale=dec[:, oc:oc + 1],
                      extra_reads=[wb_, dec])
                for dr, kt in ((0, kf), (1, kb)):
                    col0 = dr * 1024 + o * 512 + cq * 128
                    ps = next_ps(K)
                    P.mm(ps, ps[:, :TC], w3[:, col0:col0 + 128], hid2[:, tcn * TC:(tcn + 1) * TC], True, True, reads=[w3, hid2])
                    P.stt(kt, kt[:, tcn * TC:(tcn + 1) * TC], win, win[:, :TC], HY_WINDOW_SHIFT, ps, ps[:, :TC], ALU.add, ALU.mult)
                    P.op("dve", lambda e: e.tensor_reduce(out=sums[:, dr * nTC + tcn:dr * nTC + tcn + 1],
                                                          in_=kt[:, tcn * TC:(tcn + 1) * TC], axis=AX.X, op=ALU.add,
                                                          apply_absolute_value=True), reads=[kt], writes=[sums])
            P.act(sums, sums[:, 2 * nTC:2 * nTC + 1], kb, kb[:, 0:1], AF.Abs)
            P.ts("dve", sums, sums[:, 2 * nTC:2 * nTC + 1], sums, sums[:, 2 * nTC:2 * nTC + 1], -1.0, ALU.mult)
            P.op("dve", lambda e: e.tensor_reduce(out=tot[:], in_=sums[:], axis=AX.X, op=ALU.add), reads=[sums], writes=[tot])
            P.op("dve", lambda e: e.reciprocal(out=tot[:], in_=tot[:]), reads=[tot], writes=[tot])
            P.memset("dve", kb, kb[:, 0:1], 0.0)
            P.ts("dve", kf, kf[:], kf, kf[:], tot[:, 0:1], ALU.mult, extra_reads=[tot])
            P.ts("pool", kb, kb[:], kb, kb[:], tot[:, 0:1], ALU.mult, extra_reads=[tot])
            emit(o, cq, kf, kb)
    P.release_to(mark1)


def hyena_stage(K, i, I, zT, mixT, C, L, t_base, KFr, KFi, kT, tag):
    P = K.P
    nc = P.nc
    N = 2 * L
    N1 = N // 128
    R = L // 128
    G = min(512 // N1, 4)
    W = G * N1
    WT = G * 128
    mark = P.mark()
    sb = P.sb
    cn = lambda k: C[f"hy{L}_{k}"]
    tau512 = C["hy_tau512"]
    Twc, Tws, TwcT, TwsT = (cn(k) for k in ("Twc", "Tws", "TwcT", "TwsT"))
    featsD = C[f"hy{L}_featsT_dram"]

    def bf(src, nm):
        if HY_DT == F32:
            return src
        t = sb(list(src.ap.shape), HY_DT, "hyb_" + nm)
        P.copy("pool", t, t[:], src, src[:])
        return t
    F3c, F3s, F3ns = bf(C["hy_F3c"], "F3c"), bf(C["hy_F3s"], "F3s"), bf(C["hy_F3ns"], "F3ns")
    F1c, F1ns, I2c, I2ns = bf(cn("F1c"), "F1c"), bf(cn("F1ns"), "F1ns"), bf(cn("I2c"), "I2c"), bf(cn("I2ns"), "I2ns")
    NS = 2
    m1s = [sb([128, 512], F32, f"hy_m1{k}") for k in range(NS)]; m2s = [sb([128, 512], F32, f"hy_m2{k}") for k in range(NS)]
    Aprs = [sb([128, 512], HY_DT, f"hy_Apr{k}") for k in range(NS)]; Apis = [sb([128, 512], HY_DT, f"hy_Api{k}") for k in range(NS)]
    Bprs = [sb([128, 512], HY_DT, f"hy_Bpr{k}") for k in range(NS)]; Bpis = [sb([128, 512], HY_DT, f"hy_Bpi{k}") for k in range(NS)]

    def fft_fwd(U, psr, psi, sl):
        m1, m2, Apr, Api = m1s[sl], m2s[sl], Aprs[sl], Apis[sl]
        pa, pb = K.ps[4 * sl], K.ps[4 * sl + 1]
        Ut, Uf = U
        for c in range(G):
            P.mm(pa, pa[:, c * N1:(c + 1) * N1], Uf(c), F1c[:R, :], True, True, reads=[Ut, F1c])
            P.mm(pb, pb[:, c * N1:(c + 1) * N1], Uf(c), F1ns[:R, :], True, True, reads=[Ut, F1ns])
        yield
        v3 = lambda ap: ap.rearrange("q (c k) -> q c k", k=N1)
        tc_ = Twc[:, :].unsqueeze(1).broadcast_to([128, G, N1])
        ts_ = Tws[:, :].unsqueeze(1).broadcast_to([128, G, N1])
        P.tt("dve", m1, v3(m1[:, :W]), pa, v3(pa[:, :W]), Twc, tc_, ALU.mult)
        P.tt("dve", m2, v3(m2[:, :W]), pb, v3(pb[:, :W]), Tws, ts_, ALU.mult)
        P.tt("pool", Apr, Apr[:, :W], m1, m1[:, :W], m2, m2[:, :W], ALU.add)
        P.tt("dve", m1, v3(m1[:, :W]), pb, v3(pb[:, :W]), Twc, tc_, ALU.mult)
        P.tt("dve", m2, v3(m2[:, :W]), pa, v3(pa[:, :W]), Tws, ts_, ALU.mult)
        P.tt("pool", Api, Api[:, :W], m1, m1[:, :W], m2, m2[:, :W], ALU.subtract)
        yield
        P.mm(psr, psr[:, :W], F3c[:], Apr[:, :W], True, False, reads=[F3c, Apr])
        P.mm(psr, psr[:, :W], F3s[:], Api[:, :W], False, True, reads=[F3s, Api])
        P.mm(psi, psi[:, :W], F3c[:], Api[:, :W], True, False, reads=[F3c, Api])
        P.mm(psi, psi[:, :W], F3ns[:], Apr[:, :W], False, True, reads=[F3ns, Apr])
        yield

    def fft_inv(Yr, Yi, psy, sl):
        m1, m2, Bpr, Bpi = m1s[sl], m2s[sl], Bprs[sl], Bpis[sl]
        pa, pb = K.ps[4 * sl], K.ps[4 * sl + 1]
        for c in range(G):
            yr = Yr[:, c * N1:(c + 1) * N1]
            yi = Yi[:, c * N1:(c + 1) * N1]
            P.mm(pa, pa[:N1, c * 128:(c + 1) * 128], yr, F3c[:], True, False, reads=[Yr, F3c])
            P.mm(pa, pa[:N1, c * 128:(c + 1) * 128], yi, F3ns[:], False, True, reads=[Yi, F3ns])
            P.mm(pb, pb[:N1, c * 128:(c + 1) * 128], yr, F3s[:], True, False, reads=[Yr, F3s])
            P.mm(pb, pb[:N1, c * 128:(c + 1) * 128], yi, F3c[:], False, True, reads=[Yi, F3c])
        yield
        v3 = lambda ap: ap.rearrange("q (c k) -> q c k", k=128)
        tc_ = TwcT[:N1, :].unsqueeze(1).broadcast_to([N1, G, 128])
        ts_ = TwsT[:N1, :].unsqueeze(1).broadcast_to([N1, G, 128])
        P.tt("dve", m1, v3(m1[:N1, :WT]), pa, v3(pa[:N1, :WT]), TwcT, tc_, ALU.mult)
        P.tt("dve", m2, v3(m2[:N1, :WT]), pb, v3(pb[:N1, :WT]), TwsT, ts_, ALU.mult)
        P.tt("pool", Bpr, Bpr[:N1, :WT], m1, m1[:N1, :WT], m2, m2[:N1, :WT], ALU.subtract)
        P.tt("dve", m1, v3(m1[:N1, :WT]), pa, v3(pa[:N1, :WT]), TwsT, ts_, ALU.mult)
        P.tt("dve", m2, v3(m2[:N1, :WT]), pb, v3(pb[:N1, :WT]), TwcT, tc_, ALU.mult)
        P.tt("pool", Bpi, Bpi[:N1, :WT], m1, m1[:N1, :WT], m2, m2[:N1, :WT], ALU.add)
        yield
        P.mm(psy, psy[:R, :WT], I2c[:N1, :], Bpr[:N1, :WT], True, False, reads=[I2c, Bpr])
        P.mm(psy, psy[:R, :WT], I2ns[:N1, :], Bpi[:N1, :WT], False, True, reads=[I2ns, Bpi])
        yield

    def emit_kT(o, cq, kf, kb):
        r0 = o * 512 + cq * 128
        P.dma("sp", kT.ap[r0:r0 + 128, 0:L], kf[:], reads=[kf], writes=[kT])
        P.dma("sp", kT.ap[1024 + r0:1024 + r0 + 128, 0:L], kb[:], reads=[kb], writes=[kT])

    hyena_filters_td(K, i, I, C, L, emit_kT)

    mark2 = P.mark()
    Uf32 = [sb([R, G, 128], F32, f"hy_Uf{k}") for k in range(NS)]
    Ub32 = [sb([R, G, 128], F32, f"hy_Ub{k}") for k in range(NS)]
    Uf_ = [sb([R, G, 128], HY_DT, f"hy_Ufb{k}") for k in range(NS)]
    Ub_ = [sb([R, G, 128], HY_DT, f"hy_Ubb{k}") for k in range(NS)]
    Xs = [[sb([128, 512], F32, f"hy_Xs{k}{m}") for m in range(2)] for k in range(NS)]
    Ko = [[sb([128, 512], F32, f"hy_Ko{k}{m}") for m in range(2)] for k in range(NS)]

    def filt_group(gi):
        sl = gi % NS
        oc0 = gi * G
        uf, ub = Uf_[sl], Ub_[sl]
        P.dma("sp", Uf32[sl][:], kT.ap[oc0:oc0 + G, 0:L].rearrange("c (a b) -> a c b", b=128), reads=[kT], writes=[Uf32[sl]])
        P.dma("sp", Ub32[sl][:], kT.ap[1024 + oc0:1024 + oc0 + G, 0:L].rearrange("c (a b) -> a c b", b=128), reads=[kT], writes=[Ub32[sl]])
        P.copy("act", uf, uf[:], Uf32[sl], Uf32[sl][:])
        P.copy("act", ub, ub[:], Ub32[sl], Ub32[sl][:])
        pfr, pfi = K.ps[4 * sl + 2], K.ps[4 * sl + 3]
        yield from fft_fwd((uf, lambda c: uf[:, c, :]), pfr, pfi, sl)
        P.copy("act", Xs[sl][0], Xs[sl][0][:, :W], pfr, pfr[:, :W])
        P.copy("act", Xs[sl][1], Xs[sl][1][:, :W], pfi, pfi[:, :W])
        pbr, pbi = K.ps[4 * sl + 2], K.ps[4 * sl + 3]
        yield from fft_fwd((ub, lambda c: ub[:, c, :]), pbr, pbi, sl)
        kr, ki = Ko[sl]
        P.tt("dve", kr, kr[:, :W], Xs[sl][0], Xs[sl][0][:, :W], pbr, pbr[:, :W], ALU.add)
        P.tt("dve", ki, ki[:, :W], Xs[sl][1], Xs[sl][1][:, :W], pbi, pbi[:, :W], ALU.subtract)
        P.dma("pool", KFr.ap[oc0:oc0 + G, :, 0:N1].rearrange("c k2 k1 -> k2 c k1"), kr[:, :W].rearrange("q (c k) -> q c k", k=N1),
              reads=[kr], writes=[KFr])
        P.dma("pool", KFi.ap[oc0:oc0 + G, :, 0:N1].rearrange("c k2 k1 -> k2 c k1"), ki[:, :W].rearrange("q (c k) -> q c k", k=N1),
              reads=[ki], writes=[KFi])
        yield

    run_interleaved((filt_group(gi) for gi in range(1024 // G)), width=NS)
    P.release_to(mark2)

    cw = sb([R, 1536, 3], F32, "hy_cw"); cb = sb([R, 1536], F32, "hy_cb"); hb = sb([R, 1024], F32, "hy_hb")
    with nc.allow_non_contiguous_dma(reason="broadcast parameter loads"):
        for j in range(3):
            P.dma("sp", cw[:, :, j], I["ev_conv_w"].ap[i, j:j + 1, :].partition_broadcast(R), reads=[I["ev_conv_w"]], writes=[cw])
        P.dma("sp", cb[:], I["ev_conv_b"].ap[i].rearrange("(o c) -> o c", o=1).partition_broadcast(R), reads=[I["ev_conv_b"]], writes=[cb])
        P.dma("sp", hb[:], I["hy_bias"].ap[i].rearrange("o c -> (o c)").rearrange("(o c) -> o c", o=1).partition_broadcast(R),
              reads=[I["hy_bias"]], writes=[hb])
    raw = [[sb([R, G, 130], F32, f"hy_raw{k}{m}") for m in range(3)] for k in range(NS)]
    cvs = [[sb([R, G, 128], F32, f"hy_cv{k}{m}") for m in range(3)] for k in range(NS)]
    cts = [sb([R, G, 128], F32, f"hy_ct{k}") for k in range(NS)]
    ybs = [sb([R, G, 128], HY_DT, f"hy_yb{k}") for k in range(NS)]
    kfr = [[sb([128, 512], F32, f"hy_kfr{k}{o}") for o in range(2)] for k in range(NS)]
    kfi = [[sb([128, 512], F32, f"hy_kfi{k}{o}") for o in range(2)] for k in range(NS)]
    Yrs = [sb([128, 512], HY_DT, f"hy_Yr{k}") for k in range(NS)]; Yis = [sb([128, 512], HY_DT, f"hy_Yi{k}") for k in range(NS)]
    y1s = [sb([R, G, 128], F32, f"hy_y1{k}") for k in range(NS)]; youts = [sb([R, G, 128], F32, f"hy_yout{k}") for k in range(NS)]
    for k in range(NS):
        for m in range(3):
            P.memset("pool", raw[k][m], raw[k][m][:], 0.0)

    def data_group(gi):
        sl = gi % NS
        c0 = gi * G
        rw, cv, ct, yb = raw[sl], cvs[sl], cts[sl], ybs[sl]
        m1, m2 = m1s[sl], m2s[sl]
        for m in range(3):
            rows = zT.ap[m * 512 + c0:m * 512 + c0 + G, :]
            P.dma("sp", rw[m][:, :, 1:129], rows[:, t_base:t_base + L].rearrange("c (a b) -> a c b", b=128),
                  reads=[zT], writes=[rw[m]])
            if R > 1:
                with nc.allow_non_contiguous_dma(reason="1-column conv halos"):
                    P.dma("sp", rw[m][1:R, :, 0:1], rows[:, t_base + 127:t_base + L - 1].rearrange("c (a b) -> a c b", b=128)[:, :, 0:1],
                          reads=[zT], writes=[rw[m]])
                    P.dma("sp", rw[m][0:R - 1, :, 129:130], rows[:, t_base + 128:t_base + L].rearrange("c (a b) -> a c b", b=128)[:, :, 0:1],
                          reads=[zT], writes=[rw[m]])
        for o in range(2):
            oc0 = o * 512 + c0
            P.dma("sp", kfr[sl][o][:, :W].rearrange("q (c k) -> q c k", k=N1), KFr.ap[oc0:oc0 + G, :, 0:N1].rearrange("c k2 k1 -> k2 c k1"),
                  reads=[KFr], writes=[kfr[sl][o]])
            P.dma("sp", kfi[sl][o][:, :W].rearrange("q (c k) -> q c k", k=N1), KFi.ap[oc0:oc0 + G, :, 0:N1].rearrange("c k2 k1 -> k2 c k1"),
                  reads=[KFi], writes=[kfi[sl][o]])
        for m in range(3):
            ch = slice(m * 512 + c0, m * 512 + c0 + G)
            wj = lambda j: cw[:, ch, j].unsqueeze(2).broadcast_to([R, G, 128])
            P.tt("dve", cv[m], cv[m][:], rw[m], rw[m][:, :, 0:128], cw, wj(0), ALU.mult)
            P.tt("pool", ct, ct[:], rw[m], rw[m][:, :, 1:129], cw, wj(1), ALU.mult)
            P.tt("dve", cv[m], cv[m][:], cv[m], cv[m][:], ct, ct[:], ALU.add)
            P.tt("pool", ct, ct[:], rw[m], rw[m][:, :, 2:130], cw, wj(2), ALU.mult)
            P.tt("dve", cv[m], cv[m][:], cv[m], cv[m][:], ct, ct[:], ALU.add)
            P.tt("dve", cv[m], cv[m][:], cv[m], cv[m][:], cb, cb[:, ch].unsqueeze(2).broadcast_to([R, G, 128]), ALU.add)
        yield
        ycur = cv[0]
        for o in range(2):
            P.copy("act", yb, yb[:], ycur, ycur[:])
            pxr, pxi = K.ps[4 * sl + 2], K.ps[4 * sl + 3]
            yield from fft_fwd((yb, lambda c: yb[:, c, :]), pxr, pxi, sl)
            kr, ki = kfr[sl][o], kfi[sl][o]
            Yr, Yi = Yrs[sl], Yis[sl]
            P.tt("dve", m1, m1[:, :W], pxr, pxr[:, :W], kr, kr[:, :W], ALU.mult)
            P.tt("dve", m2, m2[:, :W], pxi, pxi[:, :W], ki, ki[:, :W], ALU.mult)
            P.tt("pool", Yr, Yr[:, :W], m1, m1[:, :W], m2, m2[:, :W], ALU.subtract)
            P.tt("dve", m1, m1[:, :W], pxr, pxr[:, :W], ki, ki[:, :W], ALU.mult)
            P.tt("dve", m2, m2[:, :W], pxi, pxi[:, :W], kr, kr[:, :W], ALU.mult)
            P.tt("pool", Yi, Yi[:, :W], m1, m1[:, :W], m2, m2[:, :W], ALU.add)
            yield
            py = K.ps[4 * sl + 2]
            yield from fft_inv(Yr, Yi, py, sl)
            hbo = hb[:, o * 512 + c0:o * 512 + c0 + G].unsqueeze(2).broadcast_to([R, G, 128])
            P.tt("dve", ct, ct[:], ycur, ycur[:], hb, hbo, ALU.mult)
            P.tt("dve", ct, ct[:], ct, ct[:], py, py[:R, :WT].rearrange("q (c k) -> q c k", k=128), ALU.add)
            dst = y1s[sl] if o == 0 else youts[sl]
            P.tt("dve", dst, dst[:], ct, ct[:], cv[1 + o], cv[1 + o][:], ALU.mult)
            ycur = dst
            yield
        P.dma("pool", mixT.ap[c0:c0 + G, t_base:t_base + L].rearrange("c (a b) -> a c b", b=128), ycur[:],
              reads=[ycur], writes=[mixT])
        yield

    run_interleaved((data_group(gi) for gi in range(512 // G)), width=NS)
    P.release_to(mark)


def hyena_ctx_consts():
    L, N = 256, 512
    t = np.arange(L, dtype=np.float64)[:, None]; f = np.arange(N, dtype=np.float64)[None, :]
    a = 2 * np.pi * t * f / N
    c = {"hc_Fc": np.cos(a), "hc_Fns": -np.sin(a), "hc_Ic": np.cos(a).T / N, "hc_Ins": -np.sin(a).T / N}
    return {k: np.ascontiguousarray(v, dtype=np.float32) for k, v in c.items()}


def hyena_ctx_stage(K, i, I, zT, mixT, C, Cd, t_base=0):
    P = K.P
    nc = P.nc
    sb = P.sb
    L, N = 256, 512
    mark = P.mark()
    ident = C["k_ident"]
    Fc = sb([128, 2, 512], F32, "hc_Fc"); Fns = sb([128, 2, 512], F32, "hc_Fns")
    Ic = sb([128, 4, 256], F32, "hc_Ic"); Ins = sb([128, 4, 256], F32, "hc_Ins")
    P.dma("sp", Fc[:], Cd["hc_Fc"].ap.rearrange("(k p) f -> p k f", p=128), reads=[Cd["hc_Fc"]], writes=[Fc])
    P.dma("sp", Fns[:], Cd["hc_Fns"].ap.rearrange("(k p) f -> p k f", p=128), reads=[Cd["hc_Fns"]], writes=[Fns])
    P.dma("sp", Ic[:], Cd["hc_Ic"].ap.rearrange("(k p) t -> p k t", p=128), reads=[Cd["hc_Ic"]], writes=[Ic])
    P.dma("sp", Ins[:], Cd["hc_Ins"].ap.rearrange("(k p) t -> p k t", p=128), reads=[Cd["hc_Ins"]], writes=[Ins])
    ktm = [[sb([128, 2, 512], F32, f"hc_ktm{dr}{o}") for o in range(2)] for dr in range(2)]

    def emit(o, cq, kf, kb):
        for (src, dr) in ((kf, 0), (kb, 1)):
            for tc in range(2):
                ps = next_ps(K)
                P.op("pe", lambda e: e.transpose(ps[:, 0:128], src[:, tc * 128:(tc + 1) * 128], ident[:]), reads=[src, ident], writes=[ps])
                P.copy("act", ktm[dr][o], ktm[dr][o][:, tc, cq * 128:(cq + 1) * 128], ps, ps[:, 0:128])

    hyena_filters_td(K, i, I, C, L, emit)

    def dft(x_t, fc):
        psr = next_ps(K); psi = next_ps(K)
        for tc in range(2):
            P.mm(psr, psr[:, :], Fc[:, tc, fc * 128:(fc + 1) * 128], x_t[:, tc, :], tc == 0, tc == 1, reads=[Fc, x_t])
        for tc in range(2):
            P.mm(psi, psi[:, :], Fns[:, tc, fc * 128:(fc + 1) * 128], x_t[:, tc, :], tc == 0, tc == 1, reads=[Fns, x_t])
        return psr, psi

    KFr = [sb([128, 4, 512], F32, f"hc_KFr{o}") for o in range(2)]; KFi = [sb([128, 4, 512], F32, f"hc_KFi{o}") for o in range(2)]
    Xr = sb([128, 512], F32, "hc_Xr"); Xi = sb([128, 512], F32, "hc_Xi")
    for o in range(2):
        for fc in range(4):
            pr, pi_ = dft(ktm[0][o], fc)
            P.copy("act", Xr, Xr[:], pr, pr[:, :])
            P.copy("act", Xi, Xi[:], pi_, pi_[:, :])
            pr, pi_ = dft(ktm[1][o], fc)
            P.tt("dve", KFr[o], KFr[o][:, fc, :], Xr, Xr[:], pr, pr[:, :], ALU.add)
            P.tt("dve", KFi[o], KFi[o][:, fc, :], Xi, Xi[:], pi_, pi_[:, :], ALU.subtract)
    cwt = sb([128, 12, 3], F32, "hc_cw"); cbt = sb([128, 12], F32, "hc_cb"); hbb = sb([128, 2, 512], F32, "hc_hb")
    with nc.allow_non_contiguous_dma(reason="small parameter loads"):
        for j in range(3):
            P.dma("sp", cwt[:, :, j], I["ev_conv_w"].ap[i, j].rearrange("(k p) -> p k", p=128), reads=[I["ev_conv_w"]], writes=[cwt])
        P.dma("sp", cbt[:], I["ev_conv_b"].ap[i].rearrange("(k p) -> p k", p=128), reads=[I["ev_conv_b"]], writes=[cbt])
        P.dma("sp", hbb[:].rearrange("p o c -> p (o c)"),
              I["hy_bias"].ap[i].rearrange("o c -> (o c)").rearrange("(a n) -> a n", a=1).partition_broadcast(128),
              reads=[I["hy_bias"]], writes=[hbb])
    xtm = [sb([128, 2, 512], F32, f"hc_xtm{m}") for m in range(3)]
    raw = [sb([128, 258], F32, f"hc_raw{k}") for k in range(2)]
    cv = [sb([128, 256], F32, f"hc_cv{k}") for k in range(2)]
    for k in range(2):
        P.memset("pool", raw[k], raw[k][:], 0.0)
    for m in range(3):
        for cq in range(4):
            mc = m * 4 + cq
            rw, cvb = raw[mc % 2], cv[mc % 2]
            P.dma("sp", rw[:, 1:257], zT.ap[mc * 128:(mc + 1) * 128, t_base:t_base + L], reads=[zT], writes=[rw])
            P.ts("dve", cvb, cvb[:], rw, rw[:, 0:256], cwt[:, mc, 0:1], ALU.mult, cbt[:, mc:mc + 1], ALU.add, extra_reads=[cwt, cbt])
            P.stt(cvb, cvb[:], rw, rw[:, 1:257], cwt[:, mc, 1:2], cvb, cvb[:], ALU.mult, ALU.add, extra_reads=[cwt])
            P.stt(cvb, cvb[:], rw, rw[:, 2:258], cwt[:, mc, 2:3], cvb, cvb[:], ALU.mult, ALU.add, extra_reads=[cwt])
            for tc in range(2):
                ps = next_ps(K)
                P.op("pe", lambda e: e.transpose(ps[:, 0:128], cvb[:, tc * 128:(tc + 1) * 128], ident[:]), reads=[cvb, ident], writes=[ps])
                P.copy("act", xtm[m], xtm[m][:, tc, cq * 128:(cq + 1) * 128], ps, ps[:, 0:128])
    Yr = sb([128, 4, 512], F32, "hc_Yr"); Yi = sb([128, 4, 512], F32, "hc_Yi")
    ya = sb([128, 2, 512], F32, "hc_ya"); yb_ = sb([128, 2, 512], F32, "hc_yb")
    t1 = sb([128, 512], F32, "hc_t1"); t2 = sb([128, 512], F32, "hc_t2")
    ycur = xtm[0]
    for o in range(2):
        for fc in range(4):
            pr, pi_ = dft(ycur, fc)
            P.tt("dve", t1, t1[:], pr, pr[:, :], KFr[o], KFr[o][:, fc, :], ALU.mult)
            P.tt("dve", t2, t2[:], pi_, pi_[:, :], KFi[o], KFi[o][:, fc, :], ALU.mult)
            P.tt("pool", Yr, Yr[:, fc, :], t1, t1[:], t2, t2[:], ALU.subtract)
            P.tt("dve", t1, t1[:], pr, pr[:, :], KFi[o], KFi[o][:, fc, :], ALU.mult)
            P.tt("dve", t2, t2[:], pi_, pi_[:, :], KFr[o], KFr[o][:, fc, :], ALU.mult)
            P.tt("pool", Yi, Yi[:, fc, :], t1, t1[:], t2, t2[:], ALU.add)
        dst = ya if o == 0 else yb_
        for tc in range(2):
            py = next_ps(K)
            for fc in range(4):
                P.mm(py, py[:, :], Ic[:, fc, tc * 128:(tc + 1) * 128], Yr[:, fc, :], fc == 0, False, reads=[Ic, Yr])
                P.mm(py, py[:, :], Ins[:, fc, tc * 128:(tc + 1) * 128], Yi[:, fc, :], False, fc == 3, reads=[Ins, Yi])
            P.tt("dve", t1, t1[:], ycur, ycur[:, tc, :], hbb, hbb[:, o, :], ALU.mult)
            P.tt("dve", t1, t1[:], t1, t1[:], py, py[:, :], ALU.add)
            P.tt("dve", dst, dst[:, tc, :], t1, t1[:], xtm[1 + o], xtm[1 + o][:, tc, :], ALU.mult)
        ycur = dst
    ofm = sb([128, 4, 256], F32, "hc_ofm")
    for cq in range(4):
        for tc in range(2):
            ps = next_ps(K)
            P.op("pe", lambda e: e.transpose(ps[:, 0:128], ycur[:, tc, cq * 128:(cq + 1) * 128], ident[:]), reads=[ycur, ident], writes=[ps])
            P.copy("act", ofm, ofm[:, cq, tc * 128:(tc + 1) * 128], ps, ps[:, 0:128])
    P.dma("sp", mixT.ap[0:512, t_base:t_base + L].rearrange("(k p) t -> p k t", p=128), ofm[:], reads=[ofm], writes=[mixT])
    P.release_to(mark)


MLA_SCALE = 96.0 ** -0.5
GQA_SCALE = 64.0 ** -0.5
GRID_W = 64
ROPE_BASE = 10000.0


def odd_segs():
    segs = [(0, 1184), (392, 8), (384, 8), (408, 8), (400, 8)]
    for hq in range(8):
        b = 416 + 64 * hq
        segs += [(b + 16, 16), (b, 16), (b + 48, 16), (b + 32, 16)]
    for kh in range(2):
        b = 928 + 64 * kh
        segs += [(b + 16, 16), (b, 16), (b + 48, 16), (b + 32, 16)]
    return segs


def rope_consts(L):
    out = {}
    rows = L // GRID_W
    row = np.repeat(np.arange(rows), GRID_W).astype(np.float32)
    col = np.tile(np.arange(GRID_W), rows).astype(np.float32)
    for dim, nm in ((32, "mla"), (64, "gqa")):
        nf = dim // 4
        inv = (np.float32(ROPE_BASE) ** (-np.arange(nf, dtype=np.float32) / np.float32(nf))).astype(np.float32)
        ar = (row[:, None] * inv[None, :]).astype(np.float32)
        ac = (col[:, None] * inv[None, :]).astype(np.float32)
        cr, sr, cc, sc = np.cos(ar), np.sin(ar), np.cos(ac), np.sin(ac)
        C = np.concatenate([cr, cr, cc, cc], axis=1).T
        S = np.concatenate([-sr, sr, -sc, sc], axis=1).T
        out[f"rope_{nm}_C"] = np.ascontiguousarray(C, dtype=np.float32)
        out[f"rope_{nm}_S"] = np.ascontiguousarray(S, dtype=np.float32)
    j = np.arange(128)[:, None]; r = np.arange(128)[None, :]
    out["k_mask_prev"] = np.tile((j >= r).astype(np.float32), (1, 4))
    out["k_mask_next"] = np.tile((j <= r).astype(np.float32), (1, 4))
    selM = np.zeros((96, 97), np.float32); selM[:, 96] = 1.0
    selG = np.zeros((64, 65), np.float32); selG[:, 64] = 1.0
    sel65 = np.zeros((65, 64), np.float32); sel65[64, :] = 1.0
    vs = np.zeros((1, 65), np.float32); vs[0, 64] = 1.0
    out["k_selM"] = selM; out["k_selG"] = selG; out["k_sel65"] = sel65; out["k_vsink"] = vs
    return out


def attn_core(K, A, q_t, q_ap, nq, chunks, outs):
    P = K.P
    pot = A.pot
    n = len(chunks)
    LOOK = 2

    def score(ci):
        k_t, k_ap, v_t, v_ap, mask, cols, qrows = chunks[ci]
        c0, ncol = cols if cols is not None else (0, nq)
        nk = k_ap.shape[1]
        ps = next_ps(K)
        q_use = q_ap[:, c0:c0 + ncol] if qrows is None else q_ap[qrows[0]:qrows[1], c0:c0 + ncol]
        P.mm(ps, ps[:nk, c0:c0 + ncol], k_ap, q_use, True, True, reads=[k_t, q_t])
        return ps

    pend = {}
    for ci in range(min(LOOK, n)):
        pend[ci] = score(ci)
    for ci, (k_t, k_ap, v_t, v_ap, mask, cols, qrows) in enumerate(chunks):
        c0, ncol = cols if cols is not None else (0, nq)
        nk = k_ap.shape[1]
        ps = pend.pop(ci)
        pt = A.pt[A.pti % len(A.pt)]
        A.pti += 1
        P.act(pt, pt[:nk, c0:c0 + ncol], ps, ps[:nk, c0:c0 + ncol], AF.Exp)
        if mask is not None:
            P.tt("dve", pt, pt[:nk, c0:c0 + ncol], pt, pt[:nk, c0:c0 + ncol], mask[0], mask[1], ALU.mult)
        if ci + LOOK < n:
            pend[ci + LOOK] = score(ci + LOOK)
        P.mm(pot, pot[:65, c0:c0 + ncol], v_ap, pt[:nk, c0:c0 + ncol], ci == 0, ci == n - 1, reads=[v_t, pt])
    P.copy("act", A.ot, A.ot[:65, :nq], pot, pot[:65, :nq])
    ps = next_ps(K)
    P.mm(ps, ps[:64, :nq], A.sel65[:, :], A.ot[:65, :nq], True, True, reads=[A.sel65, A.ot])
    P.op("dve", lambda e: e.reciprocal(out=A.rden[:64, :nq], in_=ps[:64, :nq]), reads=[ps], writes=[A.rden])
    ob = A.ob[A.obi % 2]
    A.obi += 1
    P.tt("dve", ob, ob[:64, :nq], A.ot, A.ot[:64, :nq], A.rden, A.rden[:64, :nq], ALU.mult)
    for (dap, c0, ncol) in outs:
        P.dma("pool", dap, ob[:64, c0:c0 + ncol], reads=[ob], writes=[A.mixT])


def odd_mixer_stage(K, i, I, zT, mixT, C, S, need_ctx):
    P = K.P
    nc = P.nc
    sb = P.sb
    NT, NCX = K.NT, K.NC
    mark = P.mark()
    tiles = ([(0, NCX, 1)] if NCX else []) + [(NCX + k * 512, min(512, K.NL - k * 512), 0) for k in range((K.NL + 511) // 512)]
    wst = sb([128, 1024], F32, "od_wst")
    wukv = sb([128, 1024], BF16, "od_wukv")
    P.dma("sp", wst[:], I["mla_w_ukv"].ap[i], reads=[I["mla_w_ukv"]], writes=[wst])
    P.copy("pool", wukv, wukv[:], wst, wst[:])
    wuq = sb([128, 2, 768], BF16, "od_wuq"); wuqs = sb([128, 2, 768], BF16, "od_wuqs")
    wst2 = sb([128, 2, 768], F32, "od_wst2")
    P.dma("sp", wst2[:], I["mla_w_uq"].ap[i].rearrange("(k p) n -> p k n", p=128), reads=[I["mla_w_uq"]], writes=[wst2])
    P.copy("pool", wuq, wuq[:], wst2, wst2[:])
    P.memset("pool", wuqs, wuqs[:], 0.0)
    for h in range(8):
        b = h * 96 + 64
        for (dst, src) in ((0, 8), (8, 0), (16, 24), (24, 16)):
            P.copy("pool", wuqs, wuqs[:, :, b + dst:b + dst + 8], wst2, wst2[:, :, b + src:b + src + 8])
    gq = sb([128, 2], F32, "od_gq"); gkv = sb([128, 1], F32, "od_gkv")
    sinkb = sb([96, 8], F32, "od_sink")
    with nc.allow_non_contiguous_dma(reason="tiny vector loads"):
        P.dma("sp", gq[:], I["mla_q_norm"].ap[i].rearrange("(k p) -> p k", p=128), reads=[I["mla_q_norm"]], writes=[gq])
        P.dma("sp", gkv[:], I["mla_kv_norm"].ap[i].rearrange("(p o) -> p o", o=1), reads=[I["mla_kv_norm"]], writes=[gkv])
        P.dma("sp", sinkb[64:96, :], I["gqa_sink"].ap[i].rearrange("(o h) -> o h", o=1).partition_broadcast(32),
              reads=[I["gqa_sink"]], writes=[sinkb])
    selM, selG, sel65, vsink = C["k_selM"], C["k_selG"], C["k_sel65"], C["k_vsink"]
    ident = C["k_ident"]
    vsb = sb([1, 65], BF16, "od_vsb")
    P.copy("dve", vsb, vsb[:], vsink, vsink[:])
    kmaxM = sb([128, 8], F32, "od_kmaxM"); kmaxG = sb([128, 2], F32, "od_kmaxG")
    P.memset("pool", kmaxM, kmaxM[:], 0.0)
    P.memset("pool", kmaxG, kmaxG[:], 0.0)
    mx1 = sb([128, 1], F32, "od_mx1")
    markp = P.mark()
    xin = [sb([128, 2, 512], F32, f"od_xin{k}") for k in range(2)]
    sq = sb([128, 2, 512], F32, "od_sq"); rstd = sb([128, 512], F32, "od_rstd")
    kvn = sb([128, 512], BF16, "od_kvn"); cqn = sb([128, 2, 512], BF16, "od_cqn")
    raw = [sb([128, 512], F32, f"od_raw{k}") for k in range(2)]; sw = [sb([128, 512], F32, f"od_sw{k}") for k in range(2)]
    tC = [sb([128, 512], F32, f"od_tC{k}") for k in range(2)]; tS = [sb([128, 512], F32, f"od_tS{k}") for k in range(2)]
    rot = sb([128, 512], F32, "od_rot"); rt2 = sb([128, 512], F32, "od_rt2")
    kf = sb([96, 512], F32, "od_kf"); kf2 = sb([96, 512], F32, "od_kf2")
    kt = [sb([97, 512], BF16, f"od_kt{k}") for k in range(2)]
    vt = [sb([128, 8, 65], BF16, f"od_vt{k}") for k in range(2)]
    vg = [sb([128, 2, 65], BF16, f"od_vg{k}") for k in range(2)]
    gvt = sb([128, 512], F32, "od_gvt")
    for k in range(2):
        P.memset("pool", kt[k], kt[k][96:97, :], 1.0)
        P.memset("pool", vt[k], vt[k][:], 1.0)
        P.memset("pool", vg[k], vg[k][:], 1.0)

    def rms(x_t, x_ap_k, nk_, n, g_t, g_ap_k, out_t, out_ap_k):
        ps = next_ps(K)
        for k in range(nk_):
            P.act(sq, sq[:, k, :n], x_t, x_ap_k(k), AF.Square)
        for k in range(nk_):
            P.mm(ps, ps[:, :n], K.ones[:], sq[:, k, :n], k == 0, k == nk_ - 1, reads=[K.ones, sq])
        P.ts("dve", rstd, rstd[:, :n], ps, ps[:, :n], 1.0 / (128 * nk_), ALU.mult, EPS, ALU.add)
        P.op("act", lambda e: e.sqrt(out=rstd[:, :n], in_=rstd[:, :n]), reads=[rstd], writes=[rstd])
        P.op("dve", lambda e: e.reciprocal(out=rstd[:, :n], in_=rstd[:, :n]), reads=[rstd], writes=[rstd])
        for k in range(nk_):
            P.stt(out_t, out_ap_k(k), x_t, x_ap_k(k), g_ap_k(k), rstd, rstd[:, :n], ALU.mult, ALU.mult, extra_reads=[g_t])

    def sumsq_max(src_t, src_ap, nrows, n, sel, dstcol_t, dstcol_ap, prow):
        P.act(kf2, kf2[:nrows, :n], src_t, src_ap, AF.Square)
        ps = next_ps(K)
        P.mm(ps, ps[:prow + 1, :n], sel[:nrows, :prow + 1], kf2[:nrows, :n], True, True, reads=[sel, kf2])
        P.op("dve", lambda e: e.reduce_max(out=mx1[prow:prow + 1, :], in_=ps[prow:prow + 1, :n], axis=AX.X), reads=[ps], writes=[mx1])
        P.tt("dve", dstcol_t, dstcol_ap, dstcol_t, dstcol_ap, mx1, mx1[prow:prow + 1, :], ALU.max)

    for it, (t0, n, is_ctx) in enumerate(tiles):
        x = xin[it % 2]
        lat0 = t0 - NCX
        P.dma("sp", x[:, 0, :n], zT.ap[256:384, t0:t0 + n], reads=[zT], writes=[x])
        rms(x, lambda k: x[:, 0, :n], 1, n, gkv, lambda k: gkv[:, 0:1], kvn, lambda k: kvn[:, :n])
        r_, s_, c_, sn_ = raw[it % 2], sw[it % 2], tC[it % 2], tS[it % 2]
        P.dma("sp", r_[64:96, :n], zT.ap[384:416, t0:t0 + n], reads=[zT], writes=[r_])
        if not is_ctx:
            P.dma("sp", s_[64:96, :n], zT.ap[1184:1216, t0:t0 + n], reads=[zT], writes=[s_])
            P.dma("sp", c_[64:96, :n], C["rope_mla_C"].ap[:, lat0:lat0 + n], reads=[C["rope_mla_C"]], writes=[c_])
            P.dma("sp", sn_[64:96, :n], C["rope_mla_S"].ap[:, lat0:lat0 + n], reads=[C["rope_mla_S"]], writes=[sn_])
            P.tt("dve", rot, rot[64:96, :n], r_, r_[64:96, :n], c_, c_[64:96, :n], ALU.mult)
            P.tt("pool", rt2, rt2[64:96, :n], s_, s_[64:96, :n], sn_, sn_[64:96, :n], ALU.mult)
            P.tt("dve", kf, kf[64:96, :n], rot, rot[64:96, :n], rt2, rt2[64:96, :n], ALU.add)
        else:
            P.copy("dve", kf, kf[64:96, :n], r_, r_[64:96, :n])
        for h in range(8):
            ps = next_ps(K)
            P.mm(ps, ps[:64, :n], wukv[:, h * 128:h * 128 + 64], kvn[:, :n], True, True, reads=[wukv, kvn])
            P.copy("act", kf, kf[0:64, :n], ps, ps[:64, :n])
            sumsq_max(kf, kf[0:96, :n], 96, n, selM, kmaxM, kmaxM[96:97, h:h + 1], 96)
            ktb = kt[h % 2]
            P.copy("pool", ktb, ktb[0:96, :n], kf, kf[0:96, :n])
            P.dma("sp", S["KM"].ap[h, :, t0:t0 + n], ktb[:, :n], reads=[ktb], writes=[S["KM"]])
        for q in range((n + 127) // 128):
            vtb = vt[q % 2]
            for half in range(2):
                ps = next_ps(K)
                P.mm(ps, ps[:, :512], kvn[:, q * 128:(q + 1) * 128], wukv[:, half * 512:(half + 1) * 512], True, True,
                     reads=[kvn, wukv])
                P.copy("act", vtb, vtb[:, half * 4:half * 4 + 4, 0:64],
                       ps, ps[:, :512].rearrange("t (h d) -> t h d", d=128)[:, :, 64:128])
            with nc.allow_non_contiguous_dma(reason="token-major V rows"):
                P.dma("sp", S["VM"].ap[:, t0 + q * 128:t0 + (q + 1) * 128, :].rearrange("h t d -> t h d"), vtb[:],
                      reads=[vtb], writes=[S["VM"]])
        for kh in range(2):
            P.dma("sp", r_[0:64, :n], zT.ap[928 + 64 * kh:928 + 64 * kh + 64, t0:t0 + n], reads=[zT], writes=[r_])
            if not is_ctx:
                P.dma("sp", s_[0:64, :n], zT.ap[1728 + 64 * kh:1728 + 64 * kh + 64, t0:t0 + n], reads=[zT], writes=[s_])
                P.dma("sp", c_[0:64, :n], C["rope_gqa_C"].ap[:, lat0:lat0 + n], reads=[C["rope_gqa_C"]], writes=[c_])
                P.dma("sp", sn_[0:64, :n], C["rope_gqa_S"].ap[:, lat0:lat0 + n], reads=[C["rope_gqa_S"]], writes=[sn_])
                P.tt("dve", rot, rot[0:64, :n], r_, r_[0:64, :n], c_, c_[0:64, :n], ALU.mult)
                P.tt("pool", rt2, rt2[0:64, :n], s_, s_[0:64, :n], sn_, sn_[0:64, :n], ALU.mult)
                P.tt("dve", kf, kf[0:64, :n], rot, rot[0:64, :n], rt2, rt2[0:64, :n], ALU.add)
            else:
                P.copy("dve", kf, kf[0:64, :n], r_, r_[0:64, :n])
            sumsq_max(kf, kf[0:64, :n], 64, n, selG, kmaxG, kmaxG[64:65, kh:kh + 1], 64)
            ktb = kt[kh % 2]
            P.copy("pool", ktb, ktb[0:64, :n], kf, kf[0:64, :n])
            P.memset("pool", ktb, ktb[64:96, :n], 0.0)
            P.memset("pool", ktb, ktb[64:65, :n], 1.0)
            P.dma("sp", S["KG"].ap[kh, :, t0:t0 + n], ktb[0:66, :n], reads=[ktb], writes=[S["KG"]])
            P.memset("pool", ktb, ktb[96:97, :n], 1.0)
        P.dma("sp", gvt[:, :n], zT.ap[1056:1184, t0:t0 + n], reads=[zT], writes=[gvt])
        for q in range((n + 127) // 128):
            ps = next_ps(K)
            P.op("pe", lambda e: e.transpose(ps[:, 0:128], gvt[:, q * 128:(q + 1) * 128], ident[:]), reads=[gvt, ident], writes=[ps])
            vgb = vg[q % 2]
            P.copy("act", vgb, vgb[:, :, 0:64], ps, ps[:, 0:128].rearrange("t (h d) -> t h d", d=64))
            with nc.allow_non_contiguous_dma(reason="token-major V rows"):
                P.dma("sp", S["VG"].ap[:, t0 + q * 128:t0 + (q + 1) * 128, :].rearrange("h t d -> t h d"), vgb[:],
                      reads=[vgb], writes=[S["VG"]])
    qf = sb([96, 512], F32, "od_qf")
    qt = [sb([97, 512], BF16, f"od_qt{k}") for k in range(2)]
    prod = sb([128, 512], F32, "od_prod")
    for it, (t0, n, is_ctx) in enumerate(tiles):
        if is_ctx and not need_ctx:
            continue
        x = xin[it % 2]
        lat0 = t0 - NCX
        P.dma("sp", x[:, :, :n], zT.ap[0:256, t0:t0 + n].rearrange("(k p) t -> p k t", p=128), reads=[zT], writes=[x])
        rms(x, lambda k: x[:, k, :n], 2, n, gq, lambda k: gq[:, k:k + 1], cqn, lambda k: cqn[:, k, :n])
        c_, sn_ = tC[it % 2], tS[it % 2]
        if not is_ctx:
            P.dma("sp", c_[64:96, :n], C["rope_mla_C"].ap[:, lat0:lat0 + n], reads=[C["rope_mla_C"]], writes=[c_])
            P.dma("sp", sn_[64:96, :n], C["rope_mla_S"].ap[:, lat0:lat0 + n], reads=[C["rope_mla_S"]], writes=[sn_])
        for h in range(8):
            ps = next_ps(K)
            for k in range(2):
                P.mm(ps, ps[:96, :n], wuq[:, k, h * 96:(h + 1) * 96], cqn[:, k, :n], k == 0, k == 1, reads=[wuq, cqn])
            if not is_ctx:
                ps2 = next_ps(K)
                for k in range(2):
                    P.mm(ps2, ps2[:96, :n], wuqs[:, k, h * 96:(h + 1) * 96], cqn[:, k, :n], k == 0, k == 1, reads=[wuqs, cqn])
                P.tt("dve", rot, rot[64:96, :n], ps, ps[64:96, :n], c_, c_[64:96, :n], ALU.mult)
                P.tt("dve", rt2, rt2[64:96, :n], ps2, ps2[64:96, :n], sn_, sn_[64:96, :n], ALU.mult)
                P.stt(qf, qf[64:96, :n], rot, rot[64:96, :n], 1.0, rt2, rt2[64:96, :n], ALU.mult, ALU.add)
                P.ts("dve", qf, qf[64:96, :n], qf, qf[64:96, :n], MLA_SCALE, ALU.mult)
                P.op("act", lambda e: e.mul(out=qf[0:64, :n], in_=ps[0:64, :n], mul=MLA_SCALE), reads=[ps], writes=[qf])
            else:
                P.op("act", lambda e: e.mul(out=qf[0:96, :n], in_=ps[0:96, :n], mul=MLA_SCALE), reads=[ps], writes=[qf])
            qtb = qt[h % 2]
            P.copy("pool", qtb, qtb[0:96, :n], qf, qf[0:96, :n])
            P.act(kf2, kf2[:96, :n], qf, qf[0:96, :n], AF.Square)
            psq = next_ps(K)
            P.mm(psq, psq[:97, :n], selM[:, :], kf2[:96, :n], True, True, reads=[selM, kf2])
            P.ts("dve", prod, prod[96:97, :n], psq, psq[96:97, :n], kmaxM[96:97, h:h + 1], ALU.mult, extra_reads=[kmaxM])
            P.op("act", lambda e: e.sqrt(out=prod[96:97, :n], in_=prod[96:97, :n]), reads=[prod], writes=[prod])
            P.ts("dve", qtb, qtb[96:97, :n], prod, prod[96:97, :n], -1.0, ALU.mult)
            P.dma("sp", S["QM"].ap[h, :, t0:t0 + n], qtb[:, :n], reads=[qtb], writes=[S["QM"]])
        r_, s_ = raw[it % 2], sw[it % 2]
        if not is_ctx:
            P.dma("sp", c_[0:64, :n], C["rope_gqa_C"].ap[:, lat0:lat0 + n], reads=[C["rope_gqa_C"]], writes=[c_])
            P.dma("sp", sn_[0:64, :n], C["rope_gqa_S"].ap[:, lat0:lat0 + n], reads=[C["rope_gqa_S"]], writes=[sn_])
        for hq in range(8):
            P.dma("sp", r_[0:64, :n], zT.ap[416 + 64 * hq:416 + 64 * hq + 64, t0:t0 + n], reads=[zT], writes=[r_])
            if not is_ctx:
                P.dma("sp", s_[0:64, :n], zT.ap[1216 + 64 * hq:1216 + 64 * hq + 64, t0:t0 + n], reads=[zT], writes=[s_])
                P.tt("dve", rot, rot[0:64, :n], r_, r_[0:64, :n], c_, c_[0:64, :n], ALU.mult)
                P.tt("pool", rt2, rt2[0:64, :n], s_, s_[0:64, :n], sn_, sn_[0:64, :n], ALU.mult)
                P.stt(qf, qf[0:64, :n], rot, rot[0:64, :n], 1.0, rt2, rt2[0:64, :n], ALU.mult, ALU.add)
                P.ts("dve", qf, qf[0:64, :n], qf, qf[0:64, :n], GQA_SCALE, ALU.mult)
            else:
                P.ts("dve", qf, qf[0:64, :n], r_, r_[0:64, :n], GQA_SCALE, ALU.mult)
            qtb = qt[hq % 2]
            P.copy("pool", qtb, qtb[0:64, :n], qf, qf[0:64, :n])
            P.memset("pool", qtb, qtb[64:96, :n], 1.0)
            P.act(kf2, kf2[:64, :n], qf, qf[0:64, :n], AF.Square)
            psq = next_ps(K)
            P.mm(psq, psq[:65, :n], selG[:, :], kf2[:64, :n], True, True, reads=[selG, kf2])
            P.ts("dve", prod, prod[64:65, :n], psq, psq[64:65, :n], kmaxG[64:65, hq // 4:hq // 4 + 1], ALU.mult, extra_reads=[kmaxG])
            P.op("act", lambda e: e.sqrt(out=prod[64:65, :n], in_=prod[64:65, :n]), reads=[prod], writes=[prod])
            P.ts("dve", qtb, qtb[64:65, :n], prod, prod[64:65, :n], -1.0, ALU.mult)
            P.dma("sp", S["QG"].ap[hq, :, t0:t0 + n], qtb[0:66, :n], reads=[qtb], writes=[S["QG"]])
    P.release_to(markp)
    A = Ctx()
    A.mixT = mixT
    A.sel65 = sel65
    K.ps_reserved = {7}
    A.pot = K.ps[7]
    A.pt = [sb([128, 512], BF16, f"at_pt{k}") for k in range(4)]
    A.pti = 0
    A.ot = sb([65, 512], F32, "at_ot"); A.rden = sb([64, 512], F32, "at_rden")
    A.ob = [sb([64, 512], F32, f"at_ob{k}") for k in range(2)]
    A.obi = 0
    NKC = NT // 128
    kres = sb([97, NT], BF16, "at_k"); vres = sb([128, NKC, 65], BF16, "at_v")
    qb_ = [sb([97, 512], BF16, f"at_q{k}") for k in range(2)]
    qi = 0
    for h in range(8):
        P.dma("sp", kres[:, :], S["KM"].ap[h, :, :], reads=[S["KM"]], writes=[kres])
        with nc.allow_non_contiguous_dma(reason="token-major V rows"):
            P.dma("sp", vres[:], S["VM"].ap[h].rearrange("(c p) d -> p c d", p=128), reads=[S["VM"]], writes=[vres])
        qlist = [(NCX + k * 512, min(512, K.NL - k * 512), False) for k in range((K.NL + 511) // 512)]
        if need_ctx and NCX:
            qlist = [(0, NCX, True)] + qlist
        for (t0, n, is_ctx) in qlist:
            qb = qb_[qi % 2]
            qi += 1
            P.dma("sp", qb[:, :n], S["QM"].ap[h, :, t0:t0 + n], reads=[S["QM"]], writes=[qb])
            kcs = range(NCX // 128) if is_ctx else range(NKC)
            chunks = [(kres, kres[:, kc * 128:(kc + 1) * 128], vres, vres[:, kc, :], None, None, None) for kc in kcs]
            attn_core(K, A, qb, qb[:, :n], n, chunks, [(mixT.ap[h * 64:(h + 1) * 64, t0:t0 + n], 0, n)])
    mprev, mnext = C["k_mask_prev"], C["k_mask_next"]
    ks2 = sb([96, 8], BF16, "at_ks2")
    P.copy("dve", ks2, ks2[64:96, :], sinkb, sinkb[64:96, :])
    P.memset("pool", ks2, ks2[64:65, :], 1.0)
    NB = K.NL // 128
    for kh in range(2):
        P.dma("sp", kres[0:66, :], S["KG"].ap[kh, :, :], reads=[S["KG"]], writes=[kres])
        with nc.allow_non_contiguous_dma(reason="token-major V rows"):
            P.dma("sp", vres[:], S["VG"].ap[kh].rearrange("(c p) d -> p c d", p=128), reads=[S["VG"]], writes=[vres])
        blocks = [(NCX + b * 128, b, False) for b in range(NB)]
        if need_ctx and NCX:
            blocks = [(cb * 128, cb, True) for cb in range(NCX // 128)] + blocks
        for (t0, b, is_ctx) in blocks:
            qb = qb_[qi % 2]
            qi += 1
            P.dma("sp", qb[0:66, :].rearrange("r (g t) -> r g t", t=128),
                  S["QG"].ap[4 * kh:4 * kh + 4, :, t0:t0 + 128].rearrange("g r t -> r g t"), reads=[S["QG"]], writes=[qb])
            chunks = []
            for cb in range(NCX // 128):
                chunks.append((kres, kres[0:66, cb * 128:(cb + 1) * 128], vres, vres[:, cb, :], None, None, None))
            if not is_ctx:
                kc0 = NCX // 128 + b
                if b > 0:
                    chunks.append((kres, kres[0:66, (kc0 - 1) * 128:kc0 * 128], vres, vres[:, kc0 - 1, :], (mprev, mprev[:, :]), None, None))
                chunks.append((kres, kres[0:66, kc0 * 128:(kc0 + 1) * 128], vres, vres[:, kc0, :], None, None, None))
                if b < NB - 1:
                    chunks.append((kres, kres[0:66, (kc0 + 1) * 128:(kc0 + 2) * 128], vres, vres[:, kc0 + 1, :], (mnext, mnext[:, :]), None, None))
            for g in range(4):
                hq = 4 * kh + g
                chunks.append((ks2, ks2[64:66, hq:hq + 1], vsb, vsb[0:1, :], None, (g * 128, 128), (64, 66)))
            outs = [(mixT.ap[512 + (4 * kh + g) * 64:512 + (4 * kh + g + 1) * 64, t0:t0 + 128], g * 128, 128) for g in range(4)]
            attn_core(K, A, qb, qb[0:66, :], 512, chunks, outs)
    K.ps_reserved = set()
    P.release_to(mark)


NCTX_FULL, NLAT_FULL, DEPTH = 256, 8192, 4
_W_SHAPES = dict(
    c_ctx=[D], mod_w=[DEPTH, D, 6 * D], mod_b=[DEPTH, 6 * D],
    norm_mix=[DEPTH, D], norm_ffn=[DEPTH, D], final_norm=[D],
    ev_w_in=[2, D, 2048], ev_conv_w=[2, 3, 1536], ev_conv_b=[2, 1536],
    hy_w1=[2, 33, 64], hy_b1=[2, 64], hy_w2=[2, 64, 64], hy_b2=[2, 64], hy_w3=[2, 64, 2048], hy_freq=[2, 64],
    hy_decay=[2, 2, 512], hy_bias=[2, 2, 512],
    s5_a_re=[2, 2, 32, 64], s5_a_im=[2, 2, 32, 64], s5_log_dt=[2, 2, 32],
    s5_b_re=[2, 2, 32, 64, 16], s5_b_im=[2, 2, 32, 64, 16], s5_c_re=[2, 2, 32, 16, 64], s5_c_im=[2, 2, 32, 16, 64],
    s5_d=[2, 512], s5_w_glu=[2, 512, 512], ev_w_out=[2, D, D],
    ff_w_gate=[2, D, 2816], ff_w_up=[2, D, 2816], ff_w_down=[2, 2816, D],
    od_w_in=[2, D, 1184], mla_q_norm=[2, 256], mla_w_uq=[2, 256, 768], mla_kv_norm=[2, 128], mla_w_ukv=[2, 128, 1024],
    gqa_sink=[2, 8], od_w_out=[2, D, D],
    moe_router=[2, D, 8], moe_w_gate=[2, 8, D, 3584], moe_w_up=[2, 8, D, 3584], moe_w_down=[2, 8, 3584, D],
)
_DRAM_ONLY_CONSTS = ("rope_mla_C", "rope_mla_S", "rope_gqa_C", "rope_gqa_S", "hy8192_featsT", "hy256_featsT",
                     "hc_Fc", "hc_Fns", "hc_Ic", "hc_Ins")


def all_consts():
    c = {}
    c.update(host_consts())
    c.update(hyena_consts(NLAT_FULL))
    c["hy256_featsT"] = hyena_consts(NCTX_FULL)["hy256_featsT"]
    c.update(hyena_ctx_consts())
    c.update(hyena_shared_consts())
    c.update(rope_consts(NLAT_FULL))
    return c


def build_program():
    P = Prog()
    nc = P.nc
    K = setup_common(P, NCTX_FULL, NLAT_FULL)
    NT = K.NT
    inp = lambda k, shp: T(nc.dram_tensor(k, list(shp), F32, kind="ExternalInput").ap())
    I = {k: inp(k, v) for k, v in _W_SHAPES.items()}
    I["hin"] = inp("hin", [D, NT])
    I["c"] = inp("c", [D])
    consts = all_consts()
    Cd = {k: inp(k, v.shape) for k, v in consts.items()}
    HALF = NLAT_FULL // 2
    I["sel"] = inp("sel", [128, 2])
    outT = T(nc.dram_tensor("outT", [D, HALF], F32, kind="ExternalOutput").ap())
    hT2 = P.dram("hT2", [D, HALF])
    zT = P.dram("zT", [2048, NT])
    mixT = P.dram("mixT", [D, NT])
    K.yS5 = P.dram("yS5", [512, NT])
    kT = P.dram("kT", [2048, NLAT_FULL])
    KFr = P.dram("KFr", [1024, 128, 128])
    KFi = P.dram("KFi", [1024, 128, 128])
    S = dict(QM=P.dram("QM", [8, 97, NT], BF16), KM=P.dram("KM", [8, 97, NT], BF16), VM=P.dram("VM", [8, NT, 65], BF16),
             QG=P.dram("QG", [8, 66, NT], BF16), KG=P.dram("KG", [2, 66, NT], BF16), VG=P.dram("VG", [2, NT, 65], BF16))

    def load_consts(keys):
        C = {}
        for k in keys:
            d = Cd[k]
            if k in _DRAM_ONLY_CONSTS:
                C[k + "_dram" if k.endswith("featsT") else k] = d
            else:
                t = P.sb(list(d.ap.shape), F32, "c_" + k)
                P.dma("sp", t[:], d.ap[:, :], reads=[d], writes=[t])
                C[k] = t
        return C

    for i0 in range((NT + 1023) // 1024):
        n = min(1024, NT - i0 * 1024)
        P.dma("sp", K.hT.ap[:, i0 * 1024:i0 * 1024 + n], I["hin"].ap[:, i0 * 1024:i0 * 1024 + n], reads=[I["hin"]], writes=[K.hT])
    mods = mods_all(K, list(range(DEPTH)), I["c"], I["c_ctx"], I["mod_w"], I["mod_b"], I["norm_mix"], I["norm_ffn"])
    for l in range(DEPTH):
        i = l // 2
        need_ctx = l < DEPTH - 1
        if l % 2 == 0:
            inproj_stage(K, mods[l], T(None, I["ev_w_in"].ap[i]), zT)
            m = P.mark()
            C = load_consts([k for k in Cd if k.startswith(f"hy{NLAT_FULL}_") or k.startswith("hy_")])
            hyena_stage(K, i, I, zT, mixT, C, NLAT_FULL, NCTX_FULL, KFr, KFi, kT, f"l{l}")
            P.release_to(m)
            m = P.mark()
            C = load_consts(["hy_tau512", "k_ident", f"hy{NCTX_FULL}_featsT"])
            hyena_ctx_stage(K, i, I, zT, mixT, C, Cd, 0)
            P.release_to(m)
            m = P.mark()
            C = load_consts(["k_tau", "k_maskB", "k_maskC", "k_ident"])
            s5_stage(K, i, I, zT, mixT, C)
            P.release_to(m)
            outproj_stage(K, mods[l], T(None, I["ev_w_out"].ap[i]), mixT)
            experts = [(T(None, I["ff_w_gate"].ap[i]), T(None, I["ff_w_up"].ap[i]), T(None, I["ff_w_down"].ap[i]))]
            ffn_stage(K, mods[l], experts, router=None, ST=1024)
        else:
            inproj_stage(K, mods[l], T(None, I["od_w_in"].ap[i]), zT, segs=odd_segs())
            m = P.mark()
            C = load_consts(["k_ident", "k_mask_prev", "k_mask_next", "k_selM", "k_selG", "k_sel65", "k_vsink",
                             "rope_mla_C", "rope_mla_S", "rope_gqa_C", "rope_gqa_S"])
            odd_mixer_stage(K, i, I, zT, mixT, C, S, need_ctx)
            P.release_to(m)
            outproj_stage(K, mods[l], T(None, I["od_w_out"].ap[i]), mixT)
            experts = [(T(None, I["moe_w_gate"].ap[i, e]), T(None, I["moe_w_up"].ap[i, e]), T(None, I["moe_w_down"].ap[i, e]))
                       for e in range(8)]
            if l < DEPTH - 1:
                ffn_stage(K, mods[l], experts, router=T(None, I["moe_router"].ap[i]), ST=1024)
            else:
                m = P.mark()
                selt = P.sb([128, 2], F32, "sel")
                P.dma("sp", selt[:], I["sel"].ap[:, :], reads=[I["sel"]], writes=[selt])
                sa = [P.sb([128, KC, 512], F32, f"sel_a{k}") for k in range(2)]
                sb_ = [P.sb([128, KC, 512], F32, f"sel_b{k}") for k in range(2)]
                for j in range(HALF // 512):
                    a_, b_ = sa[j % 2], sb_[j % 2]
                    P.dma("sp", a_[:], K.hT.ap[:, NCTX_FULL + j * 512:NCTX_FULL + (j + 1) * 512].rearrange("(k p) t -> p k t", p=128),
                          reads=[K.hT], writes=[a_])
                    P.dma("sp", b_[:], K.hT.ap[:, NCTX_FULL + HALF + j * 512:NCTX_FULL + HALF + (j + 1) * 512].rearrange("(k p) t -> p k t", p=128),
                          reads=[K.hT], writes=[b_])
                    P.ts("dve", a_, a_[:], a_, a_[:], selt[:, 0:1], ALU.mult, extra_reads=[selt])
                    P.stt(a_, a_[:], b_, b_[:], selt[:, 1:2], a_, a_[:], ALU.mult, ALU.add, extra_reads=[selt])
                    P.dma("pool", hT2.ap[:, j * 512:(j + 1) * 512].rearrange("(k p) t -> p k t", p=128), a_[:], reads=[a_], writes=[hT2])
                P.release_to(m)
                K2 = Ctx()
                K2.__dict__.update(K.__dict__)
                K2.hT, K2.NC, K2.NL, K2.NT = hT2, 0, HALF, HALF
                ffn_stage(K2, mods[l], experts, router=T(None, I["moe_router"].ap[i]), ST=1024,
                          tok_ranges=[(k * 1024, 1024, 0) for k in range(HALF // 1024)])
                final_norm_stage(K2, I["final_norm"], outT, 0, HALF)
    P.finish()
    return P


def kernel(**inputs):
    x = np.asarray(inputs["x"], dtype=np.float32)
    ctx = np.asarray(inputs["ctx"], dtype=np.float32)
    B = x.shape[0]
    P = build_program()
    shared = {k: np.ascontiguousarray(np.asarray(inputs[k], dtype=np.float32)) for k in _W_SHAPES}
    shared.update(all_consts())
    in_maps = []
    for core in range(8):
        b = core // 2
        m = dict(shared)
        m["hin"] = np.ascontiguousarray(np.concatenate([ctx[b], x[b]], axis=0).T)
        m["c"] = np.ascontiguousarray(np.asarray(inputs["c"], dtype=np.float32)[b])
        sel = np.zeros((128, 2), np.float32)
        sel[:, core % 2] = 1.0
        m["sel"] = sel
        in_maps.append(m)
    res = run_bass_kernel_spmd(P.nc, in_maps, core_ids=list(range(8)))
    out = np.empty((B, NLAT_FULL, D), dtype=np.float32)
    half = NLAT_FULL // 2
    for b in range(B):
        out[b, :half] = res.results[2 * b]["outT"].T
        out[b, half:] = res.results[2 * b + 1]["outT"].T
    return out
```

```python
import math
import numpy as np
import concourse.bass as bass
import concourse.mybir as mybir
from concourse.bass_utils import run_bass_kernel_spmd

F32 = mybir.dt.float32
BF16 = mybir.dt.bfloat16
I32 = mybir.dt.int32
AF = mybir.ActivationFunctionType
ALU = mybir.AluOpType
AX = mybir.AxisListType

D = 1024
KC = D // 128
EPS = 1e-6
HY_WINDOW_SHIFT = 0.05
HY_DT = BF16


class Trk:
    __slots__ = ("w", "r")

    def __init__(self):
        self.w = None
        self.r = {}


class T:
    def __init__(self, t, ap=None):
        self.t = t
        self.ap = ap if ap is not None else t
        self.whole = Trk()
        self.parts = {}

    def __getitem__(self, idx):
        return self.ap[idx]

    def p(self, key):
        return (self, key)


def _trks(x, for_write):
    if isinstance(x, tuple):
        t, key = x
        if key not in t.parts:
            t.parts[key] = Trk()
        return [t.whole, t.parts[key]], [t.parts[key]]
    return [x.whole] + list(x.parts.values()), [x.whole]


class Prog:
    NSLOT = 6

    def __init__(self):
        nc = bass.Bass("TRN2", target_bir_lowering=False)
        self.nc = nc
        self.E = dict(pe=nc.tensor, dve=nc.vector, act=nc.scalar, pool=nc.gpsimd, sp=nc.sync)
        self.sem = {}
        self.val = {}
        for e in ("pe", "dve", "act", "pool"):
            self.sem[e] = nc.semaphore("sem_" + e).__enter__()
            self.val[e] = 0
        self.slots = {}
        self.rr = {}
        for q in ("sp", "act", "pool"):
            self.slots[q] = []
            self.rr[q] = 0
            for i in range(self.NSLOT):
                k = f"dma_{q}{i}"
                self.sem[k] = nc.semaphore(k).__enter__()
                self.val[k] = 0
                self.slots[q].append(k)
        self.seen = {e: {} for e in self.E}
        self.n_ins = 0
        self._names = 0
        self.out_events = []
        self._stack = []

    def name(self, base):
        self._names += 1
        return f"{base}_{self._names}"

    def sb(self, shape, dt=F32, name="sb"):
        cm = self.nc.sbuf_tensor(self.name(name), list(shape), dt)
        t = T(cm.__enter__())
        self._stack.append(cm)
        return t

    def mark(self):
        return len(self._stack)

    def release_to(self, mark):
        self.barrier()
        while len(self._stack) > mark:
            self._stack.pop().__exit__(None, None, None)

    def barrier(self):
        for eng in self.E:
            for k in self.sem:
                if self.val[k] > 0:
                    self._wait(eng, (k, self.val[k]))

    def psum(self, shape, dt=F32, name="ps"):
        return T(self.nc.psum_tensor(self.name(name), list(shape), dt).__enter__())

    def dram(self, name, shape, dt=F32, kind="Internal"):
        return T(self.nc.dram_tensor(name, list(shape), dt, kind=kind).ap())

    def _wait(self, eng, ev):
        if ev is None:
            return
        key, v = ev
        if key == eng and eng == "pe":
            return
        if self.seen[eng].get(key, 0) >= v:
            return
        self.E[eng].wait_ge(self.sem[key], v)
        self.seen[eng][key] = v

    def _deps(self, eng, reads, writes):
        for x in reads:
            chk, _ = _trks(x, False)
            for tr in chk:
                self._wait(eng, tr.w)
        for x in writes:
            chk, _ = _trks(x, True)
            for tr in chk:
                self._wait(eng, tr.w)
                for k, v in tr.r.items():
                    self._wait(eng, (k, v))

    def _record(self, ev, reads, writes):
        for x in reads:
            _, upd = _trks(x, False)
            for tr in upd:
                tr.r[ev[0]] = ev[1]
        for x in writes:
            chk, upd = _trks(x, True)
            if not isinstance(x, tuple):
                x.parts.clear()
            for tr in upd:
                tr.w = ev
                tr.r = {}

    def op(self, eng, fn, reads=(), writes=()):
        self._deps(eng, reads, writes)
        ins = fn(self.E[eng])
        self.val[eng] += 1
        ins.then_inc(self.sem[eng], 1)
        ev = (eng, self.val[eng])
        self._record(ev, reads, writes)
        self.n_ins += 1
        return ev

    def dma(self, q, out, in_, reads=(), writes=(), **kw):
        self._deps(q, reads, writes)
        slot = self.slots[q][self.rr[q] % self.NSLOT]
        self.rr[q] += 1
        if self.val[slot] > 0:
            self._wait(q, (slot, self.val[slot]))
        ins = self.E[q].dma_start(out=out, in_=in_, **kw)
        self.val[slot] += 16
        ins.then_inc(self.sem[slot], 16)
        ev = (slot, self.val[slot])
        self._record(ev, reads, writes)
        self.n_ins += 1
        return ev

    def finish(self):
        for ev in self.out_events:
            self._wait("sp", ev)
        for k in self.sem:
            if self.val[k] > 0:
                self._wait("sp", (k, self.val[k]))

    def mm(self, ps, out_ap, lhsT, rhs, start, stop, reads, eng="pe"):
        return self.op("pe", lambda e: e.matmul(out_ap, lhsT, rhs, start=start, stop=stop),
                       reads=reads, writes=[ps])

    def act(self, out_t, out_ap, in_t, in_ap, func, bias=None, scale=1.0, extra_reads=(), accum_out=None):
        kw = {}
        if bias is not None:
            kw["bias"] = bias
        if accum_out is not None:
            kw["accum_out"] = accum_out
        return self.op("act", lambda e: e.activation(out=out_ap, in_=in_ap, func=func, scale=scale, **kw),
                       reads=[in_t] + list(extra_reads), writes=[out_t])

    def tt(self, eng, out_t, out_ap, a_t, a_ap, b_t, b_ap, op):
        return self.op(eng, lambda e: e.tensor_tensor(out=out_ap, in0=a_ap, in1=b_ap, op=op),
                       reads=[a_t, b_t], writes=[out_t])

    def ts(self, eng, out_t, out_ap, a_t, a_ap, s1, op0, s2=None, op1=None, extra_reads=()):
        if op1 is None:
            return self.op(eng, lambda e: e.tensor_scalar(out=out_ap, in0=a_ap, scalar1=s1, scalar2=None, op0=op0),
                           reads=[a_t] + list(extra_reads), writes=[out_t])
        return self.op(eng, lambda e: e.tensor_scalar(out=out_ap, in0=a_ap, scalar1=s1, scalar2=s2, op0=op0, op1=op1),
                       reads=[a_t] + list(extra_reads), writes=[out_t])

    def stt(self, out_t, out_ap, a_t, a_ap, scalar, b_t, b_ap, op0, op1, extra_reads=()):
        return self.op("dve", lambda e: e.scalar_tensor_tensor(out=out_ap, in0=a_ap, scalar=scalar, in1=b_ap, op0=op0, op1=op1),
                       reads=[a_t, b_t] + list(extra_reads), writes=[out_t])

    def copy(self, eng, out_t, out_ap, in_t, in_ap):
        if eng == "act":
            return self.op("act", lambda e: e.copy(out=out_ap, in_=in_ap), reads=[in_t], writes=[out_t])
        return self.op(eng, lambda e: e.tensor_copy(out=out_ap, in_=in_ap), reads=[in_t], writes=[out_t])

    def memset(self, eng, out_t, out_ap, v):
        return self.op(eng, lambda e: e.memset(out_ap, v), reads=[], writes=[out_t])


class Ctx:
    pass


def setup_common(P, NT_CTX, NT_LAT):
    K = Ctx()
    K.P = P
    K.NC = NT_CTX
    K.NL = NT_LAT
    K.NT = NT_CTX + NT_LAT
    nc = P.nc
    K.ps = [P.psum([128, 512], F32, name=f"bank{i}") for i in range(8)]
    K.ps_i = 0
    K.ps_reserved = set()
    K.ones = P.sb([128, 128], F32, "ones")
    P.memset("pool", K.ones, K.ones[:], 1.0)
    K.onesb = P.sb([128, 128], BF16, "onesb")
    P.memset("pool", K.onesb, K.onesb[:], 1.0)
    K.hT = P.dram("hT", [D, K.NT], F32)
    return K


def next_ps(K):
    while True:
        b = K.ps[K.ps_i % 8]
        K.ps_i += 1
        if (K.ps_i - 1) % 8 not in K.ps_reserved:
            return b


def dvec(ap1d, k):
    return ap1d.rearrange("(k p) -> p k", p=128)


def mods_all(K, layers, c_in, cctx_in, mod_w, mod_b, norm_mix, norm_ffn):
    P = K.P
    nc = P.nc
    res = {}
    pers = {}
    for l in layers:
        pers[l] = dict(M=P.sb([128, 48, 2], F32, f"modM{l}"), A_mix=P.sb([128, KC, 2], F32, f"A_mix{l}"),
                       A_ffn=P.sb([128, KC, 2], F32, f"A_ffn{l}"))
    mark = P.mark()
    craw = P.sb([128, KC, 2], F32, "craw")
    sT = P.sb([128, KC, 2], F32, "sT")
    modw_buf = [P.sb([128, KC, 256], F32, f"modw{i}") for i in range(2)]
    bT = P.sb([128, 48], F32, "modb")
    gm = P.sb([128, KC], F32, "gmix")
    gf = P.sb([128, KC], F32, "gffn")
    with nc.allow_non_contiguous_dma(reason="tiny vector load"):
        P.dma("sp", craw[:, :, 0], dvec(c_in.ap, KC), reads=[c_in], writes=[craw])
        P.dma("sp", craw[:, :, 1], dvec(cctx_in.ap, KC), reads=[cctx_in], writes=[craw])
    P.act(sT, sT[:], craw, craw[:], AF.Silu)
    wi = 0
    for l in layers:
        M = pers[l]["M"]
        with nc.allow_non_contiguous_dma(reason="tiny vector load"):
            P.dma("sp", bT[:], dvec(mod_b.ap[l], 48), reads=[mod_b], writes=[bT])
            P.dma("sp", gm[:], dvec(norm_mix.ap[l], KC), reads=[norm_mix], writes=[gm])
            P.dma("sp", gf[:], dvec(norm_ffn.ap[l], KC), reads=[norm_ffn], writes=[gf])
        for s in range(24):
            wb = modw_buf[wi % 2]
            wi += 1
            P.dma("sp", wb[:], mod_w.ap[l, :, s * 256:(s + 1) * 256].rearrange("(k p) n -> p k n", p=128),
                  reads=[mod_w], writes=[wb])
            ps = next_ps(K)
            for j in range(2):
                for k in range(KC):
                    P.mm(ps, ps[:, j * 2:j * 2 + 2], wb[:, k, j * 128:(j + 1) * 128], sT[:, k, :],
                         start=(k == 0), stop=(k == KC - 1), reads=[wb, sT])
            for j in range(2):
                jj = s * 2 + j
                P.ts("dve", M, M[:, jj, :], ps, ps[:, j * 2:j * 2 + 2], bT[:, jj:jj + 1], ALU.add, extra_reads=[bT])
        out = dict(pers[l])
        for nm, sc_i, g in (("mix", 1, gm), ("ffn", 4, gf)):
            A = pers[l]["A_" + nm]
            for col in range(2):
                P.stt(A, A[:, :, col], M, M[:, sc_i * 8:(sc_i + 1) * 8, col], 1.0, g, g[:], ALU.add, ALU.mult)
        out["B_mix"] = (lambda M: (lambda k, col: M[:, 0 * 8 + k, col:col + 1]))(M)
        out["G_mix"] = (lambda M: (lambda k, col: M[:, 2 * 8 + k, col:col + 1]))(M)
        out["B_ffn"] = (lambda M: (lambda k, col: M[:, 3 * 8 + k, col:col + 1]))(M)
        out["G_ffn"] = (lambda M: (lambda k, col: M[:, 5 * 8 + k, col:col + 1]))(M)
        res[l] = out
    P.release_to(mark)
    return res


def norm_tile(K, h_t, hc0, n, A, Bf, M, col, y_bf, yc0, y_f32=None, sq=None, rstd=None):
    P = K.P
    ps = next_ps(K)
    for k in range(KC):
        P.act(sq, sq[:, k, :n], h_t, h_t[:, k, hc0:hc0 + n], AF.Square)
    for k in range(KC):
        P.mm(ps, ps[:, :n], K.ones[:], sq[:, k, :n], start=(k == 0), stop=(k == KC - 1), reads=[K.ones, sq])
    P.ts("dve", rstd, rstd[:, :n], ps, ps[:, :n], 1.0 / D, ALU.mult, EPS, ALU.add)
    P.op("act", lambda e: e.sqrt(out=rstd[:, :n], in_=rstd[:, :n]), reads=[rstd], writes=[rstd])
    P.op("dve", lambda e: e.reciprocal(out=rstd[:, :n], in_=rstd[:, :n]), reads=[rstd], writes=[rstd])
    for k in range(KC):
        P.stt(sq, sq[:, k, :n], h_t, h_t[:, k, hc0:hc0 + n], A[:, k, col:col + 1], rstd, rstd[:, :n], ALU.mult, ALU.mult,
              extra_reads=[A])
        if y_f32 is not None:
            P.act(y_f32, y_f32[:, k, :n], sq, sq[:, k, :n], AF.Identity, bias=Bf(k, col), extra_reads=[M])
            P.copy("pool", y_bf, y_bf[:, k, yc0:yc0 + n], y_f32, y_f32[:, k, :n])
        else:
            P.act(y_bf, y_bf[:, k, yc0:yc0 + n], sq, sq[:, k, :n], AF.Identity, bias=Bf(k, col), extra_reads=[M])


def ffn_stage(K, mods, experts, router=None, tok_ranges=None, ST=1024, tag="ffn"):
    P = K.P
    nc = P.nc
    FF = experts[0][0].ap.shape[1]
    NE = len(experts)
    slabs = []
    f0 = 0
    while f0 < FF:
        fs = min(512, FF - f0)
        slabs.append((f0, fs))
        f0 += fs
    A, Bf, Gf, M = mods["A_ffn"], mods["B_ffn"], mods["G_ffn"], mods["M"]
    if tok_ranges is None:
        tok_ranges = [(0, K.NC, 1)] + [(K.NC + i * ST, min(ST, K.NL - i * ST), 0) for i in range((K.NL + ST - 1) // ST)]
    mark = P.mark()
    if True:
        B = Ctx()
        B.acc = P.sb([128, KC, ST], F32, "ffn_acc")
        B.y = P.sb([128, KC, ST], BF16, "ffn_y")
        B.sq = P.sb([128, KC, 512], F32, "ffn_sq")
        B.yf = P.sb([128, KC, 512], F32, "ffn_yf")
        B.rstd = P.sb([128, 512], F32, "ffn_rstd")
        B.hid = P.sb([128, 4, ST], BF16, "ffn_hid")
        B.wg = [P.sb([128, KC, 512], BF16, f"ffn_wg{i}") for i in range(2)]
        B.wu = [P.sb([128, KC, 512], BF16, f"ffn_wu{i}") for i in range(2)]
        B.wd = [P.sb([128, 4, D], BF16, f"ffn_wd{i}") for i in range(2)]
        B.sg = [P.sb([128, 512], F32, f"ffn_sg{i}") for i in range(2)]
        B.tmp = [P.sb([128, 512], F32, f"ffn_tmp{i}") for i in range(2)]
        B.gate_bc = P.sb([128, 8, ST], F32, "ffn_gatebc")
        B.rt = P.sb([128, KC, 8], F32, "ffn_router")
        B.lg = P.sb([128, 8], F32, "ffn_lg")
        B.mx = P.sb([128, 8], F32, "ffn_mx")
        B.gt = P.sb([128, 8], F32, "ffn_gt")
        B.m1 = P.sb([128, 8], F32, "ffn_m1")
        B.wv = P.sb([128, 4], F32, "ffn_wv")
        B.ident = P.sb([128, 128], F32, "ffn_ident")
        B.gcol = P.sb([128, 128], F32, "ffn_gcol")
        B.i = 0
        P.memset("pool", B.ident, B.ident[:], 0.0)
        P.op("pool", lambda e: e.affine_select(out=B.ident[:], in_=B.ident[:], pattern=[[-1, 128]], base=0,
                                                channel_multiplier=1, compare_op=ALU.not_equal, fill=1.0),
             reads=[B.ident], writes=[B.ident])
    if router is not None:
        P.dma("sp", B.rt[:], router.ap.rearrange("(k p) e -> p k e", p=128), reads=[router], writes=[B.rt])
    for (t0, n, col) in tok_ranges:
        ntile = (n + 511) // 512
        for j in range(ntile):
            c0 = j * 512
            w = min(512, n - c0)
            P.dma("sp", B.acc[:, :, c0:c0 + w], K.hT.ap[:, t0 + c0:t0 + c0 + w].rearrange("(k p) t -> p k t", p=128),
                  reads=[K.hT], writes=[B.acc])
            norm_tile(K, B.acc, c0, w, A, Bf, M, col, B.y, c0,
                      y_f32=(B.yf if router is not None else None), sq=B.sq, rstd=B.rstd)
            if router is not None:
                for q in range((w + 127) // 128):
                    qw = min(128, w - q * 128)
                    ps = next_ps(K)
                    for k in range(KC):
                        P.mm(ps, ps[:qw, 0:8], B.yf[:, k, q * 128:q * 128 + qw], B.rt[:, k, :],
                             start=(k == 0), stop=(k == KC - 1), reads=[B.yf, B.rt])
                    P.copy("dve", B.lg, B.lg[:qw, :], ps, ps[:qw, 0:8])
                    P.op("dve", lambda e: e.max(out=B.mx[:qw, :], in_=B.lg[:qw, :]), reads=[B.lg], writes=[B.mx])
                    P.tt("dve", B.wv, B.wv[:qw, 0:1], B.mx, B.mx[:qw, 1:2], B.mx, B.mx[:qw, 0:1], ALU.subtract)
                    P.act(B.wv, B.wv[:qw, 1:2], B.wv, B.wv[:qw, 0:1], AF.Exp)
                    P.ts("dve", B.wv, B.wv[:qw, 1:2], B.wv, B.wv[:qw, 1:2], 1.0, ALU.add)
                    P.op("dve", lambda e: e.reciprocal(out=B.wv[:qw, 2:3], in_=B.wv[:qw, 1:2]), reads=[B.wv], writes=[B.wv])
                    P.ts("dve", B.wv, B.wv[:qw, 3:4], B.wv, B.wv[:qw, 2:3], -1.0, ALU.mult, 1.0, ALU.add)
                    P.ts("dve", B.gt, B.gt[:qw, :], B.lg, B.lg[:qw, :], B.mx[:qw, 0:1], ALU.is_equal,
                         B.wv[:qw, 2:3], ALU.mult, extra_reads=[B.mx, B.wv])
                    P.ts("dve", B.m1, B.m1[:qw, :], B.lg, B.lg[:qw, :], B.mx[:qw, 1:2], ALU.is_equal,
                         B.wv[:qw, 3:4], ALU.mult, extra_reads=[B.mx, B.wv])
                    P.tt("dve", B.gt, B.gt[:qw, :], B.gt, B.gt[:qw, :], B.m1, B.m1[:qw, :], ALU.add)
                    for e_i in range(NE):
                        P.ts("dve", B.gcol, B.gcol[:qw, :], K.ones, K.ones[:qw, :], B.gt[:qw, e_i:e_i + 1], ALU.mult,
                             extra_reads=[B.gt])
                        ps2 = next_ps(K)
                        P.mm(ps2, ps2[:, :qw], B.gcol[:qw, :], B.ident[:qw, :qw], start=True, stop=True,
                             reads=[B.gcol, B.ident])
                        P.copy("act", B.gate_bc, B.gate_bc[:, e_i, c0 + q * 128:c0 + q * 128 + qw], ps2, ps2[:, :qw])
        for e_i, (wg, wu, wd) in enumerate(experts):
            for (f0, fs) in slabs:
                i = B.i % 2
                B.i += 1
                nfc = fs // 128
                P.dma("pool", B.wg[i][:, :, :fs], wg.ap[:, f0:f0 + fs].rearrange("(k p) f -> p k f", p=128),
                      reads=[wg], writes=[B.wg[i]])
                P.dma("pool", B.wu[i][:, :, :fs], wu.ap[:, f0:f0 + fs].rearrange("(k p) f -> p k f", p=128),
                      reads=[wu], writes=[B.wu[i]])
                P.dma("pool", B.wd[i][:, :nfc, :], wd.ap[f0:f0 + fs, :].rearrange("(c p) d -> p c d", p=128),
                      reads=[wd], writes=[B.wd[i]])
                for j in range(ntile):
                    c0 = j * 512
                    w = min(512, n - c0)
                    for fc in range(nfc):
                        pg = next_ps(K)
                        pu = next_ps(K)
                        for k in range(KC):
                            P.mm(pg, pg[:, :w], B.wg[i][:, k, fc * 128:(fc + 1) * 128], B.y[:, k, c0:c0 + w],
                                 start=(k == 0), stop=(k == KC - 1), reads=[B.wg[i], B.y])
                        for k in range(KC):
                            P.mm(pu, pu[:, :w], B.wu[i][:, k, fc * 128:(fc + 1) * 128], B.y[:, k, c0:c0 + w],
                                 start=(k == 0), stop=(k == KC - 1), reads=[B.wu[i], B.y])
                        sg = B.sg[(j * 4 + fc) % 2]
                        P.act(sg, sg[:, :w], pg, pg[:, :w], AF.Silu)
                        P.tt("dve", B.hid, B.hid[:, fc, c0:c0 + w], sg, sg[:, :w], pu, pu[:, :w], ALU.mult)
                    for oc in range(KC):
                        po = next_ps(K)
                        for fc in range(nfc):
                            P.mm(po, po[:, :w], B.wd[i][:, fc, oc * 128:(oc + 1) * 128], B.hid[:, fc, c0:c0 + w],
                                 start=(fc == 0), stop=(fc == nfc - 1), reads=[B.wd[i], B.hid])
                        if router is not None:
                            tmp = B.tmp[oc % 2]
                            P.stt(tmp, tmp[:, :w], po, po[:, :w], Gf(oc, col), B.gate_bc, B.gate_bc[:, e_i, c0:c0 + w],
                                  ALU.mult, ALU.mult, extra_reads=[M])
                            P.tt("pool", B.acc, B.acc[:, oc, c0:c0 + w], B.acc, B.acc[:, oc, c0:c0 + w], tmp, tmp[:, :w], ALU.add)
                        else:
                            P.stt(B.acc, B.acc[:, oc, c0:c0 + w], po, po[:, :w], Gf(oc, col), B.acc, B.acc[:, oc, c0:c0 + w],
                                  ALU.mult, ALU.add, extra_reads=[M])
        for j in range(ntile):
            c0 = j * 512
            w = min(512, n - c0)
            P.dma("sp", K.hT.ap[:, t0 + c0:t0 + c0 + w].rearrange("(k p) t -> p k t", p=128), B.acc[:, :, c0:c0 + w],
                  reads=[B.acc], writes=[K.hT])
    if hasattr(K, "dbg") and router is not None:
        P.dma("sp", K.dbg["gate_bc"].ap[:, :, :], B.gate_bc[:, :, :], reads=[B.gate_bc], writes=[K.dbg["gate_bc"]])
        P.dma("sp", K.dbg["ident"].ap[:, :], B.ident[:, :], reads=[B.ident], writes=[K.dbg["ident"]])
        for i_, t_ in enumerate((B.lg, B.mx, B.gt, B.m1)):
            P.dma("sp", K.dbg["small"].ap[:, i_ * 8:(i_ + 1) * 8], t_[:, :], reads=[t_], writes=[K.dbg["small"]])
        P.dma("sp", K.dbg["small"].ap[:, 32:36], B.wv[:, :], reads=[B.wv], writes=[K.dbg["small"]])
    P.release_to(mark)


def inproj_stage(K, mods, w_in, zT, segs=None):
    P = K.P
    nc = P.nc
    if segs is None:
        segs = [(0, w_in.ap.shape[1])]
    N = sum(n for _, n in segs)
    A, Bf, M = mods["A_mix"], mods["B_mix"], mods["M"]
    mark = P.mark()
    nch = (N + 127) // 128
    wst = [P.sb([128, KC, 512], F32, f"ip_wst{i}") for i in range(2)]
    wb = P.sb([128, KC, N], BF16, "ip_w")
    o0 = 0
    si = 0
    for (c0, ncol) in segs:
        for s0 in range(0, ncol, 512):
            cw = min(512, ncol - s0)
            st = wst[si % 2]
            si += 1
            with nc.allow_non_contiguous_dma(reason="weight column segments"):
                P.dma("sp", st[:, :, :cw], w_in.ap[:, c0 + s0:c0 + s0 + cw].rearrange("(k p) n -> p k n", p=128),
                      reads=[w_in], writes=[st])
            P.copy("pool", wb, wb[:, :, o0 + s0:o0 + s0 + cw], st, st[:, :, :cw])
        o0 += ncol
    h = [P.sb([128, KC, 512], F32, f"ip_h{i}") for i in range(2)]
    y = [P.sb([128, KC, 512], BF16, f"ip_y{i}") for i in range(2)]
    sq = P.sb([128, KC, 512], F32, "ip_sq")
    rstd = P.sb([128, 512], F32, "ip_rstd")
    zo = [P.sb([128, 512], F32, f"ip_zo{i}") for i in range(4)]
    ranges = []
    if K.NC:
        ranges.append((0, K.NC, 1))
    ranges += [(K.NC + i * 512, min(512, K.NL - i * 512), 0) for i in range((K.NL + 511) // 512)]
    for it, (t0, n, col) in enumerate(ranges):
        hb, yb = h[it % 2], y[it % 2]
        P.dma("sp", hb[:, :, :n], K.hT.ap[:, t0:t0 + n].rearrange("(k p) t -> p k t", p=128), reads=[K.hT], writes=[hb])
        norm_tile(K, hb, 0, n, A, Bf, M, col, yb, 0, sq=sq, rstd=rstd)
        for c in range(nch):
            m = min(128, N - c * 128)
            ps = next_ps(K)
            for k in range(KC):
                P.mm(ps, ps[:m, :n], wb[:, k, c * 128:c * 128 + m], yb[:, k, :n], start=(k == 0), stop=(k == KC - 1),
                     reads=[wb, yb])
            o = zo[c % 4]
            P.copy("act" if c % 2 else "dve", o, o[:m, :n], ps, ps[:m, :n])
            P.dma("pool" if c % 2 else "sp", zT.ap[c * 128:c * 128 + m, t0:t0 + n], o[:m, :n], reads=[o], writes=[zT])
    P.release_to(mark)


def outproj_stage(K, mods, w_out, mixT):
    P = K.P
    Gm, M = mods["G_mix"], mods["M"]
    mark = P.mark()
    wst = [P.sb([128, KC, 512], F32, f"op_wst{i}") for i in range(2)]
    wb = P.sb([128, KC, D], BF16, "op_w")
    for s in range(2):
        st = wst[s % 2]
        P.dma("sp", st[:], w_out.ap[:, s * 512:(s + 1) * 512].rearrange("(k p) n -> p k n", p=128), reads=[w_out], writes=[st])
        P.copy("pool", wb, wb[:, :, s * 512:(s + 1) * 512], st, st[:])
    mx = [P.sb([128, KC, 512], F32, f"op_m{i}") for i in range(2)]
    mb = [P.sb([128, KC, 512], BF16, f"op_mb{i}") for i in range(2)]
    h = [P.sb([128, KC, 512], F32, f"op_h{i}") for i in range(2)]
    ranges = [(0, K.NC, 1)] + [(K.NC + i * 512, min(512, K.NL - i * 512), 0) for i in range((K.NL + 511) // 512)]
    for it, (t0, n, col) in enumerate(ranges):
        m_, b_, h_ = mx[it % 2], mb[it % 2], h[it % 2]
        P.dma("sp", m_[:, :, :n], mixT.ap[:, t0:t0 + n].rearrange("(k p) t -> p k t", p=128), reads=[mixT], writes=[m_])
        P.dma("sp", h_[:, :, :n], K.hT.ap[:, t0:t0 + n].rearrange("(k p) t -> p k t", p=128), reads=[K.hT], writes=[h_])
        P.copy("pool", b_, b_[:, :, :n], m_, m_[:, :, :n])
        for c in range(KC):
            ps = next_ps(K)
            for k in range(KC):
                P.mm(ps, ps[:, :n], wb[:, k, c * 128:(c + 1) * 128], b_[:, k, :n], start=(k == 0), stop=(k == KC - 1),
                     reads=[wb, b_])
            P.stt(h_, h_[:, c, :n], ps, ps[:, :n], Gm(c, col), h_, h_[:, c, :n], ALU.mult, ALU.add, extra_reads=[M])
        P.dma("pool", K.hT.ap[:, t0:t0 + n].rearrange("(k p) t -> p k t", p=128), h_[:, :, :n], reads=[h_], writes=[K.hT])
    P.release_to(mark)


def final_norm_stage(K, gain, outT, t0_all, n_all):
    P = K.P
    nc = P.nc
    mark = P.mark()
    g = P.sb([128, KC], F32, "fn_g")
    with nc.allow_non_contiguous_dma(reason="tiny vector load"):
        P.dma("sp", g[:], dvec(gain.ap, KC), reads=[gain], writes=[g])
    h = [P.sb([128, KC, 512], F32, f"fn_h{i}") for i in range(2)]
    o = [P.sb([128, KC, 512], F32, f"fn_o{i}") for i in range(2)]
    sq = P.sb([128, KC, 512], F32, "fn_sq")
    rstd = P.sb([128, 512], F32, "fn_rstd")
    for it in range((n_all + 511) // 512):
        n = min(512, n_all - it * 512)
        t0 = t0_all + it * 512
        hb, ob = h[it % 2], o[it % 2]
        P.dma("sp", hb[:, :, :n], K.hT.ap[:, t0:t0 + n].rearrange("(k p) t -> p k t", p=128), reads=[K.hT], writes=[hb])
        ps = next_ps(K)
        for k in range(KC):
            P.act(sq, sq[:, k, :n], hb, hb[:, k, :n], AF.Square)
        for k in range(KC):
            P.mm(ps, ps[:, :n], K.ones[:], sq[:, k, :n], start=(k == 0), stop=(k == KC - 1), reads=[K.ones, sq])
        P.ts("dve", rstd, rstd[:, :n], ps, ps[:, :n], 1.0 / D, ALU.mult, EPS, ALU.add)
        P.op("act", lambda e: e.sqrt(out=rstd[:, :n], in_=rstd[:, :n]), reads=[rstd], writes=[rstd])
        P.op("dve", lambda e: e.reciprocal(out=rstd[:, :n], in_=rstd[:, :n]), reads=[rstd], writes=[rstd])
        for k in range(KC):
            P.stt(ob, ob[:, k, :n], hb, hb[:, k, :n], g[:, k:k + 1], rstd, rstd[:, :n], ALU.mult, ALU.mult, extra_reads=[g])
        ev = P.dma("pool", outT.ap[:, it * 512:it * 512 + n].rearrange("(k p) t -> p k t", p=128), ob[:, :, :n],
                   reads=[ob], writes=[outT])
        P.out_events.append(ev)
    P.release_to(mark)


MAGIC = 12582912.0
TWO_PI = 2.0 * math.pi


def host_consts():
    c = {}
    c["k_tau"] = np.tile(np.arange(128, dtype=np.float32)[None, :], (128, 1))
    q = np.arange(128)
    mC = np.zeros((128, 4, 2, 64), np.float32)
    for jj in range(4):
        for gl in range(2):
            sel = (q // 32 == jj) & ((q % 32) // 16 == gl)
            mC[sel, jj, gl, :] = 1.0
    c["k_maskC"] = mC.reshape(128, 512)
    mB = np.zeros((128, 4, 128), np.float32)
    for jj in range(4):
        for gl in range(2):
            rows = np.arange(64 * gl, 64 * gl + 64)
            cols = np.arange(32 * jj + 16 * gl, 32 * jj + 16 * gl + 16)
            mB[np.ix_(rows, [jj], cols)] = 1.0
    c["k_maskB"] = mB.reshape(128, 512)
    c["k_ident"] = np.eye(128, dtype=np.float32)
    return c


def range_reduce(P, out_t, out_ap, in_t, in_ap, tmp_t, tmp_ap, shift=0.0):
    P.ts("dve", tmp_t, tmp_ap, in_t, in_ap, 1.0 / TWO_PI, ALU.mult, shift / TWO_PI + MAGIC, ALU.add)
    P.ts("dve", tmp_t, tmp_ap, tmp_t, tmp_ap, MAGIC, ALU.subtract, -TWO_PI, ALU.mult)
    P.stt(out_t, out_ap, in_t, in_ap, shift, tmp_t, tmp_ap, ALU.add, ALU.add)


def s5_stage(K, i, I, zT, mixT, C, u_row0=1536, out_row0=512):
    P = K.P
    nc = P.nc
    TT = 128
    mark = P.mark()
    tau, maskB, maskC, ident = C["k_tau"], C["k_maskB"], C["k_maskC"], C["k_ident"]
    yS = K.yS5
    sb = P.sb
    lr = sb([128, 16], F32, "s5_lr"); li = sb([128, 16], F32, "s5_li"); dtv = sb([128, 16], F32, "s5_dt")
    wdt = sb([128, 16], F32, "s5_wdt"); rdt = sb([128, 16], F32, "s5_rdt"); nrdt = sb([128, 16], F32, "s5_nrdt")
    Er = sb([128, 16, TT], F32, "s5_Er"); Ei = sb([128, 16, TT], F32, "s5_Ei")
    Gr = sb([128, 16, TT], F32, "s5_Gr"); Gi = sb([128, 16, TT], F32, "s5_Gi")
    Hr = sb([128, 16], F32, "s5_Hr"); Hi = sb([128, 16], F32, "s5_Hi")
    BTr = sb([128, 16, 128], F32, "s5_BTr"); BTi = sb([128, 16, 128], F32, "s5_BTi")
    CTr = sb([128, 16, 128], F32, "s5_CTr"); CTin = sb([128, 16, 128], F32, "s5_CTin")
    bre = sb([128, 16, 16], F32, "s5_bre"); bim = sb([128, 16, 16], F32, "s5_bim")
    bbr = sb([128, 16, 16], F32, "s5_bbr"); bbi = sb([128, 16, 16], F32, "s5_bbi")
    cre = sb([128, 4, 64], F32, "s5_cre"); cim = sb([128, 4, 64], F32, "s5_cim")
    ex = sb([128, 128], F32, "s5_ex")
    th = sb([128, TT], F32, "s5_th"); tmp = sb([128, TT], F32, "s5_tmp"); sn = sb([128, TT], F32, "s5_sn")
    cs = sb([128, TT], F32, "s5_cs"); mg = sb([128, TT], F32, "s5_mg")
    s16 = [sb([128, 16], F32, f"s5_s16_{k}") for k in range(8)]
    u = [sb([128, 4, TT], F32, f"s5_u{k}") for k in range(3)]
    SL = 3
    t1s = [sb([128, 4, TT], F32, f"s5_t1{k}") for k in range(SL)]; t2s = [sb([128, 4, TT], F32, f"s5_t2{k}") for k in range(SL)]
    t3s = [sb([128, 4, TT], F32, f"s5_t3{k}") for k in range(SL)]; t4s = [sb([128, 4, TT], F32, f"s5_t4{k}") for k in range(SL)]
    wrs = [sb([128, 4, TT], F32, f"s5_wr{k}") for k in range(SL)]; wis = [sb([128, 4, TT], F32, f"s5_wi{k}") for k in range(SL)]
    zrs = [sb([128, 4, TT], F32, f"s5_zr{k}") for k in range(SL)]; zis = [sb([128, 4, TT], F32, f"s5_zi{k}") for k in range(SL)]
    xrs = [sb([128, 4, TT], F32, f"s5_xr{k}") for k in range(SL)]; xis = [sb([128, 4, TT], F32, f"s5_xi{k}") for k in range(SL)]
    cars = [sb([128, 4], F32, f"s5_car{k}") for k in range(4)]; cais = [sb([128, 4], F32, f"s5_cai{k}") for k in range(4)]
    c4s = [[sb([128, 4], F32, f"s5_c4_{q}{k}") for k in range(4)] for q in range(SL)]
    g1s = [sb([128, TT], F32, f"s5_g1{k}") for k in range(SL)]; g2s = [sb([128, TT], F32, f"s5_g2{k}") for k in range(SL)]
    yo = [sb([128, TT], F32, f"s5_yo{k}") for k in range(2)]
    yfs = [sb([128, 4, TT], F32, f"s5_yf{k}") for k in range(3)]
    vqs = [[sb([128, TT], F32, f"s5_v{p}{k}") for k in range(4)] for p in range(2)]
    oo = [sb([128, TT], F32, f"s5_oo{k}") for k in range(2)]
    wglu = sb([128, 4, 512], F32, "s5_wglu"); dsk = sb([128, 4], F32, "s5_dsk")
    onesT = sb([128, TT], F32, "s5_ones")
    P.memset("pool", onesT, onesT[:], 1.0)
    P.dma("sp", wglu[:], I["s5_w_glu"].ap[i].rearrange("(k p) n -> p k n", p=128), reads=[I["s5_w_glu"]], writes=[wglu])
    with nc.allow_non_contiguous_dma(reason="tiny vector load"):
        P.dma("sp", dsk[:], I["s5_d"].ap[i].rearrange("(k p) -> p k", p=128), reads=[I["s5_d"]], writes=[dsk])

    def sincos(angle_t, angle_ap, sin_t, sin_ap, cos_t, cos_ap, tmp_a, tmp_a_ap, tmp_b, tmp_b_ap):
        range_reduce(P, tmp_b, tmp_b_ap, angle_t, angle_ap, tmp_a, tmp_a_ap, 0.0)
        P.act(sin_t, sin_ap, tmp_b, tmp_b_ap, AF.Sin)
        range_reduce(P, tmp_b, tmp_b_ap, angle_t, angle_ap, tmp_a, tmp_a_ap, math.pi / 2)
        P.act(cos_t, cos_ap, tmp_b, tmp_b_ap, AF.Sin)

    chunks_ctx = [(k * TT, 1) for k in range(K.NC // TT)]
    chunks_lat = [(K.NC + k * TT, 0) for k in range(K.NL // TT)]
    for d in range(2):
        rev = (d == 1)
        with nc.allow_non_contiguous_dma(reason="small parameter loads"):
            P.dma("sp", lr[:], I["s5_a_re"].ap[i, d].rearrange("g p -> (g p)").rearrange("(j q) -> q j", q=128),
                  reads=[I["s5_a_re"]], writes=[lr])
            P.dma("sp", li[:], I["s5_a_im"].ap[i, d].rearrange("g p -> (g p)").rearrange("(j q) -> q j", q=128),
                  reads=[I["s5_a_im"]], writes=[li])
            for gl in range(2):
                P.dma("sp", dtv[64 * gl:64 * gl + 64, :],
                      I["s5_log_dt"].ap[i, d].rearrange("(j g) -> g j", g=2)[gl:gl + 1, :].partition_broadcast(64),
                      reads=[I["s5_log_dt"]], writes=[dtv])
            P.dma("sp", bre[:], I["s5_b_re"].ap[i, d].rearrange("g p c -> (g p) c").rearrange("(j q) c -> q j c", q=128),
                  reads=[I["s5_b_re"]], writes=[bre])
            P.dma("sp", bim[:], I["s5_b_im"].ap[i, d].rearrange("g p c -> (g p) c").rearrange("(j q) c -> q j c", q=128),
                  reads=[I["s5_b_im"]], writes=[bim])
            P.dma("sp", cre[:], I["s5_c_re"].ap[i, d].rearrange("g c p -> (g c) p").rearrange("(k q) p -> q k p", q=128),
                  reads=[I["s5_c_re"]], writes=[cre])
            P.dma("sp", cim[:], I["s5_c_im"].ap[i, d].rearrange("g c p -> (g c) p").rearrange("(k q) p -> q k p", q=128),
                  reads=[I["s5_c_im"]], writes=[cim])
        P.ts("dve", lr, lr[:], lr, lr[:], -1e-4, ALU.min)
        P.act(dtv, dtv[:], dtv, dtv[:], AF.Exp)
        P.tt("dve", wdt, wdt[:], li, li[:], dtv, dtv[:], ALU.mult)
        P.tt("dve", rdt, rdt[:], lr, lr[:], dtv, dtv[:], ALU.mult)
        P.ts("dve", nrdt, nrdt[:], rdt, rdt[:], -1.0, ALU.mult)
        ar, ai, a_s, a_c, a_m, ta, tb, den = s16
        sincos(wdt, wdt[:], a_s, a_s[:], a_c, a_c[:], ta, ta[:], tb, tb[:])
        P.act(a_m, a_m[:], rdt, rdt[:], AF.Exp)
        P.tt("dve", ar, ar[:], a_m, a_m[:], a_c, a_c[:], ALU.mult)
        P.tt("dve", ai, ai[:], a_m, a_m[:], a_s, a_s[:], ALU.mult)
        P.ts("dve", ar, ar[:], ar, ar[:], -1.0, ALU.add)
        P.tt("dve", den, den[:], lr, lr[:], lr, lr[:], ALU.mult)
        P.tt("dve", ta, ta[:], li, li[:], li, li[:], ALU.mult)
        P.tt("dve", den, den[:], den, den[:], ta, ta[:], ALU.add)
        P.op("dve", lambda e: e.reciprocal(out=den[:], in_=den[:]), reads=[den], writes=[den])
        P.tt("dve", ta, ta[:], ar, ar[:], lr, lr[:], ALU.mult)
        P.tt("dve", tb, tb[:], ai, ai[:], li, li[:], ALU.mult)
        P.tt("dve", ta, ta[:], ta, ta[:], tb, tb[:], ALU.add)
        P.tt("dve", a_c, a_c[:], ta, ta[:], den, den[:], ALU.mult)
        P.tt("dve", ta, ta[:], ai, ai[:], lr, lr[:], ALU.mult)
        P.tt("dve", tb, tb[:], ar, ar[:], li, li[:], ALU.mult)
        P.tt("dve", ta, ta[:], ta, ta[:], tb, tb[:], ALU.subtract)
        P.tt("dve", a_s, a_s[:], ta, ta[:], den, den[:], ALU.mult)
        P.ts("dve", a_m, a_m[:], a_s, a_s[:], -1.0, ALU.mult)
        coef_r, coef_i, ncoef_i = a_c, a_s, a_m
        P.ts("dve", ta, ta[:], wdt, wdt[:], float(TT), ALU.mult)
        sincos(ta, ta[:], Hi, Hi[:], Hr, Hr[:], tb, tb[:], den, den[:])
        P.act(ta, ta[:], rdt, rdt[:], AF.Exp, scale=float(TT))
        P.tt("dve", Hr, Hr[:], Hr, Hr[:], ta, ta[:], ALU.mult)
        P.tt("dve", Hi, Hi[:], Hi, Hi[:], ta, ta[:], ALU.mult)
        for j in range(16):
            jj = j % 4
            cc = j // 4
            P.ts("dve", th, th[:], tau, tau[:], wdt[:, j:j + 1], ALU.mult, extra_reads=[wdt])
            sincos(th, th[:], sn, sn[:], cs, cs[:], tmp, tmp[:], mg, mg[:])
            P.act(mg, mg[:], tau, tau[:], AF.Exp, scale=rdt[:, j:j + 1], extra_reads=[rdt])
            P.tt("dve", Gr, Gr[:, j, :], mg, mg[:], cs, cs[:], ALU.mult)
            P.tt("dve", Gi, Gi[:, j, :], mg, mg[:], sn, sn[:], ALU.mult)
            P.act(mg, mg[:], tau, tau[:], AF.Exp, scale=nrdt[:, j:j + 1], extra_reads=[nrdt])
            P.tt("dve", Er, Er[:, j, :], mg, mg[:], cs, cs[:], ALU.mult)
            P.stt(Ei, Ei[:, j, :], mg, mg[:], -1.0, sn, sn[:], ALU.mult, ALU.mult)
            P.ts("dve", bbr, bbr[:, j, :], bre, bre[:, j, :], coef_r[:, j:j + 1], ALU.mult, extra_reads=[coef_r])
            P.stt(bbr, bbr[:, j, :], bim, bim[:, j, :], ncoef_i[:, j:j + 1], bbr, bbr[:, j, :], ALU.mult, ALU.add,
                  extra_reads=[ncoef_i])
            P.ts("dve", bbi, bbi[:, j, :], bim, bim[:, j, :], coef_r[:, j:j + 1], ALU.mult, extra_reads=[coef_r])
            P.stt(bbi, bbi[:, j, :], bre, bre[:, j, :], coef_i[:, j:j + 1], bbi, bbi[:, j, :], ALU.mult, ALU.add,
                  extra_reads=[coef_i])
            for (src, dst, neg) in ((bbr, BTr, False), (bbi, BTi, False)):
                P.tt("dve", ex, ex[:].rearrange("q (a c) -> q a c", c=16),
                     src, src[:, j, :].unsqueeze(1).broadcast_to([128, 8, 16]),
                     maskB, maskB[:, jj * 128:(jj + 1) * 128].rearrange("q (a c) -> q a c", c=16), ALU.mult)
                ps = next_ps(K)
                P.op("pe", lambda e: e.transpose(ps[:, 0:128], ex[:], ident[:]), reads=[ex, ident], writes=[ps])
                P.copy("act", dst, dst[:, j, :], ps, ps[:, 0:128])
            for (src, dst, neg) in ((cre, CTr, False), (cim, CTin, True)):
                P.tt("dve", ex, ex[:].rearrange("q (a p) -> q a p", p=64),
                     src, src[:, cc, :].unsqueeze(1).broadcast_to([128, 2, 64]),
                     maskC, maskC[:, jj * 128:(jj + 1) * 128].rearrange("q (a p) -> q a p", p=64), ALU.mult)
                ps = next_ps(K)
                P.op("pe", lambda e: e.transpose(ps[:, 0:128], ex[:], ident[:]), reads=[ex, ident], writes=[ps])
                if neg:
                    P.ts("dve", dst, dst[:, j, :], ps, ps[:, 0:128], -1.0, ALU.mult)
                else:
                    P.copy("act", dst, dst[:, j, :], ps, ps[:, 0:128])
        for q4 in range(4):
            P.memset("pool", cars[q4], cars[q4][:], 0.0)
            P.memset("pool", cais[q4], cais[q4][:], 0.0)
        order = (chunks_ctx + chunks_lat) if not rev else (chunks_ctx[::-1] + chunks_lat[::-1])
        K.ps_reserved = {0, 1, 2, 3, 4, 5}
        done = {}

        def quad(ci_, t0, cc):
            ub = u[ci_ % 3]
            yf = yfs[ci_ % 3]
            vq = vqs[ci_ % 2]
            if cc == 0:
                P.dma("sp", ub[:], zT.ap[u_row0:u_row0 + 512, t0:t0 + TT].rearrange("(k q) t -> q k t", q=128),
                      reads=[zT], writes=[ub])
                if rev:
                    P.dma("sp", yf[:], yS.ap[:, t0:t0 + TT].rearrange("(k q) t -> q k t", q=128), reads=[yS], writes=[yf])
            s_ = (ci_ * 4 + cc) % SL
            t1, t2, t3, t4, wr, wi, zr, zi, xr, xi = (t1s[s_], t2s[s_], t3s[s_], t4s[s_], wrs[s_], wis[s_], zrs[s_],
                                                       zis[s_], xrs[s_], xis[s_])
            car, cai, c4, g1, g2 = cars[cc], cais[cc], c4s[s_], g1s[s_], g2s[s_]
            pa, pb = K.ps[2 * s_], K.ps[2 * s_ + 1]
            py = pa
            urhs = ub[:, cc, ::-1] if rev else ub[:, cc, :]
            for jj in range(4):
                j = 4 * cc + jj
                P.mm(pa, pa[:, jj * TT:(jj + 1) * TT], BTr[:, j, :], urhs, True, True, reads=[BTr, ub])
                P.mm(pb, pb[:, jj * TT:(jj + 1) * TT], BTi[:, j, :], urhs, True, True, reads=[BTi, ub])
            yield
            Wr = pa[:, :].rearrange("q (a t) -> q a t", t=TT)
            Wi = pb[:, :].rearrange("q (a t) -> q a t", t=TT)
            sl = slice(4 * cc, 4 * cc + 4)
            P.tt("dve", t1, t1[:], pa, Wr, Er, Er[:, sl, :], ALU.mult)
            P.tt("dve", t2, t2[:], pb, Wi, Ei, Ei[:, sl, :], ALU.mult)
            P.tt("pool", wr, wr[:], t1, t1[:], t2, t2[:], ALU.subtract)
            P.tt("dve", t3, t3[:], pa, Wr, Ei, Ei[:, sl, :], ALU.mult)
            P.tt("dve", t4, t4[:], pb, Wi, Er, Er[:, sl, :], ALU.mult)
            P.tt("pool", wi, wi[:], t3, t3[:], t4, t4[:], ALU.add)
            yield
            for jj in range(4):
                P.op("dve", lambda e: e.tensor_tensor_scan(out=zr[:, jj, :], data0=onesT[:], data1=wr[:, jj, :],
                                                           initial=car[:, jj:jj + 1], op0=ALU.mult, op1=ALU.add),
                     reads=[onesT, wr, car], writes=[zr])
                P.op("dve", lambda e: e.tensor_tensor_scan(out=zi[:, jj, :], data0=onesT[:], data1=wi[:, jj, :],
                                                           initial=cai[:, jj:jj + 1], op0=ALU.mult, op1=ALU.add),
                     reads=[onesT, wi, cai], writes=[zi])
            yield
            zlr = zr[:, :, TT - 1]
            zli = zi[:, :, TT - 1]
            P.tt("dve", c4[0], c4[0][:], zr, zlr, Hr, Hr[:, sl], ALU.mult)
            P.tt("dve", c4[1], c4[1][:], zi, zli, Hi, Hi[:, sl], ALU.mult)
            P.tt("dve", c4[2], c4[2][:], zi, zli, Hr, Hr[:, sl], ALU.mult)
            P.tt("dve", c4[3], c4[3][:], zr, zlr, Hi, Hi[:, sl], ALU.mult)
            P.tt("dve", car, car[:], c4[0], c4[0][:], c4[1], c4[1][:], ALU.subtract)
            P.tt("dve", cai, cai[:], c4[2], c4[2][:], c4[3], c4[3][:], ALU.add)
            P.tt("dve", t1, t1[:], zr, zr[:], Gr, Gr[:, sl, :], ALU.mult)
            P.tt("pool", t2, t2[:], zi, zi[:], Gi, Gi[:, sl, :], ALU.mult)
            P.tt("pool", t4, t4[:], zr, zr[:], Gi, Gi[:, sl, :], ALU.mult)
            P.tt("dve", t3, t3[:], zi, zi[:], Gr, Gr[:, sl, :], ALU.mult)
            P.tt("dve", xr, xr[:], t1, t1[:], t2, t2[:], ALU.subtract)
            P.tt("pool", xi, xi[:], t3, t3[:], t4, t4[:], ALU.add)
            yield
            for jj in range(4):
                j = 4 * cc + jj
                P.mm(py, py[:, :TT], CTr[:, j, :], xr[:, jj, :], jj == 0, False, reads=[CTr, xr])
                P.mm(py, py[:, :TT], CTin[:, j, :], xi[:, jj, :], False, jj == 3, reads=[CTin, xi])
            yield
            if not rev:
                yb = yo[cc % 2]
                P.copy("act", yb, yb[:], py, py[:, :TT])
                P.dma("pool", yS.ap[128 * cc:128 * cc + 128, t0:t0 + TT], yb[:], reads=[yb], writes=[yS])
            else:
                P.tt("dve", g1, g1[:], py, py[:, :TT][:, ::-1], yf, yf[:, cc, :], ALU.add)
                P.stt(g1, g1[:], ub, ub[:, cc, :], dsk[:, cc:cc + 1], g1, g1[:], ALU.mult, ALU.add, extra_reads=[dsk])
                P.tt("pool", g2, g2[:], g1, g1[:], g1, g1[:], ALU.mult)
                P.ts("dve", g2, g2[:], g2, g2[:], 0.044715, ALU.mult, 1.0, ALU.add)
                P.tt("dve", g2, g2[:], g2, g2[:], g1, g1[:], ALU.mult)
                P.act(g2, g2[:], g2, g2[:], AF.Sigmoid, scale=1.5957691216057308)
                P.tt("dve", vq[cc], vq[cc][:], g1, g1[:], g2, g2[:], ALU.mult)
                done[ci_] = done.get(ci_, 0) + 1
                if done[ci_] == 4:
                    for mc in range(4):
                        pg = next_ps(K)
                        for kc in range(4):
                            P.mm(pg, pg[:, :TT], wglu[:, kc, mc * 128:(mc + 1) * 128], vq[kc][:], kc == 0, kc == 3,
                                 reads=[wglu, vq[kc]])
                        ob = oo[mc % 2]
                        P.act(ob, ob[:], pg, pg[:, :TT], AF.Sigmoid)
                        P.tt("dve", ob, ob[:], ob, ob[:], vq[mc], vq[mc][:], ALU.mult)
                        P.dma("pool", mixT.ap[out_row0 + 128 * mc:out_row0 + 128 * mc + 128, t0:t0 + TT], ob[:],
                              reads=[ob], writes=[mixT])
            yield

        run_interleaved((quad(ci_, t0, cc) for ci_, (t0, is_ctx) in enumerate(order) for cc in range(4)), width=SL)
        K.ps_reserved = set()
    P.release_to(mark)


HY_BANDS = 16
HY_EMB = 33


def hyena_consts(L):
    N = 2 * L
    N1 = N // 128
    R = L // 128
    c = {}
    f64 = np.float64
    n1 = np.arange(R, dtype=f64)[:, None]; k1 = np.arange(N1, dtype=f64)[None, :]
    a = 2 * np.pi * n1 * k1 / N1
    c["F1c"] = np.cos(a); c["F1ns"] = -np.sin(a)
    n2 = np.arange(128, dtype=f64)[:, None]
    a = 2 * np.pi * n2 * k1 / N
    c["Twc"] = np.cos(a); c["Tws"] = np.sin(a)
    c["TwcT"] = np.cos(a).T.copy(); c["TwsT"] = np.sin(a).T.copy()
    k1c = np.arange(N1, dtype=f64)[:, None]; n1r = np.arange(R, dtype=f64)[None, :]
    a = 2 * np.pi * k1c * n1r / N1
    c["I2c"] = np.cos(a) / N; c["I2ns"] = -np.sin(a) / N
    t = np.arange(L, dtype=np.float32)
    t01 = t / np.float32(L)
    bands = np.linspace(1e-4, HY_BANDS - 1, HY_BANDS, dtype=np.float32)
    ang = (np.float32(2.0 * math.pi / L) * t[:, None]) * bands[None, :]
    feats = np.concatenate([t01[:, None], np.cos(ang), -np.sin(ang)], axis=-1)
    c["featsT"] = feats.T.copy()
    return {f"hy{L}_{k}": np.ascontiguousarray(v, dtype=np.float32) for k, v in c.items()}


def hyena_shared_consts():
    n2 = np.arange(128, dtype=np.float64)[:, None]; k2 = np.arange(128, dtype=np.float64)[None, :]
    a = 2 * np.pi * n2 * k2 / 128
    return {"hy_F3c": np.cos(a).astype(np.float32), "hy_F3s": np.sin(a).astype(np.float32),
            "hy_F3ns": (-np.sin(a)).astype(np.float32),
            "hy_tau512": np.tile(np.arange(512, dtype=np.float32)[None, :], (128, 1))}


def run_interleaved(gens, width=2):
    gens = list(gens)
    active = []
    while gens or active:
        while gens and len(active) < width:
            active.append(gens.pop(0))
        for g in list(active):
            try:
                next(g)
            except StopIteration:
                active.remove(g)


def hyena_filters_td(K, i, I, C, L, emit):
    P = K.P
    nc = P.nc
    sb = P.sb
    tau512 = C["hy_tau512"]
    featsD = C[f"hy{L}_featsT_dram"]
    mark1 = P.mark()
    w1 = sb([HY_EMB, 64], F32, "hy_w1"); w2 = sb([64, 64], F32, "hy_w2"); w3 = sb([64, 2048], F32, "hy_w3")
    b1 = sb([64, 1], F32, "hy_b1"); b2 = sb([64, 1], F32, "hy_b2"); fq = sb([64, 1], F32, "hy_fq")
    fb1 = sb([64, 1], F32, "hy_fb1"); fb2 = sb([64, 1], F32, "hy_fb2")
    dec = sb([128, 8], F32, "hy_dec"); decb = sb([128, 8], F32, "hy_decb")
    P.dma("sp", w1[:], I["hy_w1"].ap[i], reads=[I["hy_w1"]], writes=[w1])
    P.dma("sp", w2[:], I["hy_w2"].ap[i], reads=[I["hy_w2"]], writes=[w2])
    P.dma("sp", w3[:], I["hy_w3"].ap[i], reads=[I["hy_w3"]], writes=[w3])
    with nc.allow_non_contiguous_dma(reason="tiny vector load"):
        P.dma("sp", b1[:], I["hy_b1"].ap[i].rearrange("(p o) -> p o", o=1), reads=[I["hy_b1"]], writes=[b1])
        P.dma("sp", b2[:], I["hy_b2"].ap[i].rearrange("(p o) -> p o", o=1), reads=[I["hy_b2"]], writes=[b2])
        P.dma("sp", fq[:], I["hy_freq"].ap[i].rearrange("(p o) -> p o", o=1), reads=[I["hy_freq"]], writes=[fq])
        P.dma("sp", dec[:], I["hy_decay"].ap[i].rearrange("o (k p) -> p (o k)", p=128), reads=[I["hy_decay"]], writes=[dec])
    P.tt("dve", fb1, fb1[:], b1, b1[:], fq, fq[:], ALU.mult)
    P.tt("dve", fb2, fb2[:], b2, b2[:], fq, fq[:], ALU.mult)
    P.act(dec, dec[:], dec, dec[:], AF.Abs)
    P.ts("dve", dec, dec[:], dec, dec[:], -1.0 / L, ALU.mult)
    TC = min(512, L)
    nTC = L // TC
    hid1 = sb([64, L], F32, "hy_hid1"); hid2 = sb([64, L], F32, "hy_hid2")
    ft = sb([HY_EMB, TC], F32, "hy_ft")
    pre = sb([64, 512], F32, "hy_pre"); rr = sb([64, 512], F32, "hy_rr"); rt = sb([64, 512], F32, "hy_rt")
    for tcn in range(nTC):
        P.dma("sp", ft[:], featsD.ap[:, tcn * TC:(tcn + 1) * TC], reads=[featsD], writes=[ft])
        ps = next_ps(K)
        P.mm(ps, ps[:64, :TC], w1[:], ft[:], True, True, reads=[w1, ft])
        P.ts("dve", pre, pre[:, :TC], ps, ps[:64, :TC], fq[:, 0:1], ALU.mult, fb1[:, 0:1], ALU.add, extra_reads=[fq, fb1])
        range_reduce(P, rr, rr[:, :TC], pre, pre[:, :TC], rt, rt[:, :TC])
        P.act(hid1, hid1[:, tcn * TC:(tcn + 1) * TC], rr, rr[:, :TC], AF.Sin)
        ps = next_ps(K)
        P.mm(ps, ps[:64, :TC], w2[:], hid1[:, tcn * TC:(tcn + 1) * TC], True, True, reads=[w2, hid1])
        P.ts("dve", pre, pre[:, :TC], ps, ps[:64, :TC], fq[:, 0:1], ALU.mult, fb2[:, 0:1], ALU.add, extra_reads=[fq, fb2])
        range_reduce(P, rr, rr[:, :TC], pre, pre[:, :TC], rt, rt[:, :TC])
        P.act(hid2, hid2[:, tcn * TC:(tcn + 1) * TC], rr, rr[:, :TC], AF.Sin)
    kf = sb([128, L], F32, "hy_kf"); kb = sb([128, L], F32, "hy_kb")
    win = sb([128, 512], F32, "hy_win"); wb_ = sb([128, 1], F32, "hy_wb")
    sums = sb([128, 2 * nTC + 1], F32, "hy_sums"); tot = sb([128, 1], F32, "hy_tot")
    for o in range(2):
        for cq in range(4):
            oc = o * 4 + cq
            for tcn in range(nTC):
                P.ts("dve", wb_, wb_[:], dec, dec[:, oc:oc + 1], float(tcn * TC), ALU.mult)
                P.act(win, win[:, :TC], tau512, tau512[:, :TC], AF.Exp, bias=wb_[:, 0:1], scale=dec[:, oc:oc + 1],
                      extra_reads=[wb_, dec])
                for dr, kt in ((0, kf), (1, kb)):
                    col0 = dr * 1024 + o * 512 + cq * 128
                    ps = next_ps(K)
                    P.mm(ps, ps[:, :TC], w3[:, col0:col0 + 128], hid2[:, tcn * TC:(tcn + 1) * TC], True, True, reads=[w3, hid2])
                    P.stt(kt, kt[:, tcn * TC:(tcn + 1) * TC], win, win[:, :TC], HY_WINDOW_SHIFT, ps, ps[:, :TC], ALU.add, ALU.mult)
                    P.op("dve", lambda e: e.tensor_reduce(out=sums[:, dr * nTC + tcn:dr * nTC + tcn + 1],
                                                          in_=kt[:, tcn * TC:(tcn + 1) * TC], axis=AX.X, op=ALU.add,
                                                          apply_absolute_value=True), reads=[kt], writes=[sums])
            P.act(sums, sums[:, 2 * nTC:2 * nTC + 1], kb, kb[:, 0:1], AF.Abs)
            P.ts("dve", sums, sums[:, 2 * nTC:2 * nTC + 1], sums, sums[:, 2 * nTC:2 * nTC + 1], -1.0, ALU.mult)
            P.op("dve", lambda e: e.tensor_reduce(out=tot[:], in_=sums[:], axis=AX.X, op=ALU.add), reads=[sums], writes=[tot])
            P.op("dve", lambda e: e.reciprocal(out=tot[:], in_=tot[:]), reads=[tot], writes=[tot])
            P.memset("dve", kb, kb[:, 0:1], 0.0)
            P.ts("dve", kf, kf[:], kf, kf[:], tot[:, 0:1], ALU.mult, extra_reads=[tot])
            P.ts("pool", kb, kb[:], kb, kb[:], tot[:, 0:1], ALU.mult, extra_reads=[tot])
            emit(o, cq, kf, kb)
    P.release_to(mark1)


def hyena_stage(K, i, I, zT, mixT, C, L, t_base, KFr, KFi, kT, tag):
    P = K.P
    nc = P.nc
    N = 2 * L
    N1 = N // 128
    R = L // 128
    G = min(512 // N1, 4)
    W = G * N1
    WT = G * 128
    mark = P.mark()
    sb = P.sb
    cn = lambda k: C[f"hy{L}_{k}"]
    tau512 = C["hy_tau512"]
    Twc, Tws, TwcT, TwsT = (cn(k) for k in ("Twc", "Tws", "TwcT", "TwsT"))
    featsD = C[f"hy{L}_featsT_dram"]

    def bf(src, nm):
        if HY_DT == F32:
            return src
        t = sb(list(src.ap.shape), HY_DT, "hyb_" + nm)
        P.copy("pool", t, t[:], src, src[:])
        return t
    F3c, F3s, F3ns = bf(C["hy_F3c"], "F3c"), bf(C["hy_F3s"], "F3s"), bf(C["hy_F3ns"], "F3ns")
    F1c, F1ns, I2c, I2ns = bf(cn("F1c"), "F1c"), bf(cn("F1ns"), "F1ns"), bf(cn("I2c"), "I2c"), bf(cn("I2ns"), "I2ns")
    NS = 2
    m1s = [sb([128, 512], F32, f"hy_m1{k}") for k in range(NS)]; m2s = [sb([128, 512], F32, f"hy_m2{k}") for k in range(NS)]
    Aprs = [sb([128, 512], HY_DT, f"hy_Apr{k}") for k in range(NS)]; Apis = [sb([128, 512], HY_DT, f"hy_Api{k}") for k in range(NS)]
    Bprs = [sb([128, 512], HY_DT, f"hy_Bpr{k}") for k in range(NS)]; Bpis = [sb([128, 512], HY_DT, f"hy_Bpi{k}") for k in range(NS)]

    def fft_fwd(U, psr, psi, sl):
        m1, m2, Apr, Api = m1s[sl], m2s[sl], Aprs[sl], Apis[sl]
        pa, pb = K.ps[4 * sl], K.ps[4 * sl + 1]
        Ut, Uf = U
        for c in range(G):
            P.mm(pa, pa[:, c * N1:(c + 1) * N1], Uf(c), F1c[:R, :], True, True, reads=[Ut, F1c])
            P.mm(pb, pb[:, c * N1:(c + 1) * N1], Uf(c), F1ns[:R, :], True, True, reads=[Ut, F1ns])
        yield
        v3 = lambda ap: ap.rearrange("q (c k) -> q c k", k=N1)
        tc_ = Twc[:, :].unsqueeze(1).broadcast_to([128, G, N1])
        ts_ = Tws[:, :].unsqueeze(1).broadcast_to([128, G, N1])
        P.tt("dve", m1, v3(m1[:, :W]), pa, v3(pa[:, :W]), Twc, tc_, ALU.mult)
        P.tt("dve", m2, v3(m2[:, :W]), pb, v3(pb[:, :W]), Tws, ts_, ALU.mult)
        P.tt("pool", Apr, Apr[:, :W], m1, m1[:, :W], m2, m2[:, :W], ALU.add)
        P.tt("dve", m1, v3(m1[:, :W]), pb, v3(pb[:, :W]), Twc, tc_, ALU.mult)
        P.tt("dve", m2, v3(m2[:, :W]), pa, v3(pa[:, :W]), Tws, ts_, ALU.mult)
        P.tt("pool", Api, Api[:, :W], m1, m1[:, :W], m2, m2[:, :W], ALU.subtract)
        yield
        P.mm(psr, psr[:, :W], F3c[:], Apr[:, :W], True, False, reads=[F3c, Apr])
        P.mm(psr, psr[:, :W], F3s[:], Api[:, :W], False, True, reads=[F3s, Api])
        P.mm(psi, psi[:, :W], F3c[:], Api[:, :W], True, False, reads=[F3c, Api])
        P.mm(psi, psi[:, :W], F3ns[:], Apr[:, :W], False, True, reads=[F3ns, Apr])
        yield

    def fft_inv(Yr, Yi, psy, sl):
        m1, m2, Bpr, Bpi = m1s[sl], m2s[sl], Bprs[sl], Bpis[sl]
        pa, pb = K.ps[4 * sl], K.ps[4 * sl + 1]
        for c in range(G):
            yr = Yr[:, c * N1:(c + 1) * N1]
            yi = Yi[:, c * N1:(c + 1) * N1]
            P.mm(pa, pa[:N1, c * 128:(c + 1) * 128], yr, F3c[:], True, False, reads=[Yr, F3c])
            P.mm(pa, pa[:N1, c * 128:(c + 1) * 128], yi, F3ns[:], False, True, reads=[Yi, F3ns])
            P.mm(pb, pb[:N1, c * 128:(c + 1) * 128], yr, F3s[:], True, False, reads=[Yr, F3s])
            P.mm(pb, pb[:N1, c * 128:(c + 1) * 128], yi, F3c[:], False, True, reads=[Yi, F3c])
        yield
        v3 = lambda ap: ap.rearrange("q (c k) -> q c k", k=128)
        tc_ = TwcT[:N1, :].unsqueeze(1).broadcast_to([N1, G, 128])
        ts_ = TwsT[:N1, :].unsqueeze(1).broadcast_to([N1, G, 128])
        P.tt("dve", m1, v3(m1[:N1, :WT]), pa, v3(pa[:N1, :WT]), TwcT, tc_, ALU.mult)
        P.tt("dve", m2, v3(m2[:N1, :WT]), pb, v3(pb[:N1, :WT]), TwsT, ts_, ALU.mult)
        P.tt("pool", Bpr, Bpr[:N1, :WT], m1, m1[:N1, :WT], m2, m2[:N1, :WT], ALU.subtract)
        P.tt("dve", m1, v3(m1[:N1, :WT]), pa, v3(pa[:N1, :WT]), TwsT, ts_, ALU.mult)
        P.tt("dve", m2, v3(m2[:N1, :WT]), pb, v3(pb[:N1, :WT]), TwcT, tc_, ALU.mult)
        P.tt("pool", Bpi, Bpi[:N1, :WT], m1, m1[:N1, :WT], m2, m2[:N1, :WT], ALU.add)
        yield
        P.mm(psy, psy[:R, :WT], I2c[:N1, :], Bpr[:N1, :WT], True, False, reads=[I2c, Bpr])
        P.mm(psy, psy[:R, :WT], I2ns[:N1, :], Bpi[:N1, :WT], False, True, reads=[I2ns, Bpi])
        yield

    def emit_kT(o, cq, kf, kb):
        r0 = o * 512 + cq * 128
        P.dma("sp", kT.ap[r0:r0 + 128, 0:L], kf[:], reads=[kf], writes=[kT])
        P.dma("sp", kT.ap[1024 + r0:1024 + r0 + 128, 0:L], kb[:], reads=[kb], writes=[kT])

    hyena_filters_td(K, i, I, C, L, emit_kT)

    mark2 = P.mark()
    Uf32 = [sb([R, G, 128], F32, f"hy_Uf{k}") for k in range(NS)]
    Ub32 = [sb([R, G, 128], F32, f"hy_Ub{k}") for k in range(NS)]
    Uf_ = [sb([R, G, 128], HY_DT, f"hy_Ufb{k}") for k in range(NS)]
    Ub_ = [sb([R, G, 128], HY_DT, f"hy_Ubb{k}") for k in range(NS)]
    Xs = [[sb([128, 512], F32, f"hy_Xs{k}{m}") for m in range(2)] for k in range(NS)]
    Ko = [[sb([128, 512], F32, f"hy_Ko{k}{m}") for m in range(2)] for k in range(NS)]

    def filt_group(gi):
        sl = gi % NS
        oc0 = gi * G
        uf, ub = Uf_[sl], Ub_[sl]
        P.dma("sp", Uf32[sl][:], kT.ap[oc0:oc0 + G, 0:L].rearrange("c (a b) -> a c b", b=128), reads=[kT], writes=[Uf32[sl]])
        P.dma("sp", Ub32[sl][:], kT.ap[1024 + oc0:1024 + oc0 + G, 0:L].rearrange("c (a b) -> a c b", b=128), reads=[kT], writes=[Ub32[sl]])
        P.copy("act", uf, uf[:], Uf32[sl], Uf32[sl][:])
        P.copy("act", ub, ub[:], Ub32[sl], Ub32[sl][:])
        pfr, pfi = K.ps[4 * sl + 2], K.ps[4 * sl + 3]
        yield from fft_fwd((uf, lambda c: uf[:, c, :]), pfr, pfi, sl)
        P.copy("act", Xs[sl][0], Xs[sl][0][:, :W], pfr, pfr[:, :W])
        P.copy("act", Xs[sl][1], Xs[sl][1][:, :W], pfi, pfi[:, :W])
        pbr, pbi = K.ps[4 * sl + 2], K.ps[4 * sl + 3]
        yield from fft_fwd((ub, lambda c: ub[:, c, :]), pbr, pbi, sl)
        kr, ki = Ko[sl]
        P.tt("dve", kr, kr[:, :W], Xs[sl][0], Xs[sl][0][:, :W], pbr, pbr[:, :W], ALU.add)
        P.tt("dve", ki, ki[:, :W], Xs[sl][1], Xs[sl][1][:, :W], pbi, pbi[:, :W], ALU.subtract)
        P.dma("pool", KFr.ap[oc0 // 4, :, 0:W], kr[:, :W], reads=[kr], writes=[KFr])
        P.dma("pool", KFi.ap[oc0 // 4, :, 0:W], ki[:, :W], reads=[ki], writes=[KFi])
        yield

    run_interleaved((filt_group(gi) for gi in range(1024 // G)), width=NS)
    P.release_to(mark2)

    cw = sb([R, 1536, 3], F32, "hy_cw"); cb = sb([R, 1536], F32, "hy_cb"); hb = sb([R, 1024], F32, "hy_hb")
    with nc.allow_non_contiguous_dma(reason="broadcast parameter loads"):
        for j in range(3):
            P.dma("sp", cw[:, :, j], I["ev_conv_w"].ap[i, j:j + 1, :].partition_broadcast(R), reads=[I["ev_conv_w"]], writes=[cw])
        P.dma("sp", cb[:], I["ev_conv_b"].ap[i].rearrange("(o c) -> o c", o=1).partition_broadcast(R), reads=[I["ev_conv_b"]], writes=[cb])
        P.dma("sp", hb[:], I["hy_bias"].ap[i].rearrange("o c -> (o c)").rearrange("(o c) -> o c", o=1).partition_broadcast(R),
              reads=[I["hy_bias"]], writes=[hb])
    raw = [[sb([R, G, 130], F32, f"hy_raw{k}{m}") for m in range(3)] for k in range(NS)]
    cvs = [[sb([R, G, 128], F32, f"hy_cv{k}{m}") for m in range(3)] for k in range(NS)]
    cts = [sb([R, G, 128], F32, f"hy_ct{k}") for k in range(NS)]
    ybs = [sb([R, G, 128], HY_DT, f"hy_yb{k}") for k in range(NS)]
    kfr = [[sb([128, 512], F32, f"hy_kfr{k}{o}") for o in range(2)] for k in range(NS)]
    kfi = [[sb([128, 512], F32, f"hy_kfi{k}{o}") for o in range(2)] for k in range(NS)]
    Yrs = [sb([128, 512], HY_DT, f"hy_Yr{k}") for k in range(NS)]; Yis = [sb([128, 512], HY_DT, f"hy_Yi{k}") for k in range(NS)]
    y1s = [sb([R, G, 128], F32, f"hy_y1{k}") for k in range(NS)]; youts = [sb([R, G, 128], F32, f"hy_yout{k}") for k in range(NS)]
    for k in range(NS):
        for m in range(3):
            P.memset("pool", raw[k][m], raw[k][m][:], 0.0)

    def data_group(gi):
        sl = gi % NS
        c0 = gi * G
        rw, cv, ct, yb = raw[sl], cvs[sl], cts[sl], ybs[sl]
        m1, m2 = m1s[sl], m2s[sl]
        for m in range(3):
            rows = zT.ap[m * 512 + c0:m * 512 + c0 + G, :]
            P.dma("sp", rw[m][:, :, 1:129], rows[:, t_base:t_base + L].rearrange("c (a b) -> a c b", b=128),
                  reads=[zT], writes=[rw[m]])
            if R > 1:
                with nc.allow_non_contiguous_dma(reason="1-column conv halos"):
                    P.dma("sp", rw[m][1:R, :, 0:1], rows[:, t_base + 127:t_base + L - 1].rearrange("c (a b) -> a c b", b=128)[:, :, 0:1],
                          reads=[zT], writes=[rw[m]])
                    P.dma("sp", rw[m][0:R - 1, :, 129:130], rows[:, t_base + 128:t_base + L].rearrange("c (a b) -> a c b", b=128)[:, :, 0:1],
                          reads=[zT], writes=[rw[m]])
        for o in range(2):
            oc0 = o * 512 + c0
            P.dma("sp", kfr[sl][o][:, :W], KFr.ap[oc0 // 4, :, 0:W], reads=[KFr], writes=[kfr[sl][o]])
            P.dma("sp", kfi[sl][o][:, :W], KFi.ap[oc0 // 4, :, 0:W], reads=[KFi], writes=[kfi[sl][o]])
        for m in range(3):
            ch = slice(m * 512 + c0, m * 512 + c0 + G)
            wj = lambda j: cw[:, ch, j].unsqueeze(2).broadcast_to([R, G, 128])
            P.tt("dve", cv[m], cv[m][:], rw[m], rw[m][:, :, 0:128], cw, wj(0), ALU.mult)
            P.tt("pool", ct, ct[:], rw[m], rw[m][:, :, 1:129], cw, wj(1), ALU.mult)
            P.tt("dve", cv[m], cv[m][:], cv[m], cv[m][:], ct, ct[:], ALU.add)
            P.tt("pool", ct, ct[:], rw[m], rw[m][:, :, 2:130], cw, wj(2), ALU.mult)
            P.tt("dve", cv[m], cv[m][:], cv[m], cv[m][:], ct, ct[:], ALU.add)
            P.tt("dve", cv[m], cv[m][:], cv[m], cv[m][:], cb, cb[:, ch].unsqueeze(2).broadcast_to([R, G, 128]), ALU.add)
        yield
        ycur = cv[0]
        for o in range(2):
            P.copy("act", yb, yb[:], ycur, ycur[:])
            pxr, pxi = K.ps[4 * sl + 2], K.ps[4 * sl + 3]
            yield from fft_fwd((yb, lambda c: yb[:, c, :]), pxr, pxi, sl)
            kr, ki = kfr[sl][o], kfi[sl][o]
            Yr, Yi = Yrs[sl], Yis[sl]
            P.tt("dve", m1, m1[:, :W], pxr, pxr[:, :W], kr, kr[:, :W], ALU.mult)
            P.tt("dve", m2, m2[:, :W], pxi, pxi[:, :W], ki, ki[:, :W], ALU.mult)
            P.tt("pool", Yr, Yr[:, :W], m1, m1[:, :W], m2, m2[:, :W], ALU.subtract)
            P.tt("dve", m1, m1[:, :W], pxr, pxr[:, :W], ki, ki[:, :W], ALU.mult)
            P.tt("dve", m2, m2[:, :W], pxi, pxi[:, :W], kr, kr[:, :W], ALU.mult)
            P.tt("pool", Yi, Yi[:, :W], m1, m1[:, :W], m2, m2[:, :W], ALU.add)
            yield
            py = K.ps[4 * sl + 2]
            yield from fft_inv(Yr, Yi, py, sl)
            hbo = hb[:, o * 512 + c0:o * 512 + c0 + G].unsqueeze(2).broadcast_to([R, G, 128])
            P.tt("dve", ct, ct[:], ycur, ycur[:], hb, hbo, ALU.mult)
            P.tt("dve", ct, ct[:], ct, ct[:], py, py[:R, :WT].rearrange("q (c k) -> q c k", k=128), ALU.add)
            dst = y1s[sl] if o == 0 else youts[sl]
            P.tt("dve", dst, dst[:], ct, ct[:], cv[1 + o], cv[1 + o][:], ALU.mult)
            ycur = dst
            yield
        P.dma("pool", mixT.ap[c0:c0 + G, t_base:t_base + L].rearrange("c (a b) -> a c b", b=128), ycur[:],
              reads=[ycur], writes=[mixT])
        yield

    run_interleaved((data_group(gi) for gi in range(512 // G)), width=NS)
    P.release_to(mark)


def hyena_ctx_consts():
    L, N = 256, 512
    t = np.arange(L, dtype=np.float64)[:, None]; f = np.arange(N, dtype=np.float64)[None, :]
    a = 2 * np.pi * t * f / N
    c = {"hc_Fc": np.cos(a), "hc_Fns": -np.sin(a), "hc_Ic": np.cos(a).T / N, "hc_Ins": -np.sin(a).T / N}
    return {k: np.ascontiguousarray(v, dtype=np.float32) for k, v in c.items()}


def hyena_ctx_stage(K, i, I, zT, mixT, C, Cd, t_base=0):
    P = K.P
    nc = P.nc
    sb = P.sb
    L, N = 256, 512
    mark = P.mark()
    ident = C["k_ident"]
    Fc = sb([128, 2, 512], F32, "hc_Fc"); Fns = sb([128, 2, 512], F32, "hc_Fns")
    Ic = sb([128, 4, 256], F32, "hc_Ic"); Ins = sb([128, 4, 256], F32, "hc_Ins")
    P.dma("sp", Fc[:], Cd["hc_Fc"].ap.rearrange("(k p) f -> p k f", p=128), reads=[Cd["hc_Fc"]], writes=[Fc])
    P.dma("sp", Fns[:], Cd["hc_Fns"].ap.rearrange("(k p) f -> p k f", p=128), reads=[Cd["hc_Fns"]], writes=[Fns])
    P.dma("sp", Ic[:], Cd["hc_Ic"].ap.rearrange("(k p) t -> p k t", p=128), reads=[Cd["hc_Ic"]], writes=[Ic])
    P.dma("sp", Ins[:], Cd["hc_Ins"].ap.rearrange("(k p) t -> p k t", p=128), reads=[Cd["hc_Ins"]], writes=[Ins])
    ktm = [[sb([128, 2, 512], F32, f"hc_ktm{dr}{o}") for o in range(2)] for dr in range(2)]

    def emit(o, cq, kf, kb):
        for (src, dr) in ((kf, 0), (kb, 1)):
            for tc in range(2):
                ps = next_ps(K)
                P.op("pe", lambda e: e.transpose(ps[:, 0:128], src[:, tc * 128:(tc + 1) * 128], ident[:]), reads=[src, ident], writes=[ps])
                P.copy("act", ktm[dr][o], ktm[dr][o][:, tc, cq * 128:(cq + 1) * 128], ps, ps[:, 0:128])

    hyena_filters_td(K, i, I, C, L, emit)

    def dft(x_t, fc):
        psr = next_ps(K); psi = next_ps(K)
        for tc in range(2):
            P.mm(psr, psr[:, :], Fc[:, tc, fc * 128:(fc + 1) * 128], x_t[:, tc, :], tc == 0, tc == 1, reads=[Fc, x_t])
        for tc in range(2):
            P.mm(psi, psi[:, :], Fns[:, tc, fc * 128:(fc + 1) * 128], x_t[:, tc, :], tc == 0, tc == 1, reads=[Fns, x_t])
        return psr, psi

    KFr = [sb([128, 4, 512], F32, f"hc_KFr{o}") for o in range(2)]; KFi = [sb([128, 4, 512], F32, f"hc_KFi{o}") for o in range(2)]
    Xr = sb([128, 512], F32, "hc_Xr"); Xi = sb([128, 512], F32, "hc_Xi")
    for o in range(2):
        for fc in range(4):
            pr, pi_ = dft(ktm[0][o], fc)
            P.copy("act", Xr, Xr[:], pr, pr[:, :])
            P.copy("act", Xi, Xi[:], pi_, pi_[:, :])
            pr, pi_ = dft(ktm[1][o], fc)
            P.tt("dve", KFr[o], KFr[o][:, fc, :], Xr, Xr[:], pr, pr[:, :], ALU.add)
            P.tt("dve", KFi[o], KFi[o][:, fc, :], Xi, Xi[:], pi_, pi_[:, :], ALU.subtract)
    cwt = sb([128, 12, 3], F32, "hc_cw"); cbt = sb([128, 12], F32, "hc_cb"); hbb = sb([128, 2, 512], F32, "hc_hb")
    with nc.allow_non_contiguous_dma(reason="small parameter loads"):
        for j in range(3):
            P.dma("sp", cwt[:, :, j], I["ev_conv_w"].ap[i, j].rearrange("(k p) -> p k", p=128), reads=[I["ev_conv_w"]], writes=[cwt])
        P.dma("sp", cbt[:], I["ev_conv_b"].ap[i].rearrange("(k p) -> p k", p=128), reads=[I["ev_conv_b"]], writes=[cbt])
        P.dma("sp", hbb[:].rearrange("p o c -> p (o c)"),
              I["hy_bias"].ap[i].rearrange("o c -> (o c)").rearrange("(a n) -> a n", a=1).partition_broadcast(128),
              reads=[I["hy_bias"]], writes=[hbb])
    xtm = [sb([128, 2, 512], F32, f"hc_xtm{m}") for m in range(3)]
    raw = [sb([128, 258], F32, f"hc_raw{k}") for k in range(2)]
    cv = [sb([128, 256], F32, f"hc_cv{k}") for k in range(2)]
    for k in range(2):
        P.memset("pool", raw[k], raw[k][:], 0.0)
    for m in range(3):
        for cq in range(4):
            mc = m * 4 + cq
            rw, cvb = raw[mc % 2], cv[mc % 2]
            P.dma("sp", rw[:, 1:257], zT.ap[mc * 128:(mc + 1) * 128, t_base:t_base + L], reads=[zT], writes=[rw])
            P.ts("dve", cvb, cvb[:], rw, rw[:, 0:256], cwt[:, mc, 0:1], ALU.mult, cbt[:, mc:mc + 1], ALU.add, extra_reads=[cwt, cbt])
            P.stt(cvb, cvb[:], rw, rw[:, 1:257], cwt[:, mc, 1:2], cvb, cvb[:], ALU.mult, ALU.add, extra_reads=[cwt])
            P.stt(cvb, cvb[:], rw, rw[:, 2:258], cwt[:, mc, 2:3], cvb, cvb[:], ALU.mult, ALU.add, extra_reads=[cwt])
            for tc in range(2):
                ps = next_ps(K)
                P.op("pe", lambda e: e.transpose(ps[:, 0:128], cvb[:, tc * 128:(tc + 1) * 128], ident[:]), reads=[cvb, ident], writes=[ps])
                P.copy("act", xtm[m], xtm[m][:, tc, cq * 128:(cq + 1) * 128], ps, ps[:, 0:128])
    Yr = sb([128, 4, 512], F32, "hc_Yr"); Yi = sb([128, 4, 512], F32, "hc_Yi")
    ya = sb([128, 2, 512], F32, "hc_ya"); yb_ = sb([128, 2, 512], F32, "hc_yb")
    t1 = sb([128, 512], F32, "hc_t1"); t2 = sb([128, 512], F32, "hc_t2")
    ycur = xtm[0]
    for o in range(2):
        for fc in range(4):
            pr, pi_ = dft(ycur, fc)
            P.tt("dve", t1, t1[:], pr, pr[:, :], KFr[o], KFr[o][:, fc, :], ALU.mult)
            P.tt("dve", t2, t2[:], pi_, pi_[:, :], KFi[o], KFi[o][:, fc, :], ALU.mult)
            P.tt("pool", Yr, Yr[:, fc, :], t1, t1[:], t2, t2[:], ALU.subtract)
            P.tt("dve", t1, t1[:], pr, pr[:, :], KFi[o], KFi[o][:, fc, :], ALU.mult)
            P.tt("dve", t2, t2[:], pi_, pi_[:, :], KFr[o], KFr[o][:, fc, :], ALU.mult)
            P.tt("pool", Yi, Yi[:, fc, :], t1, t1[:], t2, t2[:], ALU.add)
        dst = ya if o == 0 else yb_
        for tc in range(2):
            py = next_ps(K)
            for fc in range(4):
                P.mm(py, py[:, :], Ic[:, fc, tc * 128:(tc + 1) * 128], Yr[:, fc, :], fc == 0, False, reads=[Ic, Yr])
                P.mm(py, py[:, :], Ins[:, fc, tc * 128:(tc + 1) * 128], Yi[:, fc, :], False, fc == 3, reads=[Ins, Yi])
            P.tt("dve", t1, t1[:], ycur, ycur[:, tc, :], hbb, hbb[:, o, :], ALU.mult)
            P.tt("dve", t1, t1[:], t1, t1[:], py, py[:, :], ALU.add)
            P.tt("dve", dst, dst[:, tc, :], t1, t1[:], xtm[1 + o], xtm[1 + o][:, tc, :], ALU.mult)
        ycur = dst
    ofm = sb([128, 4, 256], F32, "hc_ofm")
    for cq in range(4):
        for tc in range(2):
            ps = next_ps(K)
            P.op("pe", lambda e: e.transpose(ps[:, 0:128], ycur[:, tc, cq * 128:(cq + 1) * 128], ident[:]), reads=[ycur, ident], writes=[ps])
            P.copy("act", ofm, ofm[:, cq, tc * 128:(tc + 1) * 128], ps, ps[:, 0:128])
    P.dma("sp", mixT.ap[0:512, t_base:t_base + L].rearrange("(k p) t -> p k t", p=128), ofm[:], reads=[ofm], writes=[mixT])
    P.release_to(mark)


_DBG_SKIP_ATTN = False
MLA_SCALE = 96.0 ** -0.5
GQA_SCALE = 64.0 ** -0.5
GRID_W = 64
ROPE_BASE = 10000.0


def odd_segs():
    segs = [(0, 1184), (392, 8), (384, 8), (408, 8), (400, 8)]
    for hq in range(8):
        b = 416 + 64 * hq
        segs += [(b + 16, 16), (b, 16), (b + 48, 16), (b + 32, 16)]
    for kh in range(2):
        b = 928 + 64 * kh
        segs += [(b + 16, 16), (b, 16), (b + 48, 16), (b + 32, 16)]
    return segs


def rope_consts(L):
    out = {}
    rows = L // GRID_W
    row = np.repeat(np.arange(rows), GRID_W).astype(np.float32)
    col = np.tile(np.arange(GRID_W), rows).astype(np.float32)
    for dim, nm in ((32, "mla"), (64, "gqa")):
        nf = dim // 4
        inv = (np.float32(ROPE_BASE) ** (-np.arange(nf, dtype=np.float32) / np.float32(nf))).astype(np.float32)
        ar = (row[:, None] * inv[None, :]).astype(np.float32)
        ac = (col[:, None] * inv[None, :]).astype(np.float32)
        cr, sr, cc, sc = np.cos(ar), np.sin(ar), np.cos(ac), np.sin(ac)
        C = np.concatenate([cr, cr, cc, cc], axis=1).T
        S = np.concatenate([-sr, sr, -sc, sc], axis=1).T
        out[f"rope_{nm}_C"] = np.ascontiguousarray(C, dtype=np.float32)
        out[f"rope_{nm}_S"] = np.ascontiguousarray(S, dtype=np.float32)
    j = np.arange(128)[:, None]; r = np.arange(128)[None, :]
    out["k_mask_prev"] = np.tile((j >= r).astype(np.float32), (1, 4))
    out["k_mask_next"] = np.tile((j <= r).astype(np.float32), (1, 4))
    selM = np.zeros((96, 97), np.float32); selM[:, 96] = 1.0
    selG = np.zeros((64, 65), np.float32); selG[:, 64] = 1.0
    sel65 = np.zeros((65, 64), np.float32); sel65[64, :] = 1.0
    vs = np.zeros((1, 65), np.float32); vs[0, 64] = 1.0
    out["k_selM"] = selM; out["k_selG"] = selG; out["k_sel65"] = sel65; out["k_vsink"] = vs
    return out


def attn_core(K, A, q_t, q_ap, nq, chunks, outs):
    P = K.P
    pot = A.pot
    n = len(chunks)
    LOOK = 2

    def score(ci):
        k_t, k_ap, v_t, v_ap, mask, cols, qrows = chunks[ci]
        c0, ncol = cols if cols is not None else (0, nq)
        nk = k_ap.shape[1]
        ps = next_ps(K)
        q_use = q_ap[:, c0:c0 + ncol] if qrows is None else q_ap[qrows[0]:qrows[1], c0:c0 + ncol]
        P.mm(ps, ps[:nk, c0:c0 + ncol], k_ap, q_use, True, True, reads=[k_t, q_t])
        return ps

    pend = {}
    for ci in range(min(LOOK, n)):
        pend[ci] = score(ci)
    for ci, (k_t, k_ap, v_t, v_ap, mask, cols, qrows) in enumerate(chunks):
        c0, ncol = cols if cols is not None else (0, nq)
        nk = k_ap.shape[1]
        ps = pend.pop(ci)
        pt = A.pt[A.pti % len(A.pt)]
        A.pti += 1
        P.act(pt, pt[:nk, c0:c0 + ncol], ps, ps[:nk, c0:c0 + ncol], AF.Exp)
        if mask is not None:
            P.tt("dve", pt, pt[:nk, c0:c0 + ncol], pt, pt[:nk, c0:c0 + ncol], mask[0], mask[1], ALU.mult)
        if ci + LOOK < n:
            pend[ci + LOOK] = score(ci + LOOK)
        P.mm(pot, pot[:65, c0:c0 + ncol], v_ap, pt[:nk, c0:c0 + ncol], ci == 0, ci == n - 1, reads=[v_t, pt])
    P.copy("act", A.ot, A.ot[:65, :nq], pot, pot[:65, :nq])
    ps = next_ps(K)
    P.mm(ps, ps[:64, :nq], A.sel65[:, :], A.ot[:65, :nq], True, True, reads=[A.sel65, A.ot])
    P.op("dve", lambda e: e.reciprocal(out=A.rden[:64, :nq], in_=ps[:64, :nq]), reads=[ps], writes=[A.rden])
    ob = A.ob[A.obi % 2]
    A.obi += 1
    P.tt("dve", ob, ob[:64, :nq], A.ot, A.ot[:64, :nq], A.rden, A.rden[:64, :nq], ALU.mult)
    for (dap, c0, ncol) in outs:
        P.dma("pool", dap, ob[:64, c0:c0 + ncol], reads=[ob], writes=[A.mixT])


def odd_mixer_stage(K, i, I, zT, mixT, C, S, need_ctx):
    P = K.P
    nc = P.nc
    sb = P.sb
    NT, NCX = K.NT, K.NC
    mark = P.mark()
    tiles = ([(0, NCX, 1)] if NCX else []) + [(NCX + k * 512, min(512, K.NL - k * 512), 0) for k in range((K.NL + 511) // 512)]
    wst = sb([128, 1024], F32, "od_wst")
    wukv = sb([128, 1024], BF16, "od_wukv")
    P.dma("sp", wst[:], I["mla_w_ukv"].ap[i], reads=[I["mla_w_ukv"]], writes=[wst])
    P.copy("pool", wukv, wukv[:], wst, wst[:])
    wuq = sb([128, 2, 768], BF16, "od_wuq"); wuqs = sb([128, 2, 768], BF16, "od_wuqs")
    wst2 = sb([128, 2, 768], F32, "od_wst2")
    P.dma("sp", wst2[:], I["mla_w_uq"].ap[i].rearrange("(k p) n -> p k n", p=128), reads=[I["mla_w_uq"]], writes=[wst2])
    P.copy("pool", wuq, wuq[:], wst2, wst2[:])
    P.memset("pool", wuqs, wuqs[:], 0.0)
    for h in range(8):
        b = h * 96 + 64
        for (dst, src) in ((0, 8), (8, 0), (16, 24), (24, 16)):
            P.copy("pool", wuqs, wuqs[:, :, b + dst:b + dst + 8], wst2, wst2[:, :, b + src:b + src + 8])
    gq = sb([128, 2], F32, "od_gq"); gkv = sb([128, 1], F32, "od_gkv")
    sinkb = sb([96, 8], F32, "od_sink")
    with nc.allow_non_contiguous_dma(reason="tiny vector loads"):
        P.dma("sp", gq[:], I["mla_q_norm"].ap[i].rearrange("(k p) -> p k", p=128), reads=[I["mla_q_norm"]], writes=[gq])
        P.dma("sp", gkv[:], I["mla_kv_norm"].ap[i].rearrange("(p o) -> p o", o=1), reads=[I["mla_kv_norm"]], writes=[gkv])
        P.dma("sp", sinkb[64:96, :], I["gqa_sink"].ap[i].rearrange("(o h) -> o h", o=1).partition_broadcast(32),
              reads=[I["gqa_sink"]], writes=[sinkb])
    selM, selG, sel65, vsink = C["k_selM"], C["k_selG"], C["k_sel65"], C["k_vsink"]
    ident = C["k_ident"]
    vsb = sb([1, 65], BF16, "od_vsb")
    P.copy("dve", vsb, vsb[:], vsink, vsink[:])
    kmaxM = sb([128, 8], F32, "od_kmaxM"); kmaxG = sb([128, 2], F32, "od_kmaxG")
    P.memset("pool", kmaxM, kmaxM[:], 0.0)
    P.memset("pool", kmaxG, kmaxG[:], 0.0)
    mx1 = sb([128, 1], F32, "od_mx1")
    markp = P.mark()
    xin = [sb([128, 2, 512], F32, f"od_xin{k}") for k in range(2)]
    sq = sb([128, 2, 512], F32, "od_sq"); rstd = sb([128, 512], F32, "od_rstd")
    kvn = sb([128, 512], BF16, "od_kvn"); cqn = sb([128, 2, 512], BF16, "od_cqn")
    raw = [sb([128, 512], F32, f"od_raw{k}") for k in range(2)]; sw = [sb([128, 512], F32, f"od_sw{k}") for k in range(2)]
    tC = [sb([128, 512], F32, f"od_tC{k}") for k in range(2)]; tS = [sb([128, 512], F32, f"od_tS{k}") for k in range(2)]
    rot = sb([128, 512], F32, "od_rot"); rt2 = sb([128, 512], F32, "od_rt2")
    kf = sb([96, 512], F32, "od_kf"); kf2 = sb([96, 512], F32, "od_kf2")
    kt = [sb([97, 512], BF16, f"od_kt{k}") for k in range(2)]
    vt = [sb([128, 8, 65], BF16, f"od_vt{k}") for k in range(2)]
    vg = [sb([128, 2, 65], BF16, f"od_vg{k}") for k in range(2)]
    gvt = sb([128, 512], F32, "od_gvt")
    for k in range(2):
        P.memset("pool", kt[k], kt[k][96:97, :], 1.0)
        P.memset("pool", vt[k], vt[k][:], 1.0)
        P.memset("pool", vg[k], vg[k][:], 1.0)

    def rms(x_t, x_ap_k, nk_, n, g_t, g_ap_k, out_t, out_ap_k):
        ps = next_ps(K)
        for k in range(nk_):
            P.act(sq, sq[:, k, :n], x_t, x_ap_k(k), AF.Square)
        for k in range(nk_):
            P.mm(ps, ps[:, :n], K.ones[:], sq[:, k, :n], k == 0, k == nk_ - 1, reads=[K.ones, sq])
        P.ts("dve", rstd, rstd[:, :n], ps, ps[:, :n], 1.0 / (128 * nk_), ALU.mult, EPS, ALU.add)
        P.op("act", lambda e: e.sqrt(out=rstd[:, :n], in_=rstd[:, :n]), reads=[rstd], writes=[rstd])
        P.op("dve", lambda e: e.reciprocal(out=rstd[:, :n], in_=rstd[:, :n]), reads=[rstd], writes=[rstd])
        for k in range(nk_):
            P.stt(out_t, out_ap_k(k), x_t, x_ap_k(k), g_ap_k(k), rstd, rstd[:, :n], ALU.mult, ALU.mult, extra_reads=[g_t])

    def sumsq_max(src_t, src_ap, nrows, n, sel, dstcol_t, dstcol_ap, prow):
        P.act(kf2, kf2[:nrows, :n], src_t, src_ap, AF.Square)
        ps = next_ps(K)
        P.mm(ps, ps[:prow + 1, :n], sel[:nrows, :prow + 1], kf2[:nrows, :n], True, True, reads=[sel, kf2])
        P.op("dve", lambda e: e.reduce_max(out=mx1[prow:prow + 1, :], in_=ps[prow:prow + 1, :n], axis=AX.X), reads=[ps], writes=[mx1])
        P.tt("dve", dstcol_t, dstcol_ap, dstcol_t, dstcol_ap, mx1, mx1[prow:prow + 1, :], ALU.max)

    for it, (t0, n, is_ctx) in enumerate(tiles):
        x = xin[it % 2]
        lat0 = t0 - NCX
        P.dma("sp", x[:, 0, :n], zT.ap[256:384, t0:t0 + n], reads=[zT], writes=[x])
        rms(x, lambda k: x[:, 0, :n], 1, n, gkv, lambda k: gkv[:, 0:1], kvn, lambda k: kvn[:, :n])
        r_, s_, c_, sn_ = raw[it % 2], sw[it % 2], tC[it % 2], tS[it % 2]
        P.dma("sp", r_[64:96, :n], zT.ap[384:416, t0:t0 + n], reads=[zT], writes=[r_])
        if not is_ctx:
            P.dma("sp", s_[64:96, :n], zT.ap[1184:1216, t0:t0 + n], reads=[zT], writes=[s_])
            P.dma("sp", c_[64:96, :n], C["rope_mla_C"].ap[:, lat0:lat0 + n], reads=[C["rope_mla_C"]], writes=[c_])
            P.dma("sp", sn_[64:96, :n], C["rope_mla_S"].ap[:, lat0:lat0 + n], reads=[C["rope_mla_S"]], writes=[sn_])
            P.tt("dve", rot, rot[64:96, :n], r_, r_[64:96, :n], c_, c_[64:96, :n], ALU.mult)
            P.tt("pool", rt2, rt2[64:96, :n], s_, s_[64:96, :n], sn_, sn_[64:96, :n], ALU.mult)
            P.tt("dve", kf, kf[64:96, :n], rot, rot[64:96, :n], rt2, rt2[64:96, :n], ALU.add)
        else:
            P.copy("dve", kf, kf[64:96, :n], r_, r_[64:96, :n])
        for h in range(8):
            ps = next_ps(K)
            P.mm(ps, ps[:64, :n], wukv[:, h * 128:h * 128 + 64], kvn[:, :n], True, True, reads=[wukv, kvn])
            P.copy("act", kf, kf[0:64, :n], ps, ps[:64, :n])
            sumsq_max(kf, kf[0:96, :n], 96, n, selM, kmaxM, kmaxM[96:97, h:h + 1], 96)
            ktb = kt[h % 2]
            P.copy("pool", ktb, ktb[0:96, :n], kf, kf[0:96, :n])
            P.dma("sp", S["KM"].ap[h, :, t0:t0 + n], ktb[:, :n], reads=[ktb], writes=[S["KM"]])
        for q in range((n + 127) // 128):
            vtb = vt[q % 2]
            for half in range(2):
                ps = next_ps(K)
                P.mm(ps, ps[:, :512], kvn[:, q * 128:(q + 1) * 128], wukv[:, half * 512:(half + 1) * 512], True, True,
                     reads=[kvn, wukv])
                P.copy("act", vtb, vtb[:, half * 4:half * 4 + 4, 0:64],
                       ps, ps[:, :512].rearrange("t (h d) -> t h d", d=128)[:, :, 64:128])
            with nc.allow_non_contiguous_dma(reason="token-major V rows"):
                P.dma("sp", S["VM"].ap[:, t0 + q * 128:t0 + (q + 1) * 128, :].rearrange("h t d -> t h d"), vtb[:],
                      reads=[vtb], writes=[S["VM"]])
        for kh in range(2):
            P.dma("sp", r_[0:64, :n], zT.ap[928 + 64 * kh:928 + 64 * kh + 64, t0:t0 + n], reads=[zT], writes=[r_])
            if not is_ctx:
                P.dma("sp", s_[0:64, :n], zT.ap[1728 + 64 * kh:1728 + 64 * kh + 64, t0:t0 + n], reads=[zT], writes=[s_])
                P.dma("sp", c_[0:64, :n], C["rope_gqa_C"].ap[:, lat0:lat0 + n], reads=[C["rope_gqa_C"]], writes=[c_])
                P.dma("sp", sn_[0:64, :n], C["rope_gqa_S"].ap[:, lat0:lat0 + n], reads=[C["rope_gqa_S"]], writes=[sn_])
                P.tt("dve", rot, rot[0:64, :n], r_, r_[0:64, :n], c_, c_[0:64, :n], ALU.mult)
                P.tt("pool", rt2, rt2[0:64, :n], s_, s_[0:64, :n], sn_, sn_[0:64, :n], ALU.mult)
                P.tt("dve", kf, kf[0:64, :n], rot, rot[0:64, :n], rt2, rt2[0:64, :n], ALU.add)
            else:
                P.copy("dve", kf, kf[0:64, :n], r_, r_[0:64, :n])
            sumsq_max(kf, kf[0:64, :n], 64, n, selG, kmaxG, kmaxG[64:65, kh:kh + 1], 64)
            ktb = kt[kh % 2]
            P.copy("pool", ktb, ktb[0:64, :n], kf, kf[0:64, :n])
            P.memset("pool", ktb, ktb[64:96, :n], 0.0)
            P.memset("pool", ktb, ktb[64:65, :n], 1.0)
            P.dma("sp", S["KG"].ap[kh, :, t0:t0 + n], ktb[0:66, :n], reads=[ktb], writes=[S["KG"]])
            P.memset("pool", ktb, ktb[96:97, :n], 1.0)
        P.dma("sp", gvt[:, :n], zT.ap[1056:1184, t0:t0 + n], reads=[zT], writes=[gvt])
        for q in range((n + 127) // 128):
            ps = next_ps(K)
            P.op("pe", lambda e: e.transpose(ps[:, 0:128], gvt[:, q * 128:(q + 1) * 128], ident[:]), reads=[gvt, ident], writes=[ps])
            vgb = vg[q % 2]
            P.copy("act", vgb, vgb[:, :, 0:64], ps, ps[:, 0:128].rearrange("t (h d) -> t h d", d=64))
            with nc.allow_non_contiguous_dma(reason="token-major V rows"):
                P.dma("sp", S["VG"].ap[:, t0 + q * 128:t0 + (q + 1) * 128, :].rearrange("h t d -> t h d"), vgb[:],
                      reads=[vgb], writes=[S["VG"]])
    qf = sb([96, 512], F32, "od_qf")
    qt = [sb([97, 512], BF16, f"od_qt{k}") for k in range(2)]
    prod = sb([128, 512], F32, "od_prod")
    for it, (t0, n, is_ctx) in enumerate(tiles):
        if is_ctx and not need_ctx:
            continue
        x = xin[it % 2]
        lat0 = t0 - NCX
        P.dma("sp", x[:, :, :n], zT.ap[0:256, t0:t0 + n].rearrange("(k p) t -> p k t", p=128), reads=[zT], writes=[x])
        rms(x, lambda k: x[:, k, :n], 2, n, gq, lambda k: gq[:, k:k + 1], cqn, lambda k: cqn[:, k, :n])
        c_, sn_ = tC[it % 2], tS[it % 2]
        if not is_ctx:
            P.dma("sp", c_[64:96, :n], C["rope_mla_C"].ap[:, lat0:lat0 + n], reads=[C["rope_mla_C"]], writes=[c_])
            P.dma("sp", sn_[64:96, :n], C["rope_mla_S"].ap[:, lat0:lat0 + n], reads=[C["rope_mla_S"]], writes=[sn_])
        for h in range(8):
            ps = next_ps(K)
            for k in range(2):
                P.mm(ps, ps[:96, :n], wuq[:, k, h * 96:(h + 1) * 96], cqn[:, k, :n], k == 0, k == 1, reads=[wuq, cqn])
            if not is_ctx:
                ps2 = next_ps(K)
                for k in range(2):
                    P.mm(ps2, ps2[:96, :n], wuqs[:, k, h * 96:(h + 1) * 96], cqn[:, k, :n], k == 0, k == 1, reads=[wuqs, cqn])
                P.tt("dve", rot, rot[64:96, :n], ps, ps[64:96, :n], c_, c_[64:96, :n], ALU.mult)
                P.tt("dve", rt2, rt2[64:96, :n], ps2, ps2[64:96, :n], sn_, sn_[64:96, :n], ALU.mult)
                P.stt(qf, qf[64:96, :n], rot, rot[64:96, :n], 1.0, rt2, rt2[64:96, :n], ALU.mult, ALU.add)
                P.ts("dve", qf, qf[64:96, :n], qf, qf[64:96, :n], MLA_SCALE, ALU.mult)
                P.op("act", lambda e: e.mul(out=qf[0:64, :n], in_=ps[0:64, :n], mul=MLA_SCALE), reads=[ps], writes=[qf])
            else:
                P.op("act", lambda e: e.mul(out=qf[0:96, :n], in_=ps[0:96, :n], mul=MLA_SCALE), reads=[ps], writes=[qf])
            qtb = qt[h % 2]
            P.copy("pool", qtb, qtb[0:96, :n], qf, qf[0:96, :n])
            P.act(kf2, kf2[:96, :n], qf, qf[0:96, :n], AF.Square)
            psq = next_ps(K)
            P.mm(psq, psq[:97, :n], selM[:, :], kf2[:96, :n], True, True, reads=[selM, kf2])
            P.ts("dve", prod, prod[96:97, :n], psq, psq[96:97, :n], kmaxM[96:97, h:h + 1], ALU.mult, extra_reads=[kmaxM])
            P.op("act", lambda e: e.sqrt(out=prod[96:97, :n], in_=prod[96:97, :n]), reads=[prod], writes=[prod])
            P.ts("dve", qtb, qtb[96:97, :n], prod, prod[96:97, :n], -1.0, ALU.mult)
            P.dma("sp", S["QM"].ap[h, :, t0:t0 + n], qtb[:, :n], reads=[qtb], writes=[S["QM"]])
        r_, s_ = raw[it % 2], sw[it % 2]
        if not is_ctx:
            P.dma("sp", c_[0:64, :n], C["rope_gqa_C"].ap[:, lat0:lat0 + n], reads=[C["rope_gqa_C"]], writes=[c_])
            P.dma("sp", sn_[0:64, :n], C["rope_gqa_S"].ap[:, lat0:lat0 + n], reads=[C["rope_gqa_S"]], writes=[sn_])
        for hq in range(8):
            P.dma("sp", r_[0:64, :n], zT.ap[416 + 64 * hq:416 + 64 * hq + 64, t0:t0 + n], reads=[zT], writes=[r_])
            if not is_ctx:
                P.dma("sp", s_[0:64, :n], zT.ap[1216 + 64 * hq:1216 + 64 * hq + 64, t0:t0 + n], reads=[zT], writes=[s_])
                P.tt("dve", rot, rot[0:64, :n], r_, r_[0:64, :n], c_, c_[0:64, :n], ALU.mult)
                P.tt("pool", rt2, rt2[0:64, :n], s_, s_[0:64, :n], sn_, sn_[0:64, :n], ALU.mult)
                P.stt(qf, qf[0:64, :n], rot, rot[0:64, :n], 1.0, rt2, rt2[0:64, :n], ALU.mult, ALU.add)
                P.ts("dve", qf, qf[0:64, :n], qf, qf[0:64, :n], GQA_SCALE, ALU.mult)
            else:
                P.ts("dve", qf, qf[0:64, :n], r_, r_[0:64, :n], GQA_SCALE, ALU.mult)
            qtb = qt[hq % 2]
            P.copy("pool", qtb, qtb[0:64, :n], qf, qf[0:64, :n])
            P.memset("pool", qtb, qtb[64:96, :n], 1.0)
            P.act(kf2, kf2[:64, :n], qf, qf[0:64, :n], AF.Square)
            psq = next_ps(K)
            P.mm(psq, psq[:65, :n], selG[:, :], kf2[:64, :n], True, True, reads=[selG, kf2])
            P.ts("dve", prod, prod[64:65, :n], psq, psq[64:65, :n], kmaxG[64:65, hq // 4:hq // 4 + 1], ALU.mult, extra_reads=[kmaxG])
            P.op("act", lambda e: e.sqrt(out=prod[64:65, :n], in_=prod[64:65, :n]), reads=[prod], writes=[prod])
            P.ts("dve", qtb, qtb[64:65, :n], prod, prod[64:65, :n], -1.0, ALU.mult)
            P.dma("sp", S["QG"].ap[hq, :, t0:t0 + n], qtb[0:66, :n], reads=[qtb], writes=[S["QG"]])
    P.release_to(markp)
    if _DBG_SKIP_ATTN:
        P.release_to(mark)
        return
    A = Ctx()
    A.mixT = mixT
    A.sel65 = sel65
    K.ps_reserved = {7}
    A.pot = K.ps[7]
    A.pt = [sb([128, 512], BF16, f"at_pt{k}") for k in range(4)]
    A.pti = 0
    A.ot = sb([65, 512], F32, "at_ot"); A.rden = sb([64, 512], F32, "at_rden")
    A.ob = [sb([64, 512], F32, f"at_ob{k}") for k in range(2)]
    A.obi = 0
    NKC = NT // 128
    kres = sb([97, NT], BF16, "at_k"); vres = sb([128, NKC, 65], BF16, "at_v")
    qb_ = [sb([97, 512], BF16, f"at_q{k}") for k in range(2)]
    qi = 0
    for h in range(8):
        P.dma("sp", kres[:, :], S["KM"].ap[h, :, :], reads=[S["KM"]], writes=[kres])
        with nc.allow_non_contiguous_dma(reason="token-major V rows"):
            P.dma("sp", vres[:], S["VM"].ap[h].rearrange("(c p) d -> p c d", p=128), reads=[S["VM"]], writes=[vres])
        qlist = [(NCX + k * 512, min(512, K.NL - k * 512), False) for k in range((K.NL + 511) // 512)]
        if need_ctx and NCX:
            qlist = [(0, NCX, True)] + qlist
        for (t0, n, is_ctx) in qlist:
            qb = qb_[qi % 2]
            qi += 1
            P.dma("sp", qb[:, :n], S["QM"].ap[h, :, t0:t0 + n], reads=[S["QM"]], writes=[qb])
            kcs = range(NCX // 128) if is_ctx else range(NKC)
            chunks = [(kres, kres[:, kc * 128:(kc + 1) * 128], vres, vres[:, kc, :], None, None, None) for kc in kcs]
            attn_core(K, A, qb, qb[:, :n], n, chunks, [(mixT.ap[h * 64:(h + 1) * 64, t0:t0 + n], 0, n)])
    mprev, mnext = C["k_mask_prev"], C["k_mask_next"]
    ks2 = sb([96, 8], BF16, "at_ks2")
    P.copy("dve", ks2, ks2[64:96, :], sinkb, sinkb[64:96, :])
    P.memset("pool", ks2, ks2[64:65, :], 1.0)
    NB = K.NL // 128
    for kh in range(2):
        P.dma("sp", kres[0:66, :], S["KG"].ap[kh, :, :], reads=[S["KG"]], writes=[kres])
        with nc.allow_non_contiguous_dma(reason="token-major V rows"):
            P.dma("sp", vres[:], S["VG"].ap[kh].rearrange("(c p) d -> p c d", p=128), reads=[S["VG"]], writes=[vres])
        blocks = [(NCX + b * 128, b, False) for b in range(NB)]
        if need_ctx and NCX:
            blocks = [(cb * 128, cb, True) for cb in range(NCX // 128)] + blocks
        for (t0, b, is_ctx) in blocks:
            qb = qb_[qi % 2]
            qi += 1
            P.dma("sp", qb[0:66, :].rearrange("r (g t) -> r g t", t=128),
                  S["QG"].ap[4 * kh:4 * kh + 4, :, t0:t0 + 128].rearrange("g r t -> r g t"), reads=[S["QG"]], writes=[qb])
            chunks = []
            for cb in range(NCX // 128):
                chunks.append((kres, kres[0:66, cb * 128:(cb + 1) * 128], vres, vres[:, cb, :], None, None, None))
            if not is_ctx:
                kc0 = NCX // 128 + b
                if b > 0:
                    chunks.append((kres, kres[0:66, (kc0 - 1) * 128:kc0 * 128], vres, vres[:, kc0 - 1, :], (mprev, mprev[:, :]), None, None))
                chunks.append((kres, kres[0:66, kc0 * 128:(kc0 + 1) * 128], vres, vres[:, kc0, :], None, None, None))
                if b < NB - 1:
                    chunks.append((kres, kres[0:66, (kc0 + 1) * 128:(kc0 + 2) * 128], vres, vres[:, kc0 + 1, :], (mnext, mnext[:, :]), None, None))
            for g in range(4):
                hq = 4 * kh + g
                chunks.append((ks2, ks2[64:66, hq:hq + 1], vsb, vsb[0:1, :], None, (g * 128, 128), (64, 66)))
            outs = [(mixT.ap[512 + (4 * kh + g) * 64:512 + (4 * kh + g + 1) * 64, t0:t0 + 128], g * 128, 128) for g in range(4)]
            attn_core(K, A, qb, qb[0:66, :], 512, chunks, outs)
    K.ps_reserved = set()
    P.release_to(mark)


NCTX_FULL, NLAT_FULL, DEPTH = 256, 8192, 4
_W_SHAPES = dict(
    c_ctx=[D], mod_w=[DEPTH, D, 6 * D], mod_b=[DEPTH, 6 * D],
    norm_mix=[DEPTH, D], norm_ffn=[DEPTH, D], final_norm=[D],
    ev_w_in=[2, D, 2048], ev_conv_w=[2, 3, 1536], ev_conv_b=[2, 1536],
    hy_w1=[2, 33, 64], hy_b1=[2, 64], hy_w2=[2, 64, 64], hy_b2=[2, 64], hy_w3=[2, 64, 2048], hy_freq=[2, 64],
    hy_decay=[2, 2, 512], hy_bias=[2, 2, 512],
    s5_a_re=[2, 2, 32, 64], s5_a_im=[2, 2, 32, 64], s5_log_dt=[2, 2, 32],
    s5_b_re=[2, 2, 32, 64, 16], s5_b_im=[2, 2, 32, 64, 16], s5_c_re=[2, 2, 32, 16, 64], s5_c_im=[2, 2, 32, 16, 64],
    s5_d=[2, 512], s5_w_glu=[2, 512, 512], ev_w_out=[2, D, D],
    ff_w_gate=[2, D, 2816], ff_w_up=[2, D, 2816], ff_w_down=[2, 2816, D],
    od_w_in=[2, D, 1184], mla_q_norm=[2, 256], mla_w_uq=[2, 256, 768], mla_kv_norm=[2, 128], mla_w_ukv=[2, 128, 1024],
    gqa_sink=[2, 8], od_w_out=[2, D, D],
    moe_router=[2, D, 8], moe_w_gate=[2, 8, D, 3584], moe_w_up=[2, 8, D, 3584], moe_w_down=[2, 8, 3584, D],
)
_DRAM_ONLY_CONSTS = ("rope_mla_C", "rope_mla_S", "rope_gqa_C", "rope_gqa_S", "hy8192_featsT", "hy256_featsT",
                     "hc_Fc", "hc_Fns", "hc_Ic", "hc_Ins")


def all_consts():
    c = {}
    c.update(host_consts())
    c.update(hyena_consts(NLAT_FULL))
    c["hy256_featsT"] = hyena_consts(NCTX_FULL)["hy256_featsT"]
    c.update(hyena_ctx_consts())
    c.update(hyena_shared_consts())
    c.update(rope_consts(NLAT_FULL))
    return c


def build_program():
    P = Prog()
    nc = P.nc
    K = setup_common(P, NCTX_FULL, NLAT_FULL)
    NT = K.NT
    inp = lambda k, shp: T(nc.dram_tensor(k, list(shp), F32, kind="ExternalInput").ap())
    I = {k: inp(k, v) for k, v in _W_SHAPES.items()}
    I["hin"] = inp("hin", [D, NT])
    I["c"] = inp("c", [D])
    consts = all_consts()
    Cd = {k: inp(k, v.shape) for k, v in consts.items()}
    HALF = NLAT_FULL // 2
    I["sel"] = inp("sel", [128, 2])
    outT = T(nc.dram_tensor("outT", [D, HALF], F32, kind="ExternalOutput").ap())
    hT2 = P.dram("hT2", [D, HALF])
    zT = P.dram("zT", [2048, NT])
    mixT = P.dram("mixT", [D, NT])
    K.yS5 = P.dram("yS5", [512, NT])
    kT = P.dram("kT", [2048, NLAT_FULL])
    KFr = P.dram("KFr", [256, 128, 512])
    KFi = P.dram("KFi", [256, 128, 512])
    S = dict(QM=P.dram("QM", [8, 97, NT], BF16), KM=P.dram("KM", [8, 97, NT], BF16), VM=P.dram("VM", [8, NT, 65], BF16),
             QG=P.dram("QG", [8, 66, NT], BF16), KG=P.dram("KG", [2, 66, NT], BF16), VG=P.dram("VG", [2, NT, 65], BF16))

    def load_consts(keys):
        C = {}
        for k in keys:
            d = Cd[k]
            if k in _DRAM_ONLY_CONSTS:
                C[k + "_dram" if k.endswith("featsT") else k] = d
            else:
                t = P.sb(list(d.ap.shape), F32, "c_" + k)
                P.dma("sp", t[:], d.ap[:, :], reads=[d], writes=[t])
                C[k] = t
        return C

    for i0 in range((NT + 1023) // 1024):
        n = min(1024, NT - i0 * 1024)
        P.dma("sp", K.hT.ap[:, i0 * 1024:i0 * 1024 + n], I["hin"].ap[:, i0 * 1024:i0 * 1024 + n], reads=[I["hin"]], writes=[K.hT])
    mods = mods_all(K, list(range(DEPTH)), I["c"], I["c_ctx"], I["mod_w"], I["mod_b"], I["norm_mix"], I["norm_ffn"])
    for l in range(DEPTH):
        i = l // 2
        need_ctx = l < DEPTH - 1
        if l % 2 == 0:
            inproj_stage(K, mods[l], T(None, I["ev_w_in"].ap[i]), zT)
            m = P.mark()
            C = load_consts([k for k in Cd if k.startswith(f"hy{NLAT_FULL}_") or k.startswith("hy_")])
            hyena_stage(K, i, I, zT, mixT, C, NLAT_FULL, NCTX_FULL, KFr, KFi, kT, f"l{l}")
            P.release_to(m)
            m = P.mark()
            C = load_consts(["hy_tau512", "k_ident", f"hy{NCTX_FULL}_featsT"])
            hyena_ctx_stage(K, i, I, zT, mixT, C, Cd, 0)
            P.release_to(m)
            m = P.mark()
            C = load_consts(["k_tau", "k_maskB", "k_maskC", "k_ident"])
            s5_stage(K, i, I, zT, mixT, C)
            P.release_to(m)
            outproj_stage(K, mods[l], T(None, I["ev_w_out"].ap[i]), mixT)
            experts = [(T(None, I["ff_w_gate"].ap[i]), T(None, I["ff_w_up"].ap[i]), T(None, I["ff_w_down"].ap[i]))]
            ffn_stage(K, mods[l], experts, router=None, ST=1024)
        else:
            inproj_stage(K, mods[l], T(None, I["od_w_in"].ap[i]), zT, segs=odd_segs())
            m = P.mark()
            C = load_consts(["k_ident", "k_mask_prev", "k_mask_next", "k_selM", "k_selG", "k_sel65", "k_vsink",
                             "rope_mla_C", "rope_mla_S", "rope_gqa_C", "rope_gqa_S"])
            odd_mixer_stage(K, i, I, zT, mixT, C, S, need_ctx)
            P.release_to(m)
            outproj_stage(K, mods[l], T(None, I["od_w_out"].ap[i]), mixT)
            experts = [(T(None, I["moe_w_gate"].ap[i, e]), T(None, I["moe_w_up"].ap[i, e]), T(None, I["moe_w_down"].ap[i, e]))
                       for e in range(8)]
            if l < DEPTH - 1:
                ffn_stage(K, mods[l], experts, router=T(None, I["moe_router"].ap[i]), ST=1024)
            else:
                m = P.mark()
                selt = P.sb([128, 2], F32, "sel")
                P.dma("sp", selt[:], I["sel"].ap[:, :], reads=[I["sel"]], writes=[selt])
                sa = [P.sb([128, KC, 512], F32, f"sel_a{k}") for k in range(2)]
                sb_ = [P.sb([128, KC, 512], F32, f"sel_b{k}") for k in range(2)]
                for j in range(HALF // 512):
                    a_, b_ = sa[j % 2], sb_[j % 2]
                    P.dma("sp", a_[:], K.hT.ap[:, NCTX_FULL + j * 512:NCTX_FULL + (j + 1) * 512].rearrange("(k p) t -> p k t", p=128),
                          reads=[K.hT], writes=[a_])
                    P.dma("sp", b_[:], K.hT.ap[:, NCTX_FULL + HALF + j * 512:NCTX_FULL + HALF + (j + 1) * 512].rearrange("(k p) t -> p k t", p=128),
                          reads=[K.hT], writes=[b_])
                    P.ts("dve", a_, a_[:], a_, a_[:], selt[:, 0:1], ALU.mult, extra_reads=[selt])
                    P.stt(a_, a_[:], b_, b_[:], selt[:, 1:2], a_, a_[:], ALU.mult, ALU.add, extra_reads=[selt])
                    P.dma("pool", hT2.ap[:, j * 512:(j + 1) * 512].rearrange("(k p) t -> p k t", p=128), a_[:], reads=[a_], writes=[hT2])
                P.release_to(m)
                K2 = Ctx()
                K2.__dict__.update(K.__dict__)
                K2.hT, K2.NC, K2.NL, K2.NT = hT2, 0, HALF, HALF
                ffn_stage(K2, mods[l], experts, router=T(None, I["moe_router"].ap[i]), ST=1024,
                          tok_ranges=[(k * 1024, 1024, 0) for k in range(HALF // 1024)])
                final_norm_stage(K2, I["final_norm"], outT, 0, HALF)
    P.finish()
    return P


def kernel(**inputs):
    x = np.asarray(inputs["x"], dtype=np.float32)
    ctx = np.asarray(inputs["ctx"], dtype=np.float32)
    B = x.shape[0]
    P = build_program()
    shared = {k: np.ascontiguousarray(np.asarray(inputs[k], dtype=np.float32)) for k in _W_SHAPES}
    shared.update(all_consts())
    in_maps = []
    for core in range(8):
        b = core // 2
        m = dict(shared)
        m["hin"] = np.ascontiguousarray(np.concatenate([ctx[b], x[b]], axis=0).T)
        m["c"] = np.ascontiguousarray(np.asarray(inputs["c"], dtype=np.float32)[b])
        sel = np.zeros((128, 2), np.float32)
        sel[:, core % 2] = 1.0
        m["sel"] = sel
        in_maps.append(m)
    res = run_bass_kernel_spmd(P.nc, in_maps, core_ids=list(range(8)))
    out = np.empty((B, NLAT_FULL, D), dtype=np.float32)
    half = NLAT_FULL // 2
    for b in range(B):
        out[b, :half] = res.results[2 * b]["outT"].T
        out[b, half:] = res.results[2 * b + 1]["outT"].T
    return out
```

```python
import math
import numpy as np
import concourse.bass as bass
import concourse.mybir as mybir
from concourse.bass_utils import run_bass_kernel_spmd

F32 = mybir.dt.float32
BF16 = mybir.dt.bfloat16
I32 = mybir.dt.int32
AF = mybir.ActivationFunctionType
ALU = mybir.AluOpType
AX = mybir.AxisListType

D = 1024
KC = D // 128
EPS = 1e-6
HY_WINDOW_SHIFT = 0.05
HY_DT = BF16


class Trk:
    __slots__ = ("w", "r")

    def __init__(self):
        self.w = None
        self.r = {}


class T:
    def __init__(self, t, ap=None):
        self.t = t
        self.ap = ap if ap is not None else t
        self.whole = Trk()
        self.parts = {}

    def __getitem__(self, idx):
        return self.ap[idx]

    def p(self, key):
        return (self, key)


def _trks(x, for_write):
    if isinstance(x, tuple):
        t, key = x
        if key not in t.parts:
            t.parts[key] = Trk()
        return [t.whole, t.parts[key]], [t.parts[key]]
    return [x.whole] + list(x.parts.values()), [x.whole]


class Prog:
    NSLOT = 6

    def __init__(self):
        nc = bass.Bass("TRN2", target_bir_lowering=False)
        self.nc = nc
        self.E = dict(pe=nc.tensor, dve=nc.vector, act=nc.scalar, pool=nc.gpsimd, sp=nc.sync)
        self.sem = {}
        self.val = {}
        for e in ("pe", "dve", "act", "pool"):
            self.sem[e] = nc.semaphore("sem_" + e).__enter__()
            self.val[e] = 0
        self.slots = {}
        self.rr = {}
        for q in ("sp", "act", "pool"):
            self.slots[q] = []
            self.rr[q] = 0
            for i in range(self.NSLOT):
                k = f"dma_{q}{i}"
                self.sem[k] = nc.semaphore(k).__enter__()
                self.val[k] = 0
                self.slots[q].append(k)
        self.seen = {e: {} for e in self.E}
        self.n_ins = 0
        self._names = 0
        self.out_events = []
        self._stack = []

    def name(self, base):
        self._names += 1
        return f"{base}_{self._names}"

    def sb(self, shape, dt=F32, name="sb"):
        cm = self.nc.sbuf_tensor(self.name(name), list(shape), dt)
        t = T(cm.__enter__())
        self._stack.append(cm)
        return t

    def mark(self):
        return len(self._stack)

    def release_to(self, mark):
        self.barrier()
        while len(self._stack) > mark:
            self._stack.pop().__exit__(None, None, None)

    def barrier(self):
        for eng in self.E:
            for k in self.sem:
                if self.val[k] > 0:
                    self._wait(eng, (k, self.val[k]))

    def psum(self, shape, dt=F32, name="ps"):
        return T(self.nc.psum_tensor(self.name(name), list(shape), dt).__enter__())

    def dram(self, name, shape, dt=F32, kind="Internal"):
        return T(self.nc.dram_tensor(name, list(shape), dt, kind=kind).ap())

    def _wait(self, eng, ev):
        if ev is None:
            return
        key, v = ev
        if key == eng and eng == "pe":
            return
        if self.seen[eng].get(key, 0) >= v:
            return
        self.E[eng].wait_ge(self.sem[key], v)
        self.seen[eng][key] = v

    def _deps(self, eng, reads, writes):
        for x in reads:
            chk, _ = _trks(x, False)
            for tr in chk:
                self._wait(eng, tr.w)
        for x in writes:
            chk, _ = _trks(x, True)
            for tr in chk:
                self._wait(eng, tr.w)
                for k, v in tr.r.items():
                    self._wait(eng, (k, v))

    def _record(self, ev, reads, writes):
        for x in reads:
            _, upd = _trks(x, False)
            for tr in upd:
                tr.r[ev[0]] = ev[1]
        for x in writes:
            chk, upd = _trks(x, True)
            if not isinstance(x, tuple):
                x.parts.clear()
            for tr in upd:
                tr.w = ev
                tr.r = {}

    def op(self, eng, fn, reads=(), writes=()):
        self._deps(eng, reads, writes)
        ins = fn(self.E[eng])
        self.val[eng] += 1
        ins.then_inc(self.sem[eng], 1)
        ev = (eng, self.val[eng])
        self._record(ev, reads, writes)
        self.n_ins += 1
        return ev

    def dma(self, q, out, in_, reads=(), writes=(), **kw):
        self._deps(q, reads, writes)
        slot = self.slots[q][self.rr[q] % self.NSLOT]
        self.rr[q] += 1
        if self.val[slot] > 0:
            self._wait(q, (slot, self.val[slot]))
        ins = self.E[q].dma_start(out=out, in_=in_, **kw)
        self.val[slot] += 16
        ins.then_inc(self.sem[slot], 16)
        ev = (slot, self.val[slot])
        self._record(ev, reads, writes)
        self.n_ins += 1
        return ev

    def finish(self):
        for ev in self.out_events:
            self._wait("sp", ev)
        for k in self.sem:
            if self.val[k] > 0:
                self._wait("sp", (k, self.val[k]))

    def mm(self, ps, out_ap, lhsT, rhs, start, stop, reads, eng="pe"):
        return self.op("pe", lambda e: e.matmul(out_ap, lhsT, rhs, start=start, stop=stop),
                       reads=reads, writes=[ps])

    def act(self, out_t, out_ap, in_t, in_ap, func, bias=None, scale=1.0, extra_reads=(), accum_out=None):
        kw = {}
        if bias is not None:
            kw["bias"] = bias
        if accum_out is not None:
            kw["accum_out"] = accum_out
        return self.op("act", lambda e: e.activation(out=out_ap, in_=in_ap, func=func, scale=scale, **kw),
                       reads=[in_t] + list(extra_reads), writes=[out_t])

    def tt(self, eng, out_t, out_ap, a_t, a_ap, b_t, b_ap, op):
        return self.op(eng, lambda e: e.tensor_tensor(out=out_ap, in0=a_ap, in1=b_ap, op=op),
                       reads=[a_t, b_t], writes=[out_t])

    def ts(self, eng, out_t, out_ap, a_t, a_ap, s1, op0, s2=None, op1=None, extra_reads=()):
        if op1 is None:
            return self.op(eng, lambda e: e.tensor_scalar(out=out_ap, in0=a_ap, scalar1=s1, scalar2=None, op0=op0),
                           reads=[a_t] + list(extra_reads), writes=[out_t])
        return self.op(eng, lambda e: e.tensor_scalar(out=out_ap, in0=a_ap, scalar1=s1, scalar2=s2, op0=op0, op1=op1),
                       reads=[a_t] + list(extra_reads), writes=[out_t])

    def stt(self, out_t, out_ap, a_t, a_ap, scalar, b_t, b_ap, op0, op1, extra_reads=()):
        return self.op("dve", lambda e: e.scalar_tensor_tensor(out=out_ap, in0=a_ap, scalar=scalar, in1=b_ap, op0=op0, op1=op1),
                       reads=[a_t, b_t] + list(extra_reads), writes=[out_t])

    def copy(self, eng, out_t, out_ap, in_t, in_ap):
        if eng == "act":
            return self.op("act", lambda e: e.copy(out=out_ap, in_=in_ap), reads=[in_t], writes=[out_t])
        return self.op(eng, lambda e: e.tensor_copy(out=out_ap, in_=in_ap), reads=[in_t], writes=[out_t])

    def memset(self, eng, out_t, out_ap, v):
        return self.op(eng, lambda e: e.memset(out_ap, v), reads=[], writes=[out_t])


class Ctx:
    pass


def setup_common(P, NT_CTX, NT_LAT):
    K = Ctx()
    K.P = P
    K.NC = NT_CTX
    K.NL = NT_LAT
    K.NT = NT_CTX + NT_LAT
    nc = P.nc
    K.ps = [P.psum([128, 512], F32, name=f"bank{i}") for i in range(8)]
    K.ps_i = 0
    K.ps_reserved = set()
    K.ones = P.sb([128, 128], F32, "ones")
    P.memset("pool", K.ones, K.ones[:], 1.0)
    K.onesb = P.sb([128, 128], BF16, "onesb")
    P.memset("pool", K.onesb, K.onesb[:], 1.0)
    K.hT = P.dram("hT", [D, K.NT], F32)
    return K


def next_ps(K):
    while True:
        b = K.ps[K.ps_i % 8]
        K.ps_i += 1
        if (K.ps_i - 1) % 8 not in K.ps_reserved:
            return b


def dvec(ap1d, k):
    return ap1d.rearrange("(k p) -> p k", p=128)


def mods_all(K, layers, c_in, cctx_in, mod_w, mod_b, norm_mix, norm_ffn):
    P = K.P
    nc = P.nc
    res = {}
    pers = {}
    for l in layers:
        pers[l] = dict(M=P.sb([128, 48, 2], F32, f"modM{l}"), A_mix=P.sb([128, KC, 2], F32, f"A_mix{l}"),
                       A_ffn=P.sb([128, KC, 2], F32, f"A_ffn{l}"))
    mark = P.mark()
    craw = P.sb([128, KC, 2], F32, "craw")
    sT = P.sb([128, KC, 2], F32, "sT")
    modw_buf = [P.sb([128, KC, 256], F32, f"modw{i}") for i in range(2)]
    bT = P.sb([128, 48], F32, "modb")
    gm = P.sb([128, KC], F32, "gmix")
    gf = P.sb([128, KC], F32, "gffn")
    with nc.allow_non_contiguous_dma(reason="tiny vector load"):
        P.dma("sp", craw[:, :, 0], dvec(c_in.ap, KC), reads=[c_in], writes=[craw])
        P.dma("sp", craw[:, :, 1], dvec(cctx_in.ap, KC), reads=[cctx_in], writes=[craw])
    P.act(sT, sT[:], craw, craw[:], AF.Silu)
    wi = 0
    for l in layers:
        M = pers[l]["M"]
        with nc.allow_non_contiguous_dma(reason="tiny vector load"):
            P.dma("sp", bT[:], dvec(mod_b.ap[l], 48), reads=[mod_b], writes=[bT])
            P.dma("sp", gm[:], dvec(norm_mix.ap[l], KC), reads=[norm_mix], writes=[gm])
            P.dma("sp", gf[:], dvec(norm_ffn.ap[l], KC), reads=[norm_ffn], writes=[gf])
        for s in range(24):
            wb = modw_buf[wi % 2]
            wi += 1
            P.dma("sp", wb[:], mod_w.ap[l, :, s * 256:(s + 1) * 256].rearrange("(k p) n -> p k n", p=128),
                  reads=[mod_w], writes=[wb])
            ps = next_ps(K)
            for j in range(2):
                for k in range(KC):
                    P.mm(ps, ps[:, j * 2:j * 2 + 2], wb[:, k, j * 128:(j + 1) * 128], sT[:, k, :],
                         start=(k == 0), stop=(k == KC - 1), reads=[wb, sT])
            for j in range(2):
                jj = s * 2 + j
                P.ts("dve", M, M[:, jj, :], ps, ps[:, j * 2:j * 2 + 2], bT[:, jj:jj + 1], ALU.add, extra_reads=[bT])
        out = dict(pers[l])
        for nm, sc_i, g in (("mix", 1, gm), ("ffn", 4, gf)):
            A = pers[l]["A_" + nm]
            for col in range(2):
                P.stt(A, A[:, :, col], M, M[:, sc_i * 8:(sc_i + 1) * 8, col], 1.0, g, g[:], ALU.add, ALU.mult)
        out["B_mix"] = (lambda M: (lambda k, col: M[:, 0 * 8 + k, col:col + 1]))(M)
        out["G_mix"] = (lambda M: (lambda k, col: M[:, 2 * 8 + k, col:col + 1]))(M)
        out["B_ffn"] = (lambda M: (lambda k, col: M[:, 3 * 8 + k, col:col + 1]))(M)
        out["G_ffn"] = (lambda M: (lambda k, col: M[:, 5 * 8 + k, col:col + 1]))(M)
        res[l] = out
    P.release_to(mark)
    return res


def norm_tile(K, h_t, hc0, n, A, Bf, M, col, y_bf, yc0, y_f32=None, sq=None, rstd=None):
    P = K.P
    ps = next_ps(K)
    for k in range(KC):
        P.act(sq, sq[:, k, :n], h_t, h_t[:, k, hc0:hc0 + n], AF.Square)
    for k in range(KC):
        P.mm(ps, ps[:, :n], K.ones[:], sq[:, k, :n], start=(k == 0), stop=(k == KC - 1), reads=[K.ones, sq])
    P.ts("dve", rstd, rstd[:, :n], ps, ps[:, :n], 1.0 / D, ALU.mult, EPS, ALU.add)
    P.op("act", lambda e: e.sqrt(out=rstd[:, :n], in_=rstd[:, :n]), reads=[rstd], writes=[rstd])
    P.op("dve", lambda e: e.reciprocal(out=rstd[:, :n], in_=rstd[:, :n]), reads=[rstd], writes=[rstd])
    for k in range(KC):
        P.stt(sq, sq[:, k, :n], h_t, h_t[:, k, hc0:hc0 + n], A[:, k, col:col + 1], rstd, rstd[:, :n], ALU.mult, ALU.mult,
              extra_reads=[A])
        if y_f32 is not None:
            P.act(y_f32, y_f32[:, k, :n], sq, sq[:, k, :n], AF.Identity, bias=Bf(k, col), extra_reads=[M])
            P.copy("pool", y_bf, y_bf[:, k, yc0:yc0 + n], y_f32, y_f32[:, k, :n])
        else:
            P.act(y_bf, y_bf[:, k, yc0:yc0 + n], sq, sq[:, k, :n], AF.Identity, bias=Bf(k, col), extra_reads=[M])


def ffn_stage(K, mods, experts, router=None, tok_ranges=None, ST=1024, tag="ffn"):
    P = K.P
    nc = P.nc
    FF = experts[0][0].ap.shape[1]
    NE = len(experts)
    slabs = []
    f0 = 0
    while f0 < FF:
        fs = min(512, FF - f0)
        slabs.append((f0, fs))
        f0 += fs
    A, Bf, Gf, M = mods["A_ffn"], mods["B_ffn"], mods["G_ffn"], mods["M"]
    if tok_ranges is None:
        tok_ranges = [(0, K.NC, 1)] + [(K.NC + i * ST, min(ST, K.NL - i * ST), 0) for i in range((K.NL + ST - 1) // ST)]
    mark = P.mark()
    if True:
        B = Ctx()
        B.acc = P.sb([128, KC, ST], F32, "ffn_acc")
        B.y = P.sb([128, KC, ST], BF16, "ffn_y")
        B.sq = P.sb([128, KC, 512], F32, "ffn_sq")
        B.yf = P.sb([128, KC, 512], F32, "ffn_yf")
        B.rstd = P.sb([128, 512], F32, "ffn_rstd")
        B.hid = P.sb([128, 4, ST], BF16, "ffn_hid")
        B.wg = [P.sb([128, KC, 512], BF16, f"ffn_wg{i}") for i in range(2)]
        B.wu = [P.sb([128, KC, 512], BF16, f"ffn_wu{i}") for i in range(2)]
        B.wd = [P.sb([128, 4, D], BF16, f"ffn_wd{i}") for i in range(2)]
        B.sg = [P.sb([128, 512], F32, f"ffn_sg{i}") for i in range(2)]
        B.tmp = [P.sb([128, 512], F32, f"ffn_tmp{i}") for i in range(2)]
        B.gate_bc = P.sb([128, 8, ST], F32, "ffn_gatebc")
        B.rt = P.sb([128, KC, 8], F32, "ffn_router")
        B.lg = P.sb([128, 8], F32, "ffn_lg")
        B.mx = P.sb([128, 8], F32, "ffn_mx")
        B.gt = P.sb([128, 8], F32, "ffn_gt")
        B.m1 = P.sb([128, 8], F32, "ffn_m1")
        B.wv = P.sb([128, 4], F32, "ffn_wv")
        B.ident = P.sb([128, 128], F32, "ffn_ident")
        B.gcol = P.sb([128, 128], F32, "ffn_gcol")
        B.i = 0
        P.memset("pool", B.ident, B.ident[:], 0.0)
        P.op("pool", lambda e: e.affine_select(out=B.ident[:], in_=B.ident[:], pattern=[[-1, 128]], base=0,
                                                channel_multiplier=1, compare_op=ALU.not_equal, fill=1.0),
             reads=[B.ident], writes=[B.ident])
    if router is not None:
        P.dma("sp", B.rt[:], router.ap.rearrange("(k p) e -> p k e", p=128), reads=[router], writes=[B.rt])
    for (t0, n, col) in tok_ranges:
        ntile = (n + 511) // 512
        for j in range(ntile):
            c0 = j * 512
            w = min(512, n - c0)
            P.dma("sp", B.acc[:, :, c0:c0 + w], K.hT.ap[:, t0 + c0:t0 + c0 + w].rearrange("(k p) t -> p k t", p=128),
                  reads=[K.hT], writes=[B.acc])
            norm_tile(K, B.acc, c0, w, A, Bf, M, col, B.y, c0,
                      y_f32=(B.yf if router is not None else None), sq=B.sq, rstd=B.rstd)
            if router is not None:
                for q in range((w + 127) // 128):
                    qw = min(128, w - q * 128)
                    ps = next_ps(K)
                    for k in range(KC):
                        P.mm(ps, ps[:qw, 0:8], B.yf[:, k, q * 128:q * 128 + qw], B.rt[:, k, :],
                             start=(k == 0), stop=(k == KC - 1), reads=[B.yf, B.rt])
                    P.copy("dve", B.lg, B.lg[:qw, :], ps, ps[:qw, 0:8])
                    P.op("dve", lambda e: e.max(out=B.mx[:qw, :], in_=B.lg[:qw, :]), reads=[B.lg], writes=[B.mx])
                    P.tt("dve", B.wv, B.wv[:qw, 0:1], B.mx, B.mx[:qw, 1:2], B.mx, B.mx[:qw, 0:1], ALU.subtract)
                    P.act(B.wv, B.wv[:qw, 1:2], B.wv, B.wv[:qw, 0:1], AF.Exp)
                    P.ts("dve", B.wv, B.wv[:qw, 1:2], B.wv, B.wv[:qw, 1:2], 1.0, ALU.add)
                    P.op("dve", lambda e: e.reciprocal(out=B.wv[:qw, 2:3], in_=B.wv[:qw, 1:2]), reads=[B.wv], writes=[B.wv])
                    P.ts("dve", B.wv, B.wv[:qw, 3:4], B.wv, B.wv[:qw, 2:3], -1.0, ALU.mult, 1.0, ALU.add)
                    P.ts("dve", B.gt, B.gt[:qw, :], B.lg, B.lg[:qw, :], B.mx[:qw, 0:1], ALU.is_equal,
                         B.wv[:qw, 2:3], ALU.mult, extra_reads=[B.mx, B.wv])
                    P.ts("dve", B.m1, B.m1[:qw, :], B.lg, B.lg[:qw, :], B.mx[:qw, 1:2], ALU.is_equal,
                         B.wv[:qw, 3:4], ALU.mult, extra_reads=[B.mx, B.wv])
                    P.tt("dve", B.gt, B.gt[:qw, :], B.gt, B.gt[:qw, :], B.m1, B.m1[:qw, :], ALU.add)
                    for e_i in range(NE):
                        P.ts("dve", B.gcol, B.gcol[:qw, :], K.ones, K.ones[:qw, :], B.gt[:qw, e_i:e_i + 1], ALU.mult,
                             extra_reads=[B.gt])
                        ps2 = next_ps(K)
                        P.mm(ps2, ps2[:, :qw], B.gcol[:qw, :], B.ident[:qw, :qw], start=True, stop=True,
                             reads=[B.gcol, B.ident])
                        P.copy("act", B.gate_bc, B.gate_bc[:, e_i, c0 + q * 128:c0 + q * 128 + qw], ps2, ps2[:, :qw])
        for e_i, (wg, wu, wd) in enumerate(experts):
            for (f0, fs) in slabs:
                i = B.i % 2
                B.i += 1
                nfc = fs // 128
                P.dma("pool", B.wg[i][:, :, :fs], wg.ap[:, f0:f0 + fs].rearrange("(k p) f -> p k f", p=128),
                      reads=[wg], writes=[B.wg[i]])
                P.dma("pool", B.wu[i][:, :, :fs], wu.ap[:, f0:f0 + fs].rearrange("(k p) f -> p k f", p=128),
                      reads=[wu], writes=[B.wu[i]])
                P.dma("pool", B.wd[i][:, :nfc, :], wd.ap[f0:f0 + fs, :].rearrange("(c p) d -> p c d", p=128),
                      reads=[wd], writes=[B.wd[i]])
                for j in range(ntile):
                    c0 = j * 512
                    w = min(512, n - c0)
                    for fc in range(nfc):
                        pg = next_ps(K)
                        pu = next_ps(K)
                        for k in range(KC):
                            P.mm(pg, pg[:, :w], B.wg[i][:, k, fc * 128:(fc + 1) * 128], B.y[:, k, c0:c0 + w],
                                 start=(k == 0), stop=(k == KC - 1), reads=[B.wg[i], B.y])
                        for k in range(KC):
                            P.mm(pu, pu[:, :w], B.wu[i][:, k, fc * 128:(fc + 1) * 128], B.y[:, k, c0:c0 + w],
                                 start=(k == 0), stop=(k == KC - 1), reads=[B.wu[i], B.y])
                        sg = B.sg[(j * 4 + fc) % 2]
                        P.act(sg, sg[:, :w], pg, pg[:, :w], AF.Silu)
                        P.tt("dve", B.hid, B.hid[:, fc, c0:c0 + w], sg, sg[:, :w], pu, pu[:, :w], ALU.mult)
                    for oc in range(KC):
                        po = next_ps(K)
                        for fc in range(nfc):
                            P.mm(po, po[:, :w], B.wd[i][:, fc, oc * 128:(oc + 1) * 128], B.hid[:, fc, c0:c0 + w],
                                 start=(fc == 0), stop=(fc == nfc - 1), reads=[B.wd[i], B.hid])
                        if router is not None:
                            tmp = B.tmp[oc % 2]
                            P.stt(tmp, tmp[:, :w], po, po[:, :w], Gf(oc, col), B.gate_bc, B.gate_bc[:, e_i, c0:c0 + w],
                                  ALU.mult, ALU.mult, extra_reads=[M])
                            P.tt("pool", B.acc, B.acc[:, oc, c0:c0 + w], B.acc, B.acc[:, oc, c0:c0 + w], tmp, tmp[:, :w], ALU.add)
                        else:
                            P.stt(B.acc, B.acc[:, oc, c0:c0 + w], po, po[:, :w], Gf(oc, col), B.acc, B.acc[:, oc, c0:c0 + w],
                                  ALU.mult, ALU.add, extra_reads=[M])
        for j in range(ntile):
            c0 = j * 512
            w = min(512, n - c0)
            P.dma("sp", K.hT.ap[:, t0 + c0:t0 + c0 + w].rearrange("(k p) t -> p k t", p=128), B.acc[:, :, c0:c0 + w],
                  reads=[B.acc], writes=[K.hT])
    if hasattr(K, "dbg") and router is not None:
        P.dma("sp", K.dbg["gate_bc"].ap[:, :, :], B.gate_bc[:, :, :], reads=[B.gate_bc], writes=[K.dbg["gate_bc"]])
        P.dma("sp", K.dbg["ident"].ap[:, :], B.ident[:, :], reads=[B.ident], writes=[K.dbg["ident"]])
        for i_, t_ in enumerate((B.lg, B.mx, B.gt, B.m1)):
            P.dma("sp", K.dbg["small"].ap[:, i_ * 8:(i_ + 1) * 8], t_[:, :], reads=[t_], writes=[K.dbg["small"]])
        P.dma("sp", K.dbg["small"].ap[:, 32:36], B.wv[:, :], reads=[B.wv], writes=[K.dbg["small"]])
    P.release_to(mark)


def inproj_stage(K, mods, w_in, zT, segs=None):
    P = K.P
    nc = P.nc
    if segs is None:
        segs = [(0, w_in.ap.shape[1])]
    N = sum(n for _, n in segs)
    A, Bf, M = mods["A_mix"], mods["B_mix"], mods["M"]
    mark = P.mark()
    nch = (N + 127) // 128
    wst = [P.sb([128, KC, 512], F32, f"ip_wst{i}") for i in range(2)]
    wb = P.sb([128, KC, N], BF16, "ip_w")
    o0 = 0
    si = 0
    for (c0, ncol) in segs:
        for s0 in range(0, ncol, 512):
            cw = min(512, ncol - s0)
            st = wst[si % 2]
            si += 1
            with nc.allow_non_contiguous_dma(reason="weight column segments"):
                P.dma("sp", st[:, :, :cw], w_in.ap[:, c0 + s0:c0 + s0 + cw].rearrange("(k p) n -> p k n", p=128),
                      reads=[w_in], writes=[st])
            P.copy("pool", wb, wb[:, :, o0 + s0:o0 + s0 + cw], st, st[:, :, :cw])
        o0 += ncol
    h = [P.sb([128, KC, 512], F32, f"ip_h{i}") for i in range(2)]
    y = [P.sb([128, KC, 512], BF16, f"ip_y{i}") for i in range(2)]
    sq = P.sb([128, KC, 512], F32, "ip_sq")
    rstd = P.sb([128, 512], F32, "ip_rstd")
    zo = [P.sb([128, 512], F32, f"ip_zo{i}") for i in range(4)]
    ranges = []
    if K.NC:
        ranges.append((0, K.NC, 1))
    ranges += [(K.NC + i * 512, min(512, K.NL - i * 512), 0) for i in range((K.NL + 511) // 512)]
    for it, (t0, n, col) in enumerate(ranges):
        hb, yb = h[it % 2], y[it % 2]
        P.dma("sp", hb[:, :, :n], K.hT.ap[:, t0:t0 + n].rearrange("(k p) t -> p k t", p=128), reads=[K.hT], writes=[hb])
        norm_tile(K, hb, 0, n, A, Bf, M, col, yb, 0, sq=sq, rstd=rstd)
        for c in range(nch):
            m = min(128, N - c * 128)
            ps = next_ps(K)
            for k in range(KC):
                P.mm(ps, ps[:m, :n], wb[:, k, c * 128:c * 128 + m], yb[:, k, :n], start=(k == 0), stop=(k == KC - 1),
                     reads=[wb, yb])
            o = zo[c % 4]
            P.copy("act" if c % 2 else "dve", o, o[:m, :n], ps, ps[:m, :n])
            P.dma("pool" if c % 2 else "sp", zT.ap[c * 128:c * 128 + m, t0:t0 + n], o[:m, :n], reads=[o], writes=[zT])
    P.release_to(mark)


def outproj_stage(K, mods, w_out, mixT):
    P = K.P
    Gm, M = mods["G_mix"], mods["M"]
    mark = P.mark()
    wst = [P.sb([128, KC, 512], F32, f"op_wst{i}") for i in range(2)]
    wb = P.sb([128, KC, D], BF16, "op_w")
    for s in range(2):
        st = wst[s % 2]
        P.dma("sp", st[:], w_out.ap[:, s * 512:(s + 1) * 512].rearrange("(k p) n -> p k n", p=128), reads=[w_out], writes=[st])
        P.copy("pool", wb, wb[:, :, s * 512:(s + 1) * 512], st, st[:])
    mx = [P.sb([128, KC, 512], F32, f"op_m{i}") for i in range(2)]
    mb = [P.sb([128, KC, 512], BF16, f"op_mb{i}") for i in range(2)]
    h = [P.sb([128, KC, 512], F32, f"op_h{i}") for i in range(2)]
    ranges = [(0, K.NC, 1)] + [(K.NC + i * 512, min(512, K.NL - i * 512), 0) for i in range((K.NL + 511) // 512)]
    for it, (t0, n, col) in enumerate(ranges):
        m_, b_, h_ = mx[it % 2], mb[it % 2], h[it % 2]
        P.dma("sp", m_[:, :, :n], mixT.ap[:, t0:t0 + n].rearrange("(k p) t -> p k t", p=128), reads=[mixT], writes=[m_])
        P.dma("sp", h_[:, :, :n], K.hT.ap[:, t0:t0 + n].rearrange("(k p) t -> p k t", p=128), reads=[K.hT], writes=[h_])
        P.copy("pool", b_, b_[:, :, :n], m_, m_[:, :, :n])
        for c in range(KC):
            ps = next_ps(K)
            for k in range(KC):
                P.mm(ps, ps[:, :n], wb[:, k, c * 128:(c + 1) * 128], b_[:, k, :n], start=(k == 0), stop=(k == KC - 1),
                     reads=[wb, b_])
            P.stt(h_, h_[:, c, :n], ps, ps[:, :n], Gm(c, col), h_, h_[:, c, :n], ALU.mult, ALU.add, extra_reads=[M])
        P.dma("pool", K.hT.ap[:, t0:t0 + n].rearrange("(k p) t -> p k t", p=128), h_[:, :, :n], reads=[h_], writes=[K.hT])
    P.release_to(mark)


def outproj_half_stage(K, mods, w_out, mixT, mixH, selt_d, hT2, HALF):
    P = K.P
    Gm, M = mods["G_mix"], mods["M"]
    mark = P.mark()
    selt = P.sb([128, 2], F32, "oh_sel")
    P.dma("sp", selt[:], selt_d.ap[:, :], reads=[selt_d], writes=[selt])
    wst = [P.sb([128, KC, 512], F32, f"oh_wst{i}") for i in range(2)]
    wb = P.sb([128, KC, D], BF16, "oh_w")
    for s_ in range(2):
        st = wst[s_ % 2]
        P.dma("sp", st[:], w_out.ap[:, s_ * 512:(s_ + 1) * 512].rearrange("(k p) n -> p k n", p=128), reads=[w_out], writes=[st])
        P.copy("pool", wb, wb[:, :, s_ * 512:(s_ + 1) * 512], st, st[:])
    mx = [P.sb([128, KC, 512], F32, f"oh_m{i}") for i in range(2)]
    m2 = [P.sb([128, 4, 512], F32, f"oh_m2{i}") for i in range(2)]
    mb = [P.sb([128, KC, 512], BF16, f"oh_mb{i}") for i in range(2)]
    h = [P.sb([128, KC, 512], F32, f"oh_h{i}") for i in range(2)]
    h2 = [P.sb([128, KC, 512], F32, f"oh_h2{i}") for i in range(2)]
    NCX = K.NC
    for j in range(HALF // 512):
        m_, mm2, b_, h_, hh2 = mx[j % 2], m2[j % 2], mb[j % 2], h[j % 2], h2[j % 2]
        ta, tb = NCX + j * 512, NCX + HALF + j * 512
        P.dma("sp", m_[:, 0:4, :], mixH.ap[:, j * 512:(j + 1) * 512].rearrange("(k p) t -> p k t", p=128), reads=[mixH], writes=[m_])
        P.dma("sp", m_[:, 4:8, :], mixT.ap[512:1024, ta:ta + 512].rearrange("(k p) t -> p k t", p=128), reads=[mixT], writes=[m_])
        P.dma("sp", mm2[:], mixT.ap[512:1024, tb:tb + 512].rearrange("(k p) t -> p k t", p=128), reads=[mixT], writes=[mm2])
        P.dma("sp", h_[:], K.hT.ap[:, ta:ta + 512].rearrange("(k p) t -> p k t", p=128), reads=[K.hT], writes=[h_])
        P.dma("sp", hh2[:], K.hT.ap[:, tb:tb + 512].rearrange("(k p) t -> p k t", p=128), reads=[K.hT], writes=[hh2])
        P.ts("dve", m_, m_[:, 4:8, :], m_, m_[:, 4:8, :], selt[:, 0:1], ALU.mult, extra_reads=[selt])
        P.stt(m_, m_[:, 4:8, :], mm2, mm2[:], selt[:, 1:2], m_, m_[:, 4:8, :], ALU.mult, ALU.add, extra_reads=[selt])
        P.ts("dve", h_, h_[:], h_, h_[:], selt[:, 0:1], ALU.mult, extra_reads=[selt])
        P.stt(h_, h_[:], hh2, hh2[:], selt[:, 1:2], h_, h_[:], ALU.mult, ALU.add, extra_reads=[selt])
        P.copy("pool", b_, b_[:], m_, m_[:])
        for c in range(KC):
            ps = next_ps(K)
            for k in range(KC):
                P.mm(ps, ps[:, :], wb[:, k, c * 128:(c + 1) * 128], b_[:, k, :], start=(k == 0), stop=(k == KC - 1), reads=[wb, b_])
            P.stt(h_, h_[:, c, :], ps, ps[:, :], Gm(c, 0), h_, h_[:, c, :], ALU.mult, ALU.add, extra_reads=[M])
        P.dma("pool", hT2.ap[:, j * 512:(j + 1) * 512].rearrange("(k p) t -> p k t", p=128), h_[:], reads=[h_], writes=[hT2])
    P.release_to(mark)


def final_norm_stage(K, gain, outT, t0_all, n_all):
    P = K.P
    nc = P.nc
    mark = P.mark()
    g = P.sb([128, KC], F32, "fn_g")
    with nc.allow_non_contiguous_dma(reason="tiny vector load"):
        P.dma("sp", g[:], dvec(gain.ap, KC), reads=[gain], writes=[g])
    h = [P.sb([128, KC, 512], F32, f"fn_h{i}") for i in range(2)]
    o = [P.sb([128, KC, 512], F32, f"fn_o{i}") for i in range(2)]
    sq = P.sb([128, KC, 512], F32, "fn_sq")
    rstd = P.sb([128, 512], F32, "fn_rstd")
    for it in range((n_all + 511) // 512):
        n = min(512, n_all - it * 512)
        t0 = t0_all + it * 512
        hb, ob = h[it % 2], o[it % 2]
        P.dma("sp", hb[:, :, :n], K.hT.ap[:, t0:t0 + n].rearrange("(k p) t -> p k t", p=128), reads=[K.hT], writes=[hb])
        ps = next_ps(K)
        for k in range(KC):
            P.act(sq, sq[:, k, :n], hb, hb[:, k, :n], AF.Square)
        for k in range(KC):
            P.mm(ps, ps[:, :n], K.ones[:], sq[:, k, :n], start=(k == 0), stop=(k == KC - 1), reads=[K.ones, sq])
        P.ts("dve", rstd, rstd[:, :n], ps, ps[:, :n], 1.0 / D, ALU.mult, EPS, ALU.add)
        P.op("act", lambda e: e.sqrt(out=rstd[:, :n], in_=rstd[:, :n]), reads=[rstd], writes=[rstd])
        P.op("dve", lambda e: e.reciprocal(out=rstd[:, :n], in_=rstd[:, :n]), reads=[rstd], writes=[rstd])
        for k in range(KC):
            P.stt(ob, ob[:, k, :n], hb, hb[:, k, :n], g[:, k:k + 1], rstd, rstd[:, :n], ALU.mult, ALU.mult, extra_reads=[g])
        ev = P.dma("pool", outT.ap[:, it * 512:it * 512 + n].rearrange("(k p) t -> p k t", p=128), ob[:, :, :n],
                   reads=[ob], writes=[outT])
        P.out_events.append(ev)
    P.release_to(mark)


MAGIC = 12582912.0
TWO_PI = 2.0 * math.pi


def host_consts():
    c = {}
    c["k_tau"] = np.tile(np.arange(128, dtype=np.float32)[None, :], (128, 1))
    q = np.arange(128)
    mC = np.zeros((128, 4, 2, 64), np.float32)
    for jj in range(4):
        for gl in range(2):
            sel = (q // 32 == jj) & ((q % 32) // 16 == gl)
            mC[sel, jj, gl, :] = 1.0
    c["k_maskC"] = mC.reshape(128, 512)
    mB = np.zeros((128, 4, 128), np.float32)
    for jj in range(4):
        for gl in range(2):
            rows = np.arange(64 * gl, 64 * gl + 64)
            cols = np.arange(32 * jj + 16 * gl, 32 * jj + 16 * gl + 16)
            mB[np.ix_(rows, [jj], cols)] = 1.0
    c["k_maskB"] = mB.reshape(128, 512)
    c["k_ident"] = np.eye(128, dtype=np.float32)
    return c


def range_reduce(P, out_t, out_ap, in_t, in_ap, tmp_t, tmp_ap, shift=0.0):
    P.ts("dve", tmp_t, tmp_ap, in_t, in_ap, 1.0 / TWO_PI, ALU.mult, shift / TWO_PI + MAGIC, ALU.add)
    P.ts("dve", tmp_t, tmp_ap, tmp_t, tmp_ap, MAGIC, ALU.subtract, -TWO_PI, ALU.mult)
    P.stt(out_t, out_ap, in_t, in_ap, shift, tmp_t, tmp_ap, ALU.add, ALU.add)


def s5_stage(K, i, I, zT, mixT, C, u_row0=1536, out_row0=512):
    P = K.P
    nc = P.nc
    TT = 128
    mark = P.mark()
    tau, maskB, maskC, ident = C["k_tau"], C["k_maskB"], C["k_maskC"], C["k_ident"]
    yS = K.yS5
    sb = P.sb
    lr = sb([128, 16], F32, "s5_lr"); li = sb([128, 16], F32, "s5_li"); dtv = sb([128, 16], F32, "s5_dt")
    wdt = sb([128, 16], F32, "s5_wdt"); rdt = sb([128, 16], F32, "s5_rdt"); nrdt = sb([128, 16], F32, "s5_nrdt")
    Er = sb([128, 16, TT], F32, "s5_Er"); Ei = sb([128, 16, TT], F32, "s5_Ei")
    Gr = sb([128, 16, TT], F32, "s5_Gr"); Gi = sb([128, 16, TT], F32, "s5_Gi")
    Hr = sb([128, 16], F32, "s5_Hr"); Hi = sb([128, 16], F32, "s5_Hi")
    BTr = sb([128, 16, 128], F32, "s5_BTr"); BTi = sb([128, 16, 128], F32, "s5_BTi")
    CTr = sb([128, 16, 128], F32, "s5_CTr"); CTin = sb([128, 16, 128], F32, "s5_CTin")
    bre = sb([128, 16, 16], F32, "s5_bre"); bim = sb([128, 16, 16], F32, "s5_bim")
    bbr = sb([128, 16, 16], F32, "s5_bbr"); bbi = sb([128, 16, 16], F32, "s5_bbi")
    cre = sb([128, 4, 64], F32, "s5_cre"); cim = sb([128, 4, 64], F32, "s5_cim")
    ex = sb([128, 128], F32, "s5_ex")
    th = sb([128, TT], F32, "s5_th"); tmp = sb([128, TT], F32, "s5_tmp"); sn = sb([128, TT], F32, "s5_sn")
    cs = sb([128, TT], F32, "s5_cs"); mg = sb([128, TT], F32, "s5_mg")
    s16 = [sb([128, 16], F32, f"s5_s16_{k}") for k in range(8)]
    u = [sb([128, 4, TT], F32, f"s5_u{k}") for k in range(3)]
    SL = 3
    t1s = [sb([128, 4, TT], F32, f"s5_t1{k}") for k in range(SL)]; t2s = [sb([128, 4, TT], F32, f"s5_t2{k}") for k in range(SL)]
    t3s = [sb([128, 4, TT], F32, f"s5_t3{k}") for k in range(SL)]; t4s = [sb([128, 4, TT], F32, f"s5_t4{k}") for k in range(SL)]
    wrs = [sb([128, 4, TT], F32, f"s5_wr{k}") for k in range(SL)]; wis = [sb([128, 4, TT], F32, f"s5_wi{k}") for k in range(SL)]
    zrs = [sb([128, 4, TT], F32, f"s5_zr{k}") for k in range(SL)]; zis = [sb([128, 4, TT], F32, f"s5_zi{k}") for k in range(SL)]
    xrs = [sb([128, 4, TT], F32, f"s5_xr{k}") for k in range(SL)]; xis = [sb([128, 4, TT], F32, f"s5_xi{k}") for k in range(SL)]
    cars = [sb([128, 4], F32, f"s5_car{k}") for k in range(4)]; cais = [sb([128, 4], F32, f"s5_cai{k}") for k in range(4)]
    c4s = [[sb([128, 4], F32, f"s5_c4_{q}{k}") for k in range(4)] for q in range(SL)]
    g1s = [sb([128, TT], F32, f"s5_g1{k}") for k in range(SL)]; g2s = [sb([128, TT], F32, f"s5_g2{k}") for k in range(SL)]
    yo = [sb([128, TT], F32, f"s5_yo{k}") for k in range(2)]
    yfs = [sb([128, 4, TT], F32, f"s5_yf{k}") for k in range(3)]
    vqs = [[sb([128, TT], F32, f"s5_v{p}{k}") for k in range(4)] for p in range(2)]
    oo = [sb([128, TT], F32, f"s5_oo{k}") for k in range(2)]
    wglu = sb([128, 4, 512], F32, "s5_wglu"); dsk = sb([128, 4], F32, "s5_dsk")
    onesT = sb([128, TT], F32, "s5_ones")
    P.memset("pool", onesT, onesT[:], 1.0)
    P.dma("sp", wglu[:], I["s5_w_glu"].ap[i].rearrange("(k p) n -> p k n", p=128), reads=[I["s5_w_glu"]], writes=[wglu])
    with nc.allow_non_contiguous_dma(reason="tiny vector load"):
        P.dma("sp", dsk[:], I["s5_d"].ap[i].rearrange("(k p) -> p k", p=128), reads=[I["s5_d"]], writes=[dsk])

    def sincos(angle_t, angle_ap, sin_t, sin_ap, cos_t, cos_ap, tmp_a, tmp_a_ap, tmp_b, tmp_b_ap):
        range_reduce(P, tmp_b, tmp_b_ap, angle_t, angle_ap, tmp_a, tmp_a_ap, 0.0)
        P.act(sin_t, sin_ap, tmp_b, tmp_b_ap, AF.Sin)
        range_reduce(P, tmp_b, tmp_b_ap, angle_t, angle_ap, tmp_a, tmp_a_ap, math.pi / 2)
        P.act(cos_t, cos_ap, tmp_b, tmp_b_ap, AF.Sin)

    chunks_ctx = [(k * TT, 1) for k in range(K.NC // TT)]
    chunks_lat = [(K.NC + k * TT, 0) for k in range(K.NL // TT)]
    for d in range(2):
        rev = (d == 1)
        with nc.allow_non_contiguous_dma(reason="small parameter loads"):
            P.dma("sp", lr[:], I["s5_a_re"].ap[i, d].rearrange("g p -> (g p)").rearrange("(j q) -> q j", q=128),
                  reads=[I["s5_a_re"]], writes=[lr])
            P.dma("sp", li[:], I["s5_a_im"].ap[i, d].rearrange("g p -> (g p)").rearrange("(j q) -> q j", q=128),
                  reads=[I["s5_a_im"]], writes=[li])
            for gl in range(2):
                P.dma("sp", dtv[64 * gl:64 * gl + 64, :],
                      I["s5_log_dt"].ap[i, d].rearrange("(j g) -> g j", g=2)[gl:gl + 1, :].partition_broadcast(64),
                      reads=[I["s5_log_dt"]], writes=[dtv])
            P.dma("sp", bre[:], I["s5_b_re"].ap[i, d].rearrange("g p c -> (g p) c").rearrange("(j q) c -> q j c", q=128),
                  reads=[I["s5_b_re"]], writes=[bre])
            P.dma("sp", bim[:], I["s5_b_im"].ap[i, d].rearrange("g p c -> (g p) c").rearrange("(j q) c -> q j c", q=128),
                  reads=[I["s5_b_im"]], writes=[bim])
            P.dma("sp", cre[:], I["s5_c_re"].ap[i, d].rearrange("g c p -> (g c) p").rearrange("(k q) p -> q k p", q=128),
                  reads=[I["s5_c_re"]], writes=[cre])
            P.dma("sp", cim[:], I["s5_c_im"].ap[i, d].rearrange("g c p -> (g c) p").rearrange("(k q) p -> q k p", q=128),
                  reads=[I["s5_c_im"]], writes=[cim])
        P.ts("dve", lr, lr[:], lr, lr[:], -1e-4, ALU.min)
        P.act(dtv, dtv[:], dtv, dtv[:], AF.Exp)
        P.tt("dve", wdt, wdt[:], li, li[:], dtv, dtv[:], ALU.mult)
        P.tt("dve", rdt, rdt[:], lr, lr[:], dtv, dtv[:], ALU.mult)
        P.ts("dve", nrdt, nrdt[:], rdt, rdt[:], -1.0, ALU.mult)
        ar, ai, a_s, a_c, a_m, ta, tb, den = s16
        sincos(wdt, wdt[:], a_s, a_s[:], a_c, a_c[:], ta, ta[:], tb, tb[:])
        P.act(a_m, a_m[:], rdt, rdt[:], AF.Exp)
        P.tt("dve", ar, ar[:], a_m, a_m[:], a_c, a_c[:], ALU.mult)
        P.tt("dve", ai, ai[:], a_m, a_m[:], a_s, a_s[:], ALU.mult)
        P.ts("dve", ar, ar[:], ar, ar[:], -1.0, ALU.add)
        P.tt("dve", den, den[:], lr, lr[:], lr, lr[:], ALU.mult)
        P.tt("dve", ta, ta[:], li, li[:], li, li[:], ALU.mult)
        P.tt("dve", den, den[:], den, den[:], ta, ta[:], ALU.add)
        P.op("dve", lambda e: e.reciprocal(out=den[:], in_=den[:]), reads=[den], writes=[den])
        P.tt("dve", ta, ta[:], ar, ar[:], lr, lr[:], ALU.mult)
        P.tt("dve", tb, tb[:], ai, ai[:], li, li[:], ALU.mult)
        P.tt("dve", ta, ta[:], ta, ta[:], tb, tb[:], ALU.add)
        P.tt("dve", a_c, a_c[:], ta, ta[:], den, den[:], ALU.mult)
        P.tt("dve", ta, ta[:], ai, ai[:], lr, lr[:], ALU.mult)
        P.tt("dve", tb, tb[:], ar, ar[:], li, li[:], ALU.mult)
        P.tt("dve", ta, ta[:], ta, ta[:], tb, tb[:], ALU.subtract)
        P.tt("dve", a_s, a_s[:], ta, ta[:], den, den[:], ALU.mult)
        P.ts("dve", a_m, a_m[:], a_s, a_s[:], -1.0, ALU.mult)
        coef_r, coef_i, ncoef_i = a_c, a_s, a_m
        P.ts("dve", ta, ta[:], wdt, wdt[:], float(TT), ALU.mult)
        sincos(ta, ta[:], Hi, Hi[:], Hr, Hr[:], tb, tb[:], den, den[:])
        P.act(ta, ta[:], rdt, rdt[:], AF.Exp, scale=float(TT))
        P.tt("dve", Hr, Hr[:], Hr, Hr[:], ta, ta[:], ALU.mult)
        P.tt("dve", Hi, Hi[:], Hi, Hi[:], ta, ta[:], ALU.mult)
        for j in range(16):
            jj = j % 4
            cc = j // 4
            P.ts("dve", th, th[:], tau, tau[:], wdt[:, j:j + 1], ALU.mult, extra_reads=[wdt])
            sincos(th, th[:], sn, sn[:], cs, cs[:], tmp, tmp[:], mg, mg[:])
            P.act(mg, mg[:], tau, tau[:], AF.Exp, scale=rdt[:, j:j + 1], extra_reads=[rdt])
            P.tt("dve", Gr, Gr[:, j, :], mg, mg[:], cs, cs[:], ALU.mult)
            P.tt("dve", Gi, Gi[:, j, :], mg, mg[:], sn, sn[:], ALU.mult)
            P.act(mg, mg[:], tau, tau[:], AF.Exp, scale=nrdt[:, j:j + 1], extra_reads=[nrdt])
            P.tt("dve", Er, Er[:, j, :], mg, mg[:], cs, cs[:], ALU.mult)
            P.stt(Ei, Ei[:, j, :], mg, mg[:], -1.0, sn, sn[:], ALU.mult, ALU.mult)
            P.ts("dve", bbr, bbr[:, j, :], bre, bre[:, j, :], coef_r[:, j:j + 1], ALU.mult, extra_reads=[coef_r])
            P.stt(bbr, bbr[:, j, :], bim, bim[:, j, :], ncoef_i[:, j:j + 1], bbr, bbr[:, j, :], ALU.mult, ALU.add,
                  extra_reads=[ncoef_i])
            P.ts("dve", bbi, bbi[:, j, :], bim, bim[:, j, :], coef_r[:, j:j + 1], ALU.mult, extra_reads=[coef_r])
            P.stt(bbi, bbi[:, j, :], bre, bre[:, j, :], coef_i[:, j:j + 1], bbi, bbi[:, j, :], ALU.mult, ALU.add,
                  extra_reads=[coef_i])
            for (src, dst, neg) in ((bbr, BTr, False), (bbi, BTi, False)):
                P.tt("dve", ex, ex[:].rearrange("q (a c) -> q a c", c=16),
                     src, src[:, j, :].unsqueeze(1).broadcast_to([128, 8, 16]),
                     maskB, maskB[:, jj * 128:(jj + 1) * 128].rearrange("q (a c) -> q a c", c=16), ALU.mult)
                ps = next_ps(K)
                P.op("pe", lambda e: e.transpose(ps[:, 0:128], ex[:], ident[:]), reads=[ex, ident], writes=[ps])
                P.copy("act", dst, dst[:, j, :], ps, ps[:, 0:128])
            for (src, dst, neg) in ((cre, CTr, False), (cim, CTin, True)):
                P.tt("dve", ex, ex[:].rearrange("q (a p) -> q a p", p=64),
                     src, src[:, cc, :].unsqueeze(1).broadcast_to([128, 2, 64]),
                     maskC, maskC[:, jj * 128:(jj + 1) * 128].rearrange("q (a p) -> q a p", p=64), ALU.mult)
                ps = next_ps(K)
                P.op("pe", lambda e: e.transpose(ps[:, 0:128], ex[:], ident[:]), reads=[ex, ident], writes=[ps])
                if neg:
                    P.ts("dve", dst, dst[:, j, :], ps, ps[:, 0:128], -1.0, ALU.mult)
                else:
                    P.copy("act", dst, dst[:, j, :], ps, ps[:, 0:128])
        for q4 in range(4):
            P.memset("pool", cars[q4], cars[q4][:], 0.0)
            P.memset("pool", cais[q4], cais[q4][:], 0.0)
        order = (chunks_ctx + chunks_lat) if not rev else (chunks_ctx[::-1] + chunks_lat[::-1])
        K.ps_reserved = {0, 1, 2, 3, 4, 5}
        done = {}

        def quad(ci_, t0, cc):
            ub = u[ci_ % 3]
            yf = yfs[ci_ % 3]
            vq = vqs[ci_ % 2]
            if cc == 0:
                P.dma("sp", ub[:], zT.ap[u_row0:u_row0 + 512, t0:t0 + TT].rearrange("(k q) t -> q k t", q=128),
                      reads=[zT], writes=[ub])
                if rev:
                    P.dma("sp", yf[:], yS.ap[:, t0:t0 + TT].rearrange("(k q) t -> q k t", q=128), reads=[yS], writes=[yf])
            s_ = (ci_ * 4 + cc) % SL
            t1, t2, t3, t4, wr, wi, zr, zi, xr, xi = (t1s[s_], t2s[s_], t3s[s_], t4s[s_], wrs[s_], wis[s_], zrs[s_],
                                                       zis[s_], xrs[s_], xis[s_])
            car, cai, c4, g1, g2 = cars[cc], cais[cc], c4s[s_], g1s[s_], g2s[s_]
            pa, pb = K.ps[2 * s_], K.ps[2 * s_ + 1]
            py = pa
            urhs = ub[:, cc, ::-1] if rev else ub[:, cc, :]
            for jj in range(4):
                j = 4 * cc + jj
                P.mm(pa, pa[:, jj * TT:(jj + 1) * TT], BTr[:, j, :], urhs, True, True, reads=[BTr, ub])
                P.mm(pb, pb[:, jj * TT:(jj + 1) * TT], BTi[:, j, :], urhs, True, True, reads=[BTi, ub])
            yield
            Wr = pa[:, :].rearrange("q (a t) -> q a t", t=TT)
            Wi = pb[:, :].rearrange("q (a t) -> q a t", t=TT)
            sl = slice(4 * cc, 4 * cc + 4)
            P.tt("dve", t1, t1[:], pa, Wr, Er, Er[:, sl, :], ALU.mult)
            P.tt("dve", t2, t2[:], pb, Wi, Ei, Ei[:, sl, :], ALU.mult)
            P.tt("pool", wr, wr[:], t1, t1[:], t2, t2[:], ALU.subtract)
            P.tt("dve", t3, t3[:], pa, Wr, Ei, Ei[:, sl, :], ALU.mult)
            P.tt("dve", t4, t4[:], pb, Wi, Er, Er[:, sl, :], ALU.mult)
            P.tt("pool", wi, wi[:], t3, t3[:], t4, t4[:], ALU.add)
            yield
            for jj in range(4):
                P.op("dve", lambda e: e.tensor_tensor_scan(out=zr[:, jj, :], data0=onesT[:], data1=wr[:, jj, :],
                                                           initial=car[:, jj:jj + 1], op0=ALU.mult, op1=ALU.add),
                     reads=[onesT, wr, car], writes=[zr])
                P.op("dve", lambda e: e.tensor_tensor_scan(out=zi[:, jj, :], data0=onesT[:], data1=wi[:, jj, :],
                                                           initial=cai[:, jj:jj + 1], op0=ALU.mult, op1=ALU.add),
                     reads=[onesT, wi, cai], writes=[zi])
            yield
            zlr = zr[:, :, TT - 1]
            zli = zi[:, :, TT - 1]
            P.tt("dve", c4[0], c4[0][:], zr, zlr, Hr, Hr[:, sl], ALU.mult)
            P.tt("dve", c4[1], c4[1][:], zi, zli, Hi, Hi[:, sl], ALU.mult)
            P.tt("dve", c4[2], c4[2][:], zi, zli, Hr, Hr[:, sl], ALU.mult)
            P.tt("dve", c4[3], c4[3][:], zr, zlr, Hi, Hi[:, sl], ALU.mult)
            P.tt("dve", car, car[:], c4[0], c4[0][:], c4[1], c4[1][:], ALU.subtract)
            P.tt("dve", cai, cai[:], c4[2], c4[2][:], c4[3], c4[3][:], ALU.add)
            P.tt("dve", t1, t1[:], zr, zr[:], Gr, Gr[:, sl, :], ALU.mult)
            P.tt("pool", t2, t2[:], zi, zi[:], Gi, Gi[:, sl, :], ALU.mult)
            P.tt("pool", t4, t4[:], zr, zr[:], Gi, Gi[:, sl, :], ALU.mult)
            P.tt("dve", t3, t3[:], zi, zi[:], Gr, Gr[:, sl, :], ALU.mult)
            P.tt("dve", xr, xr[:], t1, t1[:], t2, t2[:], ALU.subtract)
            P.tt("pool", xi, xi[:], t3, t3[:], t4, t4[:], ALU.add)
            yield
            for jj in range(4):
                j = 4 * cc + jj
                P.mm(py, py[:, :TT], CTr[:, j, :], xr[:, jj, :], jj == 0, False, reads=[CTr, xr])
                P.mm(py, py[:, :TT], CTin[:, j, :], xi[:, jj, :], False, jj == 3, reads=[CTin, xi])
            yield
            if not rev:
                yb = yo[cc % 2]
                P.copy("act", yb, yb[:], py, py[:, :TT])
                P.dma("pool", yS.ap[128 * cc:128 * cc + 128, t0:t0 + TT], yb[:], reads=[yb], writes=[yS])
            else:
                P.tt("dve", g1, g1[:], py, py[:, :TT][:, ::-1], yf, yf[:, cc, :], ALU.add)
                P.stt(g1, g1[:], ub, ub[:, cc, :], dsk[:, cc:cc + 1], g1, g1[:], ALU.mult, ALU.add, extra_reads=[dsk])
                P.tt("pool", g2, g2[:], g1, g1[:], g1, g1[:], ALU.mult)
                P.ts("dve", g2, g2[:], g2, g2[:], 0.044715, ALU.mult, 1.0, ALU.add)
                P.tt("dve", g2, g2[:], g2, g2[:], g1, g1[:], ALU.mult)
                P.act(g2, g2[:], g2, g2[:], AF.Sigmoid, scale=1.5957691216057308)
                P.tt("dve", vq[cc], vq[cc][:], g1, g1[:], g2, g2[:], ALU.mult)
                done[ci_] = done.get(ci_, 0) + 1
                if done[ci_] == 4:
                    for mc in range(4):
                        pg = next_ps(K)
                        for kc in range(4):
                            P.mm(pg, pg[:, :TT], wglu[:, kc, mc * 128:(mc + 1) * 128], vq[kc][:], kc == 0, kc == 3,
                                 reads=[wglu, vq[kc]])
                        ob = oo[mc % 2]
                        P.act(ob, ob[:], pg, pg[:, :TT], AF.Sigmoid)
                        P.tt("dve", ob, ob[:], ob, ob[:], vq[mc], vq[mc][:], ALU.mult)
                        P.dma("pool", mixT.ap[out_row0 + 128 * mc:out_row0 + 128 * mc + 128, t0:t0 + TT], ob[:],
                              reads=[ob], writes=[mixT])
            yield

        run_interleaved((quad(ci_, t0, cc) for ci_, (t0, is_ctx) in enumerate(order) for cc in range(4)), width=SL)
        K.ps_reserved = set()
    P.release_to(mark)


HY_BANDS = 16
HY_EMB = 33


def hyena_consts(L):
    N = 2 * L
    N1 = N // 128
    R = L // 128
    c = {}
    f64 = np.float64
    n1 = np.arange(R, dtype=f64)[:, None]; k1 = np.arange(N1, dtype=f64)[None, :]
    a = 2 * np.pi * n1 * k1 / N1
    c["F1c"] = np.cos(a); c["F1ns"] = -np.sin(a)
    n2 = np.arange(128, dtype=f64)[:, None]
    a = 2 * np.pi * n2 * k1 / N
    c["Twc"] = np.cos(a); c["Tws"] = np.sin(a)
    c["TwcT"] = np.cos(a).T.copy(); c["TwsT"] = np.sin(a).T.copy()
    k1c = np.arange(N1, dtype=f64)[:, None]; n1r = np.arange(R, dtype=f64)[None, :]
    a = 2 * np.pi * k1c * n1r / N1
    c["I2c"] = np.cos(a) / N; c["I2ns"] = -np.sin(a) / N
    t = np.arange(L, dtype=np.float32)
    t01 = t / np.float32(L)
    bands = np.linspace(1e-4, HY_BANDS - 1, HY_BANDS, dtype=np.float32)
    ang = (np.float32(2.0 * math.pi / L) * t[:, None]) * bands[None, :]
    feats = np.concatenate([t01[:, None], np.cos(ang), -np.sin(ang)], axis=-1)
    c["featsT"] = feats.T.copy()
    return {f"hy{L}_{k}": np.ascontiguousarray(v, dtype=np.float32) for k, v in c.items()}


def hyena_shared_consts():
    n2 = np.arange(128, dtype=np.float64)[:, None]; k2 = np.arange(128, dtype=np.float64)[None, :]
    a = 2 * np.pi * n2 * k2 / 128
    return {"hy_F3c": np.cos(a).astype(np.float32), "hy_F3s": np.sin(a).astype(np.float32),
            "hy_F3ns": (-np.sin(a)).astype(np.float32),
            "hy_tau512": np.tile(np.arange(512, dtype=np.float32)[None, :], (128, 1))}


def run_interleaved(gens, width=2):
    gens = list(gens)
    active = []
    while gens or active:
        while gens and len(active) < width:
            active.append(gens.pop(0))
        for g in list(active):
            try:
                next(g)
            except StopIteration:
                active.remove(g)


def hyena_filters_td(K, i, I, C, L, emit):
    P = K.P
    nc = P.nc
    sb = P.sb
    tau512 = C["hy_tau512"]
    featsD = C[f"hy{L}_featsT_dram"]
    mark1 = P.mark()
    w1 = sb([HY_EMB, 64], F32, "hy_w1"); w2 = sb([64, 64], F32, "hy_w2"); w3 = sb([64, 2048], F32, "hy_w3")
    b1 = sb([64, 1], F32, "hy_b1"); b2 = sb([64, 1], F32, "hy_b2"); fq = sb([64, 1], F32, "hy_fq")
    fb1 = sb([64, 1], F32, "hy_fb1"); fb2 = sb([64, 1], F32, "hy_fb2")
    dec = sb([128, 8], F32, "hy_dec"); decb = sb([128, 8], F32, "hy_decb")
    P.dma("sp", w1[:], I["hy_w1"].ap[i], reads=[I["hy_w1"]], writes=[w1])
    P.dma("sp", w2[:], I["hy_w2"].ap[i], reads=[I["hy_w2"]], writes=[w2])
    P.dma("sp", w3[:], I["hy_w3"].ap[i], reads=[I["hy_w3"]], writes=[w3])
    with nc.allow_non_contiguous_dma(reason="tiny vector load"):
        P.dma("sp", b1[:], I["hy_b1"].ap[i].rearrange("(p o) -> p o", o=1), reads=[I["hy_b1"]], writes=[b1])
        P.dma("sp", b2[:], I["hy_b2"].ap[i].rearrange("(p o) -> p o", o=1), reads=[I["hy_b2"]], writes=[b2])
        P.dma("sp", fq[:], I["hy_freq"].ap[i].rearrange("(p o) -> p o", o=1), reads=[I["hy_freq"]], writes=[fq])
        P.dma("sp", dec[:], I["hy_decay"].ap[i].rearrange("o (k p) -> p (o k)", p=128), reads=[I["hy_decay"]], writes=[dec])
    P.tt("dve", fb1, fb1[:], b1, b1[:], fq, fq[:], ALU.mult)
    P.tt("dve", fb2, fb2[:], b2, b2[:], fq, fq[:], ALU.mult)
    P.act(dec, dec[:], dec, dec[:], AF.Abs)
    P.ts("dve", dec, dec[:], dec, dec[:], -1.0 / L, ALU.mult)
    TC = min(512, L)
    nTC = L // TC
    hid1 = sb([64, L], F32, "hy_hid1"); hid2 = sb([64, L], F32, "hy_hid2")
    ft = sb([HY_EMB, TC], F32, "hy_ft")
    pre = sb([64, 512], F32, "hy_pre"); rr = sb([64, 512], F32, "hy_rr"); rt = sb([64, 512], F32, "hy_rt")
    for tcn in range(nTC):
        P.dma("sp", ft[:], featsD.ap[:, tcn * TC:(tcn + 1) * TC], reads=[featsD], writes=[ft])
        ps = next_ps(K)
        P.mm(ps, ps[:64, :TC], w1[:], ft[:], True, True, reads=[w1, ft])
        P.ts("dve", pre, pre[:, :TC], ps, ps[:64, :TC], fq[:, 0:1], ALU.mult, fb1[:, 0:1], ALU.add, extra_reads=[fq, fb1])
        range_reduce(P, rr, rr[:, :TC], pre, pre[:, :TC], rt, rt[:, :TC])
        P.act(hid1, hid1[:, tcn * TC:(tcn + 1) * TC], rr, rr[:, :TC], AF.Sin)
        ps = next_ps(K)
        P.mm(ps, ps[:64, :TC], w2[:], hid1[:, tcn * TC:(tcn + 1) * TC], True, True, reads=[w2, hid1])
        P.ts("dve", pre, pre[:, :TC], ps, ps[:64, :TC], fq[:, 0:1], ALU.mult, fb2[:, 0:1], ALU.add, extra_reads=[fq, fb2])
        range_reduce(P, rr, rr[:, :TC], pre, pre[:, :TC], rt, rt[:, :TC])
        P.act(hid2, hid2[:, tcn * TC:(tcn + 1) * TC], rr, rr[:, :TC], AF.Sin)
    kf = sb([128, L], F32, "hy_kf"); kb = sb([128, L], F32, "hy_kb")
    win = sb([128, 512], F32, "hy_win"); wb_ = sb([128, 1], F32, "hy_wb")
    sums = sb([128, 2 * nTC + 1], F32, "hy_sums"); tot = sb([128, 1], F32, "hy_tot")
    for o in range(2):
        for cq in range(4):
            oc = o * 4 + cq
            for tcn in range(nTC):
                P.ts("dve", wb_, wb_[:], dec, dec[:, oc:oc + 1], float(tcn * TC), ALU.mult)
                P.act(win, win[:, :TC], tau512, tau512[:, :TC], AF.Exp, bias=wb_[:, 0:1], scale=dec[:, oc:oc + 1],
                      extra_reads=[wb_, dec])
                for dr, kt in ((0, kf), (1, kb)):
                    col0 = dr * 1024 + o * 512 + cq * 128
                    ps = next_ps(K)
                    P.mm(ps, ps[:, :TC], w3[:, col0:col0 + 128], hid2[:, tcn * TC:(tcn + 1) * TC], True, True, reads=[w3, hid2])
                    P.stt(kt, kt[:, tcn * TC:(tcn + 1) * TC], win, win[:, :TC], HY_WINDOW_SHIFT, ps, ps[:, :TC], ALU.add, ALU.mult)
                    P.op("dve", lambda e: e.tensor_reduce(out=sums[:, dr * nTC + tcn:dr * nTC + tcn + 1],
                                                          in_=kt[:, tcn * TC:(tcn + 1) * TC], axis=AX.X, op=ALU.add,
                                                          apply_absolute_value=True), reads=[kt], writes=[sums])
            P.act(sums, sums[:, 2 * nTC:2 * nTC + 1], kb, kb[:, 0:1], AF.Abs)
            P.ts("dve", sums, sums[:, 2 * nTC:2 * nTC + 1], sums, sums[:, 2 * nTC:2 * nTC + 1], -1.0, ALU.mult)
            P.op("dve", lambda e: e.tensor_reduce(out=tot[:], in_=sums[:], axis=AX.X, op=ALU.add), reads=[sums], writes=[tot])
            P.op("dve", lambda e: e.reciprocal(out=tot[:], in_=tot[:]), reads=[tot], writes=[tot])
            P.memset("dve", kb, kb[:, 0:1], 0.0)
            P.ts("dve", kf, kf[:], kf, kf[:], tot[:, 0:1], ALU.mult, extra_reads=[tot])
            P.ts("pool", kb, kb[:], kb, kb[:], tot[:, 0:1], ALU.mult, extra_reads=[tot])
            emit(o, cq, kf, kb)
    P.release_to(mark1)


def hyena_stage(K, i, I, zT, mixT, C, L, t_base, KFr, KFi, kT, tag):
    P = K.P
    nc = P.nc
    N = 2 * L
    N1 = N // 128
    R = L // 128
    G = min(512 // N1, 4)
    W = G * N1
    WT = G * 128
    mark = P.mark()
    sb = P.sb
    cn = lambda k: C[f"hy{L}_{k}"]
    tau512 = C["hy_tau512"]
    Twc, Tws, TwcT, TwsT = (cn(k) for k in ("Twc", "Tws", "TwcT", "TwsT"))
    featsD = C[f"hy{L}_featsT_dram"]

    def bf(src, nm):
        if HY_DT == F32:
            return src
        t = sb(list(src.ap.shape), HY_DT, "hyb_" + nm)
        P.copy("pool", t, t[:], src, src[:])
        return t
    F3c, F3s, F3ns = bf(C["hy_F3c"], "F3c"), bf(C["hy_F3s"], "F3s"), bf(C["hy_F3ns"], "F3ns")
    F1c, F1ns, I2c, I2ns = bf(cn("F1c"), "F1c"), bf(cn("F1ns"), "F1ns"), bf(cn("I2c"), "I2c"), bf(cn("I2ns"), "I2ns")
    NS = 2
    mq = [[sb([128, 512], HY_DT, f"hy_mq{k}{j}") for j in range(4)] for k in range(NS)]
    m1s = [sb([128, 512], F32, f"hy_m1{k}") for k in range(NS)]; m2s = [sb([128, 512], F32, f"hy_m2{k}") for k in range(NS)]

    def neg(src, nm):
        t = sb(list(src.ap.shape), HY_DT, "hyn_" + nm)
        P.ts("dve", t, t[:], src, src[:], -1.0, ALU.mult)
        return t
    F3nc = neg(F3c, "F3nc")
    I2nc = neg(I2c, "I2nc")

    def fft_fwd(U, psr, psi, sl):
        q1, q2, q3, q4 = mq[sl]
        pa, pb = K.ps[4 * sl], K.ps[4 * sl + 1]
        Ut, Uf = U
        for c in range(G):
            P.mm(pa, pa[:, c * N1:(c + 1) * N1], Uf(c), F1c[:R, :], True, True, reads=[Ut, F1c])
            P.mm(pb, pb[:, c * N1:(c + 1) * N1], Uf(c), F1ns[:R, :], True, True, reads=[Ut, F1ns])
        yield
        v3 = lambda ap: ap.rearrange("q (c k) -> q c k", k=N1)
        tc_ = Twc[:, :].unsqueeze(1).broadcast_to([128, G, N1])
        ts_ = Tws[:, :].unsqueeze(1).broadcast_to([128, G, N1])
        P.tt("dve", q1, v3(q1[:, :W]), pa, v3(pa[:, :W]), Twc, tc_, ALU.mult)
        P.tt("dve", q2, v3(q2[:, :W]), pb, v3(pb[:, :W]), Tws, ts_, ALU.mult)
        P.tt("dve", q3, v3(q3[:, :W]), pb, v3(pb[:, :W]), Twc, tc_, ALU.mult)
        P.tt("dve", q4, v3(q4[:, :W]), pa, v3(pa[:, :W]), Tws, ts_, ALU.mult)
        yield
        for idx, (w_, q_) in enumerate(((F3c, q1), (F3c, q2), (F3s, q3), (F3ns, q4))):
            P.mm(psr, psr[:, :W], w_[:], q_[:, :W], idx == 0, idx == 3, reads=[w_, q_])
        for idx, (w_, q_) in enumerate(((F3c, q3), (F3nc, q4), (F3ns, q1), (F3ns, q2))):
            P.mm(psi, psi[:, :W], w_[:], q_[:, :W], idx == 0, idx == 3, reads=[w_, q_])
        yield

    def fft_inv(Yr, Yi, psy, sl):
        q1, q2, q3, q4 = mq[sl]
        pa, pb = K.ps[4 * sl], K.ps[4 * sl + 1]
        for c in range(G):
            yr = Yr[:, c * N1:(c + 1) * N1]
            yi = Yi[:, c * N1:(c + 1) * N1]
            P.mm(pa, pa[:N1, c * 128:(c + 1) * 128], yr, F3c[:], True, False, reads=[Yr, F3c])
            P.mm(pa, pa[:N1, c * 128:(c + 1) * 128], yi, F3ns[:], False, True, reads=[Yi, F3ns])
            P.mm(pb, pb[:N1, c * 128:(c + 1) * 128], yr, F3s[:], True, False, reads=[Yr, F3s])
            P.mm(pb, pb[:N1, c * 128:(c + 1) * 128], yi, F3c[:], False, True, reads=[Yi, F3c])
        yield
        v3 = lambda ap: ap.rearrange("q (c k) -> q c k", k=128)
        tc_ = TwcT[:N1, :].unsqueeze(1).broadcast_to([N1, G, 128])
        ts_ = TwsT[:N1, :].unsqueeze(1).broadcast_to([N1, G, 128])
        P.tt("dve", q1, v3(q1[:N1, :WT]), pa, v3(pa[:N1, :WT]), TwcT, tc_, ALU.mult)
        P.tt("dve", q2, v3(q2[:N1, :WT]), pb, v3(pb[:N1, :WT]), TwsT, ts_, ALU.mult)
        P.tt("dve", q3, v3(q3[:N1, :WT]), pa, v3(pa[:N1, :WT]), TwsT, ts_, ALU.mult)
        P.tt("dve", q4, v3(q4[:N1, :WT]), pb, v3(pb[:N1, :WT]), TwcT, tc_, ALU.mult)
        yield
        for idx, (w_, q_) in enumerate(((I2c, q1), (I2nc, q2), (I2ns, q3), (I2ns, q4))):
            P.mm(psy, psy[:R, :WT], w_[:N1, :], q_[:N1, :WT], idx == 0, idx == 3, reads=[w_, q_])
        yield

    def emit_kT(o, cq, kf, kb):
        r0 = o * 512 + cq * 128
        P.dma("sp", kT.ap[r0:r0 + 128, 0:L], kf[:], reads=[kf], writes=[kT])
        P.dma("sp", kT.ap[1024 + r0:1024 + r0 + 128, 0:L], kb[:], reads=[kb], writes=[kT])

    hyena_filters_td(K, i, I, C, L, emit_kT)

    mark2 = P.mark()
    Uf32 = [sb([R, G, 128], F32, f"hy_Uf{k}") for k in range(NS)]
    Ub32 = [sb([R, G, 128], F32, f"hy_Ub{k}") for k in range(NS)]
    Uf_ = [sb([R, G, 128], HY_DT, f"hy_Ufb{k}") for k in range(NS)]
    Ub_ = [sb([R, G, 128], HY_DT, f"hy_Ubb{k}") for k in range(NS)]
    Xs = [[sb([128, 512], F32, f"hy_Xs{k}{m}") for m in range(2)] for k in range(NS)]
    Ko = [[sb([128, 512], F32, f"hy_Ko{k}{m}") for m in range(2)] for k in range(NS)]

    def filt_group(gi):
        sl = gi % NS
        oc0 = gi * G
        uf, ub = Uf_[sl], Ub_[sl]
        P.dma("sp", Uf32[sl][:], kT.ap[oc0:oc0 + G, 0:L].rearrange("c (a b) -> a c b", b=128), reads=[kT], writes=[Uf32[sl]])
        P.dma("sp", Ub32[sl][:], kT.ap[1024 + oc0:1024 + oc0 + G, 0:L].rearrange("c (a b) -> a c b", b=128), reads=[kT], writes=[Ub32[sl]])
        P.copy("act", uf, uf[:], Uf32[sl], Uf32[sl][:])
        P.copy("act", ub, ub[:], Ub32[sl], Ub32[sl][:])
        pfr, pfi = K.ps[4 * sl + 2], K.ps[4 * sl + 3]
        yield from fft_fwd((uf, lambda c: uf[:, c, :]), pfr, pfi, sl)
        P.copy("act", Xs[sl][0], Xs[sl][0][:, :W], pfr, pfr[:, :W])
        P.copy("act", Xs[sl][1], Xs[sl][1][:, :W], pfi, pfi[:, :W])
        pbr, pbi = K.ps[4 * sl + 2], K.ps[4 * sl + 3]
        yield from fft_fwd((ub, lambda c: ub[:, c, :]), pbr, pbi, sl)
        kr, ki = Ko[sl]
        P.tt("dve", kr, kr[:, :W], Xs[sl][0], Xs[sl][0][:, :W], pbr, pbr[:, :W], ALU.add)
        P.tt("dve", ki, ki[:, :W], Xs[sl][1], Xs[sl][1][:, :W], pbi, pbi[:, :W], ALU.subtract)
        P.dma("pool", KFr.ap[oc0 // 4, :, 0:W], kr[:, :W], reads=[kr], writes=[KFr])
        P.dma("pool", KFi.ap[oc0 // 4, :, 0:W], ki[:, :W], reads=[ki], writes=[KFi])
        yield

    run_interleaved((filt_group(gi) for gi in range(1024 // G)), width=NS)
    P.release_to(mark2)

    cw = sb([R, 1536, 3], F32, "hy_cw"); cb = sb([R, 1536], F32, "hy_cb"); hb = sb([R, 1024], F32, "hy_hb")
    with nc.allow_non_contiguous_dma(reason="broadcast parameter loads"):
        for j in range(3):
            P.dma("sp", cw[:, :, j], I["ev_conv_w"].ap[i, j:j + 1, :].partition_broadcast(R), reads=[I["ev_conv_w"]], writes=[cw])
        P.dma("sp", cb[:], I["ev_conv_b"].ap[i].rearrange("(o c) -> o c", o=1).partition_broadcast(R), reads=[I["ev_conv_b"]], writes=[cb])
        P.dma("sp", hb[:], I["hy_bias"].ap[i].rearrange("o c -> (o c)").rearrange("(o c) -> o c", o=1).partition_broadcast(R),
              reads=[I["hy_bias"]], writes=[hb])
    raw = [[sb([R, G, 130], F32, f"hy_raw{k}{m}") for m in range(3)] for k in range(NS)]
    cvs = [[sb([R, G, 128], F32, f"hy_cv{k}{m}") for m in range(3)] for k in range(NS)]
    cts = [sb([R, G, 128], F32, f"hy_ct{k}") for k in range(NS)]
    ybs = [sb([R, G, 128], HY_DT, f"hy_yb{k}") for k in range(NS)]
    kfr = [[sb([128, 512], F32, f"hy_kfr{k}{o}") for o in range(2)] for k in range(NS)]
    kfi = [[sb([128, 512], F32, f"hy_kfi{k}{o}") for o in range(2)] for k in range(NS)]
    Yrs = [sb([128, 512], HY_DT, f"hy_Yr{k}") for k in range(NS)]; Yis = [sb([128, 512], HY_DT, f"hy_Yi{k}") for k in range(NS)]
    y1s = [sb([R, G, 128], F32, f"hy_y1{k}") for k in range(NS)]; youts = [sb([R, G, 128], F32, f"hy_yout{k}") for k in range(NS)]
    for k in range(NS):
        for m in range(3):
            P.memset("pool", raw[k][m], raw[k][m][:], 0.0)

    def data_group(gi):
        sl = gi % NS
        c0 = gi * G
        rw, cv, ct, yb = raw[sl], cvs[sl], cts[sl], ybs[sl]
        m1, m2 = m1s[sl], m2s[sl]
        for m in range(3):
            rows = zT.ap[m * 512 + c0:m * 512 + c0 + G, :]
            P.dma("sp", rw[m][:, :, 1:129], rows[:, t_base:t_base + L].rearrange("c (a b) -> a c b", b=128),
                  reads=[zT], writes=[rw[m]])
            if R > 1:
                with nc.allow_non_contiguous_dma(reason="1-column conv halos"):
                    P.dma("sp", rw[m][1:R, :, 0:1], rows[:, t_base + 127:t_base + L - 1].rearrange("c (a b) -> a c b", b=128)[:, :, 0:1],
                          reads=[zT], writes=[rw[m]])
                    P.dma("sp", rw[m][0:R - 1, :, 129:130], rows[:, t_base + 128:t_base + L].rearrange("c (a b) -> a c b", b=128)[:, :, 0:1],
                          reads=[zT], writes=[rw[m]])
        for o in range(2):
            oc0 = o * 512 + c0
            P.dma("sp", kfr[sl][o][:, :W], KFr.ap[oc0 // 4, :, 0:W], reads=[KFr], writes=[kfr[sl][o]])
            P.dma("sp", kfi[sl][o][:, :W], KFi.ap[oc0 // 4, :, 0:W], reads=[KFi], writes=[kfi[sl][o]])
        for m in range(3):
            ch = slice(m * 512 + c0, m * 512 + c0 + G)
            wj = lambda j: cw[:, ch, j].unsqueeze(2).broadcast_to([R, G, 128])
            P.tt("dve", cv[m], cv[m][:], rw[m], rw[m][:, :, 0:128], cw, wj(0), ALU.mult)
            P.tt("pool", ct, ct[:], rw[m], rw[m][:, :, 1:129], cw, wj(1), ALU.mult)
            P.tt("dve", cv[m], cv[m][:], cv[m], cv[m][:], ct, ct[:], ALU.add)
            P.tt("pool", ct, ct[:], rw[m], rw[m][:, :, 2:130], cw, wj(2), ALU.mult)
            P.tt("dve", cv[m], cv[m][:], cv[m], cv[m][:], ct, ct[:], ALU.add)
            P.tt("dve", cv[m], cv[m][:], cv[m], cv[m][:], cb, cb[:, ch].unsqueeze(2).broadcast_to([R, G, 128]), ALU.add)
        yield
        ycur = cv[0]
        for o in range(2):
            P.copy("act", yb, yb[:], ycur, ycur[:])
            pxr, pxi = K.ps[4 * sl + 2], K.ps[4 * sl + 3]
            yield from fft_fwd((yb, lambda c: yb[:, c, :]), pxr, pxi, sl)
            kr, ki = kfr[sl][o], kfi[sl][o]
            Yr, Yi = Yrs[sl], Yis[sl]
            P.tt("dve", m1, m1[:, :W], pxr, pxr[:, :W], kr, kr[:, :W], ALU.mult)
            P.tt("dve", m2, m2[:, :W], pxi, pxi[:, :W], ki, ki[:, :W], ALU.mult)
            P.tt("pool", Yr, Yr[:, :W], m1, m1[:, :W], m2, m2[:, :W], ALU.subtract)
            P.tt("dve", m1, m1[:, :W], pxr, pxr[:, :W], ki, ki[:, :W], ALU.mult)
            P.tt("dve", m2, m2[:, :W], pxi, pxi[:, :W], kr, kr[:, :W], ALU.mult)
            P.tt("pool", Yi, Yi[:, :W], m1, m1[:, :W], m2, m2[:, :W], ALU.add)
            yield
            py = K.ps[4 * sl + 2]
            yield from fft_inv(Yr, Yi, py, sl)
            hbo = hb[:, o * 512 + c0:o * 512 + c0 + G].unsqueeze(2).broadcast_to([R, G, 128])
            P.tt("dve", ct, ct[:], ycur, ycur[:], hb, hbo, ALU.mult)
            P.tt("dve", ct, ct[:], ct, ct[:], py, py[:R, :WT].rearrange("q (c k) -> q c k", k=128), ALU.add)
            dst = y1s[sl] if o == 0 else youts[sl]
            P.tt("dve", dst, dst[:], ct, ct[:], cv[1 + o], cv[1 + o][:], ALU.mult)
            ycur = dst
            yield
        P.dma("pool", mixT.ap[c0:c0 + G, t_base:t_base + L].rearrange("c (a b) -> a c b", b=128), ycur[:],
              reads=[ycur], writes=[mixT])
        yield

    run_interleaved((data_group(gi) for gi in range(512 // G)), width=NS)
    P.release_to(mark)


def hyena_ctx_consts():
    L, N = 256, 512
    t = np.arange(L, dtype=np.float64)[:, None]; f = np.arange(N, dtype=np.float64)[None, :]
    a = 2 * np.pi * t * f / N
    c = {"hc_Fc": np.cos(a), "hc_Fns": -np.sin(a), "hc_Ic": np.cos(a).T / N, "hc_Ins": -np.sin(a).T / N}
    return {k: np.ascontiguousarray(v, dtype=np.float32) for k, v in c.items()}


def hyena_ctx_stage(K, i, I, zT, mixT, C, Cd, t_base=0):
    P = K.P
    nc = P.nc
    sb = P.sb
    L, N = 256, 512
    mark = P.mark()
    ident = C["k_ident"]
    Fc = sb([128, 2, 512], F32, "hc_Fc"); Fns = sb([128, 2, 512], F32, "hc_Fns")
    Ic = sb([128, 4, 256], F32, "hc_Ic"); Ins = sb([128, 4, 256], F32, "hc_Ins")
    P.dma("sp", Fc[:], Cd["hc_Fc"].ap.rearrange("(k p) f -> p k f", p=128), reads=[Cd["hc_Fc"]], writes=[Fc])
    P.dma("sp", Fns[:], Cd["hc_Fns"].ap.rearrange("(k p) f -> p k f", p=128), reads=[Cd["hc_Fns"]], writes=[Fns])
    P.dma("sp", Ic[:], Cd["hc_Ic"].ap.rearrange("(k p) t -> p k t", p=128), reads=[Cd["hc_Ic"]], writes=[Ic])
    P.dma("sp", Ins[:], Cd["hc_Ins"].ap.rearrange("(k p) t -> p k t", p=128), reads=[Cd["hc_Ins"]], writes=[Ins])
    ktm = [[sb([128, 2, 512], F32, f"hc_ktm{dr}{o}") for o in range(2)] for dr in range(2)]

    def emit(o, cq, kf, kb):
        for (src, dr) in ((kf, 0), (kb, 1)):
            for tc in range(2):
                ps = next_ps(K)
                P.op("pe", lambda e: e.transpose(ps[:, 0:128], src[:, tc * 128:(tc + 1) * 128], ident[:]), reads=[src, ident], writes=[ps])
                P.copy("act", ktm[dr][o], ktm[dr][o][:, tc, cq * 128:(cq + 1) * 128], ps, ps[:, 0:128])

    hyena_filters_td(K, i, I, C, L, emit)

    def dft(x_t, fc):
        psr = next_ps(K); psi = next_ps(K)
        for tc in range(2):
            P.mm(psr, psr[:, :], Fc[:, tc, fc * 128:(fc + 1) * 128], x_t[:, tc, :], tc == 0, tc == 1, reads=[Fc, x_t])
        for tc in range(2):
            P.mm(psi, psi[:, :], Fns[:, tc, fc * 128:(fc + 1) * 128], x_t[:, tc, :], tc == 0, tc == 1, reads=[Fns, x_t])
        return psr, psi

    KFr = [sb([128, 4, 512], F32, f"hc_KFr{o}") for o in range(2)]; KFi = [sb([128, 4, 512], F32, f"hc_KFi{o}") for o in range(2)]
    Xr = sb([128, 512], F32, "hc_Xr"); Xi = sb([128, 512], F32, "hc_Xi")
    for o in range(2):
        for fc in range(4):
            pr, pi_ = dft(ktm[0][o], fc)
            P.copy("act", Xr, Xr[:], pr, pr[:, :])
            P.copy("act", Xi, Xi[:], pi_, pi_[:, :])
            pr, pi_ = dft(ktm[1][o], fc)
            P.tt("dve", KFr[o], KFr[o][:, fc, :], Xr, Xr[:], pr, pr[:, :], ALU.add)
            P.tt("dve", KFi[o], KFi[o][:, fc, :], Xi, Xi[:], pi_, pi_[:, :], ALU.subtract)
    cwt = sb([128, 12, 3], F32, "hc_cw"); cbt = sb([128, 12], F32, "hc_cb"); hbb = sb([128, 2, 512], F32, "hc_hb")
    with nc.allow_non_contiguous_dma(reason="small parameter loads"):
        for j in range(3):
            P.dma("sp", cwt[:, :, j], I["ev_conv_w"].ap[i, j].rearrange("(k p) -> p k", p=128), reads=[I["ev_conv_w"]], writes=[cwt])
        P.dma("sp", cbt[:], I["ev_conv_b"].ap[i].rearrange("(k p) -> p k", p=128), reads=[I["ev_conv_b"]], writes=[cbt])
        P.dma("sp", hbb[:].rearrange("p o c -> p (o c)"),
              I["hy_bias"].ap[i].rearrange("o c -> (o c)").rearrange("(a n) -> a n", a=1).partition_broadcast(128),
              reads=[I["hy_bias"]], writes=[hbb])
    xtm = [sb([128, 2, 512], F32, f"hc_xtm{m}") for m in range(3)]
    raw = [sb([128, 258], F32, f"hc_raw{k}") for k in range(2)]
    cv = [sb([128, 256], F32, f"hc_cv{k}") for k in range(2)]
    for k in range(2):
        P.memset("pool", raw[k], raw[k][:], 0.0)
    for m in range(3):
        for cq in range(4):
            mc = m * 4 + cq
            rw, cvb = raw[mc % 2], cv[mc % 2]
            P.dma("sp", rw[:, 1:257], zT.ap[mc * 128:(mc + 1) * 128, t_base:t_base + L], reads=[zT], writes=[rw])
            P.ts("dve", cvb, cvb[:], rw, rw[:, 0:256], cwt[:, mc, 0:1], ALU.mult, cbt[:, mc:mc + 1], ALU.add, extra_reads=[cwt, cbt])
            P.stt(cvb, cvb[:], rw, rw[:, 1:257], cwt[:, mc, 1:2], cvb, cvb[:], ALU.mult, ALU.add, extra_reads=[cwt])
            P.stt(cvb, cvb[:], rw, rw[:, 2:258], cwt[:, mc, 2:3], cvb, cvb[:], ALU.mult, ALU.add, extra_reads=[cwt])
            for tc in range(2):
                ps = next_ps(K)
                P.op("pe", lambda e: e.transpose(ps[:, 0:128], cvb[:, tc * 128:(tc + 1) * 128], ident[:]), reads=[cvb, ident], writes=[ps])
                P.copy("act", xtm[m], xtm[m][:, tc, cq * 128:(cq + 1) * 128], ps, ps[:, 0:128])
    Yr = sb([128, 4, 512], F32, "hc_Yr"); Yi = sb([128, 4, 512], F32, "hc_Yi")
    ya = sb([128, 2, 512], F32, "hc_ya"); yb_ = sb([128, 2, 512], F32, "hc_yb")
    t1 = sb([128, 512], F32, "hc_t1"); t2 = sb([128, 512], F32, "hc_t2")
    ycur = xtm[0]
    for o in range(2):
        for fc in range(4):
            pr, pi_ = dft(ycur, fc)
            P.tt("dve", t1, t1[:], pr, pr[:, :], KFr[o], KFr[o][:, fc, :], ALU.mult)
            P.tt("dve", t2, t2[:], pi_, pi_[:, :], KFi[o], KFi[o][:, fc, :], ALU.mult)
            P.tt("pool", Yr, Yr[:, fc, :], t1, t1[:], t2, t2[:], ALU.subtract)
            P.tt("dve", t1, t1[:], pr, pr[:, :], KFi[o], KFi[o][:, fc, :], ALU.mult)
            P.tt("dve", t2, t2[:], pi_, pi_[:, :], KFr[o], KFr[o][:, fc, :], ALU.mult)
            P.tt("pool", Yi, Yi[:, fc, :], t1, t1[:], t2, t2[:], ALU.add)
        dst = ya if o == 0 else yb_
        for tc in range(2):
            py = next_ps(K)
            for fc in range(4):
                P.mm(py, py[:, :], Ic[:, fc, tc * 128:(tc + 1) * 128], Yr[:, fc, :], fc == 0, False, reads=[Ic, Yr])
                P.mm(py, py[:, :], Ins[:, fc, tc * 128:(tc + 1) * 128], Yi[:, fc, :], False, fc == 3, reads=[Ins, Yi])
            P.tt("dve", t1, t1[:], ycur, ycur[:, tc, :], hbb, hbb[:, o, :], ALU.mult)
            P.tt("dve", t1, t1[:], t1, t1[:], py, py[:, :], ALU.add)
            P.tt("dve", dst, dst[:, tc, :], t1, t1[:], xtm[1 + o], xtm[1 + o][:, tc, :], ALU.mult)
        ycur = dst
    ofm = sb([128, 4, 256], F32, "hc_ofm")
    for cq in range(4):
        for tc in range(2):
            ps = next_ps(K)
            P.op("pe", lambda e: e.transpose(ps[:, 0:128], ycur[:, tc, cq * 128:(cq + 1) * 128], ident[:]), reads=[ycur, ident], writes=[ps])
            P.copy("act", ofm, ofm[:, cq, tc * 128:(tc + 1) * 128], ps, ps[:, 0:128])
    P.dma("sp", mixT.ap[0:512, t_base:t_base + L].rearrange("(k p) t -> p k t", p=128), ofm[:], reads=[ofm], writes=[mixT])
    P.release_to(mark)


_DBG_SKIP_ATTN = False
MLA_SCALE = 96.0 ** -0.5
GQA_SCALE = 64.0 ** -0.5
GRID_W = 64
ROPE_BASE = 10000.0


def odd_segs():
    segs = [(0, 1184), (392, 8), (384, 8), (408, 8), (400, 8)]
    for hq in range(8):
        b = 416 + 64 * hq
        segs += [(b + 16, 16), (b, 16), (b + 48, 16), (b + 32, 16)]
    for kh in range(2):
        b = 928 + 64 * kh
        segs += [(b + 16, 16), (b, 16), (b + 48, 16), (b + 32, 16)]
    return segs


def rope_consts(L):
    out = {}
    rows = L // GRID_W
    row = np.repeat(np.arange(rows), GRID_W).astype(np.float32)
    col = np.tile(np.arange(GRID_W), rows).astype(np.float32)
    for dim, nm in ((32, "mla"), (64, "gqa")):
        nf = dim // 4
        inv = (np.float32(ROPE_BASE) ** (-np.arange(nf, dtype=np.float32) / np.float32(nf))).astype(np.float32)
        ar = (row[:, None] * inv[None, :]).astype(np.float32)
        ac = (col[:, None] * inv[None, :]).astype(np.float32)
        cr, sr, cc, sc = np.cos(ar), np.sin(ar), np.cos(ac), np.sin(ac)
        C = np.concatenate([cr, cr, cc, cc], axis=1).T
        S = np.concatenate([-sr, sr, -sc, sc], axis=1).T
        out[f"rope_{nm}_C"] = np.ascontiguousarray(C, dtype=np.float32)
        out[f"rope_{nm}_S"] = np.ascontiguousarray(S, dtype=np.float32)
    j = np.arange(128)[:, None]; r = np.arange(128)[None, :]
    out["k_mask_prev"] = np.tile((j >= r).astype(np.float32), (1, 4))
    out["k_mask_next"] = np.tile((j <= r).astype(np.float32), (1, 4))
    selM = np.zeros((96, 97), np.float32); selM[:, 96] = 1.0
    selG = np.zeros((64, 65), np.float32); selG[:, 64] = 1.0
    sel65 = np.zeros((65, 64), np.float32); sel65[64, :] = 1.0
    vs = np.zeros((1, 65), np.float32); vs[0, 64] = 1.0
    out["k_selM"] = selM; out["k_selG"] = selG; out["k_sel65"] = sel65; out["k_vsink"] = vs
    return out


def attn_core(K, A, q_t, q_ap, nq, chunks, outs):
    P = K.P
    pot = A.pot
    n = len(chunks)
    LOOK = 2

    def score(ci):
        k_t, k_ap, v_t, v_ap, mask, cols, qrows = chunks[ci]
        c0, ncol = cols if cols is not None else (0, nq)
        nk = k_ap.shape[1]
        ps = next_ps(K)
        q_use = q_ap[:, c0:c0 + ncol] if qrows is None else q_ap[qrows[0]:qrows[1], c0:c0 + ncol]
        P.mm(ps, ps[:nk, c0:c0 + ncol], k_ap, q_use, True, True, reads=[k_t, q_t])
        return ps

    pend = {}
    for ci in range(min(LOOK, n)):
        pend[ci] = score(ci)
    for ci, (k_t, k_ap, v_t, v_ap, mask, cols, qrows) in enumerate(chunks):
        c0, ncol = cols if cols is not None else (0, nq)
        nk = k_ap.shape[1]
        ps = pend.pop(ci)
        pt = A.pt[A.pti % len(A.pt)]
        A.pti += 1
        P.act(pt, pt[:nk, c0:c0 + ncol], ps, ps[:nk, c0:c0 + ncol], AF.Exp)
        if mask is not None:
            P.tt("dve", pt, pt[:nk, c0:c0 + ncol], pt, pt[:nk, c0:c0 + ncol], mask[0], mask[1], ALU.mult)
        if ci + LOOK < n:
            pend[ci + LOOK] = score(ci + LOOK)
        P.mm(pot, pot[:65, c0:c0 + ncol], v_ap, pt[:nk, c0:c0 + ncol], ci == 0, ci == n - 1, reads=[v_t, pt])
    P.copy("act", A.ot, A.ot[:65, :nq], pot, pot[:65, :nq])
    ps = next_ps(K)
    P.mm(ps, ps[:64, :nq], A.sel65[:, :], A.ot[:65, :nq], True, True, reads=[A.sel65, A.ot])
    P.op("dve", lambda e: e.reciprocal(out=A.rden[:64, :nq], in_=ps[:64, :nq]), reads=[ps], writes=[A.rden])
    ob = A.ob[A.obi % 2]
    A.obi += 1
    P.tt("dve", ob, ob[:64, :nq], A.ot, A.ot[:64, :nq], A.rden, A.rden[:64, :nq], ALU.mult)
    for (dap, c0, ncol) in outs:
        P.dma("pool", dap, ob[:64, c0:c0 + ncol], reads=[ob], writes=[A.mixT] + ([A.mixH] if A.mixH is not None else []))


def odd_mixer_stage(K, i, I, zT, mixT, C, S, need_ctx, qsel=None):
    P = K.P
    nc = P.nc
    sb = P.sb
    NT, NCX = K.NT, K.NC
    mark = P.mark()
    tiles = ([(0, NCX, 1)] if NCX else []) + [(NCX + k * 512, min(512, K.NL - k * 512), 0) for k in range((K.NL + 511) // 512)]
    wst = sb([128, 1024], F32, "od_wst")
    wukv = sb([128, 1024], BF16, "od_wukv")
    P.dma("sp", wst[:], I["mla_w_ukv"].ap[i], reads=[I["mla_w_ukv"]], writes=[wst])
    P.copy("pool", wukv, wukv[:], wst, wst[:])
    wuq = sb([128, 2, 768], BF16, "od_wuq"); wuqs = sb([128, 2, 768], BF16, "od_wuqs")
    wst2 = sb([128, 2, 768], F32, "od_wst2")
    P.dma("sp", wst2[:], I["mla_w_uq"].ap[i].rearrange("(k p) n -> p k n", p=128), reads=[I["mla_w_uq"]], writes=[wst2])
    P.copy("pool", wuq, wuq[:], wst2, wst2[:])
    P.memset("pool", wuqs, wuqs[:], 0.0)
    for h in range(8):
        b = h * 96 + 64
        for (dst, src) in ((0, 8), (8, 0), (16, 24), (24, 16)):
            P.copy("pool", wuqs, wuqs[:, :, b + dst:b + dst + 8], wst2, wst2[:, :, b + src:b + src + 8])
    gq = sb([128, 2], F32, "od_gq"); gkv = sb([128, 1], F32, "od_gkv")
    sinkb = sb([96, 8], F32, "od_sink")
    with nc.allow_non_contiguous_dma(reason="tiny vector loads"):
        P.dma("sp", gq[:], I["mla_q_norm"].ap[i].rearrange("(k p) -> p k", p=128), reads=[I["mla_q_norm"]], writes=[gq])
        P.dma("sp", gkv[:], I["mla_kv_norm"].ap[i].rearrange("(p o) -> p o", o=1), reads=[I["mla_kv_norm"]], writes=[gkv])
        P.dma("sp", sinkb[64:96, :], I["gqa_sink"].ap[i].rearrange("(o h) -> o h", o=1).partition_broadcast(32),
              reads=[I["gqa_sink"]], writes=[sinkb])
    selM, selG, sel65, vsink = C["k_selM"], C["k_selG"], C["k_sel65"], C["k_vsink"]
    ident = C["k_ident"]
    vsb = sb([1, 65], BF16, "od_vsb")
    P.copy("dve", vsb, vsb[:], vsink, vsink[:])
    kmaxM = sb([128, 8], F32, "od_kmaxM"); kmaxG = sb([128, 2], F32, "od_kmaxG")
    P.memset("pool", kmaxM, kmaxM[:], 0.0)
    P.memset("pool", kmaxG, kmaxG[:], 0.0)
    mx1 = sb([128, 1], F32, "od_mx1")
    markp = P.mark()
    xin = [sb([128, 2, 512], F32, f"od_xin{k}") for k in range(2)]
    sq = sb([128, 2, 512], F32, "od_sq"); rstd = sb([128, 512], F32, "od_rstd")
    kvn = sb([128, 512], BF16, "od_kvn"); cqn = sb([128, 2, 512], BF16, "od_cqn")
    raw = [sb([128, 512], F32, f"od_raw{k}") for k in range(2)]; sw = [sb([128, 512], F32, f"od_sw{k}") for k in range(2)]
    tC = [sb([128, 512], F32, f"od_tC{k}") for k in range(2)]; tS = [sb([128, 512], F32, f"od_tS{k}") for k in range(2)]
    rot = sb([128, 512], F32, "od_rot"); rt2 = sb([128, 512], F32, "od_rt2")
    kf = sb([96, 512], F32, "od_kf"); kf2 = sb([96, 512], F32, "od_kf2")
    kt = [sb([97, 512], BF16, f"od_kt{k}") for k in range(2)]
    vt = [sb([128, 8, 65], BF16, f"od_vt{k}") for k in range(2)]
    vg = [sb([128, 2, 65], BF16, f"od_vg{k}") for k in range(2)]
    gvt = sb([128, 512], F32, "od_gvt")
    for k in range(2):
        P.memset("pool", kt[k], kt[k][96:97, :], 1.0)
        P.memset("pool", vt[k], vt[k][:], 1.0)
        P.memset("pool", vg[k], vg[k][:], 1.0)

    def rms(x_t, x_ap_k, nk_, n, g_t, g_ap_k, out_t, out_ap_k):
        ps = next_ps(K)
        for k in range(nk_):
            P.act(sq, sq[:, k, :n], x_t, x_ap_k(k), AF.Square)
        for k in range(nk_):
            P.mm(ps, ps[:, :n], K.ones[:], sq[:, k, :n], k == 0, k == nk_ - 1, reads=[K.ones, sq])
        P.ts("dve", rstd, rstd[:, :n], ps, ps[:, :n], 1.0 / (128 * nk_), ALU.mult, EPS, ALU.add)
        P.op("act", lambda e: e.sqrt(out=rstd[:, :n], in_=rstd[:, :n]), reads=[rstd], writes=[rstd])
        P.op("dve", lambda e: e.reciprocal(out=rstd[:, :n], in_=rstd[:, :n]), reads=[rstd], writes=[rstd])
        for k in range(nk_):
            P.stt(out_t, out_ap_k(k), x_t, x_ap_k(k), g_ap_k(k), rstd, rstd[:, :n], ALU.mult, ALU.mult, extra_reads=[g_t])

    def sumsq_max(src_t, src_ap, nrows, n, sel, dstcol_t, dstcol_ap, prow):
        P.act(kf2, kf2[:nrows, :n], src_t, src_ap, AF.Square)
        ps = next_ps(K)
        P.mm(ps, ps[:prow + 1, :n], sel[:nrows, :prow + 1], kf2[:nrows, :n], True, True, reads=[sel, kf2])
        P.op("dve", lambda e: e.reduce_max(out=mx1[prow:prow + 1, :], in_=ps[prow:prow + 1, :n], axis=AX.X), reads=[ps], writes=[mx1])
        P.tt("dve", dstcol_t, dstcol_ap, dstcol_t, dstcol_ap, mx1, mx1[prow:prow + 1, :], ALU.max)

    for it, (t0, n, is_ctx) in enumerate(tiles):
        x = xin[it % 2]
        lat0 = t0 - NCX
        P.dma("sp", x[:, 0, :n], zT.ap[256:384, t0:t0 + n], reads=[zT], writes=[x])
        rms(x, lambda k: x[:, 0, :n], 1, n, gkv, lambda k: gkv[:, 0:1], kvn, lambda k: kvn[:, :n])
        r_, s_, c_, sn_ = raw[it % 2], sw[it % 2], tC[it % 2], tS[it % 2]
        P.dma("sp", r_[64:96, :n], zT.ap[384:416, t0:t0 + n], reads=[zT], writes=[r_])
        if not is_ctx:
            P.dma("sp", s_[64:96, :n], zT.ap[1184:1216, t0:t0 + n], reads=[zT], writes=[s_])
            P.dma("sp", c_[64:96, :n], C["rope_mla_C"].ap[:, lat0:lat0 + n], reads=[C["rope_mla_C"]], writes=[c_])
            P.dma("sp", sn_[64:96, :n], C["rope_mla_S"].ap[:, lat0:lat0 + n], reads=[C["rope_mla_S"]], writes=[sn_])
            P.tt("dve", rot, rot[64:96, :n], r_, r_[64:96, :n], c_, c_[64:96, :n], ALU.mult)
            P.tt("pool", rt2, rt2[64:96, :n], s_, s_[64:96, :n], sn_, sn_[64:96, :n], ALU.mult)
            P.tt("dve", kf, kf[64:96, :n], rot, rot[64:96, :n], rt2, rt2[64:96, :n], ALU.add)
        else:
            P.copy("dve", kf, kf[64:96, :n], r_, r_[64:96, :n])
        for h in range(8):
            ps = next_ps(K)
            P.mm(ps, ps[:64, :n], wukv[:, h * 128:h * 128 + 64], kvn[:, :n], True, True, reads=[wukv, kvn])
            P.copy("act", kf, kf[0:64, :n], ps, ps[:64, :n])
            sumsq_max(kf, kf[0:96, :n], 96, n, selM, kmaxM, kmaxM[96:97, h:h + 1], 96)
            ktb = kt[h % 2]
            P.copy("pool", ktb, ktb[0:96, :n], kf, kf[0:96, :n])
            P.dma("sp", S["KM"].ap[h, :, t0:t0 + n], ktb[:, :n], reads=[ktb], writes=[S["KM"]])
        for q in range((n + 127) // 128):
            vtb = vt[q % 2]
            for half in range(2):
                ps = next_ps(K)
                P.mm(ps, ps[:, :512], kvn[:, q * 128:(q + 1) * 128], wukv[:, half * 512:(half + 1) * 512], True, True,
                     reads=[kvn, wukv])
                P.copy("act", vtb, vtb[:, half * 4:half * 4 + 4, 0:64],
                       ps, ps[:, :512].rearrange("t (h d) -> t h d", d=128)[:, :, 64:128])
            with nc.allow_non_contiguous_dma(reason="token-major V rows"):
                P.dma("sp", S["VM"].ap[:, t0 + q * 128:t0 + (q + 1) * 128, :].rearrange("h t d -> t h d"), vtb[:],
                      reads=[vtb], writes=[S["VM"]])
        for kh in range(2):
            P.dma("sp", r_[0:64, :n], zT.ap[928 + 64 * kh:928 + 64 * kh + 64, t0:t0 + n], reads=[zT], writes=[r_])
            if not is_ctx:
                P.dma("sp", s_[0:64, :n], zT.ap[1728 + 64 * kh:1728 + 64 * kh + 64, t0:t0 + n], reads=[zT], writes=[s_])
                P.dma("sp", c_[0:64, :n], C["rope_gqa_C"].ap[:, lat0:lat0 + n], reads=[C["rope_gqa_C"]], writes=[c_])
                P.dma("sp", sn_[0:64, :n], C["rope_gqa_S"].ap[:, lat0:lat0 + n], reads=[C["rope_gqa_S"]], writes=[sn_])
                P.tt("dve", rot, rot[0:64, :n], r_, r_[0:64, :n], c_, c_[0:64, :n], ALU.mult)
                P.tt("pool", rt2, rt2[0:64, :n], s_, s_[0:64, :n], sn_, sn_[0:64, :n], ALU.mult)
                P.tt("dve", kf, kf[0:64, :n], rot, rot[0:64, :n], rt2, rt2[0:64, :n], ALU.add)
            else:
                P.copy("dve", kf, kf[0:64, :n], r_, r_[0:64, :n])
            sumsq_max(kf, kf[0:64, :n], 64, n, selG, kmaxG, kmaxG[64:65, kh:kh + 1], 64)
            ktb = kt[kh % 2]
            P.copy("pool", ktb, ktb[0:64, :n], kf, kf[0:64, :n])
            P.memset("pool", ktb, ktb[64:96, :n], 0.0)
            P.memset("pool", ktb, ktb[64:65, :n], 1.0)
            P.dma("sp", S["KG"].ap[kh, :, t0:t0 + n], ktb[0:66, :n], reads=[ktb], writes=[S["KG"]])
            P.memset("pool", ktb, ktb[96:97, :n], 1.0)
        P.dma("sp", gvt[:, :n], zT.ap[1056:1184, t0:t0 + n], reads=[zT], writes=[gvt])
        for q in range((n + 127) // 128):
            ps = next_ps(K)
            P.op("pe", lambda e: e.transpose(ps[:, 0:128], gvt[:, q * 128:(q + 1) * 128], ident[:]), reads=[gvt, ident], writes=[ps])
            vgb = vg[q % 2]
            P.copy("act", vgb, vgb[:, :, 0:64], ps, ps[:, 0:128].rearrange("t (h d) -> t h d", d=64))
            with nc.allow_non_contiguous_dma(reason="token-major V rows"):
                P.dma("sp", S["VG"].ap[:, t0 + q * 128:t0 + (q + 1) * 128, :].rearrange("h t d -> t h d"), vgb[:],
                      reads=[vgb], writes=[S["VG"]])
    qf = sb([96, 512], F32, "od_qf")
    qt = [sb([97, 512], BF16, f"od_qt{k}") for k in range(2)]
    prod = sb([128, 512], F32, "od_prod")
    for it, (t0, n, is_ctx) in enumerate(tiles):
        if is_ctx and not need_ctx:
            continue
        x = xin[it % 2]
        lat0 = t0 - NCX
        P.dma("sp", x[:, :, :n], zT.ap[0:256, t0:t0 + n].rearrange("(k p) t -> p k t", p=128), reads=[zT], writes=[x])
        rms(x, lambda k: x[:, k, :n], 2, n, gq, lambda k: gq[:, k:k + 1], cqn, lambda k: cqn[:, k, :n])
        c_, sn_ = tC[it % 2], tS[it % 2]
        if not is_ctx:
            P.dma("sp", c_[64:96, :n], C["rope_mla_C"].ap[:, lat0:lat0 + n], reads=[C["rope_mla_C"]], writes=[c_])
            P.dma("sp", sn_[64:96, :n], C["rope_mla_S"].ap[:, lat0:lat0 + n], reads=[C["rope_mla_S"]], writes=[sn_])
        for h in range(8):
            ps = next_ps(K)
            for k in range(2):
                P.mm(ps, ps[:96, :n], wuq[:, k, h * 96:(h + 1) * 96], cqn[:, k, :n], k == 0, k == 1, reads=[wuq, cqn])
            if not is_ctx:
                ps2 = next_ps(K)
                for k in range(2):
                    P.mm(ps2, ps2[:96, :n], wuqs[:, k, h * 96:(h + 1) * 96], cqn[:, k, :n], k == 0, k == 1, reads=[wuqs, cqn])
                P.tt("dve", rot, rot[64:96, :n], ps, ps[64:96, :n], c_, c_[64:96, :n], ALU.mult)
                P.tt("dve", rt2, rt2[64:96, :n], ps2, ps2[64:96, :n], sn_, sn_[64:96, :n], ALU.mult)
                P.stt(qf, qf[64:96, :n], rot, rot[64:96, :n], 1.0, rt2, rt2[64:96, :n], ALU.mult, ALU.add)
                P.ts("dve", qf, qf[64:96, :n], qf, qf[64:96, :n], MLA_SCALE, ALU.mult)
                P.op("act", lambda e: e.mul(out=qf[0:64, :n], in_=ps[0:64, :n], mul=MLA_SCALE), reads=[ps], writes=[qf])
            else:
                P.op("act", lambda e: e.mul(out=qf[0:96, :n], in_=ps[0:96, :n], mul=MLA_SCALE), reads=[ps], writes=[qf])
            qtb = qt[h % 2]
            P.copy("pool", qtb, qtb[0:96, :n], qf, qf[0:96, :n])
            P.act(kf2, kf2[:96, :n], qf, qf[0:96, :n], AF.Square)
            psq = next_ps(K)
            P.mm(psq, psq[:97, :n], selM[:, :], kf2[:96, :n], True, True, reads=[selM, kf2])
            P.ts("dve", prod, prod[96:97, :n], psq, psq[96:97, :n], kmaxM[96:97, h:h + 1], ALU.mult, extra_reads=[kmaxM])
            P.op("act", lambda e: e.sqrt(out=prod[96:97, :n], in_=prod[96:97, :n]), reads=[prod], writes=[prod])
            P.ts("dve", qtb, qtb[96:97, :n], prod, prod[96:97, :n], -1.0, ALU.mult)
            P.dma("sp", S["QM"].ap[h, :, t0:t0 + n], qtb[:, :n], reads=[qtb], writes=[S["QM"]])
        r_, s_ = raw[it % 2], sw[it % 2]
        if not is_ctx:
            P.dma("sp", c_[0:64, :n], C["rope_gqa_C"].ap[:, lat0:lat0 + n], reads=[C["rope_gqa_C"]], writes=[c_])
            P.dma("sp", sn_[0:64, :n], C["rope_gqa_S"].ap[:, lat0:lat0 + n], reads=[C["rope_gqa_S"]], writes=[sn_])
        for hq in range(8):
            P.dma("sp", r_[0:64, :n], zT.ap[416 + 64 * hq:416 + 64 * hq + 64, t0:t0 + n], reads=[zT], writes=[r_])
            if not is_ctx:
                P.dma("sp", s_[0:64, :n], zT.ap[1216 + 64 * hq:1216 + 64 * hq + 64, t0:t0 + n], reads=[zT], writes=[s_])
                P.tt("dve", rot, rot[0:64, :n], r_, r_[0:64, :n], c_, c_[0:64, :n], ALU.mult)
                P.tt("pool", rt2, rt2[0:64, :n], s_, s_[0:64, :n], sn_, sn_[0:64, :n], ALU.mult)
                P.stt(qf, qf[0:64, :n], rot, rot[0:64, :n], 1.0, rt2, rt2[0:64, :n], ALU.mult, ALU.add)
                P.ts("dve", qf, qf[0:64, :n], qf, qf[0:64, :n], GQA_SCALE, ALU.mult)
            else:
                P.ts("dve", qf, qf[0:64, :n], r_, r_[0:64, :n], GQA_SCALE, ALU.mult)
            qtb = qt[hq % 2]
            P.copy("pool", qtb, qtb[0:64, :n], qf, qf[0:64, :n])
            P.memset("pool", qtb, qtb[64:96, :n], 1.0)
            P.act(kf2, kf2[:64, :n], qf, qf[0:64, :n], AF.Square)
            psq = next_ps(K)
            P.mm(psq, psq[:65, :n], selG[:, :], kf2[:64, :n], True, True, reads=[selG, kf2])
            P.ts("dve", prod, prod[64:65, :n], psq, psq[64:65, :n], kmaxG[64:65, hq // 4:hq // 4 + 1], ALU.mult, extra_reads=[kmaxG])
            P.op("act", lambda e: e.sqrt(out=prod[64:65, :n], in_=prod[64:65, :n]), reads=[prod], writes=[prod])
            P.ts("dve", qtb, qtb[64:65, :n], prod, prod[64:65, :n], -1.0, ALU.mult)
            P.dma("sp", S["QG"].ap[hq, :, t0:t0 + n], qtb[0:66, :n], reads=[qtb], writes=[S["QG"]])
    P.release_to(markp)
    if _DBG_SKIP_ATTN:
        P.release_to(mark)
        return
    A = Ctx()
    A.mixT = mixT
    A.mixH = qsel[2] if qsel is not None else None
    A.sel65 = sel65
    K.ps_reserved = {7}
    A.pot = K.ps[7]
    A.pt = [sb([128, 512], BF16, f"at_pt{k}") for k in range(4)]
    A.pti = 0
    A.ot = sb([65, 512], F32, "at_ot"); A.rden = sb([64, 512], F32, "at_rden")
    A.ob = [sb([64, 512], F32, f"at_ob{k}") for k in range(2)]
    A.obi = 0
    NKC = NT // 128
    kres = sb([97, NT], BF16, "at_k"); vres = sb([128, NKC, 65], BF16, "at_v")
    qb_ = [sb([97, 512], BF16, f"at_q{k}") for k in range(2)]
    qb2_ = [sb([97, 512], BF16, f"at_q2{k}") for k in range(2)] if qsel is not None else None
    qi = 0
    for h in range(8):
        P.dma("sp", kres[:, :], S["KM"].ap[h, :, :], reads=[S["KM"]], writes=[kres])
        with nc.allow_non_contiguous_dma(reason="token-major V rows"):
            P.dma("sp", vres[:], S["VM"].ap[h].rearrange("(c p) d -> p c d", p=128), reads=[S["VM"]], writes=[vres])
        qlist = [(NCX + k * 512, min(512, K.NL - k * 512), False) for k in range((K.NL + 511) // 512)]
        if need_ctx and NCX:
            qlist = [(0, NCX, True)] + qlist
        if qsel is not None:
            selt, HALF_, mixH = qsel
            for k in range(HALF_ // 512):
                qb = qb_[qi % 2]
                qb2 = qb2_[qi % 2]
                qi += 1
                P.dma("sp", qb[:, :], S["QM"].ap[h, :, NCX + k * 512:NCX + (k + 1) * 512], reads=[S["QM"]], writes=[qb])
                P.dma("sp", qb2[:, :], S["QM"].ap[h, :, NCX + HALF_ + k * 512:NCX + HALF_ + (k + 1) * 512], reads=[S["QM"]], writes=[qb2])
                P.ts("dve", qb, qb[:, :], qb, qb[:, :], selt[:97, 0:1], ALU.mult, extra_reads=[selt])
                P.stt(qb, qb[:, :], qb2, qb2[:, :], selt[:97, 1:2], qb, qb[:, :], ALU.mult, ALU.add, extra_reads=[selt])
                chunks = [(kres, kres[:, kc * 128:(kc + 1) * 128], vres, vres[:, kc, :], None, None, None) for kc in range(NKC)]
                attn_core(K, A, qb, qb[:, :], 512, chunks, [(mixH.ap[h * 64:(h + 1) * 64, k * 512:(k + 1) * 512], 0, 512)])
            continue
        for (t0, n, is_ctx) in qlist:
            qb = qb_[qi % 2]
            qi += 1
            P.dma("sp", qb[:, :n], S["QM"].ap[h, :, t0:t0 + n], reads=[S["QM"]], writes=[qb])
            kcs = range(NCX // 128) if is_ctx else range(NKC)
            chunks = [(kres, kres[:, kc * 128:(kc + 1) * 128], vres, vres[:, kc, :], None, None, None) for kc in kcs]
            attn_core(K, A, qb, qb[:, :n], n, chunks, [(mixT.ap[h * 64:(h + 1) * 64, t0:t0 + n], 0, n)])
    mprev, mnext = C["k_mask_prev"], C["k_mask_next"]
    ks2 = sb([96, 8], BF16, "at_ks2")
    P.copy("dve", ks2, ks2[64:96, :], sinkb, sinkb[64:96, :])
    P.memset("pool", ks2, ks2[64:65, :], 1.0)
    NB = K.NL // 128
    for kh in range(2):
        P.dma("sp", kres[0:66, :], S["KG"].ap[kh, :, :], reads=[S["KG"]], writes=[kres])
        with nc.allow_non_contiguous_dma(reason="token-major V rows"):
            P.dma("sp", vres[:], S["VG"].ap[kh].rearrange("(c p) d -> p c d", p=128), reads=[S["VG"]], writes=[vres])
        blocks = [(NCX + b * 128, b, False) for b in range(NB)]
        if need_ctx and NCX:
            blocks = [(cb * 128, cb, True) for cb in range(NCX // 128)] + blocks
        for (t0, b, is_ctx) in blocks:
            qb = qb_[qi % 2]
            qi += 1
            P.dma("sp", qb[0:66, :].rearrange("r (g t) -> r g t", t=128),
                  S["QG"].ap[4 * kh:4 * kh + 4, :, t0:t0 + 128].rearrange("g r t -> r g t"), reads=[S["QG"]], writes=[qb])
            chunks = []
            for cb in range(NCX // 128):
                chunks.append((kres, kres[0:66, cb * 128:(cb + 1) * 128], vres, vres[:, cb, :], None, None, None))
            if not is_ctx:
                kc0 = NCX // 128 + b
                if b > 0:
                    chunks.append((kres, kres[0:66, (kc0 - 1) * 128:kc0 * 128], vres, vres[:, kc0 - 1, :], (mprev, mprev[:, :]), None, None))
                chunks.append((kres, kres[0:66, kc0 * 128:(kc0 + 1) * 128], vres, vres[:, kc0, :], None, None, None))
                if b < NB - 1:
                    chunks.append((kres, kres[0:66, (kc0 + 1) * 128:(kc0 + 2) * 128], vres, vres[:, kc0 + 1, :], (mnext, mnext[:, :]), None, None))
            for g in range(4):
                hq = 4 * kh + g
                chunks.append((ks2, ks2[64:66, hq:hq + 1], vsb, vsb[0:1, :], None, (g * 128, 128), (64, 66)))
            outs = [(mixT.ap[512 + (4 * kh + g) * 64:512 + (4 * kh + g + 1) * 64, t0:t0 + 128], g * 128, 128) for g in range(4)]
            attn_core(K, A, qb, qb[0:66, :], 512, chunks, outs)
    K.ps_reserved = set()
    P.release_to(mark)


NCTX_FULL, NLAT_FULL, DEPTH = 256, 8192, 4
_W_SHAPES = dict(
    c_ctx=[D], mod_w=[DEPTH, D, 6 * D], mod_b=[DEPTH, 6 * D],
    norm_mix=[DEPTH, D], norm_ffn=[DEPTH, D], final_norm=[D],
    ev_w_in=[2, D, 2048], ev_conv_w=[2, 3, 1536], ev_conv_b=[2, 1536],
    hy_w1=[2, 33, 64], hy_b1=[2, 64], hy_w2=[2, 64, 64], hy_b2=[2, 64], hy_w3=[2, 64, 2048], hy_freq=[2, 64],
    hy_decay=[2, 2, 512], hy_bias=[2, 2, 512],
    s5_a_re=[2, 2, 32, 64], s5_a_im=[2, 2, 32, 64], s5_log_dt=[2, 2, 32],
    s5_b_re=[2, 2, 32, 64, 16], s5_b_im=[2, 2, 32, 64, 16], s5_c_re=[2, 2, 32, 16, 64], s5_c_im=[2, 2, 32, 16, 64],
    s5_d=[2, 512], s5_w_glu=[2, 512, 512], ev_w_out=[2, D, D],
    ff_w_gate=[2, D, 2816], ff_w_up=[2, D, 2816], ff_w_down=[2, 2816, D],
    od_w_in=[2, D, 1184], mla_q_norm=[2, 256], mla_w_uq=[2, 256, 768], mla_kv_norm=[2, 128], mla_w_ukv=[2, 128, 1024],
    gqa_sink=[2, 8], od_w_out=[2, D, D],
    moe_router=[2, D, 8], moe_w_gate=[2, 8, D, 3584], moe_w_up=[2, 8, D, 3584], moe_w_down=[2, 8, 3584, D],
)
_DRAM_ONLY_CONSTS = ("rope_mla_C", "rope_mla_S", "rope_gqa_C", "rope_gqa_S", "hy8192_featsT", "hy256_featsT",
                     "hc_Fc", "hc_Fns", "hc_Ic", "hc_Ins")


def all_consts():
    c = {}
    c.update(host_consts())
    c.update(hyena_consts(NLAT_FULL))
    c["hy256_featsT"] = hyena_consts(NCTX_FULL)["hy256_featsT"]
    c.update(hyena_ctx_consts())
    c.update(hyena_shared_consts())
    c.update(rope_consts(NLAT_FULL))
    return c


def build_program():
    P = Prog()
    nc = P.nc
    K = setup_common(P, NCTX_FULL, NLAT_FULL)
    NT = K.NT
    inp = lambda k, shp: T(nc.dram_tensor(k, list(shp), F32, kind="ExternalInput").ap())
    I = {k: inp(k, v) for k, v in _W_SHAPES.items()}
    I["hin"] = inp("hin", [D, NT])
    I["c"] = inp("c", [D])
    consts = all_consts()
    Cd = {k: inp(k, v.shape) for k, v in consts.items()}
    HALF = NLAT_FULL // 2
    I["sel"] = inp("sel", [128, 2])
    outT = T(nc.dram_tensor("outT", [D, HALF], F32, kind="ExternalOutput").ap())
    hT2 = P.dram("hT2", [D, HALF])
    mixH = P.dram("mixH", [512, HALF])
    zT = P.dram("zT", [2048, NT])
    mixT = P.dram("mixT", [D, NT])
    K.yS5 = P.dram("yS5", [512, NT])
    kT = P.dram("kT", [2048, NLAT_FULL])
    KFr = P.dram("KFr", [256, 128, 512])
    KFi = P.dram("KFi", [256, 128, 512])
    S = dict(QM=P.dram("QM", [8, 97, NT], BF16), KM=P.dram("KM", [8, 97, NT], BF16), VM=P.dram("VM", [8, NT, 65], BF16),
             QG=P.dram("QG", [8, 66, NT], BF16), KG=P.dram("KG", [2, 66, NT], BF16), VG=P.dram("VG", [2, NT, 65], BF16))

    def load_consts(keys):
        C = {}
        for k in keys:
            d = Cd[k]
            if k in _DRAM_ONLY_CONSTS:
                C[k + "_dram" if k.endswith("featsT") else k] = d
            else:
                t = P.sb(list(d.ap.shape), F32, "c_" + k)
                P.dma("sp", t[:], d.ap[:, :], reads=[d], writes=[t])
                C[k] = t
        return C

    for i0 in range((NT + 1023) // 1024):
        n = min(1024, NT - i0 * 1024)
        P.dma("sp", K.hT.ap[:, i0 * 1024:i0 * 1024 + n], I["hin"].ap[:, i0 * 1024:i0 * 1024 + n], reads=[I["hin"]], writes=[K.hT])
    mods = mods_all(K, list(range(DEPTH)), I["c"], I["c_ctx"], I["mod_w"], I["mod_b"], I["norm_mix"], I["norm_ffn"])
    for l in range(DEPTH):
        i = l // 2
        need_ctx = l < DEPTH - 1
        if l % 2 == 0:
            inproj_stage(K, mods[l], T(None, I["ev_w_in"].ap[i]), zT)
            m = P.mark()
            C = load_consts([k for k in Cd if k.startswith(f"hy{NLAT_FULL}_") or k.startswith("hy_")])
            hyena_stage(K, i, I, zT, mixT, C, NLAT_FULL, NCTX_FULL, KFr, KFi, kT, f"l{l}")
            P.release_to(m)
            m = P.mark()
            C = load_consts(["hy_tau512", "k_ident", f"hy{NCTX_FULL}_featsT"])
            hyena_ctx_stage(K, i, I, zT, mixT, C, Cd, 0)
            P.release_to(m)
            m = P.mark()
            C = load_consts(["k_tau", "k_maskB", "k_maskC", "k_ident"])
            s5_stage(K, i, I, zT, mixT, C)
            P.release_to(m)
            outproj_stage(K, mods[l], T(None, I["ev_w_out"].ap[i]), mixT)
            experts = [(T(None, I["ff_w_gate"].ap[i]), T(None, I["ff_w_up"].ap[i]), T(None, I["ff_w_down"].ap[i]))]
            ffn_stage(K, mods[l], experts, router=None, ST=1024)
        else:
            inproj_stage(K, mods[l], T(None, I["od_w_in"].ap[i]), zT, segs=odd_segs())
            m = P.mark()
            C = load_consts(["k_ident", "k_mask_prev", "k_mask_next", "k_selM", "k_selG", "k_sel65", "k_vsink",
                             "rope_mla_C", "rope_mla_S", "rope_gqa_C", "rope_gqa_S"])
            if l < DEPTH - 1:
                odd_mixer_stage(K, i, I, zT, mixT, C, S, need_ctx)
                P.release_to(m)
                outproj_stage(K, mods[l], T(None, I["od_w_out"].ap[i]), mixT)
            else:
                selq = P.sb([128, 2], F32, "selq")
                P.dma("sp", selq[:], I["sel"].ap[:, :], reads=[I["sel"]], writes=[selq])
                odd_mixer_stage(K, i, I, zT, mixT, C, S, need_ctx, qsel=(selq, HALF, mixH))
                P.release_to(m)
                outproj_half_stage(K, mods[l], T(None, I["od_w_out"].ap[i]), mixT, mixH, I["sel"], hT2, HALF)
            experts = [(T(None, I["moe_w_gate"].ap[i, e]), T(None, I["moe_w_up"].ap[i, e]), T(None, I["moe_w_down"].ap[i, e]))
                       for e in range(8)]
            if l < DEPTH - 1:
                ffn_stage(K, mods[l], experts, router=T(None, I["moe_router"].ap[i]), ST=1024)
            else:
                K2 = Ctx()
                K2.__dict__.update(K.__dict__)
                K2.hT, K2.NC, K2.NL, K2.NT = hT2, 0, HALF, HALF
                ffn_stage(K2, mods[l], experts, router=T(None, I["moe_router"].ap[i]), ST=1024,
                          tok_ranges=[(k * 1024, 1024, 0) for k in range(HALF // 1024)])
                final_norm_stage(K2, I["final_norm"], outT, 0, HALF)
    P.finish()
    return P


def kernel(**inputs):
    x = np.asarray(inputs["x"], dtype=np.float32)
    ctx = np.asarray(inputs["ctx"], dtype=np.float32)
    B = x.shape[0]
    P = build_program()
    shared = {k: np.ascontiguousarray(np.asarray(inputs[k], dtype=np.float32)) for k in _W_SHAPES}
    shared.update(all_consts())
    in_maps = []
    for core in range(8):
        b = core // 2
        m = dict(shared)
        m["hin"] = np.ascontiguousarray(np.concatenate([ctx[b], x[b]], axis=0).T)
        m["c"] = np.ascontiguousarray(np.asarray(inputs["c"], dtype=np.float32)[b])
        sel = np.zeros((128, 2), np.float32)
        sel[:, core % 2] = 1.0
        m["sel"] = sel
        in_maps.append(m)
    res = run_bass_kernel_spmd(P.nc, in_maps, core_ids=list(range(8)))
    out = np.empty((B, NLAT_FULL, D), dtype=np.float32)
    half = NLAT_FULL // 2
    for b in range(B):
        out[b, :half] = res.results[2 * b]["outT"].T
        out[b, half:] = res.results[2 * b + 1]["outT"].T
    return out
```

```python
import math
import numpy as np
import concourse.bass as bass
import concourse.mybir as mybir
from concourse.bass_utils import run_bass_kernel_spmd

F32 = mybir.dt.float32
BF16 = mybir.dt.bfloat16
I32 = mybir.dt.int32
AF = mybir.ActivationFunctionType
ALU = mybir.AluOpType
AX = mybir.AxisListType

D = 1024
KC = D // 128
EPS = 1e-6
HY_WINDOW_SHIFT = 0.05
HY_DT = BF16


class Trk:
    __slots__ = ("w", "r")

    def __init__(self):
        self.w = None
        self.r = {}


class T:
    def __init__(self, t, ap=None):
        self.t = t
        self.ap = ap if ap is not None else t
        self.whole = Trk()
        self.parts = {}

    def __getitem__(self, idx):
        return self.ap[idx]

    def p(self, key):
        return (self, key)


def _trks(x, for_write):
    if isinstance(x, tuple):
        t, key = x
        if key not in t.parts:
            t.parts[key] = Trk()
        return [t.whole, t.parts[key]], [t.parts[key]]
    return [x.whole] + list(x.parts.values()), [x.whole]


class Prog:
    NSLOT = 6

    def __init__(self):
        nc = bass.Bass("TRN2", target_bir_lowering=False)
        self.nc = nc
        self.E = dict(pe=nc.tensor, dve=nc.vector, act=nc.scalar, pool=nc.gpsimd, sp=nc.sync)
        self.sem = {}
        self.val = {}
        for e in ("pe", "dve", "act", "pool"):
            self.sem[e] = nc.semaphore("sem_" + e).__enter__()
            self.val[e] = 0
        self.slots = {}
        self.rr = {}
        for q in ("sp", "act", "pool"):
            self.slots[q] = []
            self.rr[q] = 0
            for i in range(self.NSLOT):
                k = f"dma_{q}{i}"
                self.sem[k] = nc.semaphore(k).__enter__()
                self.val[k] = 0
                self.slots[q].append(k)
        self.seen = {e: {} for e in self.E}
        self.n_ins = 0
        self._names = 0
        self.out_events = []
        self._stack = []

    def name(self, base):
        self._names += 1
        return f"{base}_{self._names}"

    def sb(self, shape, dt=F32, name="sb"):
        cm = self.nc.sbuf_tensor(self.name(name), list(shape), dt)
        t = T(cm.__enter__())
        self._stack.append(cm)
        return t

    def mark(self):
        return len(self._stack)

    def release_to(self, mark):
        self.barrier()
        while len(self._stack) > mark:
            self._stack.pop().__exit__(None, None, None)

    def barrier(self):
        for eng in self.E:
            for k in self.sem:
                if self.val[k] > 0:
                    self._wait(eng, (k, self.val[k]))

    def psum(self, shape, dt=F32, name="ps"):
        return T(self.nc.psum_tensor(self.name(name), list(shape), dt).__enter__())

    def dram(self, name, shape, dt=F32, kind="Internal"):
        return T(self.nc.dram_tensor(name, list(shape), dt, kind=kind).ap())

    def _wait(self, eng, ev):
        if ev is None:
            return
        key, v = ev
        if key == eng and eng == "pe":
            return
        if self.seen[eng].get(key, 0) >= v:
            return
        self.E[eng].wait_ge(self.sem[key], v)
        self.seen[eng][key] = v

    def _deps(self, eng, reads, writes):
        for x in reads:
            chk, _ = _trks(x, False)
            for tr in chk:
                self._wait(eng, tr.w)
        for x in writes:
            chk, _ = _trks(x, True)
            for tr in chk:
                self._wait(eng, tr.w)
                for k, v in tr.r.items():
                    self._wait(eng, (k, v))

    def _record(self, ev, reads, writes):
        for x in reads:
            _, upd = _trks(x, False)
            for tr in upd:
                tr.r[ev[0]] = ev[1]
        for x in writes:
            chk, upd = _trks(x, True)
            if not isinstance(x, tuple):
                x.parts.clear()
            for tr in upd:
                tr.w = ev
                tr.r = {}

    def op(self, eng, fn, reads=(), writes=()):
        self._deps(eng, reads, writes)
        ins = fn(self.E[eng])
        self.val[eng] += 1
        ins.then_inc(self.sem[eng], 1)
        ev = (eng, self.val[eng])
        self._record(ev, reads, writes)
        self.n_ins += 1
        return ev

    def dma(self, q, out, in_, reads=(), writes=(), **kw):
        self._deps(q, reads, writes)
        slot = self.slots[q][self.rr[q] % self.NSLOT]
        self.rr[q] += 1
        if self.val[slot] > 0:
            self._wait(q, (slot, self.val[slot]))
        ins = self.E[q].dma_start(out=out, in_=in_, **kw)
        self.val[slot] += 16
        ins.then_inc(self.sem[slot], 16)
        ev = (slot, self.val[slot])
        self._record(ev, reads, writes)
        self.n_ins += 1
        return ev

    def finish(self):
        for ev in self.out_events:
            self._wait("sp", ev)
        for k in self.sem:
            if self.val[k] > 0:
                self._wait("sp", (k, self.val[k]))

    def mm(self, ps, out_ap, lhsT, rhs, start, stop, reads, eng="pe"):
        return self.op("pe", lambda e: e.matmul(out_ap, lhsT, rhs, start=start, stop=stop),
                       reads=reads, writes=[ps])

    def act(self, out_t, out_ap, in_t, in_ap, func, bias=None, scale=1.0, extra_reads=(), accum_out=None):
        kw = {}
        if bias is not None:
            kw["bias"] = bias
        if accum_out is not None:
            kw["accum_out"] = accum_out
        return self.op("act", lambda e: e.activation(out=out_ap, in_=in_ap, func=func, scale=scale, **kw),
                       reads=[in_t] + list(extra_reads), writes=[out_t])

    def tt(self, eng, out_t, out_ap, a_t, a_ap, b_t, b_ap, op):
        return self.op(eng, lambda e: e.tensor_tensor(out=out_ap, in0=a_ap, in1=b_ap, op=op),
                       reads=[a_t, b_t], writes=[out_t])

    def ts(self, eng, out_t, out_ap, a_t, a_ap, s1, op0, s2=None, op1=None, extra_reads=()):
        if op1 is None:
            return self.op(eng, lambda e: e.tensor_scalar(out=out_ap, in0=a_ap, scalar1=s1, scalar2=None, op0=op0),
                           reads=[a_t] + list(extra_reads), writes=[out_t])
        return self.op(eng, lambda e: e.tensor_scalar(out=out_ap, in0=a_ap, scalar1=s1, scalar2=s2, op0=op0, op1=op1),
                       reads=[a_t] + list(extra_reads), writes=[out_t])

    def stt(self, out_t, out_ap, a_t, a_ap, scalar, b_t, b_ap, op0, op1, extra_reads=()):
        return self.op("dve", lambda e: e.scalar_tensor_tensor(out=out_ap, in0=a_ap, scalar=scalar, in1=b_ap, op0=op0, op1=op1),
                       reads=[a_t, b_t] + list(extra_reads), writes=[out_t])

    def copy(self, eng, out_t, out_ap, in_t, in_ap):
        if eng == "act":
            return self.op("act", lambda e: e.copy(out=out_ap, in_=in_ap), reads=[in_t], writes=[out_t])
        return self.op(eng, lambda e: e.tensor_copy(out=out_ap, in_=in_ap), reads=[in_t], writes=[out_t])

    def memset(self, eng, out_t, out_ap, v):
        return self.op(eng, lambda e: e.memset(out_ap, v), reads=[], writes=[out_t])


class Ctx:
    pass


def setup_common(P, NT_CTX, NT_LAT):
    K = Ctx()
    K.P = P
    K.NC = NT_CTX
    K.NL = NT_LAT
    K.NT = NT_CTX + NT_LAT
    nc = P.nc
    K.ps = [P.psum([128, 512], F32, name=f"bank{i}") for i in range(8)]
    K.ps_i = 0
    K.ps_reserved = set()
    K.ones = P.sb([128, 128], F32, "ones")
    P.memset("pool", K.ones, K.ones[:], 1.0)
    K.onesb = P.sb([128, 128], BF16, "onesb")
    P.memset("pool", K.onesb, K.onesb[:], 1.0)
    K.hT = P.dram("hT", [D, K.NT], F32)
    return K


def next_ps(K):
    while True:
        b = K.ps[K.ps_i % 8]
        K.ps_i += 1
        if (K.ps_i - 1) % 8 not in K.ps_reserved:
            return b


def dvec(ap1d, k):
    return ap1d.rearrange("(k p) -> p k", p=128)


def mods_all(K, layers, c_in, cctx_in, mod_w, mod_b, norm_mix, norm_ffn):
    P = K.P
    nc = P.nc
    res = {}
    pers = {}
    for l in layers:
        pers[l] = dict(M=P.sb([128, 48, 2], F32, f"modM{l}"), A_mix=P.sb([128, KC, 2], F32, f"A_mix{l}"),
                       A_ffn=P.sb([128, KC, 2], F32, f"A_ffn{l}"))
    mark = P.mark()
    craw = P.sb([128, KC, 2], F32, "craw")
    sT = P.sb([128, KC, 2], F32, "sT")
    modw_buf = [P.sb([128, KC, 256], F32, f"modw{i}") for i in range(2)]
    bT = P.sb([128, 48], F32, "modb")
    gm = P.sb([128, KC], F32, "gmix")
    gf = P.sb([128, KC], F32, "gffn")
    with nc.allow_non_contiguous_dma(reason="tiny vector load"):
        P.dma("sp", craw[:, :, 0], dvec(c_in.ap, KC), reads=[c_in], writes=[craw])
        P.dma("sp", craw[:, :, 1], dvec(cctx_in.ap, KC), reads=[cctx_in], writes=[craw])
    P.act(sT, sT[:], craw, craw[:], AF.Silu)
    wi = 0
    for l in layers:
        M = pers[l]["M"]
        with nc.allow_non_contiguous_dma(reason="tiny vector load"):
            P.dma("sp", bT[:], dvec(mod_b.ap[l], 48), reads=[mod_b], writes=[bT])
            P.dma("sp", gm[:], dvec(norm_mix.ap[l], KC), reads=[norm_mix], writes=[gm])
            P.dma("sp", gf[:], dvec(norm_ffn.ap[l], KC), reads=[norm_ffn], writes=[gf])
        for s in range(24):
            wb = modw_buf[wi % 2]
            wi += 1
            P.dma("sp", wb[:], mod_w.ap[l, :, s * 256:(s + 1) * 256].rearrange("(k p) n -> p k n", p=128),
                  reads=[mod_w], writes=[wb])
            ps = next_ps(K)
            for j in range(2):
                for k in range(KC):
                    P.mm(ps, ps[:, j * 2:j * 2 + 2], wb[:, k, j * 128:(j + 1) * 128], sT[:, k, :],
                         start=(k == 0), stop=(k == KC - 1), reads=[wb, sT])
            for j in range(2):
                jj = s * 2 + j
                P.ts("dve", M, M[:, jj, :], ps, ps[:, j * 2:j * 2 + 2], bT[:, jj:jj + 1], ALU.add, extra_reads=[bT])
        out = dict(pers[l])
        for nm, sc_i, g in (("mix", 1, gm), ("ffn", 4, gf)):
            A = pers[l]["A_" + nm]
            for col in range(2):
                P.stt(A, A[:, :, col], M, M[:, sc_i * 8:(sc_i + 1) * 8, col], 1.0, g, g[:], ALU.add, ALU.mult)
        out["B_mix"] = (lambda M: (lambda k, col: M[:, 0 * 8 + k, col:col + 1]))(M)
        out["G_mix"] = (lambda M: (lambda k, col: M[:, 2 * 8 + k, col:col + 1]))(M)
        out["B_ffn"] = (lambda M: (lambda k, col: M[:, 3 * 8 + k, col:col + 1]))(M)
        out["G_ffn"] = (lambda M: (lambda k, col: M[:, 5 * 8 + k, col:col + 1]))(M)
        res[l] = out
    P.release_to(mark)
    return res


def norm_tile(K, h_t, hc0, n, A, Bf, M, col, y_bf, yc0, y_f32=None, sq=None, rstd=None):
    P = K.P
    ps = next_ps(K)
    for k in range(KC):
        P.act(sq, sq[:, k, :n], h_t, h_t[:, k, hc0:hc0 + n], AF.Square)
    for k in range(KC):
        P.mm(ps, ps[:, :n], K.ones[:], sq[:, k, :n], start=(k == 0), stop=(k == KC - 1), reads=[K.ones, sq])
    P.ts("dve", rstd, rstd[:, :n], ps, ps[:, :n], 1.0 / D, ALU.mult, EPS, ALU.add)
    P.op("act", lambda e: e.sqrt(out=rstd[:, :n], in_=rstd[:, :n]), reads=[rstd], writes=[rstd])
    P.op("dve", lambda e: e.reciprocal(out=rstd[:, :n], in_=rstd[:, :n]), reads=[rstd], writes=[rstd])
    for k in range(KC):
        P.stt(sq, sq[:, k, :n], h_t, h_t[:, k, hc0:hc0 + n], A[:, k, col:col + 1], rstd, rstd[:, :n], ALU.mult, ALU.mult,
              extra_reads=[A])
        if y_f32 is not None:
            P.act(y_f32, y_f32[:, k, :n], sq, sq[:, k, :n], AF.Identity, bias=Bf(k, col), extra_reads=[M])
            P.copy("pool", y_bf, y_bf[:, k, yc0:yc0 + n], y_f32, y_f32[:, k, :n])
        else:
            P.act(y_bf, y_bf[:, k, yc0:yc0 + n], sq, sq[:, k, :n], AF.Identity, bias=Bf(k, col), extra_reads=[M])


def ffn_stage(K, mods, experts, router=None, tok_ranges=None, ST=1024, tag="ffn"):
    P = K.P
    nc = P.nc
    FF = experts[0][0].ap.shape[1]
    NE = len(experts)
    slabs = []
    f0 = 0
    while f0 < FF:
        fs = min(512, FF - f0)
        slabs.append((f0, fs))
        f0 += fs
    A, Bf, Gf, M = mods["A_ffn"], mods["B_ffn"], mods["G_ffn"], mods["M"]
    if tok_ranges is None:
        tok_ranges = [(0, K.NC, 1)] + [(K.NC + i * ST, min(ST, K.NL - i * ST), 0) for i in range((K.NL + ST - 1) // ST)]
    mark = P.mark()
    if True:
        B = Ctx()
        B.acc = P.sb([128, KC, ST], F32, "ffn_acc")
        B.y = P.sb([128, KC, ST], BF16, "ffn_y")
        B.sq = P.sb([128, KC, 512], F32, "ffn_sq")
        B.yf = P.sb([128, KC, 512], F32, "ffn_yf")
        B.rstd = P.sb([128, 512], F32, "ffn_rstd")
        B.hid = P.sb([128, 4, ST], BF16, "ffn_hid")
        B.wg = [P.sb([128, KC, 512], BF16, f"ffn_wg{i}") for i in range(2)]
        B.wu = [P.sb([128, KC, 512], BF16, f"ffn_wu{i}") for i in range(2)]
        B.wd = [P.sb([128, 4, D], BF16, f"ffn_wd{i}") for i in range(2)]
        B.sg = [P.sb([128, 512], F32, f"ffn_sg{i}") for i in range(2)]
        B.tmp = [P.sb([128, 512], F32, f"ffn_tmp{i}") for i in range(2)]
        B.gate_bc = P.sb([128, 8, ST], F32, "ffn_gatebc")
        B.rt = P.sb([128, KC, 8], F32, "ffn_router")
        B.lg = P.sb([128, 8], F32, "ffn_lg")
        B.mx = P.sb([128, 8], F32, "ffn_mx")
        B.gt = P.sb([128, 8], F32, "ffn_gt")
        B.m1 = P.sb([128, 8], F32, "ffn_m1")
        B.wv = P.sb([128, 4], F32, "ffn_wv")
        B.ident = P.sb([128, 128], F32, "ffn_ident")
        B.gcol = P.sb([128, 128], F32, "ffn_gcol")
        B.i = 0
        P.memset("pool", B.ident, B.ident[:], 0.0)
        P.op("pool", lambda e: e.affine_select(out=B.ident[:], in_=B.ident[:], pattern=[[-1, 128]], base=0,
                                                channel_multiplier=1, compare_op=ALU.not_equal, fill=1.0),
             reads=[B.ident], writes=[B.ident])
    if router is not None:
        P.dma("sp", B.rt[:], router.ap.rearrange("(k p) e -> p k e", p=128), reads=[router], writes=[B.rt])
    for (t0, n, col) in tok_ranges:
        ntile = (n + 511) // 512
        for j in range(ntile):
            c0 = j * 512
            w = min(512, n - c0)
            P.dma("sp", B.acc[:, :, c0:c0 + w], K.hT.ap[:, t0 + c0:t0 + c0 + w].rearrange("(k p) t -> p k t", p=128),
                  reads=[K.hT], writes=[B.acc])
            norm_tile(K, B.acc, c0, w, A, Bf, M, col, B.y, c0,
                      y_f32=(B.yf if router is not None else None), sq=B.sq, rstd=B.rstd)
            if router is not None:
                for q in range((w + 127) // 128):
                    qw = min(128, w - q * 128)
                    ps = next_ps(K)
                    for k in range(KC):
                        P.mm(ps, ps[:qw, 0:8], B.yf[:, k, q * 128:q * 128 + qw], B.rt[:, k, :],
                             start=(k == 0), stop=(k == KC - 1), reads=[B.yf, B.rt])
                    P.copy("dve", B.lg, B.lg[:qw, :], ps, ps[:qw, 0:8])
                    P.op("dve", lambda e: e.max(out=B.mx[:qw, :], in_=B.lg[:qw, :]), reads=[B.lg], writes=[B.mx])
                    P.tt("dve", B.wv, B.wv[:qw, 0:1], B.mx, B.mx[:qw, 1:2], B.mx, B.mx[:qw, 0:1], ALU.subtract)
                    P.act(B.wv, B.wv[:qw, 1:2], B.wv, B.wv[:qw, 0:1], AF.Exp)
                    P.ts("dve", B.wv, B.wv[:qw, 1:2], B.wv, B.wv[:qw, 1:2], 1.0, ALU.add)
                    P.op("dve", lambda e: e.reciprocal(out=B.wv[:qw, 2:3], in_=B.wv[:qw, 1:2]), reads=[B.wv], writes=[B.wv])
                    P.ts("dve", B.wv, B.wv[:qw, 3:4], B.wv, B.wv[:qw, 2:3], -1.0, ALU.mult, 1.0, ALU.add)
                    P.ts("dve", B.gt, B.gt[:qw, :], B.lg, B.lg[:qw, :], B.mx[:qw, 0:1], ALU.is_equal,
                         B.wv[:qw, 2:3], ALU.mult, extra_reads=[B.mx, B.wv])
                    P.ts("dve", B.m1, B.m1[:qw, :], B.lg, B.lg[:qw, :], B.mx[:qw, 1:2], ALU.is_equal,
                         B.wv[:qw, 3:4], ALU.mult, extra_reads=[B.mx, B.wv])
                    P.tt("dve", B.gt, B.gt[:qw, :], B.gt, B.gt[:qw, :], B.m1, B.m1[:qw, :], ALU.add)
                    for e_i in range(NE):
                        P.ts("dve", B.gcol, B.gcol[:qw, :], K.ones, K.ones[:qw, :], B.gt[:qw, e_i:e_i + 1], ALU.mult,
                             extra_reads=[B.gt])
                        ps2 = next_ps(K)
                        P.mm(ps2, ps2[:, :qw], B.gcol[:qw, :], B.ident[:qw, :qw], start=True, stop=True,
                             reads=[B.gcol, B.ident])
                        P.copy("act", B.gate_bc, B.gate_bc[:, e_i, c0 + q * 128:c0 + q * 128 + qw], ps2, ps2[:, :qw])
        for e_i, (wg, wu, wd) in enumerate(experts):
            for (f0, fs) in slabs:
                i = B.i % 2
                B.i += 1
                nfc = fs // 128
                P.dma("pool", B.wg[i][:, :, :fs], wg.ap[:, f0:f0 + fs].rearrange("(k p) f -> p k f", p=128),
                      reads=[wg], writes=[B.wg[i]])
                P.dma("pool", B.wu[i][:, :, :fs], wu.ap[:, f0:f0 + fs].rearrange("(k p) f -> p k f", p=128),
                      reads=[wu], writes=[B.wu[i]])
                P.dma("pool", B.wd[i][:, :nfc, :], wd.ap[f0:f0 + fs, :].rearrange("(c p) d -> p c d", p=128),
                      reads=[wd], writes=[B.wd[i]])
                for j in range(ntile):
                    c0 = j * 512
                    w = min(512, n - c0)
                    for fc in range(nfc):
                        pg = next_ps(K)
                        pu = next_ps(K)
                        for k in range(KC):
                            P.mm(pg, pg[:, :w], B.wg[i][:, k, fc * 128:(fc + 1) * 128], B.y[:, k, c0:c0 + w],
                                 start=(k == 0), stop=(k == KC - 1), reads=[B.wg[i], B.y])
                        for k in range(KC):
                            P.mm(pu, pu[:, :w], B.wu[i][:, k, fc * 128:(fc + 1) * 128], B.y[:, k, c0:c0 + w],
                                 start=(k == 0), stop=(k == KC - 1), reads=[B.wu[i], B.y])
                        sg = B.sg[(j * 4 + fc) % 2]
                        P.act(sg, sg[:, :w], pg, pg[:, :w], AF.Silu)
                        P.tt("dve", B.hid, B.hid[:, fc, c0:c0 + w], sg, sg[:, :w], pu, pu[:, :w], ALU.mult)
                    for oc in range(KC):
                        po = next_ps(K)
                        for fc in range(nfc):
                            P.mm(po, po[:, :w], B.wd[i][:, fc, oc * 128:(oc + 1) * 128], B.hid[:, fc, c0:c0 + w],
                                 start=(fc == 0), stop=(fc == nfc - 1), reads=[B.wd[i], B.hid])
                        if router is not None:
                            tmp = B.tmp[oc % 2]
                            P.stt(tmp, tmp[:, :w], po, po[:, :w], Gf(oc, col), B.gate_bc, B.gate_bc[:, e_i, c0:c0 + w],
                                  ALU.mult, ALU.mult, extra_reads=[M])
                            P.tt("pool", B.acc, B.acc[:, oc, c0:c0 + w], B.acc, B.acc[:, oc, c0:c0 + w], tmp, tmp[:, :w], ALU.add)
                        else:
                            P.stt(B.acc, B.acc[:, oc, c0:c0 + w], po, po[:, :w], Gf(oc, col), B.acc, B.acc[:, oc, c0:c0 + w],
                                  ALU.mult, ALU.add, extra_reads=[M])
        for j in range(ntile):
            c0 = j * 512
            w = min(512, n - c0)
            P.dma("sp", K.hT.ap[:, t0 + c0:t0 + c0 + w].rearrange("(k p) t -> p k t", p=128), B.acc[:, :, c0:c0 + w],
                  reads=[B.acc], writes=[K.hT])
    if hasattr(K, "dbg") and router is not None:
        P.dma("sp", K.dbg["gate_bc"].ap[:, :, :], B.gate_bc[:, :, :], reads=[B.gate_bc], writes=[K.dbg["gate_bc"]])
        P.dma("sp", K.dbg["ident"].ap[:, :], B.ident[:, :], reads=[B.ident], writes=[K.dbg["ident"]])
        for i_, t_ in enumerate((B.lg, B.mx, B.gt, B.m1)):
            P.dma("sp", K.dbg["small"].ap[:, i_ * 8:(i_ + 1) * 8], t_[:, :], reads=[t_], writes=[K.dbg["small"]])
        P.dma("sp", K.dbg["small"].ap[:, 32:36], B.wv[:, :], reads=[B.wv], writes=[K.dbg["small"]])
    P.release_to(mark)


def inproj_stage(K, mods, w_in, zT, segs=None):
    P = K.P
    nc = P.nc
    if segs is None:
        segs = [(0, w_in.ap.shape[1])]
    N = sum(n for _, n in segs)
    A, Bf, M = mods["A_mix"], mods["B_mix"], mods["M"]
    mark = P.mark()
    nch = (N + 127) // 128
    wst = [P.sb([128, KC, 512], F32, f"ip_wst{i}") for i in range(2)]
    wb = P.sb([128, KC, N], BF16, "ip_w")
    o0 = 0
    si = 0
    for (c0, ncol) in segs:
        for s0 in range(0, ncol, 512):
            cw = min(512, ncol - s0)
            st = wst[si % 2]
            si += 1
            with nc.allow_non_contiguous_dma(reason="weight column segments"):
                P.dma("sp", st[:, :, :cw], w_in.ap[:, c0 + s0:c0 + s0 + cw].rearrange("(k p) n -> p k n", p=128),
                      reads=[w_in], writes=[st])
            P.copy("pool", wb, wb[:, :, o0 + s0:o0 + s0 + cw], st, st[:, :, :cw])
        o0 += ncol
    h = [P.sb([128, KC, 512], F32, f"ip_h{i}") for i in range(2)]
    y = [P.sb([128, KC, 512], BF16, f"ip_y{i}") for i in range(2)]
    sq = P.sb([128, KC, 512], F32, "ip_sq")
    rstd = P.sb([128, 512], F32, "ip_rstd")
    zo = [P.sb([128, 512], F32, f"ip_zo{i}") for i in range(4)]
    ranges = []
    if K.NC:
        ranges.append((0, K.NC, 1))
    ranges += [(K.NC + i * 512, min(512, K.NL - i * 512), 0) for i in range((K.NL + 511) // 512)]
    for it, (t0, n, col) in enumerate(ranges):
        hb, yb = h[it % 2], y[it % 2]
        P.dma("sp", hb[:, :, :n], K.hT.ap[:, t0:t0 + n].rearrange("(k p) t -> p k t", p=128), reads=[K.hT], writes=[hb])
        norm_tile(K, hb, 0, n, A, Bf, M, col, yb, 0, sq=sq, rstd=rstd)
        for c in range(nch):
            m = min(128, N - c * 128)
            ps = next_ps(K)
            for k in range(KC):
                P.mm(ps, ps[:m, :n], wb[:, k, c * 128:c * 128 + m], yb[:, k, :n], start=(k == 0), stop=(k == KC - 1),
                     reads=[wb, yb])
            o = zo[c % 4]
            P.copy("act" if c % 2 else "dve", o, o[:m, :n], ps, ps[:m, :n])
            P.dma("pool" if c % 2 else "sp", zT.ap[c * 128:c * 128 + m, t0:t0 + n], o[:m, :n], reads=[o], writes=[zT])
    P.release_to(mark)


def outproj_stage(K, mods, w_out, mixT):
    P = K.P
    Gm, M = mods["G_mix"], mods["M"]
    mark = P.mark()
    wst = [P.sb([128, KC, 512], F32, f"op_wst{i}") for i in range(2)]
    wb = P.sb([128, KC, D], BF16, "op_w")
    for s in range(2):
        st = wst[s % 2]
        P.dma("sp", st[:], w_out.ap[:, s * 512:(s + 1) * 512].rearrange("(k p) n -> p k n", p=128), reads=[w_out], writes=[st])
        P.copy("pool", wb, wb[:, :, s * 512:(s + 1) * 512], st, st[:])
    mx = [P.sb([128, KC, 512], F32, f"op_m{i}") for i in range(2)]
    mb = [P.sb([128, KC, 512], BF16, f"op_mb{i}") for i in range(2)]
    h = [P.sb([128, KC, 512], F32, f"op_h{i}") for i in range(2)]
    ranges = [(0, K.NC, 1)] + [(K.NC + i * 512, min(512, K.NL - i * 512), 0) for i in range((K.NL + 511) // 512)]
    for it, (t0, n, col) in enumerate(ranges):
        m_, b_, h_ = mx[it % 2], mb[it % 2], h[it % 2]
        P.dma("sp", m_[:, :, :n], mixT.ap[:, t0:t0 + n].rearrange("(k p) t -> p k t", p=128), reads=[mixT], writes=[m_])
        P.dma("sp", h_[:, :, :n], K.hT.ap[:, t0:t0 + n].rearrange("(k p) t -> p k t", p=128), reads=[K.hT], writes=[h_])
        P.copy("pool", b_, b_[:, :, :n], m_, m_[:, :, :n])
        for c in range(KC):
            ps = next_ps(K)
            for k in range(KC):
                P.mm(ps, ps[:, :n], wb[:, k, c * 128:(c + 1) * 128], b_[:, k, :n], start=(k == 0), stop=(k == KC - 1),
                     reads=[wb, b_])
            P.stt(h_, h_[:, c, :n], ps, ps[:, :n], Gm(c, col), h_, h_[:, c, :n], ALU.mult, ALU.add, extra_reads=[M])
        P.dma("pool", K.hT.ap[:, t0:t0 + n].rearrange("(k p) t -> p k t", p=128), h_[:, :, :n], reads=[h_], writes=[K.hT])
    P.release_to(mark)


def outproj_half_stage(K, mods, w_out, mixT, mixH, selt_d, hT2, HALF):
    P = K.P
    Gm, M = mods["G_mix"], mods["M"]
    mark = P.mark()
    selt = P.sb([128, 2], F32, "oh_sel")
    P.dma("sp", selt[:], selt_d.ap[:, :], reads=[selt_d], writes=[selt])
    wst = [P.sb([128, KC, 512], F32, f"oh_wst{i}") for i in range(2)]
    wb = P.sb([128, KC, D], BF16, "oh_w")
    for s_ in range(2):
        st = wst[s_ % 2]
        P.dma("sp", st[:], w_out.ap[:, s_ * 512:(s_ + 1) * 512].rearrange("(k p) n -> p k n", p=128), reads=[w_out], writes=[st])
        P.copy("pool", wb, wb[:, :, s_ * 512:(s_ + 1) * 512], st, st[:])
    mx = [P.sb([128, KC, 512], F32, f"oh_m{i}") for i in range(2)]
    m2 = [P.sb([128, 4, 512], F32, f"oh_m2{i}") for i in range(2)]
    mb = [P.sb([128, KC, 512], BF16, f"oh_mb{i}") for i in range(2)]
    h = [P.sb([128, KC, 512], F32, f"oh_h{i}") for i in range(2)]
    h2 = [P.sb([128, KC, 512], F32, f"oh_h2{i}") for i in range(2)]
    NCX = K.NC
    for j in range(HALF // 512):
        m_, mm2, b_, h_, hh2 = mx[j % 2], m2[j % 2], mb[j % 2], h[j % 2], h2[j % 2]
        ta, tb = NCX + j * 512, NCX + HALF + j * 512
        P.dma("sp", m_[:, 0:4, :], mixH.ap[:, j * 512:(j + 1) * 512].rearrange("(k p) t -> p k t", p=128), reads=[mixH], writes=[m_])
        P.dma("sp", m_[:, 4:8, :], mixT.ap[512:1024, ta:ta + 512].rearrange("(k p) t -> p k t", p=128), reads=[mixT], writes=[m_])
        P.dma("sp", mm2[:], mixT.ap[512:1024, tb:tb + 512].rearrange("(k p) t -> p k t", p=128), reads=[mixT], writes=[mm2])
        P.dma("sp", h_[:], K.hT.ap[:, ta:ta + 512].rearrange("(k p) t -> p k t", p=128), reads=[K.hT], writes=[h_])
        P.dma("sp", hh2[:], K.hT.ap[:, tb:tb + 512].rearrange("(k p) t -> p k t", p=128), reads=[K.hT], writes=[hh2])
        P.ts("dve", m_, m_[:, 4:8, :], m_, m_[:, 4:8, :], selt[:, 0:1], ALU.mult, extra_reads=[selt])
        P.stt(m_, m_[:, 4:8, :], mm2, mm2[:], selt[:, 1:2], m_, m_[:, 4:8, :], ALU.mult, ALU.add, extra_reads=[selt])
        P.ts("dve", h_, h_[:], h_, h_[:], selt[:, 0:1], ALU.mult, extra_reads=[selt])
        P.stt(h_, h_[:], hh2, hh2[:], selt[:, 1:2], h_, h_[:], ALU.mult, ALU.add, extra_reads=[selt])
        P.copy("pool", b_, b_[:], m_, m_[:])
        for c in range(KC):
            ps = next_ps(K)
            for k in range(KC):
                P.mm(ps, ps[:, :], wb[:, k, c * 128:(c + 1) * 128], b_[:, k, :], start=(k == 0), stop=(k == KC - 1), reads=[wb, b_])
            P.stt(h_, h_[:, c, :], ps, ps[:, :], Gm(c, 0), h_, h_[:, c, :], ALU.mult, ALU.add, extra_reads=[M])
        P.dma("pool", hT2.ap[:, j * 512:(j + 1) * 512].rearrange("(k p) t -> p k t", p=128), h_[:], reads=[h_], writes=[hT2])
    P.release_to(mark)


def final_norm_stage(K, gain, outT, t0_all, n_all):
    P = K.P
    nc = P.nc
    mark = P.mark()
    g = P.sb([128, KC], F32, "fn_g")
    with nc.allow_non_contiguous_dma(reason="tiny vector load"):
        P.dma("sp", g[:], dvec(gain.ap, KC), reads=[gain], writes=[g])
    h = [P.sb([128, KC, 512], F32, f"fn_h{i}") for i in range(2)]
    o = [P.sb([128, KC, 512], F32, f"fn_o{i}") for i in range(2)]
    sq = P.sb([128, KC, 512], F32, "fn_sq")
    rstd = P.sb([128, 512], F32, "fn_rstd")
    for it in range((n_all + 511) // 512):
        n = min(512, n_all - it * 512)
        t0 = t0_all + it * 512
        hb, ob = h[it % 2], o[it % 2]
        P.dma("sp", hb[:, :, :n], K.hT.ap[:, t0:t0 + n].rearrange("(k p) t -> p k t", p=128), reads=[K.hT], writes=[hb])
        ps = next_ps(K)
        for k in range(KC):
            P.act(sq, sq[:, k, :n], hb, hb[:, k, :n], AF.Square)
        for k in range(KC):
            P.mm(ps, ps[:, :n], K.ones[:], sq[:, k, :n], start=(k == 0), stop=(k == KC - 1), reads=[K.ones, sq])
        P.ts("dve", rstd, rstd[:, :n], ps, ps[:, :n], 1.0 / D, ALU.mult, EPS, ALU.add)
        P.op("act", lambda e: e.sqrt(out=rstd[:, :n], in_=rstd[:, :n]), reads=[rstd], writes=[rstd])
        P.op("dve", lambda e: e.reciprocal(out=rstd[:, :n], in_=rstd[:, :n]), reads=[rstd], writes=[rstd])
        for k in range(KC):
            P.stt(ob, ob[:, k, :n], hb, hb[:, k, :n], g[:, k:k + 1], rstd, rstd[:, :n], ALU.mult, ALU.mult, extra_reads=[g])
        ev = P.dma("pool", outT.ap[:, it * 512:it * 512 + n].rearrange("(k p) t -> p k t", p=128), ob[:, :, :n],
                   reads=[ob], writes=[outT])
        P.out_events.append(ev)
    P.release_to(mark)


MAGIC = 12582912.0
TWO_PI = 2.0 * math.pi


def host_consts():
    c = {}
    c["k_tau"] = np.tile(np.arange(128, dtype=np.float32)[None, :], (128, 1))
    q = np.arange(128)
    mC = np.zeros((128, 4, 2, 64), np.float32)
    for jj in range(4):
        for gl in range(2):
            sel = (q // 32 == jj) & ((q % 32) // 16 == gl)
            mC[sel, jj, gl, :] = 1.0
    c["k_maskC"] = mC.reshape(128, 512)
    mB = np.zeros((128, 4, 128), np.float32)
    for jj in range(4):
        for gl in range(2):
            rows = np.arange(64 * gl, 64 * gl + 64)
            cols = np.arange(32 * jj + 16 * gl, 32 * jj + 16 * gl + 16)
            mB[np.ix_(rows, [jj], cols)] = 1.0
    c["k_maskB"] = mB.reshape(128, 512)
    c["k_ident"] = np.eye(128, dtype=np.float32)
    return c


def range_reduce(P, out_t, out_ap, in_t, in_ap, tmp_t, tmp_ap, shift=0.0):
    P.ts("dve", tmp_t, tmp_ap, in_t, in_ap, 1.0 / TWO_PI, ALU.mult, shift / TWO_PI + MAGIC, ALU.add)
    P.ts("dve", tmp_t, tmp_ap, tmp_t, tmp_ap, MAGIC, ALU.subtract, -TWO_PI, ALU.mult)
    P.stt(out_t, out_ap, in_t, in_ap, shift, tmp_t, tmp_ap, ALU.add, ALU.add)


def s5_stage(K, i, I, zT, mixT, C, u_row0=1536, out_row0=512):
    P = K.P
    nc = P.nc
    TT = 128
    mark = P.mark()
    tau, maskB, maskC, ident = C["k_tau"], C["k_maskB"], C["k_maskC"], C["k_ident"]
    yS = K.yS5
    sb = P.sb
    lr = sb([128, 16], F32, "s5_lr"); li = sb([128, 16], F32, "s5_li"); dtv = sb([128, 16], F32, "s5_dt")
    wdt = sb([128, 16], F32, "s5_wdt"); rdt = sb([128, 16], F32, "s5_rdt"); nrdt = sb([128, 16], F32, "s5_nrdt")
    Er = sb([128, 16, TT], F32, "s5_Er"); Ei = sb([128, 16, TT], F32, "s5_Ei")
    Gr = sb([128, 16, TT], F32, "s5_Gr"); Gi = sb([128, 16, TT], F32, "s5_Gi")
    Hr = sb([128, 16], F32, "s5_Hr"); Hi = sb([128, 16], F32, "s5_Hi")
    BTr = sb([128, 16, 128], F32, "s5_BTr"); BTi = sb([128, 16, 128], F32, "s5_BTi")
    CTr = sb([128, 16, 128], F32, "s5_CTr"); CTin = sb([128, 16, 128], F32, "s5_CTin")
    bre = sb([128, 16, 16], F32, "s5_bre"); bim = sb([128, 16, 16], F32, "s5_bim")
    bbr = sb([128, 16, 16], F32, "s5_bbr"); bbi = sb([128, 16, 16], F32, "s5_bbi")
    cre = sb([128, 4, 64], F32, "s5_cre"); cim = sb([128, 4, 64], F32, "s5_cim")
    ex = sb([128, 128], F32, "s5_ex")
    th = sb([128, TT], F32, "s5_th"); tmp = sb([128, TT], F32, "s5_tmp"); sn = sb([128, TT], F32, "s5_sn")
    cs = sb([128, TT], F32, "s5_cs"); mg = sb([128, TT], F32, "s5_mg")
    s16 = [sb([128, 16], F32, f"s5_s16_{k}") for k in range(8)]
    u = [sb([128, 4, TT], F32, f"s5_u{k}") for k in range(3)]
    SL = 3
    t1s = [sb([128, 4, TT], F32, f"s5_t1{k}") for k in range(SL)]; t2s = [sb([128, 4, TT], F32, f"s5_t2{k}") for k in range(SL)]
    t3s = [sb([128, 4, TT], F32, f"s5_t3{k}") for k in range(SL)]; t4s = [sb([128, 4, TT], F32, f"s5_t4{k}") for k in range(SL)]
    wrs = [sb([128, 4, TT], F32, f"s5_wr{k}") for k in range(SL)]; wis = [sb([128, 4, TT], F32, f"s5_wi{k}") for k in range(SL)]
    zrs = [sb([128, 4, TT], F32, f"s5_zr{k}") for k in range(SL)]; zis = [sb([128, 4, TT], F32, f"s5_zi{k}") for k in range(SL)]
    xrs = [sb([128, 4, TT], F32, f"s5_xr{k}") for k in range(SL)]; xis = [sb([128, 4, TT], F32, f"s5_xi{k}") for k in range(SL)]
    cars = [sb([128, 4], F32, f"s5_car{k}") for k in range(4)]; cais = [sb([128, 4], F32, f"s5_cai{k}") for k in range(4)]
    c4s = [[sb([128, 4], F32, f"s5_c4_{q}{k}") for k in range(4)] for q in range(SL)]
    g1s = [sb([128, TT], F32, f"s5_g1{k}") for k in range(SL)]; g2s = [sb([128, TT], F32, f"s5_g2{k}") for k in range(SL)]
    yo = [sb([128, TT], F32, f"s5_yo{k}") for k in range(2)]
    yfs = [sb([128, 4, TT], F32, f"s5_yf{k}") for k in range(3)]
    vqs = [[sb([128, TT], F32, f"s5_v{p}{k}") for k in range(4)] for p in range(2)]
    oo = [sb([128, TT], F32, f"s5_oo{k}") for k in range(2)]
    wglu = sb([128, 4, 512], F32, "s5_wglu"); dsk = sb([128, 4], F32, "s5_dsk")
    onesT = sb([128, TT], F32, "s5_ones")
    P.memset("pool", onesT, onesT[:], 1.0)
    P.dma("sp", wglu[:], I["s5_w_glu"].ap[i].rearrange("(k p) n -> p k n", p=128), reads=[I["s5_w_glu"]], writes=[wglu])
    with nc.allow_non_contiguous_dma(reason="tiny vector load"):
        P.dma("sp", dsk[:], I["s5_d"].ap[i].rearrange("(k p) -> p k", p=128), reads=[I["s5_d"]], writes=[dsk])

    def sincos(angle_t, angle_ap, sin_t, sin_ap, cos_t, cos_ap, tmp_a, tmp_a_ap, tmp_b, tmp_b_ap):
        range_reduce(P, tmp_b, tmp_b_ap, angle_t, angle_ap, tmp_a, tmp_a_ap, 0.0)
        P.act(sin_t, sin_ap, tmp_b, tmp_b_ap, AF.Sin)
        range_reduce(P, tmp_b, tmp_b_ap, angle_t, angle_ap, tmp_a, tmp_a_ap, math.pi / 2)
        P.act(cos_t, cos_ap, tmp_b, tmp_b_ap, AF.Sin)

    chunks_ctx = [(k * TT, 1) for k in range(K.NC // TT)]
    chunks_lat = [(K.NC + k * TT, 0) for k in range(K.NL // TT)]
    for d in range(2):
        rev = (d == 1)
        with nc.allow_non_contiguous_dma(reason="small parameter loads"):
            P.dma("sp", lr[:], I["s5_a_re"].ap[i, d].rearrange("g p -> (g p)").rearrange("(j q) -> q j", q=128),
                  reads=[I["s5_a_re"]], writes=[lr])
            P.dma("sp", li[:], I["s5_a_im"].ap[i, d].rearrange("g p -> (g p)").rearrange("(j q) -> q j", q=128),
                  reads=[I["s5_a_im"]], writes=[li])
            for gl in range(2):
                P.dma("sp", dtv[64 * gl:64 * gl + 64, :],
                      I["s5_log_dt"].ap[i, d].rearrange("(j g) -> g j", g=2)[gl:gl + 1, :].partition_broadcast(64),
                      reads=[I["s5_log_dt"]], writes=[dtv])
            P.dma("sp", bre[:], I["s5_b_re"].ap[i, d].rearrange("g p c -> (g p) c").rearrange("(j q) c -> q j c", q=128),
                  reads=[I["s5_b_re"]], writes=[bre])
            P.dma("sp", bim[:], I["s5_b_im"].ap[i, d].rearrange("g p c -> (g p) c").rearrange("(j q) c -> q j c", q=128),
                  reads=[I["s5_b_im"]], writes=[bim])
            P.dma("sp", cre[:], I["s5_c_re"].ap[i, d].rearrange("g c p -> (g c) p").rearrange("(k q) p -> q k p", q=128),
                  reads=[I["s5_c_re"]], writes=[cre])
            P.dma("sp", cim[:], I["s5_c_im"].ap[i, d].rearrange("g c p -> (g c) p").rearrange("(k q) p -> q k p", q=128),
                  reads=[I["s5_c_im"]], writes=[cim])
        P.ts("dve", lr, lr[:], lr, lr[:], -1e-4, ALU.min)
        P.act(dtv, dtv[:], dtv, dtv[:], AF.Exp)
        P.tt("dve", wdt, wdt[:], li, li[:], dtv, dtv[:], ALU.mult)
        P.tt("dve", rdt, rdt[:], lr, lr[:], dtv, dtv[:], ALU.mult)
        P.ts("dve", nrdt, nrdt[:], rdt, rdt[:], -1.0, ALU.mult)
        ar, ai, a_s, a_c, a_m, ta, tb, den = s16
        sincos(wdt, wdt[:], a_s, a_s[:], a_c, a_c[:], ta, ta[:], tb, tb[:])
        P.act(a_m, a_m[:], rdt, rdt[:], AF.Exp)
        P.tt("dve", ar, ar[:], a_m, a_m[:], a_c, a_c[:], ALU.mult)
        P.tt("dve", ai, ai[:], a_m, a_m[:], a_s, a_s[:], ALU.mult)
        P.ts("dve", ar, ar[:], ar, ar[:], -1.0, ALU.add)
        P.tt("dve", den, den[:], lr, lr[:], lr, lr[:], ALU.mult)
        P.tt("dve", ta, ta[:], li, li[:], li, li[:], ALU.mult)
        P.tt("dve", den, den[:], den, den[:], ta, ta[:], ALU.add)
        P.op("dve", lambda e: e.reciprocal(out=den[:], in_=den[:]), reads=[den], writes=[den])
        P.tt("dve", ta, ta[:], ar, ar[:], lr, lr[:], ALU.mult)
        P.tt("dve", tb, tb[:], ai, ai[:], li, li[:], ALU.mult)
        P.tt("dve", ta, ta[:], ta, ta[:], tb, tb[:], ALU.add)
        P.tt("dve", a_c, a_c[:], ta, ta[:], den, den[:], ALU.mult)
        P.tt("dve", ta, ta[:], ai, ai[:], lr, lr[:], ALU.mult)
        P.tt("dve", tb, tb[:], ar, ar[:], li, li[:], ALU.mult)
        P.tt("dve", ta, ta[:], ta, ta[:], tb, tb[:], ALU.subtract)
        P.tt("dve", a_s, a_s[:], ta, ta[:], den, den[:], ALU.mult)
        P.ts("dve", a_m, a_m[:], a_s, a_s[:], -1.0, ALU.mult)
        coef_r, coef_i, ncoef_i = a_c, a_s, a_m
        P.ts("dve", ta, ta[:], wdt, wdt[:], float(TT), ALU.mult)
        sincos(ta, ta[:], Hi, Hi[:], Hr, Hr[:], tb, tb[:], den, den[:])
        P.act(ta, ta[:], rdt, rdt[:], AF.Exp, scale=float(TT))
        P.tt("dve", Hr, Hr[:], Hr, Hr[:], ta, ta[:], ALU.mult)
        P.tt("dve", Hi, Hi[:], Hi, Hi[:], ta, ta[:], ALU.mult)
        for j in range(16):
            jj = j % 4
            cc = j // 4
            P.ts("dve", th, th[:], tau, tau[:], wdt[:, j:j + 1], ALU.mult, extra_reads=[wdt])
            sincos(th, th[:], sn, sn[:], cs, cs[:], tmp, tmp[:], mg, mg[:])
            P.act(mg, mg[:], tau, tau[:], AF.Exp, scale=rdt[:, j:j + 1], extra_reads=[rdt])
            P.tt("dve", Gr, Gr[:, j, :], mg, mg[:], cs, cs[:], ALU.mult)
            P.tt("dve", Gi, Gi[:, j, :], mg, mg[:], sn, sn[:], ALU.mult)
            P.act(mg, mg[:], tau, tau[:], AF.Exp, scale=nrdt[:, j:j + 1], extra_reads=[nrdt])
            P.tt("dve", Er, Er[:, j, :], mg, mg[:], cs, cs[:], ALU.mult)
            P.stt(Ei, Ei[:, j, :], mg, mg[:], -1.0, sn, sn[:], ALU.mult, ALU.mult)
            P.ts("dve", bbr, bbr[:, j, :], bre, bre[:, j, :], coef_r[:, j:j + 1], ALU.mult, extra_reads=[coef_r])
            P.stt(bbr, bbr[:, j, :], bim, bim[:, j, :], ncoef_i[:, j:j + 1], bbr, bbr[:, j, :], ALU.mult, ALU.add,
                  extra_reads=[ncoef_i])
            P.ts("dve", bbi, bbi[:, j, :], bim, bim[:, j, :], coef_r[:, j:j + 1], ALU.mult, extra_reads=[coef_r])
            P.stt(bbi, bbi[:, j, :], bre, bre[:, j, :], coef_i[:, j:j + 1], bbi, bbi[:, j, :], ALU.mult, ALU.add,
                  extra_reads=[coef_i])
            for (src, dst, neg) in ((bbr, BTr, False), (bbi, BTi, False)):
                P.tt("dve", ex, ex[:].rearrange("q (a c) -> q a c", c=16),
                     src, src[:, j, :].unsqueeze(1).broadcast_to([128, 8, 16]),
                     maskB, maskB[:, jj * 128:(jj + 1) * 128].rearrange("q (a c) -> q a c", c=16), ALU.mult)
                ps = next_ps(K)
                P.op("pe", lambda e: e.transpose(ps[:, 0:128], ex[:], ident[:]), reads=[ex, ident], writes=[ps])
                P.copy("act", dst, dst[:, j, :], ps, ps[:, 0:128])
            for (src, dst, neg) in ((cre, CTr, False), (cim, CTin, True)):
                P.tt("dve", ex, ex[:].rearrange("q (a p) -> q a p", p=64),
                     src, src[:, cc, :].unsqueeze(1).broadcast_to([128, 2, 64]),
                     maskC, maskC[:, jj * 128:(jj + 1) * 128].rearrange("q (a p) -> q a p", p=64), ALU.mult)
                ps = next_ps(K)
                P.op("pe", lambda e: e.transpose(ps[:, 0:128], ex[:], ident[:]), reads=[ex, ident], writes=[ps])
                if neg:
                    P.ts("dve", dst, dst[:, j, :], ps, ps[:, 0:128], -1.0, ALU.mult)
                else:
                    P.copy("act", dst, dst[:, j, :], ps, ps[:, 0:128])
        for q4 in range(4):
            P.memset("pool", cars[q4], cars[q4][:], 0.0)
            P.memset("pool", cais[q4], cais[q4][:], 0.0)
        order = (chunks_ctx + chunks_lat) if not rev else (chunks_ctx[::-1] + chunks_lat[::-1])
        K.ps_reserved = {0, 1, 2, 3, 4, 5}
        done = {}

        def quad(ci_, t0, cc):
            ub = u[ci_ % 3]
            yf = yfs[ci_ % 3]
            vq = vqs[ci_ % 2]
            if cc == 0:
                P.dma("sp", ub[:], zT.ap[u_row0:u_row0 + 512, t0:t0 + TT].rearrange("(k q) t -> q k t", q=128),
                      reads=[zT], writes=[ub])
                if rev:
                    P.dma("sp", yf[:], yS.ap[:, t0:t0 + TT].rearrange("(k q) t -> q k t", q=128), reads=[yS], writes=[yf])
            s_ = (ci_ * 4 + cc) % SL
            t1, t2, t3, t4, wr, wi, zr, zi, xr, xi = (t1s[s_], t2s[s_], t3s[s_], t4s[s_], wrs[s_], wis[s_], zrs[s_],
                                                       zis[s_], xrs[s_], xis[s_])
            car, cai, c4, g1, g2 = cars[cc], cais[cc], c4s[s_], g1s[s_], g2s[s_]
            pa, pb = K.ps[2 * s_], K.ps[2 * s_ + 1]
            py = pa
            urhs = ub[:, cc, ::-1] if rev else ub[:, cc, :]
            for jj in range(4):
                j = 4 * cc + jj
                P.mm(pa, pa[:, jj * TT:(jj + 1) * TT], BTr[:, j, :], urhs, True, True, reads=[BTr, ub])
                P.mm(pb, pb[:, jj * TT:(jj + 1) * TT], BTi[:, j, :], urhs, True, True, reads=[BTi, ub])
            yield
            Wr = pa[:, :].rearrange("q (a t) -> q a t", t=TT)
            Wi = pb[:, :].rearrange("q (a t) -> q a t", t=TT)
            sl = slice(4 * cc, 4 * cc + 4)
            P.tt("dve", t1, t1[:], pa, Wr, Er, Er[:, sl, :], ALU.mult)
            P.tt("dve", t2, t2[:], pb, Wi, Ei, Ei[:, sl, :], ALU.mult)
            P.tt("pool", wr, wr[:], t1, t1[:], t2, t2[:], ALU.subtract)
            P.tt("dve", t3, t3[:], pa, Wr, Ei, Ei[:, sl, :], ALU.mult)
            P.tt("dve", t4, t4[:], pb, Wi, Er, Er[:, sl, :], ALU.mult)
            P.tt("pool", wi, wi[:], t3, t3[:], t4, t4[:], ALU.add)
            yield
            for jj in range(4):
                P.op("dve", lambda e: e.tensor_tensor_scan(out=zr[:, jj, :], data0=onesT[:], data1=wr[:, jj, :],
                                                           initial=car[:, jj:jj + 1], op0=ALU.mult, op1=ALU.add),
                     reads=[onesT, wr, car], writes=[zr])
                P.op("dve", lambda e: e.tensor_tensor_scan(out=zi[:, jj, :], data0=onesT[:], data1=wi[:, jj, :],
                                                           initial=cai[:, jj:jj + 1], op0=ALU.mult, op1=ALU.add),
                     reads=[onesT, wi, cai], writes=[zi])
            yield
            zlr = zr[:, :, TT - 1]
            zli = zi[:, :, TT - 1]
            P.tt("dve", c4[0], c4[0][:], zr, zlr, Hr, Hr[:, sl], ALU.mult)
            P.tt("dve", c4[1], c4[1][:], zi, zli, Hi, Hi[:, sl], ALU.mult)
            P.tt("dve", c4[2], c4[2][:], zi, zli, Hr, Hr[:, sl], ALU.mult)
            P.tt("dve", c4[3], c4[3][:], zr, zlr, Hi, Hi[:, sl], ALU.mult)
            P.tt("dve", car, car[:], c4[0], c4[0][:], c4[1], c4[1][:], ALU.subtract)
            P.tt("dve", cai, cai[:], c4[2], c4[2][:], c4[3], c4[3][:], ALU.add)
            P.tt("dve", t1, t1[:], zr, zr[:], Gr, Gr[:, sl, :], ALU.mult)
            P.tt("pool", t2, t2[:], zi, zi[:], Gi, Gi[:, sl, :], ALU.mult)
            P.tt("pool", t4, t4[:], zr, zr[:], Gi, Gi[:, sl, :], ALU.mult)
            P.tt("dve", t3, t3[:], zi, zi[:], Gr, Gr[:, sl, :], ALU.mult)
            P.tt("dve", xr, xr[:], t1, t1[:], t2, t2[:], ALU.subtract)
            P.tt("pool", xi, xi[:], t3, t3[:], t4, t4[:], ALU.add)
            yield
            for jj in range(4):
                j = 4 * cc + jj
                P.mm(py, py[:, :TT], CTr[:, j, :], xr[:, jj, :], jj == 0, False, reads=[CTr, xr])
                P.mm(py, py[:, :TT], CTin[:, j, :], xi[:, jj, :], False, jj == 3, reads=[CTin, xi])
            yield
            if not rev:
                yb = yo[cc % 2]
                P.copy("act", yb, yb[:], py, py[:, :TT])
                P.dma("act", yS.ap[128 * cc:128 * cc + 128, t0:t0 + TT], yb[:], reads=[yb], writes=[yS])
            else:
                P.tt("dve", g1, g1[:], py, py[:, :TT][:, ::-1], yf, yf[:, cc, :], ALU.add)
                P.stt(g1, g1[:], ub, ub[:, cc, :], dsk[:, cc:cc + 1], g1, g1[:], ALU.mult, ALU.add, extra_reads=[dsk])
                P.tt("pool", g2, g2[:], g1, g1[:], g1, g1[:], ALU.mult)
                P.ts("dve", g2, g2[:], g2, g2[:], 0.044715, ALU.mult, 1.0, ALU.add)
                P.tt("dve", g2, g2[:], g2, g2[:], g1, g1[:], ALU.mult)
                P.act(g2, g2[:], g2, g2[:], AF.Sigmoid, scale=1.5957691216057308)
                P.tt("dve", vq[cc], vq[cc][:], g1, g1[:], g2, g2[:], ALU.mult)
                done[ci_] = done.get(ci_, 0) + 1
                if done[ci_] == 4:
                    for mc in range(4):
                        pg = next_ps(K)
                        for kc in range(4):
                            P.mm(pg, pg[:, :TT], wglu[:, kc, mc * 128:(mc + 1) * 128], vq[kc][:], kc == 0, kc == 3,
                                 reads=[wglu, vq[kc]])
                        ob = oo[mc % 2]
                        P.act(ob, ob[:], pg, pg[:, :TT], AF.Sigmoid)
                        P.tt("dve", ob, ob[:], ob, ob[:], vq[mc], vq[mc][:], ALU.mult)
                        P.dma("act", mixT.ap[out_row0 + 128 * mc:out_row0 + 128 * mc + 128, t0:t0 + TT], ob[:],
                              reads=[ob], writes=[mixT])
            yield

        run_interleaved((quad(ci_, t0, cc) for ci_, (t0, is_ctx) in enumerate(order) for cc in range(4)), width=SL)
        K.ps_reserved = set()
    P.release_to(mark)


HY_BANDS = 16
HY_EMB = 33


def hyena_consts(L):
    N = 2 * L
    N1 = N // 128
    R = L // 128
    c = {}
    f64 = np.float64
    n1 = np.arange(R, dtype=f64)[:, None]; k1 = np.arange(N1, dtype=f64)[None, :]
    a = 2 * np.pi * n1 * k1 / N1
    c["F1c"] = np.cos(a); c["F1ns"] = -np.sin(a)
    n2 = np.arange(128, dtype=f64)[:, None]
    a = 2 * np.pi * n2 * k1 / N
    c["Twc"] = np.cos(a); c["Tws"] = np.sin(a)
    c["TwcT"] = np.cos(a).T.copy(); c["TwsT"] = np.sin(a).T.copy()
    k1c = np.arange(N1, dtype=f64)[:, None]; n1r = np.arange(R, dtype=f64)[None, :]
    a = 2 * np.pi * k1c * n1r / N1
    c["I2c"] = np.cos(a) / N; c["I2ns"] = -np.sin(a) / N
    t = np.arange(L, dtype=np.float32)
    t01 = t / np.float32(L)
    bands = np.linspace(1e-4, HY_BANDS - 1, HY_BANDS, dtype=np.float32)
    ang = (np.float32(2.0 * math.pi / L) * t[:, None]) * bands[None, :]
    feats = np.concatenate([t01[:, None], np.cos(ang), -np.sin(ang)], axis=-1)
    c["featsT"] = feats.T.copy()
    return {f"hy{L}_{k}": np.ascontiguousarray(v, dtype=np.float32) for k, v in c.items()}


def hyena_shared_consts():
    n2 = np.arange(128, dtype=np.float64)[:, None]; k2 = np.arange(128, dtype=np.float64)[None, :]
    a = 2 * np.pi * n2 * k2 / 128
    return {"hy_F3c": np.cos(a).astype(np.float32), "hy_F3s": np.sin(a).astype(np.float32),
            "hy_F3ns": (-np.sin(a)).astype(np.float32),
            "hy_tau512": np.tile(np.arange(512, dtype=np.float32)[None, :], (128, 1))}


def run_interleaved(gens, width=2):
    gens = list(gens)
    active = []
    while gens or active:
        while gens and len(active) < width:
            active.append(gens.pop(0))
        for g in list(active):
            try:
                next(g)
            except StopIteration:
                active.remove(g)


def hyena_filters_td(K, i, I, C, L, emit):
    P = K.P
    nc = P.nc
    sb = P.sb
    tau512 = C["hy_tau512"]
    featsD = C[f"hy{L}_featsT_dram"]
    mark1 = P.mark()
    w1 = sb([HY_EMB, 64], F32, "hy_w1"); w2 = sb([64, 64], F32, "hy_w2"); w3 = sb([64, 2048], F32, "hy_w3")
    b1 = sb([64, 1], F32, "hy_b1"); b2 = sb([64, 1], F32, "hy_b2"); fq = sb([64, 1], F32, "hy_fq")
    fb1 = sb([64, 1], F32, "hy_fb1"); fb2 = sb([64, 1], F32, "hy_fb2")
    dec = sb([128, 8], F32, "hy_dec"); decb = sb([128, 8], F32, "hy_decb")
    P.dma("sp", w1[:], I["hy_w1"].ap[i], reads=[I["hy_w1"]], writes=[w1])
    P.dma("sp", w2[:], I["hy_w2"].ap[i], reads=[I["hy_w2"]], writes=[w2])
    P.dma("sp", w3[:], I["hy_w3"].ap[i], reads=[I["hy_w3"]], writes=[w3])
    with nc.allow_non_contiguous_dma(reason="tiny vector load"):
        P.dma("sp", b1[:], I["hy_b1"].ap[i].rearrange("(p o) -> p o", o=1), reads=[I["hy_b1"]], writes=[b1])
        P.dma("sp", b2[:], I["hy_b2"].ap[i].rearrange("(p o) -> p o", o=1), reads=[I["hy_b2"]], writes=[b2])
        P.dma("sp", fq[:], I["hy_freq"].ap[i].rearrange("(p o) -> p o", o=1), reads=[I["hy_freq"]], writes=[fq])
        P.dma("sp", dec[:], I["hy_decay"].ap[i].rearrange("o (k p) -> p (o k)", p=128), reads=[I["hy_decay"]], writes=[dec])
    P.tt("dve", fb1, fb1[:], b1, b1[:], fq, fq[:], ALU.mult)
    P.tt("dve", fb2, fb2[:], b2, b2[:], fq, fq[:], ALU.mult)
    P.act(dec, dec[:], dec, dec[:], AF.Abs)
    P.ts("dve", dec, dec[:], dec, dec[:], -1.0 / L, ALU.mult)
    TC = min(512, L)
    nTC = L // TC
    hid1 = sb([64, L], F32, "hy_hid1"); hid2 = sb([64, L], F32, "hy_hid2")
    ft = sb([HY_EMB, TC], F32, "hy_ft")
    pre = sb([64, 512], F32, "hy_pre"); rr = sb([64, 512], F32, "hy_rr"); rt = sb([64, 512], F32, "hy_rt")
    for tcn in range(nTC):
        P.dma("sp", ft[:], featsD.ap[:, tcn * TC:(tcn + 1) * TC], reads=[featsD], writes=[ft])
        ps = next_ps(K)
        P.mm(ps, ps[:64, :TC], w1[:], ft[:], True, True, reads=[w1, ft])
        P.ts("dve", pre, pre[:, :TC], ps, ps[:64, :TC], fq[:, 0:1], ALU.mult, fb1[:, 0:1], ALU.add, extra_reads=[fq, fb1])
        range_reduce(P, rr, rr[:, :TC], pre, pre[:, :TC], rt, rt[:, :TC])
        P.act(hid1, hid1[:, tcn * TC:(tcn + 1) * TC], rr, rr[:, :TC], AF.Sin)
        ps = next_ps(K)
        P.mm(ps, ps[:64, :TC], w2[:], hid1[:, tcn * TC:(tcn + 1) * TC], True, True, reads=[w2, hid1])
        P.ts("dve", pre, pre[:, :TC], ps, ps[:64, :TC], fq[:, 0:1], ALU.mult, fb2[:, 0:1], ALU.add, extra_reads=[fq, fb2])
        range_reduce(P, rr, rr[:, :TC], pre, pre[:, :TC], rt, rt[:, :TC])
        P.act(hid2, hid2[:, tcn * TC:(tcn + 1) * TC], rr, rr[:, :TC], AF.Sin)
    kf = sb([128, L], F32, "hy_kf"); kb = sb([128, L], F32, "hy_kb")
    win = sb([128, 512], F32, "hy_win"); wb_ = sb([128, 1], F32, "hy_wb")
    sums = sb([128, 2 * nTC + 1], F32, "hy_sums"); tot = sb([128, 1], F32, "hy_tot")
    for o in range(2):
        for cq in range(4):
            oc = o * 4 + cq
            for tcn in range(nTC):
                P.ts("dve", wb_, wb_[:], dec, dec[:, oc:oc + 1], float(tcn * TC), ALU.mult)
                P.act(win, win[:, :TC], tau512, tau512[:, :TC], AF.Exp, bias=wb_[:, 0:1], scale=dec[:, oc:oc + 1],
                      extra_reads=[wb_, dec])
                for dr, kt in ((0, kf), (1, kb)):
                    col0 = dr * 1024 + o * 512 + cq * 128
                    ps = next_ps(K)
                    P.mm(ps, ps[:, :TC], w3[:, col0:col0 + 128], hid2[:, tcn * TC:(tcn + 1) * TC], True, True, reads=[w3, hid2])
                    P.stt(kt, kt[:, tcn * TC:(tcn + 1) * TC], win, win[:, :TC], HY_WINDOW_SHIFT, ps, ps[:, :TC], ALU.add, ALU.mult)
                    P.op("dve", lambda e: e.tensor_reduce(out=sums[:, dr * nTC + tcn:dr * nTC + tcn + 1],
                                                          in_=kt[:, tcn * TC:(tcn + 1) * TC], axis=AX.X, op=ALU.add,
                                                          apply_absolute_value=True), reads=[kt], writes=[sums])
            P.act(sums, sums[:, 2 * nTC:2 * nTC + 1], kb, kb[:, 0:1], AF.Abs)
            P.ts("dve", sums, sums[:, 2 * nTC:2 * nTC + 1], sums, sums[:, 2 * nTC:2 * nTC + 1], -1.0, ALU.mult)
            P.op("dve", lambda e: e.tensor_reduce(out=tot[:], in_=sums[:], axis=AX.X, op=ALU.add), reads=[sums], writes=[tot])
            P.op("dve", lambda e: e.reciprocal(out=tot[:], in_=tot[:]), reads=[tot], writes=[tot])
            P.memset("dve", kb, kb[:, 0:1], 0.0)
            P.ts("dve", kf, kf[:], kf, kf[:], tot[:, 0:1], ALU.mult, extra_reads=[tot])
            P.ts("pool", kb, kb[:], kb, kb[:], tot[:, 0:1], ALU.mult, extra_reads=[tot])
            emit(o, cq, kf, kb)
    P.release_to(mark1)


def hyena_stage(K, i, I, zT, mixT, C, L, t_base, KFr, KFi, kT, tag):
    P = K.P
    nc = P.nc
    N = 2 * L
    N1 = N // 128
    R = L // 128
    G = min(512 // N1, 4)
    W = G * N1
    WT = G * 128
    mark = P.mark()
    sb = P.sb
    cn = lambda k: C[f"hy{L}_{k}"]
    tau512 = C["hy_tau512"]
    Twc, Tws, TwcT, TwsT = (cn(k) for k in ("Twc", "Tws", "TwcT", "TwsT"))
    featsD = C[f"hy{L}_featsT_dram"]

    def bf(src, nm):
        if HY_DT == F32:
            return src
        t = sb(list(src.ap.shape), HY_DT, "hyb_" + nm)
        P.copy("pool", t, t[:], src, src[:])
        return t
    F3c, F3s, F3ns = bf(C["hy_F3c"], "F3c"), bf(C["hy_F3s"], "F3s"), bf(C["hy_F3ns"], "F3ns")
    F1c, F1ns, I2c, I2ns = bf(cn("F1c"), "F1c"), bf(cn("F1ns"), "F1ns"), bf(cn("I2c"), "I2c"), bf(cn("I2ns"), "I2ns")
    NS = 2
    mq = [[sb([128, 512], HY_DT, f"hy_mq{k}{j}") for j in range(4)] for k in range(NS)]
    m1s = [sb([128, 512], F32, f"hy_m1{k}") for k in range(NS)]; m2s = [sb([128, 512], F32, f"hy_m2{k}") for k in range(NS)]

    def neg(src, nm):
        t = sb(list(src.ap.shape), HY_DT, "hyn_" + nm)
        P.ts("dve", t, t[:], src, src[:], -1.0, ALU.mult)
        return t
    F3nc = neg(F3c, "F3nc")
    I2nc = neg(I2c, "I2nc")

    def fft_fwd(U, psr, psi, sl):
        q1, q2, q3, q4 = mq[sl]
        pa, pb = K.ps[4 * sl], K.ps[4 * sl + 1]
        Ut, Uf = U
        for c in range(G):
            P.mm(pa, pa[:, c * N1:(c + 1) * N1], Uf(c), F1c[:R, :], True, True, reads=[Ut, F1c])
            P.mm(pb, pb[:, c * N1:(c + 1) * N1], Uf(c), F1ns[:R, :], True, True, reads=[Ut, F1ns])
        yield
        v3 = lambda ap: ap.rearrange("q (c k) -> q c k", k=N1)
        tc_ = Twc[:, :].unsqueeze(1).broadcast_to([128, G, N1])
        ts_ = Tws[:, :].unsqueeze(1).broadcast_to([128, G, N1])
        P.tt("dve", q1, v3(q1[:, :W]), pa, v3(pa[:, :W]), Twc, tc_, ALU.mult)
        P.tt("dve", q2, v3(q2[:, :W]), pb, v3(pb[:, :W]), Tws, ts_, ALU.mult)
        P.tt("dve", q3, v3(q3[:, :W]), pb, v3(pb[:, :W]), Twc, tc_, ALU.mult)
        P.tt("dve", q4, v3(q4[:, :W]), pa, v3(pa[:, :W]), Tws, ts_, ALU.mult)
        yield
        for idx, (w_, q_) in enumerate(((F3c, q1), (F3c, q2), (F3s, q3), (F3ns, q4))):
            P.mm(psr, psr[:, :W], w_[:], q_[:, :W], idx == 0, idx == 3, reads=[w_, q_])
        for idx, (w_, q_) in enumerate(((F3c, q3), (F3nc, q4), (F3ns, q1), (F3ns, q2))):
            P.mm(psi, psi[:, :W], w_[:], q_[:, :W], idx == 0, idx == 3, reads=[w_, q_])
        yield

    def fft_inv(Yr, Yi, psy, sl):
        q1, q2, q3, q4 = mq[sl]
        pa, pb = K.ps[4 * sl], K.ps[4 * sl + 1]
        for c in range(G):
            yr = Yr[:, c * N1:(c + 1) * N1]
            yi = Yi[:, c * N1:(c + 1) * N1]
            P.mm(pa, pa[:N1, c * 128:(c + 1) * 128], yr, F3c[:], True, False, reads=[Yr, F3c])
            P.mm(pa, pa[:N1, c * 128:(c + 1) * 128], yi, F3ns[:], False, True, reads=[Yi, F3ns])
            P.mm(pb, pb[:N1, c * 128:(c + 1) * 128], yr, F3s[:], True, False, reads=[Yr, F3s])
            P.mm(pb, pb[:N1, c * 128:(c + 1) * 128], yi, F3c[:], False, True, reads=[Yi, F3c])
        yield
        v3 = lambda ap: ap.rearrange("q (c k) -> q c k", k=128)
        tc_ = TwcT[:N1, :].unsqueeze(1).broadcast_to([N1, G, 128])
        ts_ = TwsT[:N1, :].unsqueeze(1).broadcast_to([N1, G, 128])
        P.tt("dve", q1, v3(q1[:N1, :WT]), pa, v3(pa[:N1, :WT]), TwcT, tc_, ALU.mult)
        P.tt("dve", q2, v3(q2[:N1, :WT]), pb, v3(pb[:N1, :WT]), TwsT, ts_, ALU.mult)
        P.tt("dve", q3, v3(q3[:N1, :WT]), pa, v3(pa[:N1, :WT]), TwsT, ts_, ALU.mult)
        P.tt("dve", q4, v3(q4[:N1, :WT]), pb, v3(pb[:N1, :WT]), TwcT, tc_, ALU.mult)
        yield
        for idx, (w_, q_) in enumerate(((I2c, q1), (I2nc, q2), (I2ns, q3), (I2ns, q4))):
            P.mm(psy, psy[:R, :WT], w_[:N1, :], q_[:N1, :WT], idx == 0, idx == 3, reads=[w_, q_])
        yield

    def emit_kT(o, cq, kf, kb):
        r0 = o * 512 + cq * 128
        P.dma("sp", kT.ap[r0:r0 + 128, 0:L], kf[:], reads=[kf], writes=[kT])
        P.dma("sp", kT.ap[1024 + r0:1024 + r0 + 128, 0:L], kb[:], reads=[kb], writes=[kT])

    hyena_filters_td(K, i, I, C, L, emit_kT)

    mark2 = P.mark()
    Uf32 = [sb([R, G, 128], F32, f"hy_Uf{k}") for k in range(NS)]
    Ub32 = [sb([R, G, 128], F32, f"hy_Ub{k}") for k in range(NS)]
    Uf_ = [sb([R, G, 128], HY_DT, f"hy_Ufb{k}") for k in range(NS)]
    Ub_ = [sb([R, G, 128], HY_DT, f"hy_Ubb{k}") for k in range(NS)]
    Xs = [[sb([128, 512], F32, f"hy_Xs{k}{m}") for m in range(2)] for k in range(NS)]
    Ko = [[sb([128, 512], F32, f"hy_Ko{k}{m}") for m in range(2)] for k in range(NS)]

    def filt_group(gi):
        sl = gi % NS
        oc0 = gi * G
        uf, ub = Uf_[sl], Ub_[sl]
        P.dma("sp", Uf32[sl][:], kT.ap[oc0:oc0 + G, 0:L].rearrange("c (a b) -> a c b", b=128), reads=[kT], writes=[Uf32[sl]])
        P.dma("sp", Ub32[sl][:], kT.ap[1024 + oc0:1024 + oc0 + G, 0:L].rearrange("c (a b) -> a c b", b=128), reads=[kT], writes=[Ub32[sl]])
        P.copy("act", uf, uf[:], Uf32[sl], Uf32[sl][:])
        P.copy("act", ub, ub[:], Ub32[sl], Ub32[sl][:])
        pfr, pfi = K.ps[4 * sl + 2], K.ps[4 * sl + 3]
        yield from fft_fwd((uf, lambda c: uf[:, c, :]), pfr, pfi, sl)
        P.copy("act", Xs[sl][0], Xs[sl][0][:, :W], pfr, pfr[:, :W])
        P.copy("act", Xs[sl][1], Xs[sl][1][:, :W], pfi, pfi[:, :W])
        pbr, pbi = K.ps[4 * sl + 2], K.ps[4 * sl + 3]
        yield from fft_fwd((ub, lambda c: ub[:, c, :]), pbr, pbi, sl)
        kr, ki = Ko[sl]
        P.tt("dve", kr, kr[:, :W], Xs[sl][0], Xs[sl][0][:, :W], pbr, pbr[:, :W], ALU.add)
        P.tt("dve", ki, ki[:, :W], Xs[sl][1], Xs[sl][1][:, :W], pbi, pbi[:, :W], ALU.subtract)
        P.dma("pool", KFr.ap[oc0 // 4, :, 0:W], kr[:, :W], reads=[kr], writes=[KFr])
        P.dma("pool", KFi.ap[oc0 // 4, :, 0:W], ki[:, :W], reads=[ki], writes=[KFi])
        yield

    run_interleaved((filt_group(gi) for gi in range(1024 // G)), width=NS)
    P.release_to(mark2)

    cw = sb([R, 1536, 3], F32, "hy_cw"); cb = sb([R, 1536], F32, "hy_cb"); hb = sb([R, 1024], F32, "hy_hb")
    with nc.allow_non_contiguous_dma(reason="broadcast parameter loads"):
        for j in range(3):
            P.dma("sp", cw[:, :, j], I["ev_conv_w"].ap[i, j:j + 1, :].partition_broadcast(R), reads=[I["ev_conv_w"]], writes=[cw])
        P.dma("sp", cb[:], I["ev_conv_b"].ap[i].rearrange("(o c) -> o c", o=1).partition_broadcast(R), reads=[I["ev_conv_b"]], writes=[cb])
        P.dma("sp", hb[:], I["hy_bias"].ap[i].rearrange("o c -> (o c)").rearrange("(o c) -> o c", o=1).partition_broadcast(R),
              reads=[I["hy_bias"]], writes=[hb])
    raw = [[sb([R, G, 130], F32, f"hy_raw{k}{m}") for m in range(3)] for k in range(NS)]
    cvs = [[sb([R, G, 128], F32, f"hy_cv{k}{m}") for m in range(3)] for k in range(NS)]
    cts = [sb([R, G, 128], F32, f"hy_ct{k}") for k in range(NS)]
    ybs = [sb([R, G, 128], HY_DT, f"hy_yb{k}") for k in range(NS)]
    kfr = [[sb([128, 512], F32, f"hy_kfr{k}{o}") for o in range(2)] for k in range(NS)]
    kfi = [[sb([128, 512], F32, f"hy_kfi{k}{o}") for o in range(2)] for k in range(NS)]
    Yrs = [sb([128, 512], HY_DT, f"hy_Yr{k}") for k in range(NS)]; Yis = [sb([128, 512], HY_DT, f"hy_Yi{k}") for k in range(NS)]
    y1s = [sb([R, G, 128], F32, f"hy_y1{k}") for k in range(NS)]; youts = [sb([R, G, 128], F32, f"hy_yout{k}") for k in range(NS)]
    for k in range(NS):
        for m in range(3):
            P.memset("pool", raw[k][m], raw[k][m][:], 0.0)

    def data_group(gi):
        sl = gi % NS
        c0 = gi * G
        rw, cv, ct, yb = raw[sl], cvs[sl], cts[sl], ybs[sl]
        m1, m2 = m1s[sl], m2s[sl]
        for m in range(3):
            rows = zT.ap[m * 512 + c0:m * 512 + c0 + G, :]
            P.dma("sp", rw[m][:, :, 1:129], rows[:, t_base:t_base + L].rearrange("c (a b) -> a c b", b=128),
                  reads=[zT], writes=[rw[m]])
            if R > 1:
                with nc.allow_non_contiguous_dma(reason="1-column conv halos"):
                    P.dma("sp", rw[m][1:R, :, 0:1], rows[:, t_base + 127:t_base + L - 1].rearrange("c (a b) -> a c b", b=128)[:, :, 0:1],
                          reads=[zT], writes=[rw[m]])
                    P.dma("sp", rw[m][0:R - 1, :, 129:130], rows[:, t_base + 128:t_base + L].rearrange("c (a b) -> a c b", b=128)[:, :, 0:1],
                          reads=[zT], writes=[rw[m]])
        for o in range(2):
            oc0 = o * 512 + c0
            P.dma("sp", kfr[sl][o][:, :W], KFr.ap[oc0 // 4, :, 0:W], reads=[KFr], writes=[kfr[sl][o]])
            P.dma("sp", kfi[sl][o][:, :W], KFi.ap[oc0 // 4, :, 0:W], reads=[KFi], writes=[kfi[sl][o]])
        for m in range(3):
            ch = slice(m * 512 + c0, m * 512 + c0 + G)
            wj = lambda j: cw[:, ch, j].unsqueeze(2).broadcast_to([R, G, 128])
            P.tt("dve", cv[m], cv[m][:], rw[m], rw[m][:, :, 0:128], cw, wj(0), ALU.mult)
            P.tt("pool", ct, ct[:], rw[m], rw[m][:, :, 1:129], cw, wj(1), ALU.mult)
            P.tt("dve", cv[m], cv[m][:], cv[m], cv[m][:], ct, ct[:], ALU.add)
            P.tt("pool", ct, ct[:], rw[m], rw[m][:, :, 2:130], cw, wj(2), ALU.mult)
            P.tt("dve", cv[m], cv[m][:], cv[m], cv[m][:], ct, ct[:], ALU.add)
            P.tt("dve", cv[m], cv[m][:], cv[m], cv[m][:], cb, cb[:, ch].unsqueeze(2).broadcast_to([R, G, 128]), ALU.add)
        yield
        ycur = cv[0]
        for o in range(2):
            P.copy("act", yb, yb[:], ycur, ycur[:])
            pxr, pxi = K.ps[4 * sl + 2], K.ps[4 * sl + 3]
            yield from fft_fwd((yb, lambda c: yb[:, c, :]), pxr, pxi, sl)
            kr, ki = kfr[sl][o], kfi[sl][o]
            Yr, Yi = Yrs[sl], Yis[sl]
            P.tt("dve", m1, m1[:, :W], pxr, pxr[:, :W], kr, kr[:, :W], ALU.mult)
            P.tt("dve", m2, m2[:, :W], pxi, pxi[:, :W], ki, ki[:, :W], ALU.mult)
            P.tt("pool", Yr, Yr[:, :W], m1, m1[:, :W], m2, m2[:, :W], ALU.subtract)
            P.tt("dve", m1, m1[:, :W], pxr, pxr[:, :W], ki, ki[:, :W], ALU.mult)
            P.tt("dve", m2, m2[:, :W], pxi, pxi[:, :W], kr, kr[:, :W], ALU.mult)
            P.tt("pool", Yi, Yi[:, :W], m1, m1[:, :W], m2, m2[:, :W], ALU.add)
            yield
            py = K.ps[4 * sl + 2]
            yield from fft_inv(Yr, Yi, py, sl)
            hbo = hb[:, o * 512 + c0:o * 512 + c0 + G].unsqueeze(2).broadcast_to([R, G, 128])
            P.tt("dve", ct, ct[:], ycur, ycur[:], hb, hbo, ALU.mult)
            P.tt("dve", ct, ct[:], ct, ct[:], py, py[:R, :WT].rearrange("q (c k) -> q c k", k=128), ALU.add)
            dst = y1s[sl] if o == 0 else youts[sl]
            P.tt("dve", dst, dst[:], ct, ct[:], cv[1 + o], cv[1 + o][:], ALU.mult)
            ycur = dst
            yield
        P.dma("pool", mixT.ap[c0:c0 + G, t_base:t_base + L].rearrange("c (a b) -> a c b", b=128), ycur[:],
              reads=[ycur], writes=[mixT])
        yield

    run_interleaved((data_group(gi) for gi in range(512 // G)), width=NS)
    P.release_to(mark)


def hyena_ctx_consts():
    L, N = 256, 512
    t = np.arange(L, dtype=np.float64)[:, None]; f = np.arange(N, dtype=np.float64)[None, :]
    a = 2 * np.pi * t * f / N
    c = {"hc_Fc": np.cos(a), "hc_Fns": -np.sin(a), "hc_Ic": np.cos(a).T / N, "hc_Ins": -np.sin(a).T / N}
    return {k: np.ascontiguousarray(v, dtype=np.float32) for k, v in c.items()}


def hyena_ctx_stage(K, i, I, zT, mixT, C, Cd, t_base=0):
    P = K.P
    nc = P.nc
    sb = P.sb
    L, N = 256, 512
    mark = P.mark()
    ident = C["k_ident"]
    Fc = sb([128, 2, 512], F32, "hc_Fc"); Fns = sb([128, 2, 512], F32, "hc_Fns")
    Ic = sb([128, 4, 256], F32, "hc_Ic"); Ins = sb([128, 4, 256], F32, "hc_Ins")
    P.dma("sp", Fc[:], Cd["hc_Fc"].ap.rearrange("(k p) f -> p k f", p=128), reads=[Cd["hc_Fc"]], writes=[Fc])
    P.dma("sp", Fns[:], Cd["hc_Fns"].ap.rearrange("(k p) f -> p k f", p=128), reads=[Cd["hc_Fns"]], writes=[Fns])
    P.dma("sp", Ic[:], Cd["hc_Ic"].ap.rearrange("(k p) t -> p k t", p=128), reads=[Cd["hc_Ic"]], writes=[Ic])
    P.dma("sp", Ins[:], Cd["hc_Ins"].ap.rearrange("(k p) t -> p k t", p=128), reads=[Cd["hc_Ins"]], writes=[Ins])
    ktm = [[sb([128, 2, 512], F32, f"hc_ktm{dr}{o}") for o in range(2)] for dr in range(2)]

    def emit(o, cq, kf, kb):
        for (src, dr) in ((kf, 0), (kb, 1)):
            for tc in range(2):
                ps = next_ps(K)
                P.op("pe", lambda e: e.transpose(ps[:, 0:128], src[:, tc * 128:(tc + 1) * 128], ident[:]), reads=[src, ident], writes=[ps])
                P.copy("act", ktm[dr][o], ktm[dr][o][:, tc, cq * 128:(cq + 1) * 128], ps, ps[:, 0:128])

    hyena_filters_td(K, i, I, C, L, emit)

    def dft(x_t, fc):
        psr = next_ps(K); psi = next_ps(K)
        for tc in range(2):
            P.mm(psr, psr[:, :], Fc[:, tc, fc * 128:(fc + 1) * 128], x_t[:, tc, :], tc == 0, tc == 1, reads=[Fc, x_t])
        for tc in range(2):
            P.mm(psi, psi[:, :], Fns[:, tc, fc * 128:(fc + 1) * 128], x_t[:, tc, :], tc == 0, tc == 1, reads=[Fns, x_t])
        return psr, psi

    KFr = [sb([128, 4, 512], F32, f"hc_KFr{o}") for o in range(2)]; KFi = [sb([128, 4, 512], F32, f"hc_KFi{o}") for o in range(2)]
    Xr = sb([128, 512], F32, "hc_Xr"); Xi = sb([128, 512], F32, "hc_Xi")
    for o in range(2):
        for fc in range(4):
            pr, pi_ = dft(ktm[0][o], fc)
            P.copy("act", Xr, Xr[:], pr, pr[:, :])
            P.copy("act", Xi, Xi[:], pi_, pi_[:, :])
            pr, pi_ = dft(ktm[1][o], fc)
            P.tt("dve", KFr[o], KFr[o][:, fc, :], Xr, Xr[:], pr, pr[:, :], ALU.add)
            P.tt("dve", KFi[o], KFi[o][:, fc, :], Xi, Xi[:], pi_, pi_[:, :], ALU.subtract)
    cwt = sb([128, 12, 3], F32, "hc_cw"); cbt = sb([128, 12], F32, "hc_cb"); hbb = sb([128, 2, 512], F32, "hc_hb")
    with nc.allow_non_contiguous_dma(reason="small parameter loads"):
        for j in range(3):
            P.dma("sp", cwt[:, :, j], I["ev_conv_w"].ap[i, j].rearrange("(k p) -> p k", p=128), reads=[I["ev_conv_w"]], writes=[cwt])
        P.dma("sp", cbt[:], I["ev_conv_b"].ap[i].rearrange("(k p) -> p k", p=128), reads=[I["ev_conv_b"]], writes=[cbt])
        P.dma("sp", hbb[:].rearrange("p o c -> p (o c)"),
              I["hy_bias"].ap[i].rearrange("o c -> (o c)").rearrange("(a n) -> a n", a=1).partition_broadcast(128),
              reads=[I["hy_bias"]], writes=[hbb])
    xtm = [sb([128, 2, 512], F32, f"hc_xtm{m}") for m in range(3)]
    raw = [sb([128, 258], F32, f"hc_raw{k}") for k in range(2)]
    cv = [sb([128, 256], F32, f"hc_cv{k}") for k in range(2)]
    for k in range(2):
        P.memset("pool", raw[k], raw[k][:], 0.0)
    for m in range(3):
        for cq in range(4):
            mc = m * 4 + cq
            rw, cvb = raw[mc % 2], cv[mc % 2]
            P.dma("sp", rw[:, 1:257], zT.ap[mc * 128:(mc + 1) * 128, t_base:t_base + L], reads=[zT], writes=[rw])
            P.ts("dve", cvb, cvb[:], rw, rw[:, 0:256], cwt[:, mc, 0:1], ALU.mult, cbt[:, mc:mc + 1], ALU.add, extra_reads=[cwt, cbt])
            P.stt(cvb, cvb[:], rw, rw[:, 1:257], cwt[:, mc, 1:2], cvb, cvb[:], ALU.mult, ALU.add, extra_reads=[cwt])
            P.stt(cvb, cvb[:], rw, rw[:, 2:258], cwt[:, mc, 2:3], cvb, cvb[:], ALU.mult, ALU.add, extra_reads=[cwt])
            for tc in range(2):
                ps = next_ps(K)
                P.op("pe", lambda e: e.transpose(ps[:, 0:128], cvb[:, tc * 128:(tc + 1) * 128], ident[:]), reads=[cvb, ident], writes=[ps])
                P.copy("act", xtm[m], xtm[m][:, tc, cq * 128:(cq + 1) * 128], ps, ps[:, 0:128])
    Yr = sb([128, 4, 512], F32, "hc_Yr"); Yi = sb([128, 4, 512], F32, "hc_Yi")
    ya = sb([128, 2, 512], F32, "hc_ya"); yb_ = sb([128, 2, 512], F32, "hc_yb")
    t1 = sb([128, 512], F32, "hc_t1"); t2 = sb([128, 512], F32, "hc_t2")
    ycur = xtm[0]
    for o in range(2):
        for fc in range(4):
            pr, pi_ = dft(ycur, fc)
            P.tt("dve", t1, t1[:], pr, pr[:, :], KFr[o], KFr[o][:, fc, :], ALU.mult)
            P.tt("dve", t2, t2[:], pi_, pi_[:, :], KFi[o], KFi[o][:, fc, :], ALU.mult)
            P.tt("pool", Yr, Yr[:, fc, :], t1, t1[:], t2, t2[:], ALU.subtract)
            P.tt("dve", t1, t1[:], pr, pr[:, :], KFi[o], KFi[o][:, fc, :], ALU.mult)
            P.tt("dve", t2, t2[:], pi_, pi_[:, :], KFr[o], KFr[o][:, fc, :], ALU.mult)
            P.tt("pool", Yi, Yi[:, fc, :], t1, t1[:], t2, t2[:], ALU.add)
        dst = ya if o == 0 else yb_
        for tc in range(2):
            py = next_ps(K)
            for fc in range(4):
                P.mm(py, py[:, :], Ic[:, fc, tc * 128:(tc + 1) * 128], Yr[:, fc, :], fc == 0, False, reads=[Ic, Yr])
                P.mm(py, py[:, :], Ins[:, fc, tc * 128:(tc + 1) * 128], Yi[:, fc, :], False, fc == 3, reads=[Ins, Yi])
            P.tt("dve", t1, t1[:], ycur, ycur[:, tc, :], hbb, hbb[:, o, :], ALU.mult)
            P.tt("dve", t1, t1[:], t1, t1[:], py, py[:, :], ALU.add)
            P.tt("dve", dst, dst[:, tc, :], t1, t1[:], xtm[1 + o], xtm[1 + o][:, tc, :], ALU.mult)
        ycur = dst
    ofm = sb([128, 4, 256], F32, "hc_ofm")
    for cq in range(4):
        for tc in range(2):
            ps = next_ps(K)
            P.op("pe", lambda e: e.transpose(ps[:, 0:128], ycur[:, tc, cq * 128:(cq + 1) * 128], ident[:]), reads=[ycur, ident], writes=[ps])
            P.copy("act", ofm, ofm[:, cq, tc * 128:(tc + 1) * 128], ps, ps[:, 0:128])
    P.dma("sp", mixT.ap[0:512, t_base:t_base + L].rearrange("(k p) t -> p k t", p=128), ofm[:], reads=[ofm], writes=[mixT])
    P.release_to(mark)


_DBG_SKIP_ATTN = False
MLA_SCALE = 96.0 ** -0.5
GQA_SCALE = 64.0 ** -0.5
GRID_W = 64
ROPE_BASE = 10000.0


def odd_segs():
    segs = [(0, 1184), (392, 8), (384, 8), (408, 8), (400, 8)]
    for hq in range(8):
        b = 416 + 64 * hq
        segs += [(b + 16, 16), (b, 16), (b + 48, 16), (b + 32, 16)]
    for kh in range(2):
        b = 928 + 64 * kh
        segs += [(b + 16, 16), (b, 16), (b + 48, 16), (b + 32, 16)]
    return segs


def rope_consts(L):
    out = {}
    rows = L // GRID_W
    row = np.repeat(np.arange(rows), GRID_W).astype(np.float32)
    col = np.tile(np.arange(GRID_W), rows).astype(np.float32)
    for dim, nm in ((32, "mla"), (64, "gqa")):
        nf = dim // 4
        inv = (np.float32(ROPE_BASE) ** (-np.arange(nf, dtype=np.float32) / np.float32(nf))).astype(np.float32)
        ar = (row[:, None] * inv[None, :]).astype(np.float32)
        ac = (col[:, None] * inv[None, :]).astype(np.float32)
        cr, sr, cc, sc = np.cos(ar), np.sin(ar), np.cos(ac), np.sin(ac)
        C = np.concatenate([cr, cr, cc, cc], axis=1).T
        S = np.concatenate([-sr, sr, -sc, sc], axis=1).T
        out[f"rope_{nm}_C"] = np.ascontiguousarray(C, dtype=np.float32)
        out[f"rope_{nm}_S"] = np.ascontiguousarray(S, dtype=np.float32)
    j = np.arange(128)[:, None]; r = np.arange(128)[None, :]
    out["k_mask_prev"] = np.tile((j >= r).astype(np.float32), (1, 4))
    out["k_mask_next"] = np.tile((j <= r).astype(np.float32), (1, 4))
    selM = np.zeros((96, 97), np.float32); selM[:, 96] = 1.0
    selG = np.zeros((64, 65), np.float32); selG[:, 64] = 1.0
    sel65 = np.zeros((65, 64), np.float32); sel65[64, :] = 1.0
    vs = np.zeros((1, 65), np.float32); vs[0, 64] = 1.0
    out["k_selM"] = selM; out["k_selG"] = selG; out["k_sel65"] = sel65; out["k_vsink"] = vs
    return out


def attn_core(K, A, q_t, q_ap, nq, chunks, outs):
    P = K.P
    pot = A.pot
    n = len(chunks)
    LOOK = 3

    def score(ci):
        k_t, k_ap, v_t, v_ap, mask, cols, qrows = chunks[ci]
        c0, ncol = cols if cols is not None else (0, nq)
        nk = k_ap.shape[1]
        ps = next_ps(K)
        q_use = q_ap[:, c0:c0 + ncol] if qrows is None else q_ap[qrows[0]:qrows[1], c0:c0 + ncol]
        P.mm(ps, ps[:nk, c0:c0 + ncol], k_ap, q_use, True, True, reads=[k_t, q_t])
        return ps

    pend = {}
    for ci in range(min(LOOK, n)):
        pend[ci] = score(ci)
    for ci, (k_t, k_ap, v_t, v_ap, mask, cols, qrows) in enumerate(chunks):
        c0, ncol = cols if cols is not None else (0, nq)
        nk = k_ap.shape[1]
        ps = pend.pop(ci)
        pt = A.pt[A.pti % len(A.pt)]
        A.pti += 1
        P.act(pt, pt[:nk, c0:c0 + ncol], ps, ps[:nk, c0:c0 + ncol], AF.Exp)
        if mask is not None:
            P.tt("dve", pt, pt[:nk, c0:c0 + ncol], pt, pt[:nk, c0:c0 + ncol], mask[0], mask[1], ALU.mult)
        if ci + LOOK < n:
            pend[ci + LOOK] = score(ci + LOOK)
        P.mm(pot, pot[:65, c0:c0 + ncol], v_ap, pt[:nk, c0:c0 + ncol], ci == 0, ci == n - 1, reads=[v_t, pt])
    P.copy("act", A.ot, A.ot[:65, :nq], pot, pot[:65, :nq])
    ps = next_ps(K)
    P.mm(ps, ps[:64, :nq], A.sel65[:, :], A.ot[:65, :nq], True, True, reads=[A.sel65, A.ot])
    P.op("dve", lambda e: e.reciprocal(out=A.rden[:64, :nq], in_=ps[:64, :nq]), reads=[ps], writes=[A.rden])
    ob = A.ob[A.obi % 2]
    A.obi += 1
    P.tt("dve", ob, ob[:64, :nq], A.ot, A.ot[:64, :nq], A.rden, A.rden[:64, :nq], ALU.mult)
    for (dap, c0, ncol) in outs:
        P.dma("pool", dap, ob[:64, c0:c0 + ncol], reads=[ob], writes=[A.mixT] + ([A.mixH] if A.mixH is not None else []))


def odd_mixer_stage(K, i, I, zT, mixT, C, S, need_ctx, qsel=None):
    P = K.P
    nc = P.nc
    sb = P.sb
    NT, NCX = K.NT, K.NC
    mark = P.mark()
    tiles = ([(0, NCX, 1)] if NCX else []) + [(NCX + k * 512, min(512, K.NL - k * 512), 0) for k in range((K.NL + 511) // 512)]
    wst = sb([128, 1024], F32, "od_wst")
    wukv = sb([128, 1024], BF16, "od_wukv")
    P.dma("sp", wst[:], I["mla_w_ukv"].ap[i], reads=[I["mla_w_ukv"]], writes=[wst])
    P.copy("pool", wukv, wukv[:], wst, wst[:])
    wuq = sb([128, 2, 768], BF16, "od_wuq"); wuqs = sb([128, 2, 768], BF16, "od_wuqs")
    wst2 = sb([128, 2, 768], F32, "od_wst2")
    P.dma("sp", wst2[:], I["mla_w_uq"].ap[i].rearrange("(k p) n -> p k n", p=128), reads=[I["mla_w_uq"]], writes=[wst2])
    P.copy("pool", wuq, wuq[:], wst2, wst2[:])
    P.memset("pool", wuqs, wuqs[:], 0.0)
    for h in range(8):
        b = h * 96 + 64
        for (dst, src) in ((0, 8), (8, 0), (16, 24), (24, 16)):
            P.copy("pool", wuqs, wuqs[:, :, b + dst:b + dst + 8], wst2, wst2[:, :, b + src:b + src + 8])
    gq = sb([128, 2], F32, "od_gq"); gkv = sb([128, 1], F32, "od_gkv")
    sinkb = sb([96, 8], F32, "od_sink")
    with nc.allow_non_contiguous_dma(reason="tiny vector loads"):
        P.dma("sp", gq[:], I["mla_q_norm"].ap[i].rearrange("(k p) -> p k", p=128), reads=[I["mla_q_norm"]], writes=[gq])
        P.dma("sp", gkv[:], I["mla_kv_norm"].ap[i].rearrange("(p o) -> p o", o=1), reads=[I["mla_kv_norm"]], writes=[gkv])
        P.dma("sp", sinkb[64:96, :], I["gqa_sink"].ap[i].rearrange("(o h) -> o h", o=1).partition_broadcast(32),
              reads=[I["gqa_sink"]], writes=[sinkb])
    selM, selG, sel65, vsink = C["k_selM"], C["k_selG"], C["k_sel65"], C["k_vsink"]
    ident = C["k_ident"]
    vsb = sb([1, 65], BF16, "od_vsb")
    P.copy("dve", vsb, vsb[:], vsink, vsink[:])
    kmaxM = sb([128, 8], F32, "od_kmaxM"); kmaxG = sb([128, 2], F32, "od_kmaxG")
    P.memset("pool", kmaxM, kmaxM[:], 0.0)
    P.memset("pool", kmaxG, kmaxG[:], 0.0)
    mx1 = sb([128, 1], F32, "od_mx1")
    markp = P.mark()
    xin = [sb([128, 2, 512], F32, f"od_xin{k}") for k in range(2)]
    sq = sb([128, 2, 512], F32, "od_sq"); rstd = sb([128, 512], F32, "od_rstd")
    kvn = sb([128, 512], BF16, "od_kvn"); cqn = sb([128, 2, 512], BF16, "od_cqn")
    raw = [sb([128, 512], F32, f"od_raw{k}") for k in range(2)]; sw = [sb([128, 512], F32, f"od_sw{k}") for k in range(2)]
    tC = [sb([128, 512], F32, f"od_tC{k}") for k in range(2)]; tS = [sb([128, 512], F32, f"od_tS{k}") for k in range(2)]
    rot = sb([128, 512], F32, "od_rot"); rt2 = sb([128, 512], F32, "od_rt2")
    kfs = [sb([96, 512], F32, f"od_kf{k}") for k in range(2)]; kf2s = [sb([96, 512], F32, f"od_kf2{k}") for k in range(2)]
    mx1s = [sb([128, 1], F32, f"od_mx1{k}") for k in range(2)]
    kt = [sb([97, 512], BF16, f"od_kt{k}") for k in range(2)]
    vt = [sb([128, 8, 65], BF16, f"od_vt{k}") for k in range(2)]
    vg = [sb([128, 2, 65], BF16, f"od_vg{k}") for k in range(2)]
    gvt = sb([128, 512], F32, "od_gvt")
    for k in range(2):
        P.memset("pool", kt[k], kt[k][96:97, :], 1.0)
        P.memset("pool", vt[k], vt[k][:], 1.0)
        P.memset("pool", vg[k], vg[k][:], 1.0)

    def rms(x_t, x_ap_k, nk_, n, g_t, g_ap_k, out_t, out_ap_k):
        ps = next_ps(K)
        for k in range(nk_):
            P.act(sq, sq[:, k, :n], x_t, x_ap_k(k), AF.Square)
        for k in range(nk_):
            P.mm(ps, ps[:, :n], K.ones[:], sq[:, k, :n], k == 0, k == nk_ - 1, reads=[K.ones, sq])
        P.ts("dve", rstd, rstd[:, :n], ps, ps[:, :n], 1.0 / (128 * nk_), ALU.mult, EPS, ALU.add)
        P.op("act", lambda e: e.sqrt(out=rstd[:, :n], in_=rstd[:, :n]), reads=[rstd], writes=[rstd])
        P.op("dve", lambda e: e.reciprocal(out=rstd[:, :n], in_=rstd[:, :n]), reads=[rstd], writes=[rstd])
        for k in range(nk_):
            P.stt(out_t, out_ap_k(k), x_t, x_ap_k(k), g_ap_k(k), rstd, rstd[:, :n], ALU.mult, ALU.mult, extra_reads=[g_t])

    def sumsq_max(src_t, src_ap, nrows, n, sel, dstcol_t, dstcol_ap, prow, par=0):
        kf2 = kf2s[par]
        mx1 = mx1s[par]
        P.act(kf2, kf2[:nrows, :n], src_t, src_ap, AF.Square)
        ps = next_ps(K)
        P.mm(ps, ps[:prow + 1, :n], sel[:nrows, :prow + 1], kf2[:nrows, :n], True, True, reads=[sel, kf2])
        P.op("dve", lambda e: e.reduce_max(out=mx1[prow:prow + 1, :], in_=ps[prow:prow + 1, :n], axis=AX.X), reads=[ps], writes=[mx1])
        P.tt("dve", dstcol_t, dstcol_ap, dstcol_t, dstcol_ap, mx1, mx1[prow:prow + 1, :], ALU.max)

    for it, (t0, n, is_ctx) in enumerate(tiles):
        x = xin[it % 2]
        lat0 = t0 - NCX
        P.dma("sp", x[:, 0, :n], zT.ap[256:384, t0:t0 + n], reads=[zT], writes=[x])
        rms(x, lambda k: x[:, 0, :n], 1, n, gkv, lambda k: gkv[:, 0:1], kvn, lambda k: kvn[:, :n])
        r_, s_, c_, sn_ = raw[it % 2], sw[it % 2], tC[it % 2], tS[it % 2]
        P.dma("sp", r_[64:96, :n], zT.ap[384:416, t0:t0 + n], reads=[zT], writes=[r_])
        if not is_ctx:
            P.dma("sp", s_[64:96, :n], zT.ap[1184:1216, t0:t0 + n], reads=[zT], writes=[s_])
            P.dma("sp", c_[64:96, :n], C["rope_mla_C"].ap[:, lat0:lat0 + n], reads=[C["rope_mla_C"]], writes=[c_])
            P.dma("sp", sn_[64:96, :n], C["rope_mla_S"].ap[:, lat0:lat0 + n], reads=[C["rope_mla_S"]], writes=[sn_])
            P.tt("dve", rot, rot[64:96, :n], r_, r_[64:96, :n], c_, c_[64:96, :n], ALU.mult)
            P.tt("pool", rt2, rt2[64:96, :n], s_, s_[64:96, :n], sn_, sn_[64:96, :n], ALU.mult)
            P.tt("dve", kfs[0], kfs[0][64:96, :n], rot, rot[64:96, :n], rt2, rt2[64:96, :n], ALU.add)
            P.tt("pool", kfs[1], kfs[1][64:96, :n], rot, rot[64:96, :n], rt2, rt2[64:96, :n], ALU.add)
        else:
            P.copy("dve", kfs[0], kfs[0][64:96, :n], r_, r_[64:96, :n])
            P.copy("pool", kfs[1], kfs[1][64:96, :n], r_, r_[64:96, :n])
        for h in range(8):
            kf = kfs[h % 2]
            ps = next_ps(K)
            P.mm(ps, ps[:64, :n], wukv[:, h * 128:h * 128 + 64], kvn[:, :n], True, True, reads=[wukv, kvn])
            P.copy("act", kf, kf[0:64, :n], ps, ps[:64, :n])
            sumsq_max(kf, kf[0:96, :n], 96, n, selM, kmaxM, kmaxM[96:97, h:h + 1], 96, par=h % 2)
            ktb = kt[h % 2]
            P.copy("pool", ktb, ktb[0:96, :n], kf, kf[0:96, :n])
            P.dma("sp", S["KM"].ap[h, :, t0:t0 + n], ktb[:, :n], reads=[ktb], writes=[S["KM"]])
        for q in range((n + 127) // 128):
            vtb = vt[q % 2]
            for half in range(2):
                ps = next_ps(K)
                P.mm(ps, ps[:, :512], kvn[:, q * 128:(q + 1) * 128], wukv[:, half * 512:(half + 1) * 512], True, True,
                     reads=[kvn, wukv])
                P.copy("act", vtb, vtb[:, half * 4:half * 4 + 4, 0:64],
                       ps, ps[:, :512].rearrange("t (h d) -> t h d", d=128)[:, :, 64:128])
            with nc.allow_non_contiguous_dma(reason="token-major V rows"):
                P.dma("sp", S["VM"].ap[:, t0 + q * 128:t0 + (q + 1) * 128, :].rearrange("h t d -> t h d"), vtb[:],
                      reads=[vtb], writes=[S["VM"]])
        for kh in range(2):
            kf = kfs[kh % 2]
            P.dma("sp", r_[0:64, :n], zT.ap[928 + 64 * kh:928 + 64 * kh + 64, t0:t0 + n], reads=[zT], writes=[r_])
            if not is_ctx:
                P.dma("sp", s_[0:64, :n], zT.ap[1728 + 64 * kh:1728 + 64 * kh + 64, t0:t0 + n], reads=[zT], writes=[s_])
                P.dma("sp", c_[0:64, :n], C["rope_gqa_C"].ap[:, lat0:lat0 + n], reads=[C["rope_gqa_C"]], writes=[c_])
                P.dma("sp", sn_[0:64, :n], C["rope_gqa_S"].ap[:, lat0:lat0 + n], reads=[C["rope_gqa_S"]], writes=[sn_])
                P.tt("dve", rot, rot[0:64, :n], r_, r_[0:64, :n], c_, c_[0:64, :n], ALU.mult)
                P.tt("pool", rt2, rt2[0:64, :n], s_, s_[0:64, :n], sn_, sn_[0:64, :n], ALU.mult)
                P.tt("dve", kf, kf[0:64, :n], rot, rot[0:64, :n], rt2, rt2[0:64, :n], ALU.add)
            else:
                P.copy("dve", kf, kf[0:64, :n], r_, r_[0:64, :n])
            sumsq_max(kf, kf[0:64, :n], 64, n, selG, kmaxG, kmaxG[64:65, kh:kh + 1], 64, par=kh % 2)
            ktb = kt[kh % 2]
            P.copy("pool", ktb, ktb[0:64, :n], kf, kf[0:64, :n])
            P.memset("pool", ktb, ktb[64:96, :n], 0.0)
            P.memset("pool", ktb, ktb[64:65, :n], 1.0)
            P.dma("sp", S["KG"].ap[kh, :, t0:t0 + n], ktb[0:66, :n], reads=[ktb], writes=[S["KG"]])
            P.memset("pool", ktb, ktb[96:97, :n], 1.0)
        P.dma("sp", gvt[:, :n], zT.ap[1056:1184, t0:t0 + n], reads=[zT], writes=[gvt])
        for q in range((n + 127) // 128):
            ps = next_ps(K)
            P.op("pe", lambda e: e.transpose(ps[:, 0:128], gvt[:, q * 128:(q + 1) * 128], ident[:]), reads=[gvt, ident], writes=[ps])
            vgb = vg[q % 2]
            P.copy("act", vgb, vgb[:, :, 0:64], ps, ps[:, 0:128].rearrange("t (h d) -> t h d", d=64))
            with nc.allow_non_contiguous_dma(reason="token-major V rows"):
                P.dma("sp", S["VG"].ap[:, t0 + q * 128:t0 + (q + 1) * 128, :].rearrange("h t d -> t h d"), vgb[:],
                      reads=[vgb], writes=[S["VG"]])
    qfs = [sb([96, 512], F32, f"od_qf{k}") for k in range(2)]
    qt = [sb([97, 512], BF16, f"od_qt{k}") for k in range(2)]
    prods = [sb([128, 512], F32, f"od_prod{k}") for k in range(2)]
    for it, (t0, n, is_ctx) in enumerate(tiles):
        if is_ctx and not need_ctx:
            continue
        x = xin[it % 2]
        lat0 = t0 - NCX
        P.dma("sp", x[:, :, :n], zT.ap[0:256, t0:t0 + n].rearrange("(k p) t -> p k t", p=128), reads=[zT], writes=[x])
        rms(x, lambda k: x[:, k, :n], 2, n, gq, lambda k: gq[:, k:k + 1], cqn, lambda k: cqn[:, k, :n])
        c_, sn_ = tC[it % 2], tS[it % 2]
        if not is_ctx:
            P.dma("sp", c_[64:96, :n], C["rope_mla_C"].ap[:, lat0:lat0 + n], reads=[C["rope_mla_C"]], writes=[c_])
            P.dma("sp", sn_[64:96, :n], C["rope_mla_S"].ap[:, lat0:lat0 + n], reads=[C["rope_mla_S"]], writes=[sn_])
        for h in range(8):
            qf, kf2, prod = qfs[h % 2], kf2s[h % 2], prods[h % 2]
            ps = next_ps(K)
            for k in range(2):
                P.mm(ps, ps[:96, :n], wuq[:, k, h * 96:(h + 1) * 96], cqn[:, k, :n], k == 0, k == 1, reads=[wuq, cqn])
            if not is_ctx:
                ps2 = next_ps(K)
                for k in range(2):
                    P.mm(ps2, ps2[:96, :n], wuqs[:, k, h * 96:(h + 1) * 96], cqn[:, k, :n], k == 0, k == 1, reads=[wuqs, cqn])
                P.tt("dve", rot, rot[64:96, :n], ps, ps[64:96, :n], c_, c_[64:96, :n], ALU.mult)
                P.tt("dve", rt2, rt2[64:96, :n], ps2, ps2[64:96, :n], sn_, sn_[64:96, :n], ALU.mult)
                P.stt(qf, qf[64:96, :n], rot, rot[64:96, :n], 1.0, rt2, rt2[64:96, :n], ALU.mult, ALU.add)
                P.ts("dve", qf, qf[64:96, :n], qf, qf[64:96, :n], MLA_SCALE, ALU.mult)
                P.op("act", lambda e: e.mul(out=qf[0:64, :n], in_=ps[0:64, :n], mul=MLA_SCALE), reads=[ps], writes=[qf])
            else:
                P.op("act", lambda e: e.mul(out=qf[0:96, :n], in_=ps[0:96, :n], mul=MLA_SCALE), reads=[ps], writes=[qf])
            qtb = qt[h % 2]
            P.copy("pool", qtb, qtb[0:96, :n], qf, qf[0:96, :n])
            P.act(kf2, kf2[:96, :n], qf, qf[0:96, :n], AF.Square)
            psq = next_ps(K)
            P.mm(psq, psq[:97, :n], selM[:, :], kf2[:96, :n], True, True, reads=[selM, kf2])
            P.ts("dve", prod, prod[96:97, :n], psq, psq[96:97, :n], kmaxM[96:97, h:h + 1], ALU.mult, extra_reads=[kmaxM])
            P.op("act", lambda e: e.sqrt(out=prod[96:97, :n], in_=prod[96:97, :n]), reads=[prod], writes=[prod])
            P.ts("dve", qtb, qtb[96:97, :n], prod, prod[96:97, :n], -1.0, ALU.mult)
            P.dma("sp", S["QM"].ap[h, :, t0:t0 + n], qtb[:, :n], reads=[qtb], writes=[S["QM"]])
        r_, s_ = raw[it % 2], sw[it % 2]
        if not is_ctx:
            P.dma("sp", c_[0:64, :n], C["rope_gqa_C"].ap[:, lat0:lat0 + n], reads=[C["rope_gqa_C"]], writes=[c_])
            P.dma("sp", sn_[0:64, :n], C["rope_gqa_S"].ap[:, lat0:lat0 + n], reads=[C["rope_gqa_S"]], writes=[sn_])
        for hq in range(8):
            qf, kf2, prod = qfs[hq % 2], kf2s[hq % 2], prods[hq % 2]
            P.dma("sp", r_[0:64, :n], zT.ap[416 + 64 * hq:416 + 64 * hq + 64, t0:t0 + n], reads=[zT], writes=[r_])
            if not is_ctx:
                P.dma("sp", s_[0:64, :n], zT.ap[1216 + 64 * hq:1216 + 64 * hq + 64, t0:t0 + n], reads=[zT], writes=[s_])
                P.tt("dve", rot, rot[0:64, :n], r_, r_[0:64, :n], c_, c_[0:64, :n], ALU.mult)
                P.tt("pool", rt2, rt2[0:64, :n], s_, s_[0:64, :n], sn_, sn_[0:64, :n], ALU.mult)
                P.stt(qf, qf[0:64, :n], rot, rot[0:64, :n], 1.0, rt2, rt2[0:64, :n], ALU.mult, ALU.add)
                P.ts("dve", qf, qf[0:64, :n], qf, qf[0:64, :n], GQA_SCALE, ALU.mult)
            else:
                P.ts("dve", qf, qf[0:64, :n], r_, r_[0:64, :n], GQA_SCALE, ALU.mult)
            qtb = qt[hq % 2]
            P.copy("pool", qtb, qtb[0:64, :n], qf, qf[0:64, :n])
            P.memset("pool", qtb, qtb[64:96, :n], 1.0)
            P.act(kf2, kf2[:64, :n], qf, qf[0:64, :n], AF.Square)
            psq = next_ps(K)
            P.mm(psq, psq[:65, :n], selG[:, :], kf2[:64, :n], True, True, reads=[selG, kf2])
            P.ts("dve", prod, prod[64:65, :n], psq, psq[64:65, :n], kmaxG[64:65, hq // 4:hq // 4 + 1], ALU.mult, extra_reads=[kmaxG])
            P.op("act", lambda e: e.sqrt(out=prod[64:65, :n], in_=prod[64:65, :n]), reads=[prod], writes=[prod])
            P.ts("dve", qtb, qtb[64:65, :n], prod, prod[64:65, :n], -1.0, ALU.mult)
            P.dma("sp", S["QG"].ap[hq, :, t0:t0 + n], qtb[0:66, :n], reads=[qtb], writes=[S["QG"]])
    P.release_to(markp)
    if _DBG_SKIP_ATTN:
        P.release_to(mark)
        return
    A = Ctx()
    A.mixT = mixT
    A.mixH = qsel[2] if qsel is not None else None
    A.sel65 = sel65
    K.ps_reserved = {7}
    A.pot = K.ps[7]
    A.pt = [sb([128, 512], BF16, f"at_pt{k}") for k in range(5)]
    A.pti = 0
    A.ot = sb([65, 512], F32, "at_ot"); A.rden = sb([64, 512], F32, "at_rden")
    A.ob = [sb([64, 512], F32, f"at_ob{k}") for k in range(2)]
    A.obi = 0
    NKC = NT // 128
    kres = sb([97, NT], BF16, "at_k"); vres = sb([128, NKC, 65], BF16, "at_v")
    qb_ = [sb([97, 512], BF16, f"at_q{k}") for k in range(2)]
    qb2_ = [sb([97, 512], BF16, f"at_q2{k}") for k in range(2)] if qsel is not None else None
    qi = 0
    for h in range(8):
        P.dma("sp", kres[:, :], S["KM"].ap[h, :, :], reads=[S["KM"]], writes=[kres])
        with nc.allow_non_contiguous_dma(reason="token-major V rows"):
            P.dma("sp", vres[:], S["VM"].ap[h].rearrange("(c p) d -> p c d", p=128), reads=[S["VM"]], writes=[vres])
        qlist = [(NCX + k * 512, min(512, K.NL - k * 512), False) for k in range((K.NL + 511) // 512)]
        if need_ctx and NCX:
            qlist = [(0, NCX, True)] + qlist
        if qsel is not None:
            selt, HALF_, mixH = qsel
            for k in range(HALF_ // 512):
                qb = qb_[qi % 2]
                qb2 = qb2_[qi % 2]
                qi += 1
                P.dma("sp", qb[:, :], S["QM"].ap[h, :, NCX + k * 512:NCX + (k + 1) * 512], reads=[S["QM"]], writes=[qb])
                P.dma("sp", qb2[:, :], S["QM"].ap[h, :, NCX + HALF_ + k * 512:NCX + HALF_ + (k + 1) * 512], reads=[S["QM"]], writes=[qb2])
                P.ts("dve", qb, qb[:, :], qb, qb[:, :], selt[:97, 0:1], ALU.mult, extra_reads=[selt])
                P.stt(qb, qb[:, :], qb2, qb2[:, :], selt[:97, 1:2], qb, qb[:, :], ALU.mult, ALU.add, extra_reads=[selt])
                chunks = [(kres, kres[:, kc * 128:(kc + 1) * 128], vres, vres[:, kc, :], None, None, None) for kc in range(NKC)]
                attn_core(K, A, qb, qb[:, :], 512, chunks, [(mixH.ap[h * 64:(h + 1) * 64, k * 512:(k + 1) * 512], 0, 512)])
            continue
        for (t0, n, is_ctx) in qlist:
            qb = qb_[qi % 2]
            qi += 1
            P.dma("sp", qb[:, :n], S["QM"].ap[h, :, t0:t0 + n], reads=[S["QM"]], writes=[qb])
            kcs = range(NCX // 128) if is_ctx else range(NKC)
            chunks = [(kres, kres[:, kc * 128:(kc + 1) * 128], vres, vres[:, kc, :], None, None, None) for kc in kcs]
            attn_core(K, A, qb, qb[:, :n], n, chunks, [(mixT.ap[h * 64:(h + 1) * 64, t0:t0 + n], 0, n)])
    mprev, mnext = C["k_mask_prev"], C["k_mask_next"]
    ks2 = sb([96, 8], BF16, "at_ks2")
    P.copy("dve", ks2, ks2[64:96, :], sinkb, sinkb[64:96, :])
    P.memset("pool", ks2, ks2[64:65, :], 1.0)
    NB = K.NL // 128
    for kh in range(2):
        P.dma("sp", kres[0:66, :], S["KG"].ap[kh, :, :], reads=[S["KG"]], writes=[kres])
        with nc.allow_non_contiguous_dma(reason="token-major V rows"):
            P.dma("sp", vres[:], S["VG"].ap[kh].rearrange("(c p) d -> p c d", p=128), reads=[S["VG"]], writes=[vres])
        blocks = [(NCX + b * 128, b, False) for b in range(NB)]
        if need_ctx and NCX:
            blocks = [(cb * 128, cb, True) for cb in range(NCX // 128)] + blocks
        for (t0, b, is_ctx) in blocks:
            qb = qb_[qi % 2]
            qi += 1
            P.dma("sp", qb[0:66, :].rearrange("r (g t) -> r g t", t=128),
                  S["QG"].ap[4 * kh:4 * kh + 4, :, t0:t0 + 128].rearrange("g r t -> r g t"), reads=[S["QG"]], writes=[qb])
            chunks = []
            for cb in range(NCX // 128):
                chunks.append((kres, kres[0:66, cb * 128:(cb + 1) * 128], vres, vres[:, cb, :], None, None, None))
            if not is_ctx:
                kc0 = NCX // 128 + b
                if b > 0:
                    chunks.append((kres, kres[0:66, (kc0 - 1) * 128:kc0 * 128], vres, vres[:, kc0 - 1, :], (mprev, mprev[:, :]), None, None))
                chunks.append((kres, kres[0:66, kc0 * 128:(kc0 + 1) * 128], vres, vres[:, kc0, :], None, None, None))
                if b < NB - 1:
                    chunks.append((kres, kres[0:66, (kc0 + 1) * 128:(kc0 + 2) * 128], vres, vres[:, kc0 + 1, :], (mnext, mnext[:, :]), None, None))
            for g in range(4):
                hq = 4 * kh + g
                chunks.append((ks2, ks2[64:66, hq:hq + 1], vsb, vsb[0:1, :], None, (g * 128, 128), (64, 66)))
            outs = [(mixT.ap[512 + (4 * kh + g) * 64:512 + (4 * kh + g + 1) * 64, t0:t0 + 128], g * 128, 128) for g in range(4)]
            attn_core(K, A, qb, qb[0:66, :], 512, chunks, outs)
    K.ps_reserved = set()
    P.release_to(mark)


NCTX_FULL, NLAT_FULL, DEPTH = 256, 8192, 4
_W_SHAPES = dict(
    c_ctx=[D], mod_w=[DEPTH, D, 6 * D], mod_b=[DEPTH, 6 * D],
    norm_mix=[DEPTH, D], norm_ffn=[DEPTH, D], final_norm=[D],
    ev_w_in=[2, D, 2048], ev_conv_w=[2, 3, 1536], ev_conv_b=[2, 1536],
    hy_w1=[2, 33, 64], hy_b1=[2, 64], hy_w2=[2, 64, 64], hy_b2=[2, 64], hy_w3=[2, 64, 2048], hy_freq=[2, 64],
    hy_decay=[2, 2, 512], hy_bias=[2, 2, 512],
    s5_a_re=[2, 2, 32, 64], s5_a_im=[2, 2, 32, 64], s5_log_dt=[2, 2, 32],
    s5_b_re=[2, 2, 32, 64, 16], s5_b_im=[2, 2, 32, 64, 16], s5_c_re=[2, 2, 32, 16, 64], s5_c_im=[2, 2, 32, 16, 64],
    s5_d=[2, 512], s5_w_glu=[2, 512, 512], ev_w_out=[2, D, D],
    ff_w_gate=[2, D, 2816], ff_w_up=[2, D, 2816], ff_w_down=[2, 2816, D],
    od_w_in=[2, D, 1184], mla_q_norm=[2, 256], mla_w_uq=[2, 256, 768], mla_kv_norm=[2, 128], mla_w_ukv=[2, 128, 1024],
    gqa_sink=[2, 8], od_w_out=[2, D, D],
    moe_router=[2, D, 8], moe_w_gate=[2, 8, D, 3584], moe_w_up=[2, 8, D, 3584], moe_w_down=[2, 8, 3584, D],
)
_DRAM_ONLY_CONSTS = ("rope_mla_C", "rope_mla_S", "rope_gqa_C", "rope_gqa_S", "hy8192_featsT", "hy256_featsT",
                     "hc_Fc", "hc_Fns", "hc_Ic", "hc_Ins")


def all_consts():
    c = {}
    c.update(host_consts())
    c.update(hyena_consts(NLAT_FULL))
    c["hy256_featsT"] = hyena_consts(NCTX_FULL)["hy256_featsT"]
    c.update(hyena_ctx_consts())
    c.update(hyena_shared_consts())
    c.update(rope_consts(NLAT_FULL))
    return c


def build_program():
    P = Prog()
    nc = P.nc
    K = setup_common(P, NCTX_FULL, NLAT_FULL)
    NT = K.NT
    inp = lambda k, shp: T(nc.dram_tensor(k, list(shp), F32, kind="ExternalInput").ap())
    I = {k: inp(k, v) for k, v in _W_SHAPES.items()}
    I["hin"] = inp("hin", [D, NT])
    I["c"] = inp("c", [D])
    consts = all_consts()
    Cd = {k: inp(k, v.shape) for k, v in consts.items()}
    HALF = NLAT_FULL // 2
    I["sel"] = inp("sel", [128, 2])
    outT = T(nc.dram_tensor("outT", [D, HALF], F32, kind="ExternalOutput").ap())
    hT2 = P.dram("hT2", [D, HALF])
    mixH = P.dram("mixH", [512, HALF])
    zT = P.dram("zT", [2048, NT])
    mixT = P.dram("mixT", [D, NT])
    K.yS5 = P.dram("yS5", [512, NT])
    kT = P.dram("kT", [2048, NLAT_FULL])
    KFr = P.dram("KFr", [256, 128, 512])
    KFi = P.dram("KFi", [256, 128, 512])
    S = dict(QM=P.dram("QM", [8, 97, NT], BF16), KM=P.dram("KM", [8, 97, NT], BF16), VM=P.dram("VM", [8, NT, 65], BF16),
             QG=P.dram("QG", [8, 66, NT], BF16), KG=P.dram("KG", [2, 66, NT], BF16), VG=P.dram("VG", [2, NT, 65], BF16))

    def load_consts(keys):
        C = {}
        for k in keys:
            d = Cd[k]
            if k in _DRAM_ONLY_CONSTS:
                C[k + "_dram" if k.endswith("featsT") else k] = d
            else:
                t = P.sb(list(d.ap.shape), F32, "c_" + k)
                P.dma("sp", t[:], d.ap[:, :], reads=[d], writes=[t])
                C[k] = t
        return C

    for i0 in range((NT + 1023) // 1024):
        n = min(1024, NT - i0 * 1024)
        P.dma("sp", K.hT.ap[:, i0 * 1024:i0 * 1024 + n], I["hin"].ap[:, i0 * 1024:i0 * 1024 + n], reads=[I["hin"]], writes=[K.hT])
    mods = mods_all(K, list(range(DEPTH)), I["c"], I["c_ctx"], I["mod_w"], I["mod_b"], I["norm_mix"], I["norm_ffn"])
    for l in range(DEPTH):
        i = l // 2
        need_ctx = l < DEPTH - 1
        if l % 2 == 0:
            inproj_stage(K, mods[l], T(None, I["ev_w_in"].ap[i]), zT)
            m = P.mark()
            C = load_consts([k for k in Cd if k.startswith(f"hy{NLAT_FULL}_") or k.startswith("hy_")])
            hyena_stage(K, i, I, zT, mixT, C, NLAT_FULL, NCTX_FULL, KFr, KFi, kT, f"l{l}")
            P.release_to(m)
            m = P.mark()
            C = load_consts(["hy_tau512", "k_ident", f"hy{NCTX_FULL}_featsT"])
            hyena_ctx_stage(K, i, I, zT, mixT, C, Cd, 0)
            P.release_to(m)
            m = P.mark()
            C = load_consts(["k_tau", "k_maskB", "k_maskC", "k_ident"])
            s5_stage(K, i, I, zT, mixT, C)
            P.release_to(m)
            outproj_stage(K, mods[l], T(None, I["ev_w_out"].ap[i]), mixT)
            experts = [(T(None, I["ff_w_gate"].ap[i]), T(None, I["ff_w_up"].ap[i]), T(None, I["ff_w_down"].ap[i]))]
            ffn_stage(K, mods[l], experts, router=None, ST=1024)
        else:
            inproj_stage(K, mods[l], T(None, I["od_w_in"].ap[i]), zT, segs=odd_segs())
            m = P.mark()
            C = load_consts(["k_ident", "k_mask_prev", "k_mask_next", "k_selM", "k_selG", "k_sel65", "k_vsink",
                             "rope_mla_C", "rope_mla_S", "rope_gqa_C", "rope_gqa_S"])
            if l < DEPTH - 1:
                odd_mixer_stage(K, i, I, zT, mixT, C, S, need_ctx)
                P.release_to(m)
                outproj_stage(K, mods[l], T(None, I["od_w_out"].ap[i]), mixT)
            else:
                selq = P.sb([128, 2], F32, "selq")
                P.dma("sp", selq[:], I["sel"].ap[:, :], reads=[I["sel"]], writes=[selq])
                odd_mixer_stage(K, i, I, zT, mixT, C, S, need_ctx, qsel=(selq, HALF, mixH))
                P.release_to(m)
                outproj_half_stage(K, mods[l], T(None, I["od_w_out"].ap[i]), mixT, mixH, I["sel"], hT2, HALF)
            experts = [(T(None, I["moe_w_gate"].ap[i, e]), T(None, I["moe_w_up"].ap[i, e]), T(None, I["moe_w_down"].ap[i, e]))
                       for e in range(8)]
            if l < DEPTH - 1:
                ffn_stage(K, mods[l], experts, router=T(None, I["moe_router"].ap[i]), ST=1024)
            else:
                K2 = Ctx()
                K2.__dict__.update(K.__dict__)
                K2.hT, K2.NC, K2.NL, K2.NT = hT2, 0, HALF, HALF
                ffn_stage(K2, mods[l], experts, router=T(None, I["moe_router"].ap[i]), ST=1024,
                          tok_ranges=[(k * 1024, 1024, 0) for k in range(HALF // 1024)])
                final_norm_stage(K2, I["final_norm"], outT, 0, HALF)
    P.finish()
    return P


def kernel(**inputs):
    x = np.asarray(inputs["x"], dtype=np.float32)
    ctx = np.asarray(inputs["ctx"], dtype=np.float32)
    B = x.shape[0]
    P = build_program()
    shared = {k: np.ascontiguousarray(np.asarray(inputs[k], dtype=np.float32)) for k in _W_SHAPES}
    shared.update(all_consts())
    in_maps = []
    for core in range(8):
        b = core // 2
        m = dict(shared)
        m["hin"] = np.ascontiguousarray(np.concatenate([ctx[b], x[b]], axis=0).T)
        m["c"] = np.ascontiguousarray(np.asarray(inputs["c"], dtype=np.float32)[b])
        sel = np.zeros((128, 2), np.float32)
        sel[:, core % 2] = 1.0
        m["sel"] = sel
        in_maps.append(m)
    res = run_bass_kernel_spmd(P.nc, in_maps, core_ids=list(range(8)))
    out = np.empty((B, NLAT_FULL, D), dtype=np.float32)
    half = NLAT_FULL // 2
    for b in range(B):
        out[b, :half] = res.results[2 * b]["outT"].T
        out[b, half:] = res.results[2 * b + 1]["outT"].T
    return out
```
